# Optimizing a Trainium2 kernel written in Bass

```python
import math
import jax, jax.numpy as jnp
from jax import lax
import numpy as np

D_MODEL = 1024
BATCH = 16
SEQ = 2048
DEPTH = 2

CONV_CH = 512
CONV_GROUPS = 8
CONV_WIDTH = 31
ATT_HEADS = 8
ATT_HEAD_DIM = 64
IDX_HEADS = 8
IDX_HEAD_DIM = 64
IDX_TOPK = 256
Q_BLOCK = 128
GLA_HEADS = 4
GLA_DK = 128
GLA_DV = 256
GLA_GATE_RANK = 16
GLA_TAU = 16.0
GLA_CHUNK = 64
ROPE_THETA = 500000.0
ROPE_FRAC = 4
D_FF = -(-8 * D_MODEL // (3 * 256)) * 256
NORM_EPS = 1e-6

AB_SIZES = (2 * CONV_CH, ATT_HEADS * ATT_HEAD_DIM, ATT_HEAD_DIM, ATT_HEAD_DIM,
            IDX_HEADS * IDX_HEAD_DIM, IDX_HEAD_DIM, IDX_HEADS)
AB_IN = sum(AB_SIZES)
AB_OUT = CONV_CH + ATT_HEADS * ATT_HEAD_DIM
C_SIZES = (GLA_HEADS * GLA_DK, GLA_HEADS * GLA_DK, GLA_HEADS * GLA_DV,
           GLA_HEADS * GLA_DV, GLA_GATE_RANK)
C_IN = sum(C_SIZES)
C_OUT = GLA_HEADS * GLA_DV

kernel_name = "hybrid_conv_dsa_gla_trunk"


def split_cols(u, sizes):
    outs, off = [], 0
    for s in sizes:
        outs.append(u[..., off:off + s])
        off += s
    return outs


def rms_norm(x, g):
    xf = x.astype(jnp.float32)
    y = xf * lax.rsqrt(jnp.mean(xf * xf, axis=-1, keepdims=True) + NORM_EPS)
    return (y * g.astype(jnp.float32)).astype(x.dtype)


def rope_tables(seq, head_dim):
    rot = head_dim // ROPE_FRAC
    inv = ROPE_THETA ** (-jnp.arange(0, rot, 2, dtype=jnp.float32) / rot)
    ang = jnp.arange(seq, dtype=jnp.float32)[:, None] * inv[None, :]
    return jnp.cos(ang), jnp.sin(ang)


def apply_rope(x, cos, sin):
    r = cos.shape[-1]
    xf = x.astype(jnp.float32)
    x1, x2, xp = xf[..., :r], xf[..., r:2 * r], xf[..., 2 * r:]
    out = jnp.concatenate([x1 * cos - x2 * sin, x2 * cos + x1 * sin, xp], axis=-1)
    return out.astype(x.dtype)


def conv_module(u, conv_w, conv_b, ln_g, ln_b):
    a, gate = u[..., :CONV_CH], u[..., CONV_CH:]
    h = a * jax.nn.sigmoid(gate)
    h = lax.conv_general_dilated(h, conv_w, window_strides=(1,),
                                 padding=[(CONV_WIDTH - 1, 0)],
                                 dimension_numbers=("NWC", "WIO", "NWC"),
                                 feature_group_count=CONV_CH) + conv_b
    hf = h.astype(jnp.float32)
    mu = jnp.mean(hf, axis=-1, keepdims=True)
    var = jnp.mean(jnp.square(hf - mu), axis=-1, keepdims=True)
    hf = (hf - mu) * lax.rsqrt(var + NORM_EPS) * ln_g.astype(jnp.float32) + ln_b.astype(jnp.float32)
    return jax.nn.silu(hf).astype(u.dtype)


def dsa_attention(q, k, v, iq, ik, iw, cos, sin):
    b_, s_, h_, dh = q.shape
    n_sel = min(IDX_TOPK, s_ // 4)
    q = apply_rope(q, cos[:, None, :], sin[:, None, :])
    k = apply_rope(k, cos, sin)
    iq = apply_rope(iq, cos[:, None, :], sin[:, None, :])
    ik = apply_rope(ik, cos, sin)
    nb = s_ // Q_BLOCK

    def to_blocks(t):
        return jnp.moveaxis(t.reshape(b_, nb, Q_BLOCK, *t.shape[2:]), 1, 0)

    key_pos = jnp.arange(s_, dtype=jnp.int32)
    q_pos = key_pos.reshape(nb, Q_BLOCK)
    w_scale = (IDX_HEADS ** -0.5) * (IDX_HEAD_DIM ** -0.5)
    att_scale = dh ** -0.5

    def block(args):
        qb, iqb, iwb, tpos = args
        logits = jnp.einsum("bqhd,bsd->bqsh", iqb, ik, preferred_element_type=jnp.float32)
        score = jnp.einsum("bqsh,bqh->bqs", jax.nn.relu(logits),
                           iwb.astype(jnp.float32)) * w_scale
        causal = key_pos[None, :] <= tpos[:, None]
        score = jnp.where(causal[None], score, -jnp.inf)
        _, idx = lax.top_k(score, n_sel)
        valid = idx <= tpos[None, :, None]
        kg = jax.vmap(lambda kk, ii: kk[ii])(k, idx)
        vg = jax.vmap(lambda vv, ii: vv[ii])(v, idx)
        s = jnp.einsum("bqhd,bqkd->bhqk", qb, kg, preferred_element_type=jnp.float32) * att_scale
        s = jnp.where(valid[:, None], s, -jnp.inf)
        p = jax.nn.softmax(s, axis=-1)
        return jnp.einsum("bhqk,bqkd->bqhd", p.astype(vg.dtype), vg)

    out = lax.map(block, (to_blocks(q), to_blocks(iq), to_blocks(iw), q_pos))
    return jnp.moveaxis(out, 0, 1).reshape(b_, s_, h_ * dh)


def gla(q, k, v, r, gate_lr, g2_w, g2_b, onorm_g):
    b_, s_, h_, dk = q.shape
    dv = v.shape[-1]
    log_a = jax.nn.log_sigmoid((gate_lr @ g2_w + g2_b).astype(jnp.float32)) / GLA_TAU
    log_a = log_a.reshape(b_, s_, h_, dk)
    nc, c = s_ // GLA_CHUNK, GLA_CHUNK

    def chunks(t):
        return t.astype(jnp.float32).reshape(b_, nc, c, h_, t.shape[-1]).transpose(1, 0, 3, 2, 4)

    qc = chunks(q) * (dk ** -0.5)
    kc, vc, lc = chunks(k), chunks(v), chunks(log_a)
    bcum = jnp.cumsum(lc, axis=3)
    b_last = bcum[..., -1:, :]
    q_t = qc * jnp.exp(bcum)
    k_t = kc * jnp.exp(-bcum)
    k_s = kc * jnp.exp(b_last - bcum)
    mask = jnp.tril(jnp.ones((c, c), jnp.float32))
    att = jnp.einsum("nbhid,nbhjd->nbhij", q_t, k_t) * mask
    o_intra = jnp.einsum("nbhij,nbhjv->nbhiv", att, vc)

    def step(state, inp):
        q_n, k_n, v_n, decay = inp
        o = jnp.einsum("bhid,bhdv->bhiv", q_n, state)
        state = state * decay[:, :, 0, :, None] + jnp.einsum("bhjd,bhjv->bhdv", k_n, v_n)
        return state, o

    s0 = jnp.zeros((b_, h_, dk, dv), jnp.float32)
    _, o_inter = lax.scan(step, s0, (q_t, k_s, vc, jnp.exp(b_last)))
    o = (o_intra + o_inter).transpose(1, 0, 3, 2, 4).reshape(b_, s_, h_, dv)
    o = o * lax.rsqrt(jnp.mean(o * o, axis=-1, keepdims=True) + NORM_EPS) \
        * onorm_g.astype(jnp.float32).reshape(h_, dv)
    o = o.astype(r.dtype) * jax.nn.silu(r)
    return o.reshape(b_, s_, h_ * dv)


def setup_inputs(seed: int = 0) -> dict:
    key = jax.random.key(seed)
    ks = iter(jax.random.split(key, 32))
    n_even = (DEPTH + 1) // 2
    n_odd = DEPTH // 2

    def nrm(shape, scale):
        return jax.random.normal(next(ks), shape, jnp.float32) * scale

    def gain(shape):
        return 1.0 + nrm(shape, 0.02)

    res_scale = (2.0 * DEPTH) ** -0.5
    return {
        "x": nrm((BATCH, SEQ, D_MODEL), 1.0),
        "norm_mix_g": gain((DEPTH, D_MODEL)),
        "ab_w_in": nrm((n_even, D_MODEL, AB_IN), D_MODEL ** -0.5),
        "ab_conv_w": nrm((n_even, CONV_WIDTH, 1, CONV_CH), CONV_WIDTH ** -0.5),
        "ab_conv_b": nrm((n_even, CONV_CH), 0.02),
        "ab_ln_g": gain((n_even, CONV_CH)),
        "ab_ln_b": nrm((n_even, CONV_CH), 0.02),
        "ab_w_out": nrm((n_even, AB_OUT, D_MODEL), AB_OUT ** -0.5 * res_scale),
        "c_w_in": nrm((n_odd, D_MODEL, C_IN), D_MODEL ** -0.5),
        "c_gate_w": nrm((n_odd, GLA_GATE_RANK, GLA_HEADS * GLA_DK), GLA_GATE_RANK ** -0.5),
        "c_gate_b": nrm((n_odd, GLA_HEADS * GLA_DK), 0.02),
        "c_onorm_g": gain((n_odd, GLA_HEADS * GLA_DV)),
        "c_w_out": nrm((n_odd, C_OUT, D_MODEL), C_OUT ** -0.5 * res_scale),
        "norm_ffn_g": gain((DEPTH, D_MODEL)),
        "ffn_w_gate": nrm((DEPTH, D_MODEL, D_FF), D_MODEL ** -0.5),
        "ffn_w_up": nrm((DEPTH, D_MODEL, D_FF), D_MODEL ** -0.5),
        "ffn_w_down": nrm((DEPTH, D_FF, D_MODEL), D_FF ** -0.5 * res_scale),
        "final_norm_g": gain((D_MODEL,)),
    }


def reference(x, norm_mix_g, ab_w_in, ab_conv_w, ab_conv_b, ab_ln_g, ab_ln_b, ab_w_out,
              c_w_in, c_gate_w, c_gate_b, c_onorm_g, c_w_out,
              norm_ffn_g, ffn_w_gate, ffn_w_up, ffn_w_down, final_norm_g):
    b_, s_, _ = x.shape
    cos, sin = rope_tables(s_, ATT_HEAD_DIM)
    h = x
    for layer in range(DEPTH):
        hn = rms_norm(h, norm_mix_g[layer])
        if layer % 2 == 0:
            i = layer // 2
            u = hn @ ab_w_in[i]
            ua, q, k, v, iq, ik, iw = split_cols(u, AB_SIZES)
            ya = conv_module(ua, ab_conv_w[i], ab_conv_b[i], ab_ln_g[i], ab_ln_b[i])
            yb = dsa_attention(q.reshape(b_, s_, ATT_HEADS, ATT_HEAD_DIM), k, v,
                               iq.reshape(b_, s_, IDX_HEADS, IDX_HEAD_DIM), ik, iw, cos, sin)
            y = jnp.concatenate([ya, yb], axis=-1) @ ab_w_out[i]
        else:
            i = layer // 2
            u = hn @ c_w_in[i]
            q, k, v, r, glr = split_cols(u, C_SIZES)
            yc = gla(q.reshape(b_, s_, GLA_HEADS, GLA_DK), k.reshape(b_, s_, GLA_HEADS, GLA_DK),
                     v.reshape(b_, s_, GLA_HEADS, GLA_DV), r.reshape(b_, s_, GLA_HEADS, GLA_DV),
                     glr, c_gate_w[i], c_gate_b[i], c_onorm_g[i])
            y = yc @ c_w_out[i]
        h = h + y
        hn = rms_norm(h, norm_ffn_g[layer])
        h = h + (jax.nn.silu(hn @ ffn_w_gate[layer]) * (hn @ ffn_w_up[layer])) @ ffn_w_down[layer]
    return rms_norm(h, final_norm_g)
```

```python
import contextlib
import math
import numpy as np
import ml_dtypes
import concourse.bass as bass
import concourse.mybir as mybir
from concourse.bass_utils import run_bass_kernel_spmd

F32 = mybir.dt.float32
BF16 = mybir.dt.bfloat16
ALU = mybir.AluOpType
AF = mybir.ActivationFunctionType
AX = mybir.AxisListType

ENGS = ["sync", "scalar", "vector", "gpsimd", "tensor"]
D = 1024
SEQ = 2048
NT = 16
EPS = 1e-6
DFF = 2816
NFF = 22
NIT = 18
TOPK = 256
DEBUG_STOP = 99
SAME_SYNC = True


class Sched:
    def __init__(self, nc, stack, tag=""):
        self.nc = nc
        self.stack = stack
        self.tag = tag
        self.q = {e: [] for e in ENGS}
        self.sem = {}
        self.cnt = {}
        self.waited = {e: {} for e in ENGS}
        self.lastw = {}
        self.readers = {}
        self.pe_pending = False
        self.out_deps = {}
        for e in ENGS:
            self._mksem("E_" + e)

    def _mksem(self, name):
        if name not in self.sem:
            hw_name = "%s_s%d" % (self.tag, len(self.sem))
            self.sem[name] = self.nc.alloc_semaphore(name=hw_name)
            self.cnt[name] = 0
        return name

    def op(self, eng, fn, reads=(), writes=(), dma=None, mark=True, is_out=False, force_self=False):
        deps = {}

        def add(d):
            if d[1] > deps.get(d[0], 0):
                deps[d[0]] = d[1]

        for k in reads:
            if k in self.lastw:
                add(self.lastw[k])
        for k in writes:
            if k in self.lastw:
                add(self.lastw[k])
            for s_, v_ in self.readers.get(k, {}).items():
                add((s_, v_))
        if dma is not None:
            s = self._mksem("D_" + str(dma))
            if self.cnt[s] > 0:
                add((s, self.cnt[s]))
        own = "E_" + eng
        need = []
        for s_, v_ in deps.items():
            if s_ == own and (eng == "tensor" or not (SAME_SYNC or force_self)):
                continue
            if self.waited[eng].get(s_, 0) >= v_:
                continue
            need.append((s_, v_))
        attach = None
        if eng == "tensor":
            k0 = reads[0] if len(reads) else None
            if k0 is not None and k0 in self.lastw and self.lastw[k0][0] != own:
                attach = self.lastw[k0]
        else:
            selfs = [d_ for d_ in need if d_[0] == own]
            attach = selfs[0] if selfs else (need[-1] if need else None)
        standalone = [d_ for d_ in need if attach is None or d_[0] != attach[0]]
        if attach is not None:
            for d_ in need:
                if d_[0] == attach[0] and d_[1] > attach[1]:
                    attach = d_
        for d_ in need:
            self.waited[eng][d_[0]] = max(self.waited[eng].get(d_[0], 0), d_[1])
        if attach is not None:
            self.waited[eng][attach[0]] = max(self.waited[eng].get(attach[0], 0), attach[1])
        if dma is not None:
            self.cnt[s] += 16
            dep = (s, self.cnt[s])
            self.q[eng].append(("op", fn, s, 16, standalone, attach))
        else:
            s = own
            if mark:
                self.cnt[s] += 1
                dep = (s, self.cnt[s])
                self.q[eng].append(("op", fn, s, 1, standalone, attach))
                if eng == "tensor":
                    self.pe_pending = False
            else:
                assert eng == "tensor"
                dep = (s, self.cnt[s] + 1)
                self.q[eng].append(("op", fn, None, 0, standalone, attach))
                self.pe_pending = True
        for k in writes:
            self.lastw[k] = dep
            self.readers[k] = {}
        for k in reads:
            r = self.readers.setdefault(k, {})
            if dep[1] > r.get(dep[0], 0):
                r[dep[0]] = dep[1]
        if is_out:
            if dep[1] > self.out_deps.get(dep[0], 0):
                self.out_deps[dep[0]] = dep[1]
        return dep

    def finalize(self):
        assert not self.pe_pending, "unmarked PE op at end of phase"
        tail = []
        for s_, v_ in self.out_deps.items():
            if self.waited["sync"].get(s_, 0) < v_:
                tail.append((s_, v_))
        nc = self.nc
        with nc.Block() as block:
            for eng in ENGS:
                items = self.q[eng]

                def body(e, items=items, eng=eng):
                    for it in items:
                        for (ws, wv) in it[4]:
                            e.wait_ge(self.sem[ws], wv)
                        ins = it[1](e)
                        if it[5] is not None:
                            ins._wait_ge(self.sem[it[5][0]], it[5][1])
                        if it[2] is not None:
                            ins.then_inc(self.sem[it[2]], it[3])
                    if eng == "sync":
                        for (ws, wv) in tail:
                            e.wait_ge(self.sem[ws], wv)

                getattr(block, eng)(body)


class Phase:
    def __init__(self, nc, tag):
        self.nc = nc
        self.tag = tag
        self.st = contextlib.ExitStack()
        self.st.enter_context(nc.cleanup_on_exit())
        self.S = Sched(nc, self.st, tag)

    def sb(self, name, shape, dt):
        return self.st.enter_context(self.nc.sbuf_tensor(self.tag + "_" + name, shape, dt))

    def ps(self, name, shape, dt=F32):
        return self.st.enter_context(self.nc.psum_tensor(self.tag + "_" + name, shape, dt))

    def dma(self, eng, out, in_, r=(), w=(), key=None, is_out=False):
        return self.S.op(eng, lambda e: e.dma_start(out=out, in_=in_), r, w, dma=key, is_out=is_out)

    def mm(self, out, lhsT, rhs, start, stop, r, w, mark=None):
        if mark is None:
            mark = stop
        return self.S.op("tensor", lambda e: e.matmul(out, lhsT=lhsT, rhs=rhs, start=start, stop=stop), r, w, mark=mark)

    def tr(self, out, in_, ident, r, w, mark=True):
        return self.S.op("tensor", lambda e: e.transpose(out=out, in_=in_, identity=ident), r, w, mark=mark)

    def act(self, out, in_, func, r, w, **kw):
        return self.S.op("scalar", lambda e: e.activation(out=out, in_=in_, func=func, **kw), r, w)

    def tt(self, eng, out, in0, in1, op, r, w):
        return self.S.op(eng, lambda e: e.tensor_tensor(out=out, in0=in0, in1=in1, op=op), r, w)

    def ts(self, eng, out, in0, s1, op0, r, w, s2=None, op1=None, accum_out=None):
        if op1 is None:
            return self.S.op(eng, lambda e: e.tensor_scalar(out=out, in0=in0, scalar1=s1, scalar2=None, op0=op0), r, w)
        return self.S.op(eng, lambda e: e.tensor_scalar(out=out, in0=in0, scalar1=s1, scalar2=s2, op0=op0, op1=op1,
                                                       accum_out=accum_out), r, w)

    def stt(self, out, in0, scalar, in1, op0, op1, r, w):
        return self.S.op("vector", lambda e: e.scalar_tensor_tensor(out=out, in0=in0, scalar=scalar, in1=in1,
                                                                     op0=op0, op1=op1), r, w)

    def copy(self, eng, out, in_, r, w):
        if eng == "scalar":
            return self.S.op(eng, lambda e: e.copy(out=out, in_=in_), r, w)
        return self.S.op(eng, lambda e: e.tensor_copy(out=out, in_=in_), r, w)

    def recip(self, out, in_, r, w):
        return self.S.op("vector", lambda e: e.reciprocal(out=out, in_=in_), r, w)

    def memset(self, eng, ap, val, w):
        return self.S.op(eng, lambda e: e.memset(ap, val), (), w)

    def reduce(self, out, in_, op, r, w):
        return self.S.op("vector", lambda e: e.tensor_reduce(out=out, in_=in_, axis=AX.X, op=op), r, w)

    def close(self):
        self.S.finalize()
        self.st.close()


class Build:
    def __init__(self, nseq, ext=None):
        self.nc = bass.Bass("TRN2", target_bir_lowering=False)
        self.NSEQ = nseq
        self.t = {}
        self.ext = ext or {}
        self.kinds = {}

    def dram(self, name, shape, dt, kind="Internal"):
        kind = self.ext.get(name, kind)
        self.kinds[name] = (kind, list(shape), dt)
        self.t[name] = self.nc.dram_tensor(name, list(shape), dt, kind=kind).ap()
        return self.t[name]


def rms_to_hnT(P, xt, xkey, gbc, eps_t, stat, skey, junk, hn, hkey, ps_tr, pkey, identb, dst, dkey, sl):
    P.act(junk[:], xt, AF.Square, [xkey], ["junkA", skey], accum_out=stat[:, 0:1])
    P.act(stat[:, 1:2], stat[:, 0:1], AF.Sqrt, [skey, "eps"], [skey], scale=1.0 / D, bias=eps_t[:, 0:1])
    P.recip(stat[:, 2:3], stat[:, 1:2], [skey], [skey])
    P.stt(hn, xt, stat[:, 2:3], gbc, ALU.mult, ALU.mult, [xkey, skey, "gbc"], [hkey])
    for k in range(8):
        P.tr(ps_tr[:, k, :], hn[:, k * 128:(k + 1) * 128], identb[:], [hkey, "identb"], [pkey], mark=(k == 7))
    P.copy("scalar", dst[:, 0:8, sl], ps_tr[:, 0:8, :], [pkey], [dkey])


def phase_A(B):
    nc, d, NSEQ = B.nc, B.t, B.NSEQ
    P = Phase(nc, "A")
    Wt = P.sb("Wt", [128, 8, 2376], BF16)
    Wsrc = d["ab_w_in"].rearrange("(k p) n -> p k n", p=128)
    segs = [(0, 1024, 0), (1024, 1536, 1024), (1664, 2176, 1536), (1536, 1600, 2048), (1536, 1600, 2112),
            (2176, 2240, 2176), (2176, 2240, 2240), (1600, 1664, 2304), (2240, 2248, 2368)]
    WK = []
    for i, (c0, c1, d0) in enumerate(segs):
        P.dma("gpsimd", Wt[:, :, d0:d0 + c1 - c0], Wsrc[:, :, c0:c1], w=[("W", i)], key=("W", i))
        WK.append(("W", i))
    identb = P.sb("identb", [128, 128], BF16)
    P.dma("sync", identb[:], d["ident_bf"], w=["identb"], key="identb")
    gbc = P.sb("gbc", [128, D], F32)
    P.dma("sync", gbc[:], d["g_mix0"], w=["gbc"], key="gbc")
    cos = P.sb("cos", [128, NT, 128], F32)
    sin = P.sb("sin", [128, NT, 128], F32)
    P.dma("sync", cos[:], d["cos128"].rearrange("(t p) c -> p t c", p=128), w=["cos"], key="cos")
    P.dma("sync", sin[:], d["sin128"].rearrange("(t p) c -> p t c", p=128), w=["sin"], key="sin")
    eps_t = P.sb("eps", [128, 1], F32)
    P.memset("vector", eps_t[:], EPS, ["eps"])

    xt = [P.sb("xt%d" % i, [128, D], F32) for i in range(2)]
    junk = P.sb("junk", [128, D], F32)
    stat = [P.sb("stat%d" % i, [128, 4], F32) for i in range(2)]
    hn = [P.sb("hn%d" % i, [128, D], BF16) for i in range(2)]
    hnT = [P.sb("hnT%d" % i, [128, 8, 512], BF16) for i in range(2)]
    ps_tr = [P.ps("ptr%d" % i, [128, 8, 128], BF16) for i in range(2)]
    ps_a = P.ps("psa", [128, 512])
    ps_g = P.ps("psg", [128, 512])
    ps_q = P.ps("psq", [128, 1024])
    ps_c = P.ps("psc", [128, 512])
    sig = [P.sb("sig%d" % i, [128, 512], F32) for i in range(2)]
    hc = [P.sb("hc%d" % i, [128, 512], BF16) for i in range(2)]
    tmp = [P.sb("tmp%d" % i, [128, 128], F32) for i in range(4)]
    qr = [P.sb("qr%d" % i, [128, 1024], BF16) for i in range(2)]
    kr = [P.sb("kr%d" % i, [128, 256], BF16) for i in range(2)]
    vaug = [P.sb("vaug%d" % i, [128, 256], BF16) for i in range(2)]
    iwt = [P.sb("iw%d" % i, [128, 8], F32) for i in range(2)]
    for i in range(2):
        P.memset("gpsimd", vaug[i][:, 64:192], 1.0, [("vaug", i)])
    qTg = [P.sb("qTg%d" % i, [128, 8, 512], BF16) for i in range(2)]
    kTg = [P.sb("kTg%d" % i, [128, 2, 512], BF16) for i in range(2)]
    w_scale = (8 ** -0.5) * (64 ** -0.5)

    def load_x(b, tt):
        xs = tt % 2
        r0 = b * SEQ + tt * 128
        P.dma("sync", xt[xs][:], d["x"][r0:r0 + 128, :], w=[("x", xs)], key=("x", xs))

    n_tiles = NSEQ * NT
    if DEBUG_STOP <= 1:
        P.dma("sync", d["iw"][0:128, :], iwt[0][:], r=[("iw", 0), "cos", "sin", "gbc", "identb"] + WK, key=("iw", 0), is_out=True)
        P.close()
        return
    load_x(0, 0)
    for gi in range(NSEQ * 4):
        b, g = gi // 4, gi % 4
        gs = gi % 2
        if DEBUG_STOP <= 6 and gi >= max(1, DEBUG_STOP - 3):
            break
        for t4 in range(4):
            tt = g * 4 + t4
            ti = gi * 4 + t4
            if ti + 1 < n_tiles and not (DEBUG_STOP <= 6 and ti + 1 >= 4 * max(1, DEBUG_STOP - 3)):
                load_x((ti + 1) // NT, (ti + 1) % NT)
            xs = tt % 2
            rms_to_hnT(P, xt[xs][:], ("x", xs), gbc[:], eps_t, stat[xs], ("st", xs), junk, hn[xs][:], ("hn", xs),
                       ps_tr[0], "ptr0", identb, hnT[gs], ("hnT", gs), slice(t4 * 128, (t4 + 1) * 128))
        if DEBUG_STOP <= 2:
            P.dma("sync", d["qT"][b, :, :, 0:512].rearrange("c p t -> p c t"), hnT[gs][:, 0:4, :], r=[("hnT", gs)], key=("qTg", gs), is_out=True)
            break
        for c in range(4):
            for k in range(8):
                P.mm(ps_a[:, :], Wt[:, k, c * 128:(c + 1) * 128], hnT[gs][:, k, :], k == 0, k == 7,
                     [("hnT", gs)] + WK, ["psa"])
            for k in range(8):
                P.mm(ps_g[:, :], Wt[:, k, 512 + c * 128:512 + (c + 1) * 128], hnT[gs][:, k, :], k == 0, k == 7,
                     [("hnT", gs)] + WK, ["psg"])
            s2 = c % 2
            P.act(sig[s2][:], ps_g[:, :], AF.Sigmoid, ["psg"], [("sig", s2)])
            P.tt("vector", hc[s2][:], ps_a[:, :], sig[s2][:], ALU.mult, ["psa", ("sig", s2)], [("hc", s2)])
            P.dma("sync", d["hconvT"][b, c, :, g * 512:(g + 1) * 512], hc[s2][:], r=[("hc", s2)], key=("hc", s2), is_out=True)
        if DEBUG_STOP <= 3:
            break
        for t4 in range(4):
            tt = g * 4 + t4
            r0 = b * SEQ + tt * 128
            tok = slice(t4 * 128, (t4 + 1) * 128)
            s2 = t4 % 2
            for k in range(8):
                P.mm(ps_q[:, 0:512], hnT[gs][:, k, tok], Wt[:, k, 1024:1536], k == 0, k == 7, [("hnT", gs)] + WK, ["psq"], mark=False)
            for k in range(8):
                P.mm(ps_q[:, 512:1024], hnT[gs][:, k, tok], Wt[:, k, 1536:2048], k == 0, k == 7, [("hnT", gs)] + WK, ["psq"], mark=False)
            for k in range(8):
                P.mm(ps_c[:, 0:328], hnT[gs][:, k, tok], Wt[:, k, 2048:2376], k == 0, k == 7, [("hnT", gs)] + WK, ["psq"])
            for (src, nh, dst, dkey) in ((ps_q[:, 0:512], 8, qr[s2][:, 0:512], ("qr", s2)), (ps_q[:, 512:1024], 8, qr[s2][:, 512:1024], ("qr", s2)),
                                         (ps_c[:, 0:256], 4, kr[s2][:, 0:256], ("kr", s2))):
                sv = src.rearrange("p (h e) -> p h e", e=64)
                dv = dst.rearrange("p (h e) -> p h e", e=64)
                x1, x2 = sv[:, :, 0:8], sv[:, :, 8:16]
                cv = cos[:, tt, 0:nh * 8].rearrange("p (h e) -> p h e", e=8)
                sn = sin[:, tt, 0:nh * 8].rearrange("p (h e) -> p h e", e=8)
                tv = [tmp[i][:, 0:nh * 8].rearrange("p (h e) -> p h e", e=8) for i in range(4)]
                P.tt("vector", tv[0], x1, cv, ALU.mult, ["psq", "cos"], ["tmp0"])
                P.tt("vector", tv[1], x2, sn, ALU.mult, ["psq", "sin"], ["tmp1"])
                P.tt("vector", dv[:, :, 0:8], tv[0], tv[1], ALU.subtract, ["tmp0", "tmp1"], [dkey])
                P.tt("vector", tv[2], x2, cv, ALU.mult, ["psq", "cos"], ["tmp2"])
                P.tt("vector", tv[3], x1, sn, ALU.mult, ["psq", "sin"], ["tmp3"])
                P.tt("vector", dv[:, :, 8:16], tv[2], tv[3], ALU.add, ["tmp2", "tmp3"], [dkey])
                P.copy("scalar", dv[:, :, 16:64], sv[:, :, 16:64], ["psq"], [dkey])
            P.copy("scalar", vaug[s2][:, 0:64], ps_c[:, 256:320], ["psq"], [("vaug", s2)])
            P.copy("scalar", vaug[s2][:, 192:256], ps_c[:, 256:320], ["psq"], [("vaug", s2)])
            P.ts("vector", iwt[s2][:], ps_c[:, 320:328], w_scale, ALU.mult, ["psq"], [("iw", s2)])
            P.dma("sync", d["vaug"][r0:r0 + 128, :], vaug[s2][:], r=[("vaug", s2)], key=("vaug", s2), is_out=True)
            P.dma("sync", d["iw"][r0:r0 + 128, :], iwt[s2][:], r=[("iw", s2)], key=("iw", s2), is_out=True)
            for k in range(8):
                P.tr(ps_tr[1][:, k, :], qr[s2][:, k * 128:(k + 1) * 128], identb[:], [("qr", s2), "identb"], ["ptr1"], mark=(k == 7))
            P.copy("scalar", qTg[gs][:, 0:8, tok], ps_tr[1][:, 0:8, :], ["ptr1"], [("qTg", gs)])
            for k in range(2):
                P.tr(ps_tr[1][:, k, :], kr[s2][:, k * 128:(k + 1) * 128], identb[:], [("kr", s2), "identb"], ["ptr1"], mark=(k == 1))
            P.copy("scalar", kTg[gs][:, 0:2, tok], ps_tr[1][:, 0:2, :], ["ptr1"], [("kTg", gs)])
        gsl = slice(g * 512, (g + 1) * 512)
        P.dma("sync", d["qT"][b, :, :, gsl].rearrange("c p t -> p c t"), qTg[gs][:, 0:4, :], r=[("qTg", gs)], key=("qTg", gs), is_out=True)
        P.dma("sync", d["iqT"][b, :, :, gsl].rearrange("c p t -> p c t"), qTg[gs][:, 4:8, :], r=[("qTg", gs)], key=("iqTg", gs), is_out=True)
        P.dma("sync", d["kkT"][b, :, :, gsl].rearrange("c p t -> p c t"), kTg[gs][:, :, :], r=[("kTg", gs)], key=("kTg", gs), is_out=True)
    P.close()


def phase_B(B):
    nc, d, NSEQ = B.nc, B.t, B.NSEQ
    P = Phase(nc, "B")
    identb = P.sb("identb", [128, 128], BF16)
    P.dma("sync", identb[:], d["ident_bf"], w=["identb"], key="identb")
    identf = P.sb("identf", [128, 128], F32)
    P.dma("sync", identf[:], d["ident_f32"], w=["identf"], key="identf")
    ones = P.sb("ones", [128, 128], F32)
    P.memset("vector", ones[:], 1.0, ["ones"])
    eps_t = P.sb("eps", [128, 1], F32)
    P.memset("vector", eps_t[:], EPS, ["eps"])
    cw = P.sb("cw", [31, 512], F32)
    P.dma("sync", cw[:], d["ab_conv_w"], w=["cw"], key="cw")
    prm = P.sb("prm", [128, 12], F32)
    P.dma("sync", prm[:], d["conv_prm"], w=["prm"], key="prm")
    ps_w = P.ps("psw", [128, 4, 32])
    for c in range(4):
        P.tr(ps_w[:, c, 0:31], cw[0:31, c * 128:(c + 1) * 128], identf[0:31, 0:31], ["cw", "identf"], ["psw"], mark=(c == 3))
    wT = P.sb("wT", [128, 4, 32], F32)
    P.copy("vector", wT[:, :, 0:31], ps_w[:, :, 0:31], ["psw"], ["wT"])
    diag = P.sb("diag", [128, 124, 128], BF16)
    for c in range(4):
        for j in range(31):
            i = c * 31 + j
            P.ts("vector" if i % 2 == 0 else "gpsimd", diag[:, i, :], identb[:], wT[:, c, j:j + 1], ALU.mult,
                 ["identb", "wT"], [("diag", c)])
    hbuf = P.sb("hbuf", [128, 4, 30 + SEQ], BF16)
    P.memset("gpsimd", hbuf[:, :, 0:30], 0.0, ["hbuf"])
    ps_c = [P.ps("psc%d" % i, [128, 512]) for i in range(2)]
    ps_s1 = P.ps("ps1", [128, 512])
    ps_s2 = P.ps("ps2", [128, 512])
    hcs = P.sb("hcs", [128, 4, 512], F32)
    sqs = P.sb("sqs", [128, 4, 512], F32)
    m = P.sb("m", [128, 512], F32)
    msq = P.sb("msq", [128, 512], F32)
    var = P.sb("var", [128, 512], F32)
    rstd = P.sb("rstd", [128, 512], F32)
    z = [P.sb("z%d" % i, [128, 512], F32) for i in range(2)]
    yst = [P.sb("yst%d" % i, [128, 4, 512], BF16) for i in range(2)]
    for b in range(NSEQ):
        for c in range(4):
            P.dma("sync", hbuf[:, c, 30:30 + SEQ], d["hconvT"][b, c, :, :], w=["hbuf"], key=("hbuf", c))
        for g in range(4):
            ys = g % 2
            for c in range(4):
                pc = ps_c[c % 2]
                pk = ("psc", c % 2)
                for j in range(31):
                    P.mm(pc[:, :], diag[:, c * 31 + j, :], hbuf[:, c, g * 512 + j:g * 512 + j + 512], j == 0, j == 30,
                         [("diag", c), "hbuf"], [pk])
                P.act(hcs[:, c, :], pc[:, :], AF.Identity, [pk, "prm"], [("hcs", c)], bias=prm[:, c:c + 1], scale=1.0)
                P.act(sqs[:, c, :], pc[:, :], AF.Square, [pk, "prm"], [("sqs", c)], bias=prm[:, c:c + 1], scale=1.0)
            for c in range(4):
                P.mm(ps_s1[:, :], ones[:], hcs[:, c, :], c == 0, c == 3, [("hcs", c), "ones"], ["ps1"])
            for c in range(4):
                P.mm(ps_s2[:, :], ones[:], sqs[:, c, :], c == 0, c == 3, [("sqs", c), "ones"], ["ps2"])
            P.act(m[:], ps_s1[:, :], AF.Copy, ["ps1"], ["m"], scale=1.0 / 512)
            P.tt("gpsimd", msq[:], m[:], m[:], ALU.mult, ["m"], ["msq"])
            P.stt(var[:], ps_s2[:, :], 1.0 / 512, msq[:], ALU.mult, ALU.subtract, ["ps2", "msq"], ["var"])
            P.act(var[:], var[:], AF.Sqrt, ["var", "eps"], ["var"], bias=eps_t[:, 0:1], scale=1.0)
            P.recip(rstd[:], var[:], ["var"], ["rstd"])
            for c in range(4):
                zz = z[c % 2]
                zk = ("z", c % 2)
                P.tt("gpsimd", zz[:], hcs[:, c, :], m[:], ALU.subtract, [("hcs", c), "m"], [zk])
                P.tt("vector", zz[:], zz[:], rstd[:], ALU.mult, [zk, "rstd"], [zk])
                P.act(yst[ys][:, c, :], zz[:], AF.Silu, [zk, "prm"], [("yst", ys)], scale=prm[:, 4 + c:5 + c], bias=prm[:, 8 + c:9 + c])
            P.dma("sync", d["yabT"][b, 0:4, :, g * 512:(g + 1) * 512].rearrange("c p t -> p c t"), yst[ys][:],
                  r=[("yst", ys)], key=("yst", ys), is_out=True)
    P.close()


def phase_C(B):
    nc, d, NSEQ = B.nc, B.t, B.NSEQ
    P = Phase(nc, "C")
    identb = P.sb("identb", [128, 128], BF16)
    P.dma("sync", identb[:], d["ident_bf"], w=["identb"], key="identb")
    negm = P.sb("negm", [128, 128], F32)
    P.dma("sync", negm[:], d["negmask"], w=["negm"], key="negm")
    pow2 = P.sb("pow2", [128, NIT], F32)
    P.dma("sync", pow2[:], d["pow2"], w=["pow2"], key="pow2")
    thr0 = P.sb("thr0", [128, 1], F32)
    P.memset("vector", thr0[:], -1e29, ["thr0"])
    qT = P.sb("qT", [128, 4, SEQ], BF16)
    iqT = P.sb("iqT", [128, 4, SEQ], BF16)
    kkT = P.sb("kkT", [128, 2, SEQ], BF16)
    vaug = P.sb("vaug", [128, NT, 256], BF16)
    iw = P.sb("iw", [128, NT, 8], F32)
    ybT = P.sb("ybT", [128, 4, SEQ], BF16)
    maskT = [P.sb("maskT%d" % i, [128, NT, 512], BF16) for i in range(2)]
    score = [P.sb("score%d" % i, [128, SEQ], F32) for i in range(2)]
    junk = P.sb("junk", [128, SEQ], BF16)
    rl = [P.sb("rl%d" % i, [128, 512], F32) for i in range(2)]
    mk = [P.sb("mk%d" % i, [128, SEQ], BF16) for i in range(2)]
    st = [P.sb("st%d" % i, [128, 8 + NIT], F32) for i in range(2)]
    pe = [P.sb("pe%d" % i, [128, 512], BF16) for i in range(2)]
    pm = [P.sb("pm%d" % i, [128, 512], BF16) for i in range(2)]
    rc = [P.sb("rc%d" % i, [128, 512], F32) for i in range(2)]
    ps_lg = [P.ps("plg%d" % i, [128, 512]) for i in range(2)]
    ps_tr = [P.ps("ptr%d" % i, [128, 8, 128], BF16) for i in range(2)]
    ps_s = [P.ps("pss%d" % i, [128, 512]) for i in range(2)]
    ps_o = [P.ps("pso%d" % i, [128, 512]) for i in range(2)]
    att_scale = 64 ** -0.5
    n_lg = 0
    n_s = 0
    n_o = 0
    n_tr = 0
    for b in range(NSEQ):
        rs = slice(b * SEQ, (b + 1) * SEQ)
        P.dma("sync", qT[:], d["qT"][b].rearrange("c p t -> p c t"), w=["qT"], key="qT")
        P.dma("sync", iqT[:], d["iqT"][b].rearrange("c p t -> p c t"), w=["iqT"], key="iqT")
        P.dma("sync", kkT[:], d["kkT"][b].rearrange("c p t -> p c t"), w=["kkT"], key="kkT")
        P.dma("sync", vaug[:], d["vaug"][rs, :].rearrange("(t p) c -> p t c", p=128), w=["vaug"], key="vaug")
        P.dma("sync", iw[:], d["iw"][rs, :].rearrange("(t p) c -> p t c", p=128), w=["iw"], key="iw")
        for G in range(4):
            mG = G % 2
            for q4 in range(4):
                qb = 4 * G + q4
                nk = 128 * (qb + 1)
                nkb = (nk + 511) // 512
                s2 = qb % 2
                sc = score[s2]
                sk = ("score", s2)
                qsl = slice(qb * 128, (qb + 1) * 128)
                for h in range(8):
                    c, base = h // 2, 64 * (h % 2)
                    for kb in range(nkb):
                        n = min(512, nk - kb * 512)
                        r = n_lg % 2
                        n_lg += 1
                        P.mm(ps_lg[r][:, 0:n], iqT[base:base + 64, c, qsl], kkT[base:base + 64, 1, kb * 512:kb * 512 + n],
                             True, True, ["iqT", "kkT"], [("plg", r)])
                        P.act(rl[r][:, 0:n], ps_lg[r][:, 0:n], AF.Relu, [("plg", r)], [("rl", r)])
                        dst = sc[:, kb * 512:kb * 512 + n]
                        if h == 0:
                            P.ts("vector", dst, rl[r][:, 0:n], iw[:, qb, 0:1], ALU.mult, [("rl", r), "iw"], [sk])
                        else:
                            P.stt(dst, rl[r][:, 0:n], iw[:, qb, h:h + 1], dst, ALU.mult, ALU.add, [("rl", r), "iw", sk], [sk])
                stt_ = st[s2]
                stk = ("st", s2)
                if qb >= 2:
                    P.reduce(stt_[:, 0:1], sc[:, 0:nk], ALU.max, [sk], [stk])
                    P.reduce(stt_[:, 1:2], sc[:, 0:nk], ALU.min, [sk], [stk])
                P.tt("vector", sc[:, qsl], sc[:, qsl], negm[:], ALU.add, [sk, "negm"], [sk])
                if qb >= 2:
                    P.tt("vector", stt_[:, 2:3], stt_[:, 0:1], stt_[:, 1:2], ALU.subtract, [stk], [stk])
                    P.ts("vector", stt_[:, 8:8 + NIT], pow2[:], stt_[:, 2:3], ALU.mult, ["pow2", stk], [stk])
                    lo = stt_[:, 1:2]
                    for i in range(NIT):
                        stp = stt_[:, 8 + i:9 + i]
                        P.tt("vector", stt_[:, 3:4], lo, stp, ALU.add, [stk], [stk])
                        P.ts("vector", junk[:, 0:nk], sc[:, 0:nk], stt_[:, 3:4], ALU.is_ge, [sk, stk], ["junk", stk],
                             s2=0.0, op1=ALU.add, accum_out=stt_[:, 4:5])
                        P.stt(stt_[:, 5:6], stt_[:, 4:5], TOPK - 0.5, stp, ALU.is_ge, ALU.mult, [stk], [stk])
                        P.tt("vector", lo, lo, stt_[:, 5:6], ALU.add, [stk], [stk])
                    thr = lo
                else:
                    thr = thr0[:, 0:1]
                P.ts("vector", mk[s2][:, 0:nk], sc[:, 0:nk], thr, ALU.is_ge, [sk, stk, "thr0"], [("mk", s2)])
                kt = 0
                while kt <= qb:
                    n = min(8, qb + 1 - kt)
                    r = n_tr % 2
                    n_tr += 1
                    for j in range(n):
                        P.tr(ps_tr[r][:, j, :], mk[s2][:, (kt + j) * 128:(kt + j + 1) * 128], identb[:], [("mk", s2), "identb"],
                             [("ptr", r)], mark=(j == n - 1))
                    P.copy("scalar", maskT[mG][:, kt:kt + n, q4 * 128:(q4 + 1) * 128], ps_tr[r][:, 0:n, :], [("ptr", r)], [("maskT", mG)])
                    kt += n
            nkt = 4 * G + 4
            gsl0 = G * 512
            for c in range(4):
                for e in range(2):
                    base = 64 * e
                    o = n_o % 2
                    n_o += 1
                    for kt in range(nkt):
                        q0 = max(0, kt - 4 * G) * 128
                        N = 512 - q0
                        r = n_s % 2
                        n_s += 1
                        P.mm(ps_s[r][:, 0:N], kkT[base:base + 64, 0, kt * 128:(kt + 1) * 128], qT[base:base + 64, c, gsl0 + q0:gsl0 + 512],
                             True, True, ["kkT", "qT"], [("pss", r)])
                        P.act(pe[r][:, 0:N], ps_s[r][:, 0:N], AF.Exp, [("pss", r)], [("pe", r)], scale=att_scale)
                        P.tt("gpsimd", pm[r][:, 0:N], pe[r][:, 0:N], maskT[mG][:, kt, q0:512], ALU.mult, [("pe", r), ("maskT", mG)], [("pm", r)])
                        P.mm(ps_o[o][:, q0:512], vaug[:, kt, e * 128:(e + 1) * 128], pm[r][:, 0:N], kt == 0, kt == nkt - 1,
                             [("pm", r), "vaug"], [("pso", o)])
                    so, ss_ = (slice(0, 64), slice(64, 128)) if e == 0 else (slice(64, 128), slice(0, 64))
                    P.recip(rc[o][ss_, :], ps_o[o][ss_, :], [("pso", o)], [("rc", o)])
                    P.tt("vector", ybT[so, c, gsl0:gsl0 + 512], ps_o[o][so, :], rc[o][ss_, :], ALU.mult, [("pso", o), ("rc", o)], ["ybT"])
        P.dma("sync", d["yabT"][b, 4:8, :, :].rearrange("c p t -> p c t"), ybT[:], r=["ybT"], key="ybT", is_out=True)
    P.close()


def phase_D(B, tag, yT, wname, resid, gname, hmid, hnT_name):
    nc, d, NSEQ = B.nc, B.t, B.NSEQ
    P = Phase(nc, tag)
    Wo = P.sb("Wo", [128, 8, D], BF16)
    P.dma("gpsimd", Wo[:], d[wname].rearrange("(k p) n -> p k n", p=128), w=["Wo"], key="Wo")
    identb = P.sb("identb", [128, 128], BF16)
    P.dma("sync", identb[:], d["ident_bf"], w=["identb"], key="identb")
    gbc = P.sb("gbc", [128, D], F32)
    P.dma("sync", gbc[:], d[gname], w=["gbc"], key="gbc")
    eps_t = P.sb("eps", [128, 1], F32)
    P.memset("vector", eps_t[:], EPS, ["eps"])
    yt = [P.sb("yt%d" % i, [128, 8, 512], BF16) for i in range(2)]
    xt = [P.sb("xt%d" % i, [128, D], F32) for i in range(2)]
    hm = [P.sb("hm%d" % i, [128, D], F32) for i in range(2)]
    junk = P.sb("junk", [128, D], F32)
    stat = [P.sb("stat%d" % i, [128, 4], F32) for i in range(2)]
    hn = [P.sb("hn%d" % i, [128, D], BF16) for i in range(2)]
    hnTg = [P.sb("hnTg%d" % i, [128, 8, 512], BF16) for i in range(2)]
    ps_o = [P.ps("pso%d" % i, [128, 1024]) for i in range(2)]
    ps_tr = [P.ps("ptr%d" % i, [128, 8, 128], BF16) for i in range(2)]
    ngr = NSEQ * 4

    def load_y(gi):
        b, g = gi // 4, gi % 4
        P.dma("sync", yt[gi % 2][:], d[yT][b, :, :, g * 512:(g + 1) * 512].rearrange("c p t -> p c t"), w=[("yt", gi % 2)], key=("yt", gi % 2))

    load_y(0)
    for gi in range(ngr):
        b, g = gi // 4, gi % 4
        gs = gi % 2
        if gi + 1 < ngr:
            load_y(gi + 1)
        for t4 in range(4):
            tt = g * 4 + t4
            r0 = b * SEQ + tt * 128
            tok = slice(t4 * 128, (t4 + 1) * 128)
            s2 = t4 % 2
            P.dma("sync", xt[s2][:], d[resid][r0:r0 + 128, :], w=[("x", s2)], key=("x", s2))
            for half in range(2):
                for c in range(8):
                    P.mm(ps_o[s2][:, half * 512:(half + 1) * 512], yt[gs][:, c, tok], Wo[:, c, half * 512:(half + 1) * 512],
                         c == 0, c == 7, [("yt", gs), "Wo"], [("pso", s2)], mark=(c == 7 and half == 1))
            for half in range(2):
                hs_ = slice(half * 512, (half + 1) * 512)
                P.tt("vector", hm[s2][:, hs_], ps_o[s2][:, hs_], xt[s2][:, hs_], ALU.add, [("pso", s2), ("x", s2)], [("hm", s2)])
            P.dma("sync", d[hmid][r0:r0 + 128, :], hm[s2][:], r=[("hm", s2)], key=("hm", s2), is_out=True)
            rms_to_hnT(P, hm[s2][:], ("hm", s2), gbc[:], eps_t, stat[s2], ("st", s2), junk, hn[s2][:], ("hn", s2),
                       ps_tr[s2], ("ptr", s2), identb, hnTg[gs], ("hnTg", gs), tok)
            if DEBUG_STOP == 77 and gi == 0:
                if t4 == 0:
                    dlog = P.sb("dlog", [128, 16], F32)
                    dhn = P.sb("dhn", [128, 4, D], BF16)
                P.copy("gpsimd", dlog[:, t4 * 4:(t4 + 1) * 4], stat[s2][:, 0:4], [("st", s2), ("hn", s2)], ["dlog"])
                P.copy("gpsimd", dhn[:, t4, :], hn[s2][:], [("hn", s2)], ["dhn"])
                if t4 == 3:
                    P.dma("sync", d["dbg"], dlog[:], r=["dlog"], key="dlog", is_out=True)
                    P.dma("sync", d["dbg2"].rearrange("t p c -> p t c"), dhn[:], r=["dhn"], key="dhn", is_out=True)
        P.dma("sync", d[hnT_name][:, :, gi * 512:(gi + 1) * 512].rearrange("c p t -> p c t"), hnTg[gs][:], r=[("hnTg", gs)],
              key=("hnTg", gs), is_out=True)
    P.close()


def phase_F(B, tag, layer, f0, h_in, h_out, hnT_name, final_g=None):
    nc, d, NSEQ = B.nc, B.t, B.NSEQ
    P = Phase(nc, tag)
    nf = 11
    Wg = P.sb("Wg", [128, 8, nf * 128], BF16)
    Wu = P.sb("Wu", [128, 8, nf * 128], BF16)
    Wd = P.sb("Wd", [128, nf, D], BF16)
    cs = slice(f0 * 128, (f0 + nf) * 128)
    P.dma("gpsimd", Wg[:], d["ffn_w_gate%d" % layer].rearrange("(k p) n -> p k n", p=128)[:, :, cs], w=["Wg"], key="Wg")
    P.dma("gpsimd", Wu[:], d["ffn_w_up%d" % layer].rearrange("(k p) n -> p k n", p=128)[:, :, cs], w=["Wu"], key="Wu")
    P.dma("gpsimd", Wd[:], d["ffn_w_down%d" % layer].rearrange("(f p) n -> p f n", p=128)[:, f0:f0 + nf, :], w=["Wd"], key="Wd")
    if final_g is not None:
        gbc = P.sb("gbc", [128, D], F32)
        P.dma("sync", gbc[:], d[final_g], w=["gbc"], key="gbc")
        eps_t = P.sb("eps", [128, 1], F32)
        P.memset("vector", eps_t[:], EPS, ["eps"])
        junk = P.sb("junk", [128, D], F32)
        stat = [P.sb("stat%d" % i, [128, 4], F32) for i in range(2)]
    hnTg = [P.sb("hnTg%d" % i, [128, 8, 512], BF16) for i in range(2)]
    actT = [P.sb("actT%d" % i, [128, nf, 512], BF16) for i in range(2)]
    sg = [P.sb("sg%d" % i, [128, 512], F32) for i in range(2)]
    hin = [P.sb("hin%d" % i, [128, D], F32) for i in range(2)]
    hout = [P.sb("hout%d" % i, [128, D], F32) for i in range(2)]
    ps_g = [P.ps("psg%d" % i, [128, 512]) for i in range(2)]
    ps_u = [P.ps("psu%d" % i, [128, 512]) for i in range(2)]
    ps_d = [P.ps("psd%d" % i, [128, 1024]) for i in range(2)]
    ngr = NSEQ * 4

    def load_h(gi):
        P.dma("sync", hnTg[gi % 2][:], d[hnT_name][:, :, gi * 512:(gi + 1) * 512].rearrange("c p t -> p c t"),
              w=[("hnTg", gi % 2)], key=("hnTg", gi % 2))

    load_h(0)
    for gi in range(ngr):
        gs = gi % 2
        if gi + 1 < ngr:
            load_h(gi + 1)
        for f in range(nf):
            s2 = f % 2
            for k in range(8):
                P.mm(ps_g[s2][:, :], Wg[:, k, f * 128:(f + 1) * 128], hnTg[gs][:, k, :], k == 0, k == 7, [("hnTg", gs), "Wg"], [("psg", s2)])
            for k in range(8):
                P.mm(ps_u[s2][:, :], Wu[:, k, f * 128:(f + 1) * 128], hnTg[gs][:, k, :], k == 0, k == 7, [("hnTg", gs), "Wu"], [("psu", s2)])
            P.act(sg[s2][:], ps_g[s2][:, :], AF.Silu, [("psg", s2)], [("sg", s2)])
            P.tt("vector", actT[gs][:, f, :], ps_u[s2][:, :], sg[s2][:], ALU.mult, [("psu", s2), ("sg", s2)], [("actT", gs)])
        for t4 in range(4):
            r0 = gi * 512 + t4 * 128
            tok = slice(t4 * 128, (t4 + 1) * 128)
            s2 = t4 % 2
            P.dma("sync", hin[s2][:], d[h_in][r0:r0 + 128, :], w=[("hin", s2)], key=("hin", s2))
            for half in range(2):
                for f in range(nf):
                    P.mm(ps_d[s2][:, half * 512:(half + 1) * 512], actT[gs][:, f, tok], Wd[:, f, half * 512:(half + 1) * 512],
                         f == 0, f == nf - 1, [("actT", gs), "Wd"], [("psd", s2)], mark=(f == nf - 1 and half == 1))
            for half in range(2):
                hs_ = slice(half * 512, (half + 1) * 512)
                P.tt("vector", hout[s2][:, hs_], ps_d[s2][:, hs_], hin[s2][:, hs_], ALU.add, [("psd", s2), ("hin", s2)], [("hout", s2)])
            if final_g is not None:
                sk = ("st", s2)
                P.act(junk[:], hout[s2][:], AF.Square, [("hout", s2)], ["junkA", sk], accum_out=stat[s2][:, 0:1])
                P.act(stat[s2][:, 1:2], stat[s2][:, 0:1], AF.Sqrt, [sk], [sk], scale=1.0 / D, bias=eps_t[:, 0:1])
                P.recip(stat[s2][:, 2:3], stat[s2][:, 1:2], [sk], [sk])
                P.stt(hout[s2][:], hout[s2][:], stat[s2][:, 2:3], gbc[:], ALU.mult, ALU.mult, [("hout", s2), sk, "gbc"], [("hout", s2)])
            P.dma("sync", d[h_out][r0:r0 + 128, :], hout[s2][:], r=[("hout", s2)], key=("hout", s2), is_out=True)
    P.close()


def phase_E(B):
    nc, d, NSEQ = B.nc, B.t, B.NSEQ
    P = Phase(nc, "E")
    Wc = P.sb("Wc", [128, 8, 3104], BF16)
    Wsrc = d["c_w_in"].rearrange("(k p) n -> p k n", p=128)
    WK = []
    P.memset("gpsimd", Wc[:, :, 3088:3104], 0.0, [("W", 2)])
    for i, (c0, c1) in enumerate(((0, 1024), (1024, 2048), (2048, 3088))):
        P.dma("gpsimd", Wc[:, :, c0:c1], Wsrc[:, :, c0:c1], w=[("W", i)], key=("W", i))
        WK.append(("W", i))
    identb = P.sb("identb", [128, 128], BF16)
    P.dma("sync", identb[:], d["ident_bf"], w=["identb"], key="identb")
    gbc = P.sb("gbc", [128, D], F32)
    P.dma("sync", gbc[:], d["g_mix1"], w=["gbc"], key="gbc")
    g2w = P.sb("g2w", [128, 512], F32)
    P.memset("vector", g2w[:], 0.0, ["g2w"])
    P.dma("sync", g2w[0:16, :], d["c_gate_w"], w=["g2w"], key="g2w")
    g2b = P.sb("g2b", [128, 512], F32)
    P.dma("sync", g2b[:], d["c_gate_b_bc"], w=["g2b"], key="g2b")
    eps_t = P.sb("eps", [128, 1], F32)
    P.memset("vector", eps_t[:], EPS, ["eps"])
    one_t = P.sb("one", [128, 1], F32)
    P.memset("vector", one_t[:], 1.0, ["one"])
    xt = [P.sb("xt%d" % i, [128, D], F32) for i in range(2)]
    junk = P.sb("junk", [128, D], F32)
    stat = [P.sb("stat%d" % i, [128, 4], F32) for i in range(2)]
    hn = [P.sb("hn%d" % i, [128, D], BF16) for i in range(2)]
    hnT = [P.sb("hnT%d" % i, [128, 8, 512], BF16) for i in range(2)]
    qkT = [P.sb("qkT%d" % i, [128, 8, 512], BF16) for i in range(2)]
    glrT = [P.sb("glrT%d" % i, [128, 512], F32) for i in range(2)]
    for i in range(2):
        P.memset("vector", glrT[i][:], 0.0, [("glrT", i)])
    kst = [P.sb("kst%d" % i, [128, 512], BF16) for i in range(2)]
    vst = [P.sb("vst%d" % i, [128, 1024], BF16) for i in range(2)]
    rst = [P.sb("rst%d" % i, [128, 1024], F32) for i in range(2)]
    lat = [P.sb("lat%d" % i, [128, 512], F32) for i in range(2)]
    lt1 = P.sb("lt1", [128, 512], F32)
    lt2 = P.sb("lt2", [128, 512], F32)
    ps_tr = P.ps("ptr", [128, 8, 128], BF16)
    ps_f = [P.ps("psf%d" % i, [128, 512]) for i in range(2)]
    ps_t = [P.ps("pst%d" % i, [128, 512]) for i in range(3)]
    ps_l = P.ps("psl", [128, 512])
    n_t = 0
    n_f = 0
    n_tiles = NSEQ * NT

    def load_x(ti):
        P.dma("sync", xt[ti % 2][:], d["h1"][ti * 128:(ti + 1) * 128, :], w=[("x", ti % 2)], key=("x", ti % 2))

    load_x(0)
    for gi in range(NSEQ * 4):
        b, g = gi // 4, gi % 4
        gs = gi % 2
        for t4 in range(4):
            ti = gi * 4 + t4
            if ti + 1 < n_tiles:
                load_x(ti + 1)
            xs = ti % 2
            rms_to_hnT(P, xt[xs][:], ("x", xs), gbc[:], eps_t, stat[xs], ("st", xs), junk, hn[xs][:], ("hn", xs),
                       ps_tr, "ptr", identb, hnT[gs], ("hnT", gs), slice(t4 * 128, (t4 + 1) * 128))
        for oc in range(8):
            r = n_f % 2
            n_f += 1
            for k in range(8):
                P.mm(ps_f[r][:, :], Wc[:, k, oc * 128:(oc + 1) * 128], hnT[gs][:, k, :], k == 0, k == 7, [("hnT", gs)] + WK, [("psf", r)])
            if oc < 4:
                P.act(qkT[gs][:, oc, :], ps_f[r][:, :], AF.Copy, [("psf", r)], [("qkT", gs)], scale=128 ** -0.5)
            else:
                P.copy("vector", qkT[gs][:, oc, :], ps_f[r][:, :], [("psf", r)], [("qkT", gs)])
        r = n_f % 2
        n_f += 1
        for k in range(8):
            P.mm(ps_f[r][0:32, :], Wc[:, k, 3072:3104], hnT[gs][:, k, :], k == 0, k == 7, [("hnT", gs)] + WK, [("psf", r)])
        P.copy("vector", glrT[gs][0:32, :], ps_f[r][0:32, :], [("psf", r)], [("glrT", gs)])
        gsl = slice(g * 512, (g + 1) * 512)
        P.dma("sync", d["c_qT"][b, :, :, gsl].rearrange("c p t -> p c t"), qkT[gs][:, 0:4, :], r=[("qkT", gs)], key=("qTo", gs), is_out=True)
        P.dma("sync", d["c_kT"][b, :, :, gsl].rearrange("c p t -> p c t"), qkT[gs][:, 4:8, :], r=[("qkT", gs)], key=("kTo", gs), is_out=True)
        for t4 in range(4):
            r0 = gi * 512 + t4 * 128
            tok = slice(t4 * 128, (t4 + 1) * 128)
            s2 = t4 % 2
            jobs = [(512, 1024, kst[s2][:, :], ("kst", s2), "k"),
                    (1024, 1536, vst[s2][:, 0:512], ("vst", s2), "v"), (1536, 2048, vst[s2][:, 512:1024], ("vst", s2), "v"),
                    (2048, 2560, rst[s2][:, 0:512], ("rst", s2), "r"), (2560, 3072, rst[s2][:, 512:1024], ("rst", s2), "r")]
            for (c0, c1, dst, dk, kind) in jobs:
                r = n_t % 3
                n_t += 1
                for k in range(8):
                    P.mm(ps_t[r][:, :], hnT[gs][:, k, tok], Wc[:, k, c0:c1], k == 0, k == 7, [("hnT", gs)] + WK, [("pst", r)])
                if kind == "r":
                    P.act(dst, ps_t[r][:, :], AF.Silu, [("pst", r)], [dk])
                elif kind == "v":
                    P.copy("vector", dst, ps_t[r][:, :], [("pst", r)], [dk])
                else:
                    P.copy("scalar", dst, ps_t[r][:, :], [("pst", r)], [dk])
            P.dma("sync", d["c_ktok"][r0:r0 + 128, :], kst[s2][:], r=[("kst", s2)], key=("kst", s2), is_out=True)
            P.dma("sync", d["c_v"][r0:r0 + 128, :], vst[s2][:], r=[("vst", s2)], key=("vst", s2), is_out=True)
            P.dma("sync", d["c_sr"][r0:r0 + 128, :], rst[s2][:], r=[("rst", s2)], key=("rst", s2), is_out=True)
            P.mm(ps_l[:, :], glrT[gs][:, tok], g2w[:, :], True, True, [("glrT", gs), "g2w"], ["psl"])
            P.tt("vector", lt1[:], ps_l[:, :], g2b[:], ALU.add, ["psl", "g2b"], ["lt1"])
            P.act(lt2[:], lt1[:], AF.Exp, ["lt1"], ["lt2"], scale=-1.0)
            P.act(lt1[:], lt2[:], AF.Ln, ["lt2", "one"], ["lt1"], bias=one_t[:, 0:1], scale=1.0)
            P.ts("gpsimd", lat[s2][:], lt1[:], -1.0 / 16.0, ALU.mult, ["lt1"], [("lat", s2)])
            P.dma("sync", d["c_la"][r0:r0 + 128, :], lat[s2][:], r=[("lat", s2)], key=("lat", s2), is_out=True)
    P.close()


def phase_G(B):
    nc, d, NSEQ = B.nc, B.t, B.NSEQ
    P = Phase(nc, "G")
    identb = P.sb("identb", [128, 128], BF16)
    P.dma("sync", identb[:], d["ident_bf"], w=["identb"], key="identb")
    U = P.sb("U", [128, 128], F32)
    SL = P.sb("SL", [128, 128], F32)
    P.dma("sync", U[:], d["tri_u"], w=["U"], key="U")
    P.dma("sync", SL[:], d["tri_sl"], w=["SL"], key="SL")
    gon = P.sb("gon", [128, D], F32)
    P.dma("sync", gon[:], d["c_onorm_g_bc"], w=["gon"], key="gon")
    eps_t = P.sb("eps", [128, 1], F32)
    P.memset("vector", eps_t[:], EPS, ["eps"])
    qT = P.sb("qT", [128, 4, SEQ], BF16)
    kT = P.sb("kT", [128, 4, SEQ], BF16)
    la = [P.sb("la%d" % i, [128, 512], F32) for i in range(2)]
    kt_ = [P.sb("ktk%d" % i, [128, 512], BF16) for i in range(2)]
    vt = [P.sb("vt%d" % i, [128, 1024], BF16) for i in range(2)]
    sr = [P.sb("sr%d" % i, [128, 1024], F32) for i in range(2)]
    eb = [P.sb("eb%d" % i, [128, 512], F32) for i in range(2)]
    enb = P.sb("enb", [128, 512], F32)
    erv = P.sb("erv", [128, 512], F32)
    qtT = [P.sb("qtT%d" % i, [128, 512], BF16) for i in range(2)]
    ktT = [P.sb("ktT%d" % i, [128, 512], BF16) for i in range(2)]
    ks = [P.sb("ks%d" % i, [128, 512], BF16) for i in range(2)]
    attm = [P.sb("attm%d" % i, [128, 128], BF16) for i in range(2)]
    state = P.sb("state", [128, 4, 256], F32)
    stb = P.sb("stb", [128, 4, 256], BF16)
    gr = [P.sb("gr%d" % i, [128, 1024], F32) for i in range(2)]
    stat = [P.sb("stat%d" % i, [128, 12], F32) for i in range(2)]
    junk = P.sb("junk", [128, 256], F32)
    yc = [P.sb("yc%d" % i, [128, 1024], BF16) for i in range(2)]
    ycT = [P.sb("ycT%d" % i, [128, 8, 512], BF16) for i in range(2)]
    ps_b = P.ps("psb", [128, 512])
    ps_r = P.ps("psr", [128, 512])
    ps_a = P.ps("psa", [128, 4, 128])
    ps_o = P.ps("pso", [128, 1024])
    ps_kv = P.ps("pskv", [128, 1024])
    ps_tr = P.ps("ptr", [128, 8, 128], BF16)
    nch = NSEQ * NT

    def load_chunk(ci):
        s2 = ci % 2
        r0 = ci * 128
        P.dma("sync", la[s2][:], d["c_la"][r0:r0 + 128, :], w=[("la", s2)], key=("la", s2))
        P.dma("sync", kt_[s2][:], d["c_ktok"][r0:r0 + 128, :], w=[("ktk", s2)], key=("ktk", s2))
        P.dma("sync", vt[s2][:], d["c_v"][r0:r0 + 128, :], w=[("vt", s2)], key=("vt", s2))
        P.dma("sync", sr[s2][:], d["c_sr"][r0:r0 + 128, :], w=[("sr", s2)], key=("sr", s2))

    load_chunk(0)
    for ci in range(nch):
        b, c = ci // NT, ci % NT
        s2 = ci % 2
        if c == 0:
            P.dma("sync", qT[:], d["c_qT"][b].rearrange("c p t -> p c t"), w=["qT"], key="qT")
            P.dma("sync", kT[:], d["c_kT"][b].rearrange("c p t -> p c t"), w=["kT"], key="kT")
            P.memset("vector", state[:], 0.0, [("state", h) for h in range(4)])
            P.memset("gpsimd", stb[:], 0.0, [("stb", h) for h in range(4)])
        if ci + 1 < nch:
            load_chunk(ci + 1)
        csl = slice(c * 128, (c + 1) * 128)
        for h in range(4):
            P.mm(ps_b[:, h * 128:(h + 1) * 128], la[s2][:, h * 128:(h + 1) * 128], U[:], True, True, [("la", s2), "U"], ["psb"], mark=(h == 3))
        P.mm(ps_r[:, :], SL[:], la[s2][:, :], True, True, [("la", s2), "SL"], ["psr"])
        P.act(eb[s2][:], ps_b[:, :], AF.Exp, ["psb"], [("eb", s2)])
        P.act(enb[:], ps_b[:, :], AF.Exp, ["psb"], ["enb"], scale=-1.0)
        P.act(erv[:], ps_r[:, :], AF.Exp, ["psr"], ["erv"])
        qv = qT[:, :, csl]
        kv_ = kT[:, :, csl]
        P.tt("vector", qtT[s2][:].rearrange("p (h t) -> p h t", t=128), qv, eb[s2][:].rearrange("p (h t) -> p h t", t=128), ALU.mult,
             ["qT", ("eb", s2)], [("qtT", s2)])
        P.tt("gpsimd", ktT[s2][:].rearrange("p (h t) -> p h t", t=128), kv_, enb[:].rearrange("p (h t) -> p h t", t=128), ALU.mult,
             ["kT", "enb"], [("ktT", s2)])
        P.tt("gpsimd", ks[s2][:], kt_[s2][:], erv[:], ALU.mult, [("ktk", s2), "erv"], [("ks", s2)])
        P.tt("gpsimd", gr[s2][:], sr[s2][:], gon[:], ALU.mult, [("sr", s2), "gon"], [("gr", s2)])
        for h in range(4):
            hs = slice(h * 128, (h + 1) * 128)
            vs = slice(h * 256, (h + 1) * 256)
            a2 = h % 2
            P.mm(ps_a[:, h, :], ktT[s2][:, hs], qtT[s2][:, hs], True, True, [("ktT", s2), ("qtT", s2)], [("psa", h)])
            P.tt("vector", attm[a2][:], ps_a[:, h, :], U[:], ALU.mult, [("psa", h), "U"], [("attm", a2)])
            P.mm(ps_o[:, vs], attm[a2][:], vt[s2][:, vs], True, False, [("attm", a2), ("vt", s2)], [("pso", h)], mark=False)
            P.mm(ps_o[:, vs], qtT[s2][:, hs], stb[:, h, :], False, True, [("qtT", s2), ("stb", h)], [("pso", h)])
            P.mm(ps_kv[:, vs], ks[s2][:, hs], vt[s2][:, vs], True, True, [("ks", s2), ("vt", s2)], [("pskv", h)])
            P.stt(state[:, h, :], state[:, h, :], eb[s2][:, h * 128 + 127:h * 128 + 128], ps_kv[:, vs], ALU.mult, ALU.add,
                  [("state", h), ("eb", s2), ("pskv", h)], [("state", h)])
            P.copy("scalar", stb[:, h, :], state[:, h, :], [("state", h)], [("stb", h)])
        sk = ("st", s2)
        for h in range(4):
            vs = slice(h * 256, (h + 1) * 256)
            P.act(junk[:], ps_o[:, vs], AF.Square, [("pso", h)], ["junkA", sk], accum_out=stat[s2][:, h:h + 1])
        P.act(stat[s2][:, 4:8], stat[s2][:, 0:4], AF.Sqrt, [sk], [sk], scale=1.0 / 256, bias=eps_t[:, 0:1])
        P.recip(stat[s2][:, 8:12], stat[s2][:, 4:8], [sk], [sk])
        for h in range(4):
            vs = slice(h * 256, (h + 1) * 256)
            P.stt(yc[s2][:, vs], ps_o[:, vs], stat[s2][:, 8 + h:9 + h], gr[s2][:, vs], ALU.mult, ALU.mult,
                  [("pso", h), sk, ("gr", s2)], [("yc", s2)])
        for k in range(8):
            P.tr(ps_tr[:, k, :], yc[s2][:, k * 128:(k + 1) * 128], identb[:], [("yc", s2), "identb"], ["ptr"], mark=(k == 7))
        g4 = (ci // 4) % 2
        P.copy("scalar", ycT[g4][:, 0:8, (ci % 4) * 128:(ci % 4 + 1) * 128], ps_tr[:, 0:8, :], ["ptr"], [("ycT", g4)])
        if ci % 4 == 3:
            g = c // 4
            P.dma("sync", d["ycT"][b, :, :, g * 512:(g + 1) * 512].rearrange("c p t -> p c t"), ycT[g4][:], r=[("ycT", g4)],
                  key=("ycT", g4), is_out=True)
    P.close()


def declare(B):
    NSEQ = B.NSEQ
    T = NSEQ * SEQ
    X = "ExternalInput"
    B.dram("x", [T, D], F32, X)
    B.dram("ab_w_in", [D, 2248], F32, X)
    B.dram("ab_conv_w", [31, 512], F32, X)
    B.dram("conv_prm", [128, 12], F32, X)
    B.dram("ab_w_out", [D, D], F32, X)
    B.dram("c_w_in", [D, 3088], F32, X)
    B.dram("c_gate_w", [16, 512], F32, X)
    B.dram("c_gate_b_bc", [128, 512], F32, X)
    B.dram("c_onorm_g_bc", [128, D], F32, X)
    B.dram("c_w_out", [D, D], F32, X)
    for l in range(2):
        B.dram("ffn_w_gate%d" % l, [D, DFF], F32, X)
        B.dram("ffn_w_up%d" % l, [D, DFF], F32, X)
        B.dram("ffn_w_down%d" % l, [DFF, D], F32, X)
        B.dram("g_mix%d" % l, [128, D], F32, X)
        B.dram("g_ffn%d" % l, [128, D], F32, X)
    B.dram("g_final", [128, D], F32, X)
    B.dram("ident_bf", [128, 128], BF16, X)
    B.dram("ident_f32", [128, 128], F32, X)
    B.dram("cos128", [SEQ, 128], F32, X)
    B.dram("sin128", [SEQ, 128], F32, X)
    B.dram("negmask", [128, 128], F32, X)
    B.dram("pow2", [128, NIT], F32, X)
    B.dram("tri_u", [128, 128], F32, X)
    B.dram("tri_sl", [128, 128], F32, X)
    B.dram("hconvT", [NSEQ, 4, 128, SEQ], BF16)
    B.dram("qT", [NSEQ, 4, 128, SEQ], BF16)
    B.dram("iqT", [NSEQ, 4, 128, SEQ], BF16)
    B.dram("kkT", [NSEQ, 2, 128, SEQ], BF16)
    B.dram("vaug", [T, 256], BF16)
    B.dram("iw", [T, 8], F32)
    B.dram("yabT", [NSEQ, 8, 128, SEQ], BF16)
    B.dram("h_mid0", [T, D], F32)
    B.dram("hnT", [8, 128, T], BF16)
    B.dram("h_half", [T, D], F32)
    B.dram("h1", [T, D], F32)
    B.dram("c_qT", [NSEQ, 4, 128, SEQ], BF16)
    B.dram("c_kT", [NSEQ, 4, 128, SEQ], BF16)
    B.dram("c_ktok", [T, 512], BF16)
    B.dram("c_v", [T, D], BF16)
    B.dram("c_sr", [T, D], F32)
    B.dram("c_la", [T, 512], F32)
    B.dram("ycT", [NSEQ, 8, 128, SEQ], BF16)
    B.dram("h_mid1", [T, D], F32)
    B.dram("out", [T, D], F32, "ExternalOutput")
    if DEBUG_STOP == 77:
        B.dram("dbg", [128, 16], F32, "ExternalOutput")
        B.dram("dbg2", [4, 128, D], BF16, "ExternalOutput")


PHASES = {
    "A": phase_A,
    "B": phase_B,
    "C": phase_C,
    "D0": lambda B: phase_D(B, "D0", "yabT", "ab_w_out", "x", "g_ffn0", "h_mid0", "hnT"),
    "F0a": lambda B: phase_F(B, "F0a", 0, 0, "h_mid0", "h_half", "hnT"),
    "F0b": lambda B: phase_F(B, "F0b", 0, 11, "h_half", "h1", "hnT"),
    "E": phase_E,
    "G": phase_G,
    "D1": lambda B: phase_D(B, "D1", "ycT", "c_w_out", "h1", "g_ffn1", "h_mid1", "hnT"),
    "F1a": lambda B: phase_F(B, "F1a", 1, 0, "h_mid1", "h_half", "hnT"),
    "F1b": lambda B: phase_F(B, "F1b", 1, 11, "h_half", "out", "hnT", final_g="g_final"),
}
ORDER = ["A", "B", "C", "D0", "F0a", "F0b", "E", "G", "D1", "F1a", "F1b"]


def host_consts():
    bf = ml_dtypes.bfloat16
    c = {}
    c["ident_bf"] = np.eye(128, dtype=np.float32).astype(bf)
    c["ident_f32"] = np.eye(128, dtype=np.float32)
    rot = 16
    inv = (500000.0 ** (-np.arange(0, rot, 2, dtype=np.float32) / np.float32(rot))).astype(np.float32)
    ang = (np.arange(SEQ, dtype=np.float32)[:, None] * inv[None, :]).astype(np.float32)
    c["cos128"] = np.ascontiguousarray(np.tile(np.cos(ang).astype(np.float32), (1, 16)))
    c["sin128"] = np.ascontiguousarray(np.tile(np.sin(ang).astype(np.float32), (1, 16)))
    i = np.arange(128)
    c["negmask"] = np.where(i[None, :] <= i[:, None], 0.0, -1e30).astype(np.float32)
    c["pow2"] = np.ascontiguousarray(np.broadcast_to((0.5 ** np.arange(1, NIT + 1)).astype(np.float32), (128, NIT)))
    c["tri_u"] = (i[:, None] <= i[None, :]).astype(np.float32)
    c["tri_sl"] = (i[:, None] > i[None, :]).astype(np.float32)
    return c


def host_weights(inp):
    f = lambda a: np.ascontiguousarray(np.asarray(a, dtype=np.float32))
    bc = lambda v, n: np.ascontiguousarray(np.broadcast_to(np.asarray(v, np.float32).reshape(1, -1), (128, n)))
    w = {}
    w["ab_w_in"] = f(inp["ab_w_in"][0])
    w["ab_conv_w"] = f(inp["ab_conv_w"][0].reshape(31, 512))
    prm = np.concatenate([np.asarray(inp[k][0], np.float32).reshape(4, 128).T for k in ("ab_conv_b", "ab_ln_g", "ab_ln_b")], axis=1)
    w["conv_prm"] = f(prm)
    w["ab_w_out"] = f(inp["ab_w_out"][0])
    w["c_w_in"] = f(inp["c_w_in"][0])
    w["c_gate_w"] = f(inp["c_gate_w"][0])
    w["c_gate_b_bc"] = bc(inp["c_gate_b"][0], 512)
    w["c_onorm_g_bc"] = bc(inp["c_onorm_g"][0], D)
    w["c_w_out"] = f(inp["c_w_out"][0])
    for l in range(2):
        w["ffn_w_gate%d" % l] = f(inp["ffn_w_gate"][l])
        w["ffn_w_up%d" % l] = f(inp["ffn_w_up"][l])
        w["ffn_w_down%d" % l] = f(inp["ffn_w_down"][l])
        w["g_mix%d" % l] = bc(inp["norm_mix_g"][l], D)
        w["g_ffn%d" % l] = bc(inp["norm_ffn_g"][l], D)
    w["g_final"] = bc(inp["final_norm_g"], D)
    return w


def build_program(nseq, phases, ext=None):
    B = Build(nseq, ext)
    declare(B)
    for p in phases:
        PHASES[p](B)
    return B


def kernel(**inp):
    n = 8
    nseq = 2
    x = np.asarray(inp["x"], dtype=np.float32)
    B = build_program(nseq, ORDER)
    shared = host_consts()
    shared.update(host_weights(inp))
    in_maps = []
    for c in range(n):
        m = dict(shared)
        m["x"] = np.ascontiguousarray(x[c * nseq:(c + 1) * nseq].reshape(nseq * SEQ, D))
        in_maps.append(m)
    res = run_bass_kernel_spmd(B.nc, in_maps, core_ids=list(range(n)))
    out = np.stack([np.asarray(r["out"], dtype=np.float32).reshape(nseq, SEQ, D) for r in res.results], axis=0)
    return out.reshape(16, SEQ, D)
```

```python
import contextlib
import math
import numpy as np
import ml_dtypes
import concourse.bass as bass
import concourse.mybir as mybir
from concourse.bass_utils import run_bass_kernel_spmd

F32 = mybir.dt.float32
BF16 = mybir.dt.bfloat16
ALU = mybir.AluOpType
AF = mybir.ActivationFunctionType
AX = mybir.AxisListType

ENGS = ["sync", "scalar", "vector", "gpsimd", "tensor"]
D = 1024
SEQ = 2048
NT = 16
EPS = 1e-6
DFF = 2816
NFF = 22
NIT = 16
TOPK = 256
DEBUG_STOP = 99
SAME_SYNC = True


class Sched:
    def __init__(self, nc, stack, tag=""):
        self.nc = nc
        self.stack = stack
        self.tag = tag
        self.q = {e: [] for e in ENGS}
        self.sem = {}
        self.cnt = {}
        self.waited = {e: {} for e in ENGS}
        self.lastw = {}
        self.readers = {}
        self.pe_pending = False
        self.out_deps = {}
        for e in ENGS:
            self._mksem("E_" + e)

    def _mksem(self, name):
        if name not in self.sem:
            hw_name = "%s_s%d" % (self.tag, len(self.sem))
            self.sem[name] = self.nc.alloc_semaphore(name=hw_name)
            self.cnt[name] = 0
        return name

    def op(self, eng, fn, reads=(), writes=(), dma=None, mark=True, is_out=False, force_self=False):
        deps = {}

        def add(d):
            if d[1] > deps.get(d[0], 0):
                deps[d[0]] = d[1]

        for k in reads:
            if k in self.lastw:
                add(self.lastw[k])
        for k in writes:
            if k in self.lastw:
                add(self.lastw[k])
            for s_, v_ in self.readers.get(k, {}).items():
                add((s_, v_))
        if dma is not None:
            s = self._mksem("D_" + str(dma))
            if self.cnt[s] > 0:
                add((s, self.cnt[s]))
        own = "E_" + eng
        need = []
        for s_, v_ in deps.items():
            if s_ == own and (eng == "tensor" or not (SAME_SYNC or force_self)):
                continue
            if self.waited[eng].get(s_, 0) >= v_:
                continue
            need.append((s_, v_))
        attach = None
        if eng == "tensor":
            k0 = reads[0] if len(reads) else None
            if k0 is not None and k0 in self.lastw and self.lastw[k0][0] != own:
                attach = self.lastw[k0]
        else:
            selfs = [d_ for d_ in need if d_[0] == own]
            attach = selfs[0] if selfs else (need[-1] if need else None)
        standalone = [d_ for d_ in need if attach is None or d_[0] != attach[0]]
        if attach is not None:
            for d_ in need:
                if d_[0] == attach[0] and d_[1] > attach[1]:
                    attach = d_
        for d_ in need:
            self.waited[eng][d_[0]] = max(self.waited[eng].get(d_[0], 0), d_[1])
        if attach is not None:
            self.waited[eng][attach[0]] = max(self.waited[eng].get(attach[0], 0), attach[1])
        if dma is not None:
            self.cnt[s] += 16
            dep = (s, self.cnt[s])
            self.q[eng].append(("op", fn, s, 16, standalone, attach))
        else:
            s = own
            if mark:
                self.cnt[s] += 1
                dep = (s, self.cnt[s])
                self.q[eng].append(("op", fn, s, 1, standalone, attach))
                if eng == "tensor":
                    self.pe_pending = False
            else:
                assert eng == "tensor"
                dep = (s, self.cnt[s] + 1)
                self.q[eng].append(("op", fn, None, 0, standalone, attach))
                self.pe_pending = True
        for k in writes:
            self.lastw[k] = dep
            self.readers[k] = {}
        for k in reads:
            r = self.readers.setdefault(k, {})
            if dep[1] > r.get(dep[0], 0):
                r[dep[0]] = dep[1]
        if is_out:
            if dep[1] > self.out_deps.get(dep[0], 0):
                self.out_deps[dep[0]] = dep[1]
        return dep

    def finalize(self):
        assert not self.pe_pending, "unmarked PE op at end of phase"
        tail = []
        for s_, v_ in self.out_deps.items():
            if self.waited["sync"].get(s_, 0) < v_:
                tail.append((s_, v_))
        nc = self.nc
        with nc.Block() as block:
            for eng in ENGS:
                items = self.q[eng]

                def body(e, items=items, eng=eng):
                    for it in items:
                        for (ws, wv) in it[4]:
                            e.wait_ge(self.sem[ws], wv)
                        ins = it[1](e)
                        if it[5] is not None:
                            ins._wait_ge(self.sem[it[5][0]], it[5][1])
                        if it[2] is not None:
                            ins.then_inc(self.sem[it[2]], it[3])
                    if eng == "sync":
                        for (ws, wv) in tail:
                            e.wait_ge(self.sem[ws], wv)

                getattr(block, eng)(body)


class Phase:
    def __init__(self, nc, tag):
        self.nc = nc
        self.tag = tag
        self.st = contextlib.ExitStack()
        self.st.enter_context(nc.cleanup_on_exit())
        self.S = Sched(nc, self.st, tag)

    def sb(self, name, shape, dt):
        return self.st.enter_context(self.nc.sbuf_tensor(self.tag + "_" + name, shape, dt))

    def ps(self, name, shape, dt=F32):
        return self.st.enter_context(self.nc.psum_tensor(self.tag + "_" + name, shape, dt))

    def dma(self, eng, out, in_, r=(), w=(), key=None, is_out=False):
        return self.S.op(eng, lambda e: e.dma_start(out=out, in_=in_), r, w, dma=key, is_out=is_out)

    def mm(self, out, lhsT, rhs, start, stop, r, w, mark=None):
        if mark is None:
            mark = stop
        return self.S.op("tensor", lambda e: e.matmul(out, lhsT=lhsT, rhs=rhs, start=start, stop=stop), r, w, mark=mark)

    def tr(self, out, in_, ident, r, w, mark=True):
        return self.S.op("tensor", lambda e: e.transpose(out=out, in_=in_, identity=ident), r, w, mark=mark)

    def act(self, out, in_, func, r, w, **kw):
        return self.S.op("scalar", lambda e: e.activation(out=out, in_=in_, func=func, **kw), r, w)

    def tt(self, eng, out, in0, in1, op, r, w):
        return self.S.op(eng, lambda e: e.tensor_tensor(out=out, in0=in0, in1=in1, op=op), r, w)

    def ts(self, eng, out, in0, s1, op0, r, w, s2=None, op1=None, accum_out=None):
        if op1 is None:
            return self.S.op(eng, lambda e: e.tensor_scalar(out=out, in0=in0, scalar1=s1, scalar2=None, op0=op0), r, w)
        return self.S.op(eng, lambda e: e.tensor_scalar(out=out, in0=in0, scalar1=s1, scalar2=s2, op0=op0, op1=op1,
                                                       accum_out=accum_out), r, w)

    def stt(self, out, in0, scalar, in1, op0, op1, r, w):
        return self.S.op("vector", lambda e: e.scalar_tensor_tensor(out=out, in0=in0, scalar=scalar, in1=in1,
                                                                     op0=op0, op1=op1), r, w)

    def copy(self, eng, out, in_, r, w):
        if eng == "scalar":
            return self.S.op(eng, lambda e: e.copy(out=out, in_=in_), r, w)
        return self.S.op(eng, lambda e: e.tensor_copy(out=out, in_=in_), r, w)

    def recip(self, out, in_, r, w):
        return self.S.op("vector", lambda e: e.reciprocal(out=out, in_=in_), r, w)

    def memset(self, eng, ap, val, w):
        return self.S.op(eng, lambda e: e.memset(ap, val), (), w)

    def reduce(self, out, in_, op, r, w):
        return self.S.op("vector", lambda e: e.tensor_reduce(out=out, in_=in_, axis=AX.X, op=op), r, w)

    def close(self):
        self.S.finalize()
        self.st.close()


class Build:
    def __init__(self, nseq, ext=None):
        self.nc = bass.Bass("TRN2", target_bir_lowering=False)
        self.NSEQ = nseq
        self.t = {}
        self.ext = ext or {}
        self.kinds = {}

    def dram(self, name, shape, dt, kind="Internal"):
        kind = self.ext.get(name, kind)
        self.kinds[name] = (kind, list(shape), dt)
        self.t[name] = self.nc.dram_tensor(name, list(shape), dt, kind=kind).ap()
        return self.t[name]


def rms_to_hnT(P, xt, xkey, gbc, eps_t, stat, skey, junk, hn, hkey, ps_tr, pkey, identb, dst, dkey, sl):
    P.act(junk[:], xt, AF.Square, [xkey], ["junkA", skey], accum_out=stat[:, 0:1])
    P.act(stat[:, 1:2], stat[:, 0:1], AF.Sqrt, [skey, "eps"], [skey], scale=1.0 / D, bias=eps_t[:, 0:1])
    P.recip(stat[:, 2:3], stat[:, 1:2], [skey], [skey])
    P.stt(hn, xt, stat[:, 2:3], gbc, ALU.mult, ALU.mult, [xkey, skey, "gbc"], [hkey])
    for k in range(8):
        P.tr(ps_tr[:, k, :], hn[:, k * 128:(k + 1) * 128], identb[:], [hkey, "identb"], [pkey], mark=(k == 7))
    P.copy("scalar", dst[:, 0:8, sl], ps_tr[:, 0:8, :], [pkey], [dkey])


def phase_A(B):
    nc, d, NSEQ = B.nc, B.t, B.NSEQ
    P = Phase(nc, "A")
    Wt = P.sb("Wt", [128, 8, 2376], BF16)
    Wsrc = d["ab_w_in"].rearrange("(k p) n -> p k n", p=128)
    segs = [(0, 1024, 0), (1024, 1536, 1024), (1664, 2176, 1536), (1536, 1600, 2048), (1536, 1600, 2112),
            (2176, 2240, 2176), (2176, 2240, 2240), (1600, 1664, 2304), (2240, 2248, 2368)]
    WK = []
    for i, (c0, c1, d0) in enumerate(segs):
        P.dma("gpsimd", Wt[:, :, d0:d0 + c1 - c0], Wsrc[:, :, c0:c1], w=[("W", i)], key=("W", i))
        WK.append(("W", i))
    identb = P.sb("identb", [128, 128], BF16)
    P.dma("sync", identb[:], d["ident_bf"], w=["identb"], key="identb")
    gbc = P.sb("gbc", [128, D], F32)
    P.dma("sync", gbc[:], d["g_mix0"], w=["gbc"], key="gbc")
    cos = P.sb("cos", [128, NT, 128], F32)
    sin = P.sb("sin", [128, NT, 128], F32)
    P.dma("sync", cos[:], d["cos128"].rearrange("(t p) c -> p t c", p=128), w=["cos"], key="cos")
    P.dma("sync", sin[:], d["sin128"].rearrange("(t p) c -> p t c", p=128), w=["sin"], key="sin")
    eps_t = P.sb("eps", [128, 1], F32)
    P.memset("vector", eps_t[:], EPS, ["eps"])

    xt = [P.sb("xt%d" % i, [128, D], F32) for i in range(2)]
    junk = P.sb("junk", [128, D], F32)
    stat = [P.sb("stat%d" % i, [128, 4], F32) for i in range(2)]
    hn = [P.sb("hn%d" % i, [128, D], BF16) for i in range(2)]
    hnT = [P.sb("hnT%d" % i, [128, 8, 512], BF16) for i in range(2)]
    ps_tr = [P.ps("ptr%d" % i, [128, 8, 128], BF16) for i in range(2)]
    ps_a = P.ps("psa", [128, 512])
    ps_g = P.ps("psg", [128, 512])
    ps_q = P.ps("psq", [128, 1024])
    ps_c = P.ps("psc", [128, 512])
    sig = [P.sb("sig%d" % i, [128, 512], F32) for i in range(2)]
    hc = [P.sb("hc%d" % i, [128, 512], BF16) for i in range(2)]
    tmp = [P.sb("tmp%d" % i, [128, 128], F32) for i in range(4)]
    qr = [P.sb("qr%d" % i, [128, 1024], BF16) for i in range(2)]
    kr = [P.sb("kr%d" % i, [128, 256], BF16) for i in range(2)]
    vaug = [P.sb("vaug%d" % i, [128, 256], BF16) for i in range(2)]
    iwt = [P.sb("iw%d" % i, [128, 8], F32) for i in range(2)]
    for i in range(2):
        P.memset("gpsimd", vaug[i][:, 64:192], 1.0, [("vaug", i)])
    qTg = [P.sb("qTg%d" % i, [128, 8, 512], BF16) for i in range(2)]
    kTg = [P.sb("kTg%d" % i, [128, 2, 512], BF16) for i in range(2)]
    w_scale = (8 ** -0.5) * (64 ** -0.5)

    def load_x(b, tt):
        xs = tt % 2
        r0 = b * SEQ + tt * 128
        P.dma("sync", xt[xs][:], d["x"][r0:r0 + 128, :], w=[("x", xs)], key=("x", xs))

    n_tiles = NSEQ * NT
    if DEBUG_STOP <= 1:
        P.dma("sync", d["iw"][0:128, :], iwt[0][:], r=[("iw", 0), "cos", "sin", "gbc", "identb"] + WK, key=("iw", 0), is_out=True)
        P.close()
        return
    load_x(0, 0)
    for gi in range(NSEQ * 4):
        b, g = gi // 4, gi % 4
        gs = gi % 2
        if DEBUG_STOP <= 6 and gi >= max(1, DEBUG_STOP - 3):
            break
        for t4 in range(4):
            tt = g * 4 + t4
            ti = gi * 4 + t4
            if ti + 1 < n_tiles and not (DEBUG_STOP <= 6 and ti + 1 >= 4 * max(1, DEBUG_STOP - 3)):
                load_x((ti + 1) // NT, (ti + 1) % NT)
            xs = tt % 2
            rms_to_hnT(P, xt[xs][:], ("x", xs), gbc[:], eps_t, stat[xs], ("st", xs), junk, hn[xs][:], ("hn", xs),
                       ps_tr[0], "ptr0", identb, hnT[gs], ("hnT", gs), slice(t4 * 128, (t4 + 1) * 128))
        if DEBUG_STOP <= 2:
            P.dma("sync", d["qT"][b, :, :, 0:512].rearrange("c p t -> p c t"), hnT[gs][:, 0:4, :], r=[("hnT", gs)], key=("qTg", gs), is_out=True)
            break
        for c in range(4):
            for k in range(8):
                P.mm(ps_a[:, :], Wt[:, k, c * 128:(c + 1) * 128], hnT[gs][:, k, :], k == 0, k == 7,
                     [("hnT", gs)] + WK, ["psa"])
            for k in range(8):
                P.mm(ps_g[:, :], Wt[:, k, 512 + c * 128:512 + (c + 1) * 128], hnT[gs][:, k, :], k == 0, k == 7,
                     [("hnT", gs)] + WK, ["psg"])
            s2 = c % 2
            P.act(sig[s2][:], ps_g[:, :], AF.Sigmoid, ["psg"], [("sig", s2)])
            P.tt("vector", hc[s2][:], ps_a[:, :], sig[s2][:], ALU.mult, ["psa", ("sig", s2)], [("hc", s2)])
            P.dma("sync", d["hconvT"][b, c, :, g * 512:(g + 1) * 512], hc[s2][:], r=[("hc", s2)], key=("hc", s2), is_out=True)
        if DEBUG_STOP <= 3:
            break
        for t4 in range(4):
            tt = g * 4 + t4
            r0 = b * SEQ + tt * 128
            tok = slice(t4 * 128, (t4 + 1) * 128)
            s2 = t4 % 2
            for k in range(8):
                P.mm(ps_q[:, 0:512], hnT[gs][:, k, tok], Wt[:, k, 1024:1536], k == 0, k == 7, [("hnT", gs)] + WK, ["psq"], mark=False)
            for k in range(8):
                P.mm(ps_q[:, 512:1024], hnT[gs][:, k, tok], Wt[:, k, 1536:2048], k == 0, k == 7, [("hnT", gs)] + WK, ["psq"], mark=False)
            for k in range(8):
                P.mm(ps_c[:, 0:328], hnT[gs][:, k, tok], Wt[:, k, 2048:2376], k == 0, k == 7, [("hnT", gs)] + WK, ["psq"])
            for (src, nh, dst, dkey) in ((ps_q[:, 0:512], 8, qr[s2][:, 0:512], ("qr", s2)), (ps_q[:, 512:1024], 8, qr[s2][:, 512:1024], ("qr", s2)),
                                         (ps_c[:, 0:256], 4, kr[s2][:, 0:256], ("kr", s2))):
                sv = src.rearrange("p (h e) -> p h e", e=64)
                dv = dst.rearrange("p (h e) -> p h e", e=64)
                x1, x2 = sv[:, :, 0:8], sv[:, :, 8:16]
                cv = cos[:, tt, 0:nh * 8].rearrange("p (h e) -> p h e", e=8)
                sn = sin[:, tt, 0:nh * 8].rearrange("p (h e) -> p h e", e=8)
                tv = [tmp[i][:, 0:nh * 8].rearrange("p (h e) -> p h e", e=8) for i in range(4)]
                P.tt("vector", tv[0], x1, cv, ALU.mult, ["psq", "cos"], ["tmp0"])
                P.tt("vector", tv[1], x2, sn, ALU.mult, ["psq", "sin"], ["tmp1"])
                P.tt("vector", dv[:, :, 0:8], tv[0], tv[1], ALU.subtract, ["tmp0", "tmp1"], [dkey])
                P.tt("vector", tv[2], x2, cv, ALU.mult, ["psq", "cos"], ["tmp2"])
                P.tt("vector", tv[3], x1, sn, ALU.mult, ["psq", "sin"], ["tmp3"])
                P.tt("vector", dv[:, :, 8:16], tv[2], tv[3], ALU.add, ["tmp2", "tmp3"], [dkey])
                P.copy("scalar", dv[:, :, 16:64], sv[:, :, 16:64], ["psq"], [dkey])
            P.copy("scalar", vaug[s2][:, 0:64], ps_c[:, 256:320], ["psq"], [("vaug", s2)])
            P.copy("scalar", vaug[s2][:, 192:256], ps_c[:, 256:320], ["psq"], [("vaug", s2)])
            P.ts("vector", iwt[s2][:], ps_c[:, 320:328], w_scale, ALU.mult, ["psq"], [("iw", s2)])
            P.dma("sync", d["vaug"][r0:r0 + 128, :], vaug[s2][:], r=[("vaug", s2)], key=("vaug", s2), is_out=True)
            P.dma("sync", d["iw"][r0:r0 + 128, :], iwt[s2][:], r=[("iw", s2)], key=("iw", s2), is_out=True)
            for k in range(8):
                P.tr(ps_tr[1][:, k, :], qr[s2][:, k * 128:(k + 1) * 128], identb[:], [("qr", s2), "identb"], ["ptr1"], mark=(k == 7))
            P.copy("scalar", qTg[gs][:, 0:8, tok], ps_tr[1][:, 0:8, :], ["ptr1"], [("qTg", gs)])
            for k in range(2):
                P.tr(ps_tr[1][:, k, :], kr[s2][:, k * 128:(k + 1) * 128], identb[:], [("kr", s2), "identb"], ["ptr1"], mark=(k == 1))
            P.copy("scalar", kTg[gs][:, 0:2, tok], ps_tr[1][:, 0:2, :], ["ptr1"], [("kTg", gs)])
        gsl = slice(g * 512, (g + 1) * 512)
        P.dma("sync", d["qT"][b, :, :, gsl].rearrange("c p t -> p c t"), qTg[gs][:, 0:4, :], r=[("qTg", gs)], key=("qTg", gs), is_out=True)
        P.dma("sync", d["iqT"][b, :, :, gsl].rearrange("c p t -> p c t"), qTg[gs][:, 4:8, :], r=[("qTg", gs)], key=("iqTg", gs), is_out=True)
        P.dma("sync", d["kkT"][b, :, :, gsl].rearrange("c p t -> p c t"), kTg[gs][:, :, :], r=[("kTg", gs)], key=("kTg", gs), is_out=True)
    P.close()


def phase_B(B):
    nc, d, NSEQ = B.nc, B.t, B.NSEQ
    P = Phase(nc, "B")
    identb = P.sb("identb", [128, 128], BF16)
    P.dma("sync", identb[:], d["ident_bf"], w=["identb"], key="identb")
    identf = P.sb("identf", [128, 128], F32)
    P.dma("sync", identf[:], d["ident_f32"], w=["identf"], key="identf")
    ones = P.sb("ones", [128, 128], F32)
    P.memset("vector", ones[:], 1.0, ["ones"])
    eps_t = P.sb("eps", [128, 1], F32)
    P.memset("vector", eps_t[:], EPS, ["eps"])
    cw = P.sb("cw", [31, 512], F32)
    P.dma("sync", cw[:], d["ab_conv_w"], w=["cw"], key="cw")
    prm = P.sb("prm", [128, 12], F32)
    P.dma("sync", prm[:], d["conv_prm"], w=["prm"], key="prm")
    ps_w = P.ps("psw", [128, 4, 32])
    for c in range(4):
        P.tr(ps_w[:, c, 0:31], cw[0:31, c * 128:(c + 1) * 128], identf[0:31, 0:31], ["cw", "identf"], ["psw"], mark=(c == 3))
    wT = P.sb("wT", [128, 4, 32], F32)
    P.copy("vector", wT[:, :, 0:31], ps_w[:, :, 0:31], ["psw"], ["wT"])
    diag = P.sb("diag", [128, 124, 128], BF16)
    for c in range(4):
        for j in range(31):
            i = c * 31 + j
            P.ts("vector" if i % 2 == 0 else "gpsimd", diag[:, i, :], identb[:], wT[:, c, j:j + 1], ALU.mult,
                 ["identb", "wT"], [("diag", c)])
    hbuf = P.sb("hbuf", [128, 4, 30 + SEQ], BF16)
    P.memset("gpsimd", hbuf[:, :, 0:30], 0.0, ["hbuf"])
    ps_c = [P.ps("psc%d" % i, [128, 512]) for i in range(2)]
    ps_s1 = P.ps("ps1", [128, 512])
    ps_s2 = P.ps("ps2", [128, 512])
    hcs = P.sb("hcs", [128, 4, 512], F32)
    sqs = P.sb("sqs", [128, 4, 512], F32)
    m = P.sb("m", [128, 512], F32)
    msq = P.sb("msq", [128, 512], F32)
    var = P.sb("var", [128, 512], F32)
    rstd = P.sb("rstd", [128, 512], F32)
    z = [P.sb("z%d" % i, [128, 512], F32) for i in range(2)]
    yst = [P.sb("yst%d" % i, [128, 4, 512], BF16) for i in range(2)]
    for b in range(NSEQ):
        for c in range(4):
            P.dma("sync", hbuf[:, c, 30:30 + SEQ], d["hconvT"][b, c, :, :], w=["hbuf"], key=("hbuf", c))
        for g in range(4):
            ys = g % 2
            for c in range(4):
                pc = ps_c[c % 2]
                pk = ("psc", c % 2)
                for j in range(31):
                    P.mm(pc[:, :], diag[:, c * 31 + j, :], hbuf[:, c, g * 512 + j:g * 512 + j + 512], j == 0, j == 30,
                         [("diag", c), "hbuf"], [pk])
                P.act(hcs[:, c, :], pc[:, :], AF.Identity, [pk, "prm"], [("hcs", c)], bias=prm[:, c:c + 1], scale=1.0)
                P.act(sqs[:, c, :], pc[:, :], AF.Square, [pk, "prm"], [("sqs", c)], bias=prm[:, c:c + 1], scale=1.0)
            for c in range(4):
                P.mm(ps_s1[:, :], ones[:], hcs[:, c, :], c == 0, c == 3, [("hcs", c), "ones"], ["ps1"])
            for c in range(4):
                P.mm(ps_s2[:, :], ones[:], sqs[:, c, :], c == 0, c == 3, [("sqs", c), "ones"], ["ps2"])
            P.act(m[:], ps_s1[:, :], AF.Copy, ["ps1"], ["m"], scale=1.0 / 512)
            P.tt("gpsimd", msq[:], m[:], m[:], ALU.mult, ["m"], ["msq"])
            P.stt(var[:], ps_s2[:, :], 1.0 / 512, msq[:], ALU.mult, ALU.subtract, ["ps2", "msq"], ["var"])
            P.act(var[:], var[:], AF.Sqrt, ["var", "eps"], ["var"], bias=eps_t[:, 0:1], scale=1.0)
            P.recip(rstd[:], var[:], ["var"], ["rstd"])
            for c in range(4):
                zz = z[c % 2]
                zk = ("z", c % 2)
                P.tt("gpsimd", zz[:], hcs[:, c, :], m[:], ALU.subtract, [("hcs", c), "m"], [zk])
                P.tt("vector", zz[:], zz[:], rstd[:], ALU.mult, [zk, "rstd"], [zk])
                P.act(yst[ys][:, c, :], zz[:], AF.Silu, [zk, "prm"], [("yst", ys)], scale=prm[:, 4 + c:5 + c], bias=prm[:, 8 + c:9 + c])
            P.dma("sync", d["yabT"][b, 0:4, :, g * 512:(g + 1) * 512].rearrange("c p t -> p c t"), yst[ys][:],
                  r=[("yst", ys)], key=("yst", ys), is_out=True)
    P.close()


def phase_C(B):
    nc, d, NSEQ = B.nc, B.t, B.NSEQ
    P = Phase(nc, "C")
    identb = P.sb("identb", [128, 128], BF16)
    P.dma("sync", identb[:], d["ident_bf"], w=["identb"], key="identb")
    negm = P.sb("negm", [128, 128], F32)
    P.dma("sync", negm[:], d["negmask"], w=["negm"], key="negm")
    pow2 = P.sb("pow2", [128, NIT], F32)
    P.dma("sync", pow2[:], d["pow2"], w=["pow2"], key="pow2")
    thr0 = P.sb("thr0", [128, 1], F32)
    P.memset("vector", thr0[:], -1e29, ["thr0"])
    qT = [P.sb("qT%d" % i, [128, 4, SEQ], BF16) for i in range(2)]
    iqT = [P.sb("iqT%d" % i, [128, 4, SEQ], BF16) for i in range(2)]
    kkT = [P.sb("kkT%d" % i, [128, 2, SEQ], BF16) for i in range(2)]
    vaug = [P.sb("vaug%d" % i, [128, NT, 256], BF16) for i in range(2)]
    iw = [P.sb("iw%d" % i, [128, NT, 8], F32) for i in range(2)]
    ybg = [P.sb("ybg%d" % i, [128, 512], BF16) for i in range(2)]
    maskT = [P.sb("maskT%d" % i, [128, NT, 512], BF16) for i in range(2)]
    score = [P.sb("score%d" % i, [128, SEQ], F32) for i in range(2)]
    junk = P.sb("junk", [128, SEQ], BF16)
    rl = [P.sb("rl%d" % i, [128, 512], F32) for i in range(2)]
    mk = [P.sb("mk%d" % i, [128, SEQ], BF16) for i in range(2)]
    st = [P.sb("st%d" % i, [128, 8 + NIT], F32) for i in range(2)]
    pe = [P.sb("pe%d" % i, [128, 512], BF16) for i in range(2)]
    pm = [P.sb("pm%d" % i, [128, 512], BF16) for i in range(2)]
    rc = [P.sb("rc%d" % i, [128, 512], F32) for i in range(2)]
    ps_lg = [P.ps("plg%d" % i, [128, 512]) for i in range(2)]
    ps_tr = [P.ps("ptr%d" % i, [128, 8, 128], BF16) for i in range(2)]
    ps_s = [P.ps("pss%d" % i, [128, 512]) for i in range(2)]
    ps_o = [P.ps("pso%d" % i, [128, 512]) for i in range(2)]
    att_scale = 64 ** -0.5
    ctr = {"lg": 0, "s": 0, "o": 0, "tr": 0, "yb": 0}

    def load_seq(b):
        p = b % 2
        rs = slice(b * SEQ, (b + 1) * SEQ)
        P.dma("sync", iqT[p][:], d["iqT"][b].rearrange("c p t -> p c t"), w=[("iqT", p)], key=("iqT", p))
        P.dma("sync", kkT[p][:], d["kkT"][b].rearrange("c p t -> p c t"), w=[("kkT", p)], key=("kkT", p))
        P.dma("sync", iw[p][:], d["iw"][rs, :].rearrange("(t p) c -> p t c", p=128), w=[("iw", p)], key=("iw", p))
        P.dma("sync", qT[p][:], d["qT"][b].rearrange("c p t -> p c t"), w=[("qT", p)], key=("qT", p))
        P.dma("sync", vaug[p][:], d["vaug"][rs, :].rearrange("(t p) c -> p t c", p=128), w=[("vaug", p)], key=("vaug", p))

    def score_unit(b, G, q4):
        p = b % 2
        mG = (b * 4 + G) % 2
        qb = 4 * G + q4
        nk = 128 * (qb + 1)
        nkb = (nk + 511) // 512
        s2 = qb % 2
        sc = score[s2]
        sk = ("score", s2)
        qsl = slice(qb * 128, (qb + 1) * 128)
        for h in range(8):
            c, base = h // 2, 64 * (h % 2)
            for kb in range(nkb):
                n = min(512, nk - kb * 512)
                r = ctr["lg"] % 2
                ctr["lg"] += 1
                P.mm(ps_lg[r][:, 0:n], iqT[p][base:base + 64, c, qsl], kkT[p][base:base + 64, 1, kb * 512:kb * 512 + n],
                     True, True, [("iqT", p), ("kkT", p)], [("plg", r)])
                P.act(rl[r][:, 0:n], ps_lg[r][:, 0:n], AF.Relu, [("plg", r)], [("rl", r)])
                dst = sc[:, kb * 512:kb * 512 + n]
                if h == 0:
                    P.ts("vector", dst, rl[r][:, 0:n], iw[p][:, qb, 0:1], ALU.mult, [("rl", r), ("iw", p)], [sk])
                else:
                    P.stt(dst, rl[r][:, 0:n], iw[p][:, qb, h:h + 1], dst, ALU.mult, ALU.add, [("rl", r), ("iw", p), sk], [sk])
        stt_ = st[s2]
        stk = ("st", s2)
        if qb >= 2:
            P.reduce(stt_[:, 0:1], sc[:, 0:nk], ALU.max, [sk], [stk])
            P.reduce(stt_[:, 1:2], sc[:, 0:nk], ALU.min, [sk], [stk])
        P.tt("vector", sc[:, qsl], sc[:, qsl], negm[:], ALU.add, [sk, "negm"], [sk])
        if qb >= 2:
            P.tt("vector", stt_[:, 2:3], stt_[:, 0:1], stt_[:, 1:2], ALU.subtract, [stk], [stk])
            P.ts("vector", stt_[:, 8:8 + NIT], pow2[:], stt_[:, 2:3], ALU.mult, ["pow2", stk], [stk])
            lo = stt_[:, 1:2]
            for i in range(NIT):
                stp = stt_[:, 8 + i:9 + i]
                P.tt("vector", stt_[:, 3:4], lo, stp, ALU.add, [stk], [stk])
                P.ts("vector", junk[:, 0:nk], sc[:, 0:nk], stt_[:, 3:4], ALU.is_ge, [sk, stk], ["junk", stk],
                     s2=0.0, op1=ALU.add, accum_out=stt_[:, 4:5])
                P.stt(stt_[:, 5:6], stt_[:, 4:5], TOPK - 0.5, stp, ALU.is_ge, ALU.mult, [stk], [stk])
                P.tt("vector", lo, lo, stt_[:, 5:6], ALU.add, [stk], [stk])
            thr = lo
        else:
            thr = thr0[:, 0:1]
        P.ts("vector", mk[s2][:, 0:nk], sc[:, 0:nk], thr, ALU.is_ge, [sk, stk, "thr0"], [("mk", s2)])
        kt = 0
        while kt <= qb:
            n = min(8, qb + 1 - kt)
            r = ctr["tr"] % 2
            ctr["tr"] += 1
            for j in range(n):
                P.tr(ps_tr[r][:, j, :], mk[s2][:, (kt + j) * 128:(kt + j + 1) * 128], identb[:], [("mk", s2), "identb"],
                     [("ptr", r)], mark=(j == n - 1))
            P.copy("scalar", maskT[mG][:, kt:kt + n, q4 * 128:(q4 + 1) * 128], ps_tr[r][:, 0:n, :], [("ptr", r)], [("maskT", mG)])
            kt += n

    def attn_chunk(b, G, c):
        p = b % 2
        mG = (b * 4 + G) % 2
        nkt = 4 * G + 4
        gsl0 = G * 512
        ys = ctr["yb"] % 2
        ctr["yb"] += 1
        for e in range(2):
            base = 64 * e
            o = ctr["o"] % 2
            ctr["o"] += 1
            for kt in range(nkt):
                q0 = max(0, kt - 4 * G) * 128
                N = 512 - q0
                r = ctr["s"] % 2
                ctr["s"] += 1
                P.mm(ps_s[r][:, 0:N], kkT[p][base:base + 64, 0, kt * 128:(kt + 1) * 128], qT[p][base:base + 64, c, gsl0 + q0:gsl0 + 512],
                     True, True, [("kkT", p), ("qT", p)], [("pss", r)])
                P.act(pe[r][:, 0:N], ps_s[r][:, 0:N], AF.Exp, [("pss", r)], [("pe", r)], scale=att_scale)
                P.tt("gpsimd", pm[r][:, 0:N], pe[r][:, 0:N], maskT[mG][:, kt, q0:512], ALU.mult, [("pe", r), ("maskT", mG)], [("pm", r)])
                P.mm(ps_o[o][:, q0:512], vaug[p][:, kt, e * 128:(e + 1) * 128], pm[r][:, 0:N], kt == 0, kt == nkt - 1,
                     [("pm", r), ("vaug", p)], [("pso", o)])
            so, ss_ = (slice(0, 64), slice(64, 128)) if e == 0 else (slice(64, 128), slice(0, 64))
            P.act(rc[o][ss_, :], ps_o[o][ss_, :], AF.Ln, [("pso", o)], [("rc", o)])
            P.act(rc[o][ss_, :], rc[o][ss_, :], AF.Exp, [("rc", o)], [("rc", o)], scale=-1.0)
            P.tt("vector", ybg[ys][so, :], ps_o[o][so, :], rc[o][ss_, :], ALU.mult, [("pso", o), ("rc", o)], [("ybg", ys)])
        P.dma("sync", d["yabT"][b, 4 + c, :, gsl0:gsl0 + 512], ybg[ys][:], r=[("ybg", ys)], key=("ybg", ys), is_out=True)

    units = [(b, G, q4) for b in range(NSEQ) for G in range(4) for q4 in range(4)]
    load_seq(0)
    for i, (b, G, q4) in enumerate(units):
        if G == 0 and q4 == 0 and b + 1 < NSEQ:
            load_seq(b + 1)
        score_unit(b, G, q4)
        gprev = i // 4 - 1
        if gprev >= 0:
            attn_chunk(gprev // 4, gprev % 4, i % 4)
    glast = len(units) // 4 - 1
    for c in range(4):
        attn_chunk(glast // 4, glast % 4, c)
    P.close()


def phase_D(B, tag, yT, wname, resid, gname, hmid, hnT_name):
    nc, d, NSEQ = B.nc, B.t, B.NSEQ
    P = Phase(nc, tag)
    Wo = P.sb("Wo", [128, 8, D], BF16)
    P.dma("gpsimd", Wo[:], d[wname].rearrange("(k p) n -> p k n", p=128), w=["Wo"], key="Wo")
    identb = P.sb("identb", [128, 128], BF16)
    P.dma("sync", identb[:], d["ident_bf"], w=["identb"], key="identb")
    gbc = P.sb("gbc", [128, D], F32)
    P.dma("sync", gbc[:], d[gname], w=["gbc"], key="gbc")
    eps_t = P.sb("eps", [128, 1], F32)
    P.memset("vector", eps_t[:], EPS, ["eps"])
    yt = [P.sb("yt%d" % i, [128, 8, 512], BF16) for i in range(2)]
    xt = [P.sb("xt%d" % i, [128, D], F32) for i in range(2)]
    hm = [P.sb("hm%d" % i, [128, D], F32) for i in range(2)]
    junk = P.sb("junk", [128, D], F32)
    stat = [P.sb("stat%d" % i, [128, 4], F32) for i in range(2)]
    hn = [P.sb("hn%d" % i, [128, D], BF16) for i in range(2)]
    hnTg = [P.sb("hnTg%d" % i, [128, 8, 512], BF16) for i in range(2)]
    ps_o = [P.ps("pso%d" % i, [128, 1024]) for i in range(2)]
    ps_tr = [P.ps("ptr%d" % i, [128, 8, 128], BF16) for i in range(2)]
    ngr = NSEQ * 4

    def load_y(gi):
        b, g = gi // 4, gi % 4
        P.dma("sync", yt[gi % 2][:], d[yT][b, :, :, g * 512:(g + 1) * 512].rearrange("c p t -> p c t"), w=[("yt", gi % 2)], key=("yt", gi % 2))

    load_y(0)
    for gi in range(ngr):
        b, g = gi // 4, gi % 4
        gs = gi % 2
        if gi + 1 < ngr:
            load_y(gi + 1)
        for t4 in range(4):
            tt = g * 4 + t4
            r0 = b * SEQ + tt * 128
            tok = slice(t4 * 128, (t4 + 1) * 128)
            s2 = t4 % 2
            P.dma("sync", xt[s2][:], d[resid][r0:r0 + 128, :], w=[("x", s2)], key=("x", s2))
            for half in range(2):
                for c in range(8):
                    P.mm(ps_o[s2][:, half * 512:(half + 1) * 512], yt[gs][:, c, tok], Wo[:, c, half * 512:(half + 1) * 512],
                         c == 0, c == 7, [("yt", gs), "Wo"], [("pso", s2)], mark=(c == 7 and half == 1))
            for half in range(2):
                hs_ = slice(half * 512, (half + 1) * 512)
                P.tt("vector", hm[s2][:, hs_], ps_o[s2][:, hs_], xt[s2][:, hs_], ALU.add, [("pso", s2), ("x", s2)], [("hm", s2)])
            P.dma("sync", d[hmid][r0:r0 + 128, :], hm[s2][:], r=[("hm", s2)], key=("hm", s2), is_out=True)
            rms_to_hnT(P, hm[s2][:], ("hm", s2), gbc[:], eps_t, stat[s2], ("st", s2), junk, hn[s2][:], ("hn", s2),
                       ps_tr[s2], ("ptr", s2), identb, hnTg[gs], ("hnTg", gs), tok)
            if DEBUG_STOP == 77 and gi == 0:
                if t4 == 0:
                    dlog = P.sb("dlog", [128, 16], F32)
                    dhn = P.sb("dhn", [128, 4, D], BF16)
                P.copy("gpsimd", dlog[:, t4 * 4:(t4 + 1) * 4], stat[s2][:, 0:4], [("st", s2), ("hn", s2)], ["dlog"])
                P.copy("gpsimd", dhn[:, t4, :], hn[s2][:], [("hn", s2)], ["dhn"])
                if t4 == 3:
                    P.dma("sync", d["dbg"], dlog[:], r=["dlog"], key="dlog", is_out=True)
                    P.dma("sync", d["dbg2"].rearrange("t p c -> p t c"), dhn[:], r=["dhn"], key="dhn", is_out=True)
        P.dma("sync", d[hnT_name][:, :, gi * 512:(gi + 1) * 512].rearrange("c p t -> p c t"), hnTg[gs][:], r=[("hnTg", gs)],
              key=("hnTg", gs), is_out=True)
    P.close()


def phase_F(B, tag, layer, f0, h_in, h_out, hnT_name, final_g=None):
    nc, d, NSEQ = B.nc, B.t, B.NSEQ
    P = Phase(nc, tag)
    nf = 11
    Wg = P.sb("Wg", [128, 8, nf * 128], BF16)
    Wu = P.sb("Wu", [128, 8, nf * 128], BF16)
    Wd = P.sb("Wd", [128, nf, D], BF16)
    cs = slice(f0 * 128, (f0 + nf) * 128)
    P.dma("gpsimd", Wg[:], d["ffn_w_gate%d" % layer].rearrange("(k p) n -> p k n", p=128)[:, :, cs], w=["Wg"], key="Wg")
    P.dma("gpsimd", Wu[:], d["ffn_w_up%d" % layer].rearrange("(k p) n -> p k n", p=128)[:, :, cs], w=["Wu"], key="Wu")
    P.dma("gpsimd", Wd[:], d["ffn_w_down%d" % layer].rearrange("(f p) n -> p f n", p=128)[:, f0:f0 + nf, :], w=["Wd"], key="Wd")
    if final_g is not None:
        gbc = P.sb("gbc", [128, D], F32)
        P.dma("sync", gbc[:], d[final_g], w=["gbc"], key="gbc")
        eps_t = P.sb("eps", [128, 1], F32)
        P.memset("vector", eps_t[:], EPS, ["eps"])
        junk = P.sb("junk", [128, D], F32)
        stat = [P.sb("stat%d" % i, [128, 4], F32) for i in range(2)]
    hnTg = [P.sb("hnTg%d" % i, [128, 8, 512], BF16) for i in range(2)]
    actT = [P.sb("actT%d" % i, [128, nf, 512], BF16) for i in range(2)]
    sg = [P.sb("sg%d" % i, [128, 512], F32) for i in range(2)]
    hin = [P.sb("hin%d" % i, [128, D], F32) for i in range(2)]
    hout = [P.sb("hout%d" % i, [128, D], F32) for i in range(2)]
    ps_g = [P.ps("psg%d" % i, [128, 512]) for i in range(2)]
    ps_u = [P.ps("psu%d" % i, [128, 512]) for i in range(2)]
    ps_d = [P.ps("psd%d" % i, [128, 1024]) for i in range(2)]
    ngr = NSEQ * 4

    def load_h(gi):
        P.dma("sync", hnTg[gi % 2][:], d[hnT_name][:, :, gi * 512:(gi + 1) * 512].rearrange("c p t -> p c t"),
              w=[("hnTg", gi % 2)], key=("hnTg", gi % 2))

    load_h(0)
    for gi in range(ngr):
        gs = gi % 2
        if gi + 1 < ngr:
            load_h(gi + 1)
        for f in range(nf):
            s2 = f % 2
            for k in range(8):
                P.mm(ps_g[s2][:, :], Wg[:, k, f * 128:(f + 1) * 128], hnTg[gs][:, k, :], k == 0, k == 7, [("hnTg", gs), "Wg"], [("psg", s2)])
            for k in range(8):
                P.mm(ps_u[s2][:, :], Wu[:, k, f * 128:(f + 1) * 128], hnTg[gs][:, k, :], k == 0, k == 7, [("hnTg", gs), "Wu"], [("psu", s2)])
            P.act(sg[s2][:], ps_g[s2][:, :], AF.Silu, [("psg", s2)], [("sg", s2)])
            P.tt("vector", actT[gs][:, f, :], ps_u[s2][:, :], sg[s2][:], ALU.mult, [("psu", s2), ("sg", s2)], [("actT", gs)])
        for t4 in range(4):
            r0 = gi * 512 + t4 * 128
            tok = slice(t4 * 128, (t4 + 1) * 128)
            s2 = t4 % 2
            P.dma("sync", hin[s2][:], d[h_in][r0:r0 + 128, :], w=[("hin", s2)], key=("hin", s2))
            for half in range(2):
                for f in range(nf):
                    P.mm(ps_d[s2][:, half * 512:(half + 1) * 512], actT[gs][:, f, tok], Wd[:, f, half * 512:(half + 1) * 512],
                         f == 0, f == nf - 1, [("actT", gs), "Wd"], [("psd", s2)], mark=(f == nf - 1 and half == 1))
            for half in range(2):
                hs_ = slice(half * 512, (half + 1) * 512)
                P.tt("vector", hout[s2][:, hs_], ps_d[s2][:, hs_], hin[s2][:, hs_], ALU.add, [("psd", s2), ("hin", s2)], [("hout", s2)])
            if final_g is not None:
                sk = ("st", s2)
                P.act(junk[:], hout[s2][:], AF.Square, [("hout", s2)], ["junkA", sk], accum_out=stat[s2][:, 0:1])
                P.act(stat[s2][:, 1:2], stat[s2][:, 0:1], AF.Sqrt, [sk], [sk], scale=1.0 / D, bias=eps_t[:, 0:1])
                P.recip(stat[s2][:, 2:3], stat[s2][:, 1:2], [sk], [sk])
                P.stt(hout[s2][:], hout[s2][:], stat[s2][:, 2:3], gbc[:], ALU.mult, ALU.mult, [("hout", s2), sk, "gbc"], [("hout", s2)])
            P.dma("sync", d[h_out][r0:r0 + 128, :], hout[s2][:], r=[("hout", s2)], key=("hout", s2), is_out=True)
    P.close()


def phase_E(B):
    nc, d, NSEQ = B.nc, B.t, B.NSEQ
    P = Phase(nc, "E")
    Wc = P.sb("Wc", [128, 8, 3104], BF16)
    Wsrc = d["c_w_in"].rearrange("(k p) n -> p k n", p=128)
    WK = []
    P.memset("gpsimd", Wc[:, :, 3088:3104], 0.0, [("W", 2)])
    for i, (c0, c1) in enumerate(((0, 1024), (1024, 2048), (2048, 3088))):
        P.dma("gpsimd", Wc[:, :, c0:c1], Wsrc[:, :, c0:c1], w=[("W", i)], key=("W", i))
        WK.append(("W", i))
    identb = P.sb("identb", [128, 128], BF16)
    P.dma("sync", identb[:], d["ident_bf"], w=["identb"], key="identb")
    gbc = P.sb("gbc", [128, D], F32)
    P.dma("sync", gbc[:], d["g_mix1"], w=["gbc"], key="gbc")
    g2w = P.sb("g2w", [128, 512], F32)
    P.memset("vector", g2w[:], 0.0, ["g2w"])
    P.dma("sync", g2w[0:16, :], d["c_gate_w"], w=["g2w"], key="g2w")
    g2b = P.sb("g2b", [128, 512], F32)
    P.dma("sync", g2b[:], d["c_gate_b_bc"], w=["g2b"], key="g2b")
    eps_t = P.sb("eps", [128, 1], F32)
    P.memset("vector", eps_t[:], EPS, ["eps"])
    one_t = P.sb("one", [128, 1], F32)
    P.memset("vector", one_t[:], 1.0, ["one"])
    xt = [P.sb("xt%d" % i, [128, D], F32) for i in range(2)]
    junk = P.sb("junk", [128, D], F32)
    stat = [P.sb("stat%d" % i, [128, 4], F32) for i in range(2)]
    hn = [P.sb("hn%d" % i, [128, D], BF16) for i in range(2)]
    hnT = [P.sb("hnT%d" % i, [128, 8, 512], BF16) for i in range(2)]
    qkT = [P.sb("qkT%d" % i, [128, 8, 512], BF16) for i in range(2)]
    glrT = [P.sb("glrT%d" % i, [128, 512], F32) for i in range(2)]
    for i in range(2):
        P.memset("vector", glrT[i][:], 0.0, [("glrT", i)])
    kst = [P.sb("kst%d" % i, [128, 512], BF16) for i in range(2)]
    vst = [P.sb("vst%d" % i, [128, 1024], BF16) for i in range(2)]
    rst = [P.sb("rst%d" % i, [128, 1024], F32) for i in range(2)]
    lat = [P.sb("lat%d" % i, [128, 512], F32) for i in range(2)]
    lt1 = P.sb("lt1", [128, 512], F32)
    lt2 = P.sb("lt2", [128, 512], F32)
    ps_tr = P.ps("ptr", [128, 8, 128], BF16)
    ps_f = [P.ps("psf%d" % i, [128, 512]) for i in range(2)]
    ps_t = [P.ps("pst%d" % i, [128, 512]) for i in range(3)]
    ps_l = P.ps("psl", [128, 512])
    n_t = 0
    n_f = 0
    n_tiles = NSEQ * NT

    def load_x(ti):
        P.dma("sync", xt[ti % 2][:], d["h1"][ti * 128:(ti + 1) * 128, :], w=[("x", ti % 2)], key=("x", ti % 2))

    load_x(0)
    for gi in range(NSEQ * 4):
        b, g = gi // 4, gi % 4
        gs = gi % 2
        for t4 in range(4):
            ti = gi * 4 + t4
            if ti + 1 < n_tiles:
                load_x(ti + 1)
            xs = ti % 2
            rms_to_hnT(P, xt[xs][:], ("x", xs), gbc[:], eps_t, stat[xs], ("st", xs), junk, hn[xs][:], ("hn", xs),
                       ps_tr, "ptr", identb, hnT[gs], ("hnT", gs), slice(t4 * 128, (t4 + 1) * 128))
        for oc in range(8):
            r = n_f % 2
            n_f += 1
            for k in range(8):
                P.mm(ps_f[r][:, :], Wc[:, k, oc * 128:(oc + 1) * 128], hnT[gs][:, k, :], k == 0, k == 7, [("hnT", gs)] + WK, [("psf", r)])
            if oc < 4:
                P.act(qkT[gs][:, oc, :], ps_f[r][:, :], AF.Copy, [("psf", r)], [("qkT", gs)], scale=128 ** -0.5)
            else:
                P.copy("vector", qkT[gs][:, oc, :], ps_f[r][:, :], [("psf", r)], [("qkT", gs)])
        r = n_f % 2
        n_f += 1
        for k in range(8):
            P.mm(ps_f[r][0:32, :], Wc[:, k, 3072:3104], hnT[gs][:, k, :], k == 0, k == 7, [("hnT", gs)] + WK, [("psf", r)])
        P.copy("vector", glrT[gs][0:32, :], ps_f[r][0:32, :], [("psf", r)], [("glrT", gs)])
        gsl = slice(g * 512, (g + 1) * 512)
        P.dma("sync", d["c_qT"][b, :, :, gsl].rearrange("c p t -> p c t"), qkT[gs][:, 0:4, :], r=[("qkT", gs)], key=("qTo", gs), is_out=True)
        P.dma("sync", d["c_kT"][b, :, :, gsl].rearrange("c p t -> p c t"), qkT[gs][:, 4:8, :], r=[("qkT", gs)], key=("kTo", gs), is_out=True)
        for t4 in range(4):
            r0 = gi * 512 + t4 * 128
            tok = slice(t4 * 128, (t4 + 1) * 128)
            s2 = t4 % 2
            jobs = [(512, 1024, kst[s2][:, :], ("kst", s2), "k"),
                    (1024, 1536, vst[s2][:, 0:512], ("vst", s2), "v"), (1536, 2048, vst[s2][:, 512:1024], ("vst", s2), "v"),
                    (2048, 2560, rst[s2][:, 0:512], ("rst", s2), "r"), (2560, 3072, rst[s2][:, 512:1024], ("rst", s2), "r")]
            for (c0, c1, dst, dk, kind) in jobs:
                r = n_t % 3
                n_t += 1
                for k in range(8):
                    P.mm(ps_t[r][:, :], hnT[gs][:, k, tok], Wc[:, k, c0:c1], k == 0, k == 7, [("hnT", gs)] + WK, [("pst", r)])
                if kind == "r":
                    P.act(dst, ps_t[r][:, :], AF.Silu, [("pst", r)], [dk])
                elif kind == "v":
                    P.copy("vector", dst, ps_t[r][:, :], [("pst", r)], [dk])
                else:
                    P.copy("scalar", dst, ps_t[r][:, :], [("pst", r)], [dk])
            P.dma("sync", d["c_ktok"][r0:r0 + 128, :], kst[s2][:], r=[("kst", s2)], key=("kst", s2), is_out=True)
            P.dma("sync", d["c_v"][r0:r0 + 128, :], vst[s2][:], r=[("vst", s2)], key=("vst", s2), is_out=True)
            P.dma("sync", d["c_sr"][r0:r0 + 128, :], rst[s2][:], r=[("rst", s2)], key=("rst", s2), is_out=True)
            P.mm(ps_l[:, :], glrT[gs][:, tok], g2w[:, :], True, True, [("glrT", gs), "g2w"], ["psl"])
            P.tt("vector", lt1[:], ps_l[:, :], g2b[:], ALU.add, ["psl", "g2b"], ["lt1"])
            P.act(lt2[:], lt1[:], AF.Exp, ["lt1"], ["lt2"], scale=-1.0)
            P.act(lt1[:], lt2[:], AF.Ln, ["lt2", "one"], ["lt1"], bias=one_t[:, 0:1], scale=1.0)
            P.ts("gpsimd", lat[s2][:], lt1[:], -1.0 / 16.0, ALU.mult, ["lt1"], [("lat", s2)])
            P.dma("sync", d["c_la"][r0:r0 + 128, :], lat[s2][:], r=[("lat", s2)], key=("lat", s2), is_out=True)
    P.close()


def phase_G(B):
    nc, d, NSEQ = B.nc, B.t, B.NSEQ
    P = Phase(nc, "G")
    identb = P.sb("identb", [128, 128], BF16)
    P.dma("sync", identb[:], d["ident_bf"], w=["identb"], key="identb")
    U = P.sb("U", [128, 128], F32)
    SL = P.sb("SL", [128, 128], F32)
    P.dma("sync", U[:], d["tri_u"], w=["U"], key="U")
    P.dma("sync", SL[:], d["tri_sl"], w=["SL"], key="SL")
    gon = P.sb("gon", [128, D], F32)
    P.dma("sync", gon[:], d["c_onorm_g_bc"], w=["gon"], key="gon")
    eps_t = P.sb("eps", [128, 1], F32)
    P.memset("vector", eps_t[:], EPS, ["eps"])
    qT = P.sb("qT", [128, 4, SEQ], BF16)
    kT = P.sb("kT", [128, 4, SEQ], BF16)
    la = [P.sb("la%d" % i, [128, 512], F32) for i in range(2)]
    kt_ = [P.sb("ktk%d" % i, [128, 512], BF16) for i in range(2)]
    vt = [P.sb("vt%d" % i, [128, 1024], BF16) for i in range(2)]
    sr = [P.sb("sr%d" % i, [128, 1024], F32) for i in range(2)]
    eb = [P.sb("eb%d" % i, [128, 512], F32) for i in range(2)]
    enb = P.sb("enb", [128, 512], F32)
    erv = P.sb("erv", [128, 512], F32)
    qtT = [P.sb("qtT%d" % i, [128, 512], BF16) for i in range(2)]
    ktT = [P.sb("ktT%d" % i, [128, 512], BF16) for i in range(2)]
    ks = [P.sb("ks%d" % i, [128, 512], BF16) for i in range(2)]
    attm = [P.sb("attm%d" % i, [128, 128], BF16) for i in range(2)]
    state = P.sb("state", [128, 4, 256], F32)
    stb = P.sb("stb", [128, 4, 256], BF16)
    gr = [P.sb("gr%d" % i, [128, 1024], F32) for i in range(2)]
    stat = [P.sb("stat%d" % i, [128, 12], F32) for i in range(2)]
    junk = P.sb("junk", [128, 256], F32)
    yc = [P.sb("yc%d" % i, [128, 1024], BF16) for i in range(2)]
    ycT = [P.sb("ycT%d" % i, [128, 8, 512], BF16) for i in range(2)]
    ps_b = P.ps("psb", [128, 512])
    ps_r = P.ps("psr", [128, 512])
    ps_a = P.ps("psa", [128, 4, 128])
    ps_o = P.ps("pso", [128, 1024])
    ps_kv = P.ps("pskv", [128, 1024])
    ps_tr = P.ps("ptr", [128, 8, 128], BF16)
    nch = NSEQ * NT

    def load_chunk(ci):
        s2 = ci % 2
        r0 = ci * 128
        P.dma("sync", la[s2][:], d["c_la"][r0:r0 + 128, :], w=[("la", s2)], key=("la", s2))
        P.dma("sync", kt_[s2][:], d["c_ktok"][r0:r0 + 128, :], w=[("ktk", s2)], key=("ktk", s2))
        P.dma("sync", vt[s2][:], d["c_v"][r0:r0 + 128, :], w=[("vt", s2)], key=("vt", s2))
        P.dma("sync", sr[s2][:], d["c_sr"][r0:r0 + 128, :], w=[("sr", s2)], key=("sr", s2))

    load_chunk(0)
    for ci in range(nch):
        b, c = ci // NT, ci % NT
        s2 = ci % 2
        if c == 0:
            P.dma("sync", qT[:], d["c_qT"][b].rearrange("c p t -> p c t"), w=["qT"], key="qT")
            P.dma("sync", kT[:], d["c_kT"][b].rearrange("c p t -> p c t"), w=["kT"], key="kT")
            P.memset("vector", state[:], 0.0, [("state", h) for h in range(4)])
            P.memset("gpsimd", stb[:], 0.0, [("stb", h) for h in range(4)])
        if ci + 1 < nch:
            load_chunk(ci + 1)
        csl = slice(c * 128, (c + 1) * 128)
        for h in range(4):
            P.mm(ps_b[:, h * 128:(h + 1) * 128], la[s2][:, h * 128:(h + 1) * 128], U[:], True, True, [("la", s2), "U"], ["psb"], mark=(h == 3))
        P.mm(ps_r[:, :], SL[:], la[s2][:, :], True, True, [("la", s2), "SL"], ["psr"])
        P.act(eb[s2][:], ps_b[:, :], AF.Exp, ["psb"], [("eb", s2)])
        P.act(enb[:], ps_b[:, :], AF.Exp, ["psb"], ["enb"], scale=-1.0)
        P.act(erv[:], ps_r[:, :], AF.Exp, ["psr"], ["erv"])
        qv = qT[:, :, csl]
        kv_ = kT[:, :, csl]
        P.tt("vector", qtT[s2][:].rearrange("p (h t) -> p h t", t=128), qv, eb[s2][:].rearrange("p (h t) -> p h t", t=128), ALU.mult,
             ["qT", ("eb", s2)], [("qtT", s2)])
        P.tt("gpsimd", ktT[s2][:].rearrange("p (h t) -> p h t", t=128), kv_, enb[:].rearrange("p (h t) -> p h t", t=128), ALU.mult,
             ["kT", "enb"], [("ktT", s2)])
        P.tt("gpsimd", ks[s2][:], kt_[s2][:], erv[:], ALU.mult, [("ktk", s2), "erv"], [("ks", s2)])
        P.tt("gpsimd", gr[s2][:], sr[s2][:], gon[:], ALU.mult, [("sr", s2), "gon"], [("gr", s2)])
        for h in range(4):
            hs = slice(h * 128, (h + 1) * 128)
            vs = slice(h * 256, (h + 1) * 256)
            a2 = h % 2
            P.mm(ps_a[:, h, :], ktT[s2][:, hs], qtT[s2][:, hs], True, True, [("ktT", s2), ("qtT", s2)], [("psa", h)])
            P.tt("vector", attm[a2][:], ps_a[:, h, :], U[:], ALU.mult, [("psa", h), "U"], [("attm", a2)])
            P.mm(ps_o[:, vs], attm[a2][:], vt[s2][:, vs], True, False, [("attm", a2), ("vt", s2)], [("pso", h)], mark=False)
            P.mm(ps_o[:, vs], qtT[s2][:, hs], stb[:, h, :], False, True, [("qtT", s2), ("stb", h)], [("pso", h)])
            P.mm(ps_kv[:, vs], ks[s2][:, hs], vt[s2][:, vs], True, True, [("ks", s2), ("vt", s2)], [("pskv", h)])
            P.stt(state[:, h, :], state[:, h, :], eb[s2][:, h * 128 + 127:h * 128 + 128], ps_kv[:, vs], ALU.mult, ALU.add,
                  [("state", h), ("eb", s2), ("pskv", h)], [("state", h)])
            P.copy("scalar", stb[:, h, :], state[:, h, :], [("state", h)], [("stb", h)])
        sk = ("st", s2)
        for h in range(4):
            vs = slice(h * 256, (h + 1) * 256)
            P.act(junk[:], ps_o[:, vs], AF.Square, [("pso", h)], ["junkA", sk], accum_out=stat[s2][:, h:h + 1])
        P.act(stat[s2][:, 4:8], stat[s2][:, 0:4], AF.Sqrt, [sk], [sk], scale=1.0 / 256, bias=eps_t[:, 0:1])
        P.recip(stat[s2][:, 8:12], stat[s2][:, 4:8], [sk], [sk])
        for h in range(4):
            vs = slice(h * 256, (h + 1) * 256)
            P.stt(yc[s2][:, vs], ps_o[:, vs], stat[s2][:, 8 + h:9 + h], gr[s2][:, vs], ALU.mult, ALU.mult,
                  [("pso", h), sk, ("gr", s2)], [("yc", s2)])
        for k in range(8):
            P.tr(ps_tr[:, k, :], yc[s2][:, k * 128:(k + 1) * 128], identb[:], [("yc", s2), "identb"], ["ptr"], mark=(k == 7))
        g4 = (ci // 4) % 2
        P.copy("scalar", ycT[g4][:, 0:8, (ci % 4) * 128:(ci % 4 + 1) * 128], ps_tr[:, 0:8, :], ["ptr"], [("ycT", g4)])
        if ci % 4 == 3:
            g = c // 4
            P.dma("sync", d["ycT"][b, :, :, g * 512:(g + 1) * 512].rearrange("c p t -> p c t"), ycT[g4][:], r=[("ycT", g4)],
                  key=("ycT", g4), is_out=True)
    P.close()


def declare(B):
    NSEQ = B.NSEQ
    T = NSEQ * SEQ
    X = "ExternalInput"
    B.dram("x", [T, D], F32, X)
    B.dram("ab_w_in", [D, 2248], F32, X)
    B.dram("ab_conv_w", [31, 512], F32, X)
    B.dram("conv_prm", [128, 12], F32, X)
    B.dram("ab_w_out", [D, D], F32, X)
    B.dram("c_w_in", [D, 3088], F32, X)
    B.dram("c_gate_w", [16, 512], F32, X)
    B.dram("c_gate_b_bc", [128, 512], F32, X)
    B.dram("c_onorm_g_bc", [128, D], F32, X)
    B.dram("c_w_out", [D, D], F32, X)
    for l in range(2):
        B.dram("ffn_w_gate%d" % l, [D, DFF], F32, X)
        B.dram("ffn_w_up%d" % l, [D, DFF], F32, X)
        B.dram("ffn_w_down%d" % l, [DFF, D], F32, X)
        B.dram("g_mix%d" % l, [128, D], F32, X)
        B.dram("g_ffn%d" % l, [128, D], F32, X)
    B.dram("g_final", [128, D], F32, X)
    B.dram("ident_bf", [128, 128], BF16, X)
    B.dram("ident_f32", [128, 128], F32, X)
    B.dram("cos128", [SEQ, 128], F32, X)
    B.dram("sin128", [SEQ, 128], F32, X)
    B.dram("negmask", [128, 128], F32, X)
    B.dram("pow2", [128, NIT], F32, X)
    B.dram("tri_u", [128, 128], F32, X)
    B.dram("tri_sl", [128, 128], F32, X)
    B.dram("hconvT", [NSEQ, 4, 128, SEQ], BF16)
    B.dram("qT", [NSEQ, 4, 128, SEQ], BF16)
    B.dram("iqT", [NSEQ, 4, 128, SEQ], BF16)
    B.dram("kkT", [NSEQ, 2, 128, SEQ], BF16)
    B.dram("vaug", [T, 256], BF16)
    B.dram("iw", [T, 8], F32)
    B.dram("yabT", [NSEQ, 8, 128, SEQ], BF16)
    B.dram("h_mid0", [T, D], F32)
    B.dram("hnT", [8, 128, T], BF16)
    B.dram("h_half", [T, D], F32)
    B.dram("h1", [T, D], F32)
    B.dram("c_qT", [NSEQ, 4, 128, SEQ], BF16)
    B.dram("c_kT", [NSEQ, 4, 128, SEQ], BF16)
    B.dram("c_ktok", [T, 512], BF16)
    B.dram("c_v", [T, D], BF16)
    B.dram("c_sr", [T, D], F32)
    B.dram("c_la", [T, 512], F32)
    B.dram("ycT", [NSEQ, 8, 128, SEQ], BF16)
    B.dram("h_mid1", [T, D], F32)
    B.dram("out", [T, D], F32, "ExternalOutput")
    if DEBUG_STOP == 77:
        B.dram("dbg", [128, 16], F32, "ExternalOutput")
        B.dram("dbg2", [4, 128, D], BF16, "ExternalOutput")


PHASES = {
    "A": phase_A,
    "B": phase_B,
    "C": phase_C,
    "D0": lambda B: phase_D(B, "D0", "yabT", "ab_w_out", "x", "g_ffn0", "h_mid0", "hnT"),
    "F0a": lambda B: phase_F(B, "F0a", 0, 0, "h_mid0", "h_half", "hnT"),
    "F0b": lambda B: phase_F(B, "F0b", 0, 11, "h_half", "h1", "hnT"),
    "E": phase_E,
    "G": phase_G,
    "D1": lambda B: phase_D(B, "D1", "ycT", "c_w_out", "h1", "g_ffn1", "h_mid1", "hnT"),
    "F1a": lambda B: phase_F(B, "F1a", 1, 0, "h_mid1", "h_half", "hnT"),
    "F1b": lambda B: phase_F(B, "F1b", 1, 11, "h_half", "out", "hnT", final_g="g_final"),
}
ORDER = ["A", "B", "C", "D0", "F0a", "F0b", "E", "G", "D1", "F1a", "F1b"]


def host_consts():
    bf = ml_dtypes.bfloat16
    c = {}
    c["ident_bf"] = np.eye(128, dtype=np.float32).astype(bf)
    c["ident_f32"] = np.eye(128, dtype=np.float32)
    rot = 16
    inv = (500000.0 ** (-np.arange(0, rot, 2, dtype=np.float32) / np.float32(rot))).astype(np.float32)
    ang = (np.arange(SEQ, dtype=np.float32)[:, None] * inv[None, :]).astype(np.float32)
    c["cos128"] = np.ascontiguousarray(np.tile(np.cos(ang).astype(np.float32), (1, 16)))
    c["sin128"] = np.ascontiguousarray(np.tile(np.sin(ang).astype(np.float32), (1, 16)))
    i = np.arange(128)
    c["negmask"] = np.where(i[None, :] <= i[:, None], 0.0, -1e30).astype(np.float32)
    c["pow2"] = np.ascontiguousarray(np.broadcast_to((0.5 ** np.arange(1, NIT + 1)).astype(np.float32), (128, NIT)))
    c["tri_u"] = (i[:, None] <= i[None, :]).astype(np.float32)
    c["tri_sl"] = (i[:, None] > i[None, :]).astype(np.float32)
    return c


def host_weights(inp):
    f = lambda a: np.ascontiguousarray(np.asarray(a, dtype=np.float32))
    bc = lambda v, n: np.ascontiguousarray(np.broadcast_to(np.asarray(v, np.float32).reshape(1, -1), (128, n)))
    w = {}
    w["ab_w_in"] = f(inp["ab_w_in"][0])
    w["ab_conv_w"] = f(inp["ab_conv_w"][0].reshape(31, 512))
    prm = np.concatenate([np.asarray(inp[k][0], np.float32).reshape(4, 128).T for k in ("ab_conv_b", "ab_ln_g", "ab_ln_b")], axis=1)
    w["conv_prm"] = f(prm)
    w["ab_w_out"] = f(inp["ab_w_out"][0])
    w["c_w_in"] = f(inp["c_w_in"][0])
    w["c_gate_w"] = f(inp["c_gate_w"][0])
    w["c_gate_b_bc"] = bc(inp["c_gate_b"][0], 512)
    w["c_onorm_g_bc"] = bc(inp["c_onorm_g"][0], D)
    w["c_w_out"] = f(inp["c_w_out"][0])
    for l in range(2):
        w["ffn_w_gate%d" % l] = f(inp["ffn_w_gate"][l])
        w["ffn_w_up%d" % l] = f(inp["ffn_w_up"][l])
        w["ffn_w_down%d" % l] = f(inp["ffn_w_down"][l])
        w["g_mix%d" % l] = bc(inp["norm_mix_g"][l], D)
        w["g_ffn%d" % l] = bc(inp["norm_ffn_g"][l], D)
    w["g_final"] = bc(inp["final_norm_g"], D)
    return w


def build_program(nseq, phases, ext=None):
    B = Build(nseq, ext)
    declare(B)
    for p in phases:
        PHASES[p](B)
    return B


def kernel(**inp):
    n = 8
    nseq = 2
    x = np.asarray(inp["x"], dtype=np.float32)
    B = build_program(nseq, ORDER)
    shared = host_consts()
    shared.update(host_weights(inp))
    in_maps = []
    for c in range(n):
        m = dict(shared)
        m["x"] = np.ascontiguousarray(x[c * nseq:(c + 1) * nseq].reshape(nseq * SEQ, D))
        in_maps.append(m)
    res = run_bass_kernel_spmd(B.nc, in_maps, core_ids=list(range(n)))
    out = np.stack([np.asarray(r["out"], dtype=np.float32).reshape(nseq, SEQ, D) for r in res.results], axis=0)
    return out.reshape(16, SEQ, D)
```

```python
import contextlib
import math
import numpy as np
import ml_dtypes
import concourse.bass as bass
import concourse.mybir as mybir
from concourse.bass_utils import run_bass_kernel_spmd

F32 = mybir.dt.float32
BF16 = mybir.dt.bfloat16
ALU = mybir.AluOpType
AF = mybir.ActivationFunctionType
AX = mybir.AxisListType

ENGS = ["sync", "scalar", "vector", "gpsimd", "tensor"]
D = 1024
SEQ = 2048
NT = 16
EPS = 1e-6
DFF = 2816
NFF = 22
NIT = 16
TOPK = 256
DEBUG_STOP = 99
SAME_SYNC = True


class Sched:
    def __init__(self, nc, stack, tag=""):
        self.nc = nc
        self.stack = stack
        self.tag = tag
        self.q = {e: [] for e in ENGS}
        self.sem = {}
        self.cnt = {}
        self.waited = {e: {} for e in ENGS}
        self.lastw = {}
        self.readers = {}
        self.pe_pending = False
        self.out_deps = {}
        for e in ENGS:
            self._mksem("E_" + e)

    def _mksem(self, name):
        if name not in self.sem:
            hw_name = "%s_s%d" % (self.tag, len(self.sem))
            self.sem[name] = self.nc.alloc_semaphore(name=hw_name)
            self.cnt[name] = 0
        return name

    def op(self, eng, fn, reads=(), writes=(), dma=None, mark=True, is_out=False, force_self=False):
        deps = {}

        def add(d):
            if d[1] > deps.get(d[0], 0):
                deps[d[0]] = d[1]

        for k in reads:
            if k in self.lastw:
                add(self.lastw[k])
        for k in writes:
            if k in self.lastw:
                add(self.lastw[k])
            for s_, v_ in self.readers.get(k, {}).items():
                add((s_, v_))
        if dma is not None:
            s = self._mksem("D_" + str(dma))
            if self.cnt[s] > 0:
                add((s, self.cnt[s]))
        own = "E_" + eng
        need = []
        for s_, v_ in deps.items():
            if s_ == own and (eng == "tensor" or not (SAME_SYNC or force_self)):
                continue
            if self.waited[eng].get(s_, 0) >= v_:
                continue
            need.append((s_, v_))
        attach = None
        if eng == "tensor":
            k0 = reads[0] if len(reads) else None
            if k0 is not None and k0 in self.lastw and self.lastw[k0][0] != own:
                attach = self.lastw[k0]
        else:
            selfs = [d_ for d_ in need if d_[0] == own]
            attach = selfs[0] if selfs else (need[-1] if need else None)
        standalone = [d_ for d_ in need if attach is None or d_[0] != attach[0]]
        if attach is not None:
            for d_ in need:
                if d_[0] == attach[0] and d_[1] > attach[1]:
                    attach = d_
        for d_ in need:
            self.waited[eng][d_[0]] = max(self.waited[eng].get(d_[0], 0), d_[1])
        if attach is not None:
            self.waited[eng][attach[0]] = max(self.waited[eng].get(attach[0], 0), attach[1])
        if dma is not None:
            self.cnt[s] += 16
            dep = (s, self.cnt[s])
            self.q[eng].append(("op", fn, s, 16, standalone, attach))
        else:
            s = own
            if mark:
                self.cnt[s] += 1
                dep = (s, self.cnt[s])
                self.q[eng].append(("op", fn, s, 1, standalone, attach))
                if eng == "tensor":
                    self.pe_pending = False
            else:
                assert eng == "tensor"
                dep = (s, self.cnt[s] + 1)
                self.q[eng].append(("op", fn, None, 0, standalone, attach))
                self.pe_pending = True
        for k in writes:
            self.lastw[k] = dep
            self.readers[k] = {}
        for k in reads:
            r = self.readers.setdefault(k, {})
            if dep[1] > r.get(dep[0], 0):
                r[dep[0]] = dep[1]
        if is_out:
            if dep[1] > self.out_deps.get(dep[0], 0):
                self.out_deps[dep[0]] = dep[1]
        return dep

    def finalize(self):
        assert not self.pe_pending, "unmarked PE op at end of phase"
        tail = []
        for s_, v_ in self.out_deps.items():
            if self.waited["sync"].get(s_, 0) < v_:
                tail.append((s_, v_))
        nc = self.nc
        with nc.Block() as block:
            for eng in ENGS:
                items = self.q[eng]

                def body(e, items=items, eng=eng):
                    for it in items:
                        for (ws, wv) in it[4]:
                            e.wait_ge(self.sem[ws], wv)
                        ins = it[1](e)
                        if it[5] is not None:
                            ins._wait_ge(self.sem[it[5][0]], it[5][1])
                        if it[2] is not None:
                            ins.then_inc(self.sem[it[2]], it[3])
                    if eng == "sync":
                        for (ws, wv) in tail:
                            e.wait_ge(self.sem[ws], wv)

                getattr(block, eng)(body)


class Phase:
    def __init__(self, nc, tag):
        self.nc = nc
        self.tag = tag
        self.st = contextlib.ExitStack()
        self.st.enter_context(nc.cleanup_on_exit())
        self.S = Sched(nc, self.st, tag)

    def sb(self, name, shape, dt):
        return self.st.enter_context(self.nc.sbuf_tensor(self.tag + "_" + name, shape, dt))

    def ps(self, name, shape, dt=F32):
        return self.st.enter_context(self.nc.psum_tensor(self.tag + "_" + name, shape, dt))

    def dma(self, eng, out, in_, r=(), w=(), key=None, is_out=False):
        return self.S.op(eng, lambda e: e.dma_start(out=out, in_=in_), r, w, dma=key, is_out=is_out)

    def mm(self, out, lhsT, rhs, start, stop, r, w, mark=None):
        if mark is None:
            mark = stop
        return self.S.op("tensor", lambda e: e.matmul(out, lhsT=lhsT, rhs=rhs, start=start, stop=stop), r, w, mark=mark)

    def tr(self, out, in_, ident, r, w, mark=True):
        return self.S.op("tensor", lambda e: e.transpose(out=out, in_=in_, identity=ident), r, w, mark=mark)

    def act(self, out, in_, func, r, w, **kw):
        return self.S.op("scalar", lambda e: e.activation(out=out, in_=in_, func=func, **kw), r, w)

    def tt(self, eng, out, in0, in1, op, r, w):
        return self.S.op(eng, lambda e: e.tensor_tensor(out=out, in0=in0, in1=in1, op=op), r, w)

    def ts(self, eng, out, in0, s1, op0, r, w, s2=None, op1=None, accum_out=None):
        if op1 is None:
            return self.S.op(eng, lambda e: e.tensor_scalar(out=out, in0=in0, scalar1=s1, scalar2=None, op0=op0), r, w)
        return self.S.op(eng, lambda e: e.tensor_scalar(out=out, in0=in0, scalar1=s1, scalar2=s2, op0=op0, op1=op1,
                                                       accum_out=accum_out), r, w)

    def stt(self, out, in0, scalar, in1, op0, op1, r, w):
        return self.S.op("vector", lambda e: e.scalar_tensor_tensor(out=out, in0=in0, scalar=scalar, in1=in1,
                                                                     op0=op0, op1=op1), r, w)

    def copy(self, eng, out, in_, r, w):
        if eng == "scalar":
            return self.S.op(eng, lambda e: e.copy(out=out, in_=in_), r, w)
        return self.S.op(eng, lambda e: e.tensor_copy(out=out, in_=in_), r, w)

    def recip(self, out, in_, r, w):
        return self.S.op("vector", lambda e: e.reciprocal(out=out, in_=in_), r, w)

    def memset(self, eng, ap, val, w):
        return self.S.op(eng, lambda e: e.memset(ap, val), (), w)

    def reduce(self, out, in_, op, r, w):
        return self.S.op("vector", lambda e: e.tensor_reduce(out=out, in_=in_, axis=AX.X, op=op), r, w)

    def close(self):
        self.S.finalize()
        self.st.close()


class Build:
    def __init__(self, nseq, ext=None):
        self.nc = bass.Bass("TRN2", target_bir_lowering=False)
        self.NSEQ = nseq
        self.t = {}
        self.ext = ext or {}
        self.kinds = {}

    def dram(self, name, shape, dt, kind="Internal"):
        kind = self.ext.get(name, kind)
        self.kinds[name] = (kind, list(shape), dt)
        self.t[name] = self.nc.dram_tensor(name, list(shape), dt, kind=kind).ap()
        return self.t[name]


def rms_to_hnT(P, xt, xkey, gbc, eps_t, stat, skey, junk, hn, hkey, ps_tr, pkey, identb, dst, dkey, sl):
    P.act(junk[:], xt, AF.Square, [xkey], ["junkA", skey], accum_out=stat[:, 0:1])
    P.act(stat[:, 1:2], stat[:, 0:1], AF.Sqrt, [skey, "eps"], [skey], scale=1.0 / D, bias=eps_t[:, 0:1])
    P.recip(stat[:, 2:3], stat[:, 1:2], [skey], [skey])
    P.stt(hn, xt, stat[:, 2:3], gbc, ALU.mult, ALU.mult, [xkey, skey, "gbc"], [hkey])
    for k in range(8):
        P.tr(ps_tr[:, k, :], hn[:, k * 128:(k + 1) * 128], identb[:], [hkey, "identb"], [pkey], mark=(k == 7))
    P.copy("scalar", dst[:, 0:8, sl], ps_tr[:, 0:8, :], [pkey], [dkey])


def phase_A(B):
    nc, d, NSEQ = B.nc, B.t, B.NSEQ
    P = Phase(nc, "A")
    Wt = P.sb("Wt", [128, 8, 2376], BF16)
    Wsrc = d["ab_w_in"].rearrange("(k p) n -> p k n", p=128)
    segs = [(0, 1024, 0), (1024, 1536, 1024), (1664, 2176, 1536), (1536, 1600, 2048), (1536, 1600, 2112),
            (2176, 2240, 2176), (2176, 2240, 2240), (1600, 1664, 2304), (2240, 2248, 2368)]
    WK = []
    for i, (c0, c1, d0) in enumerate(segs):
        P.dma("gpsimd", Wt[:, :, d0:d0 + c1 - c0], Wsrc[:, :, c0:c1], w=[("W", i)], key=("W", i))
        WK.append(("W", i))
    identb = P.sb("identb", [128, 128], BF16)
    P.dma("sync", identb[:], d["ident_bf"], w=["identb"], key="identb")
    gbc = P.sb("gbc", [128, D], F32)
    P.dma("sync", gbc[:], d["g_mix0"], w=["gbc"], key="gbc")
    cos = P.sb("cos", [128, NT, 128], F32)
    sin = P.sb("sin", [128, NT, 128], F32)
    P.dma("sync", cos[:], d["cos128"].rearrange("(t p) c -> p t c", p=128), w=["cos"], key="cos")
    P.dma("sync", sin[:], d["sin128"].rearrange("(t p) c -> p t c", p=128), w=["sin"], key="sin")
    eps_t = P.sb("eps", [128, 1], F32)
    P.memset("vector", eps_t[:], EPS, ["eps"])

    xt = [P.sb("xt%d" % i, [128, D], F32) for i in range(2)]
    junk = P.sb("junk", [128, D], F32)
    stat = [P.sb("stat%d" % i, [128, 4], F32) for i in range(2)]
    hn = [P.sb("hn%d" % i, [128, D], BF16) for i in range(2)]
    hnT = [P.sb("hnT%d" % i, [128, 8, 512], BF16) for i in range(2)]
    ps_tr = [P.ps("ptr%d" % i, [128, 8, 128], BF16) for i in range(2)]
    ps_a = P.ps("psa", [128, 512])
    ps_g = P.ps("psg", [128, 512])
    ps_q = P.ps("psq", [128, 1024])
    ps_c = P.ps("psc", [128, 512])
    sig = [P.sb("sig%d" % i, [128, 512], F32) for i in range(2)]
    hc = [P.sb("hc%d" % i, [128, 512], BF16) for i in range(2)]
    tmp = [P.sb("tmp%d" % i, [128, 128], F32) for i in range(4)]
    qr = [P.sb("qr%d" % i, [128, 1024], BF16) for i in range(2)]
    kr = [P.sb("kr%d" % i, [128, 256], BF16) for i in range(2)]
    vaug = [P.sb("vaug%d" % i, [128, 256], BF16) for i in range(2)]
    iwt = [P.sb("iw%d" % i, [128, 8], F32) for i in range(2)]
    for i in range(2):
        P.memset("gpsimd", vaug[i][:, 64:192], 1.0, [("vaug", i)])
    qTg = [P.sb("qTg%d" % i, [128, 8, 512], BF16) for i in range(2)]
    kTg = [P.sb("kTg%d" % i, [128, 2, 512], BF16) for i in range(2)]
    w_scale = (8 ** -0.5) * (64 ** -0.5)

    def load_x(b, tt):
        xs = tt % 2
        r0 = b * SEQ + tt * 128
        P.dma("sync", xt[xs][:], d["x"][r0:r0 + 128, :], w=[("x", xs)], key=("x", xs))

    n_tiles = NSEQ * NT
    if DEBUG_STOP <= 1:
        P.dma("sync", d["iw"][0:128, :], iwt[0][:], r=[("iw", 0), "cos", "sin", "gbc", "identb"] + WK, key=("iw", 0), is_out=True)
        P.close()
        return
    load_x(0, 0)
    for gi in range(NSEQ * 4):
        b, g = gi // 4, gi % 4
        gs = gi % 2
        if DEBUG_STOP <= 6 and gi >= max(1, DEBUG_STOP - 3):
            break
        for t4 in range(4):
            tt = g * 4 + t4
            ti = gi * 4 + t4
            if ti + 1 < n_tiles and not (DEBUG_STOP <= 6 and ti + 1 >= 4 * max(1, DEBUG_STOP - 3)):
                load_x((ti + 1) // NT, (ti + 1) % NT)
            xs = tt % 2
            rms_to_hnT(P, xt[xs][:], ("x", xs), gbc[:], eps_t, stat[xs], ("st", xs), junk, hn[xs][:], ("hn", xs),
                       ps_tr[0], "ptr0", identb, hnT[gs], ("hnT", gs), slice(t4 * 128, (t4 + 1) * 128))
        if DEBUG_STOP <= 2:
            P.dma("sync", d["qT"][b, :, :, 0:512].rearrange("c p t -> p c t"), hnT[gs][:, 0:4, :], r=[("hnT", gs)], key=("qTg", gs), is_out=True)
            break
        for c in range(4):
            for k in range(8):
                P.mm(ps_a[:, :], Wt[:, k, c * 128:(c + 1) * 128], hnT[gs][:, k, :], k == 0, k == 7,
                     [("hnT", gs)] + WK, ["psa"])
            for k in range(8):
                P.mm(ps_g[:, :], Wt[:, k, 512 + c * 128:512 + (c + 1) * 128], hnT[gs][:, k, :], k == 0, k == 7,
                     [("hnT", gs)] + WK, ["psg"])
            s2 = c % 2
            P.act(sig[s2][:], ps_g[:, :], AF.Sigmoid, ["psg"], [("sig", s2)])
            P.tt("vector", hc[s2][:], ps_a[:, :], sig[s2][:], ALU.mult, ["psa", ("sig", s2)], [("hc", s2)])
            P.dma("sync", d["hconvT"][b, c, :, g * 512:(g + 1) * 512], hc[s2][:], r=[("hc", s2)], key=("hc", s2), is_out=True)
        if DEBUG_STOP <= 3:
            break
        for t4 in range(4):
            tt = g * 4 + t4
            r0 = b * SEQ + tt * 128
            tok = slice(t4 * 128, (t4 + 1) * 128)
            s2 = t4 % 2
            for k in range(8):
                P.mm(ps_q[:, 0:512], hnT[gs][:, k, tok], Wt[:, k, 1024:1536], k == 0, k == 7, [("hnT", gs)] + WK, ["psq"], mark=False)
            for k in range(8):
                P.mm(ps_q[:, 512:1024], hnT[gs][:, k, tok], Wt[:, k, 1536:2048], k == 0, k == 7, [("hnT", gs)] + WK, ["psq"], mark=False)
            for k in range(8):
                P.mm(ps_c[:, 0:328], hnT[gs][:, k, tok], Wt[:, k, 2048:2376], k == 0, k == 7, [("hnT", gs)] + WK, ["psq"])
            for (src, nh, dst, dkey) in ((ps_q[:, 0:512], 8, qr[s2][:, 0:512], ("qr", s2)), (ps_q[:, 512:1024], 8, qr[s2][:, 512:1024], ("qr", s2)),
                                         (ps_c[:, 0:256], 4, kr[s2][:, 0:256], ("kr", s2))):
                sv = src.rearrange("p (h e) -> p h e", e=64)
                dv = dst.rearrange("p (h e) -> p h e", e=64)
                x1, x2 = sv[:, :, 0:8], sv[:, :, 8:16]
                cv = cos[:, tt, 0:nh * 8].rearrange("p (h e) -> p h e", e=8)
                sn = sin[:, tt, 0:nh * 8].rearrange("p (h e) -> p h e", e=8)
                tv = [tmp[i][:, 0:nh * 8].rearrange("p (h e) -> p h e", e=8) for i in range(4)]
                P.tt("vector", tv[0], x1, cv, ALU.mult, ["psq", "cos"], ["tmp0"])
                P.tt("vector", tv[1], x2, sn, ALU.mult, ["psq", "sin"], ["tmp1"])
                P.tt("vector", dv[:, :, 0:8], tv[0], tv[1], ALU.subtract, ["tmp0", "tmp1"], [dkey])
                P.tt("vector", tv[2], x2, cv, ALU.mult, ["psq", "cos"], ["tmp2"])
                P.tt("vector", tv[3], x1, sn, ALU.mult, ["psq", "sin"], ["tmp3"])
                P.tt("vector", dv[:, :, 8:16], tv[2], tv[3], ALU.add, ["tmp2", "tmp3"], [dkey])
                P.copy("scalar", dv[:, :, 16:64], sv[:, :, 16:64], ["psq"], [dkey])
            P.copy("scalar", vaug[s2][:, 0:64], ps_c[:, 256:320], ["psq"], [("vaug", s2)])
            P.copy("scalar", vaug[s2][:, 192:256], ps_c[:, 256:320], ["psq"], [("vaug", s2)])
            P.ts("vector", iwt[s2][:], ps_c[:, 320:328], w_scale, ALU.mult, ["psq"], [("iw", s2)])
            P.dma("sync", d["vaug"][r0:r0 + 128, :], vaug[s2][:], r=[("vaug", s2)], key=("vaug", s2), is_out=True)
            P.dma("sync", d["iw"][r0:r0 + 128, :], iwt[s2][:], r=[("iw", s2)], key=("iw", s2), is_out=True)
            for k in range(8):
                P.tr(ps_tr[1][:, k, :], qr[s2][:, k * 128:(k + 1) * 128], identb[:], [("qr", s2), "identb"], ["ptr1"], mark=(k == 7))
            P.copy("scalar", qTg[gs][:, 0:8, tok], ps_tr[1][:, 0:8, :], ["ptr1"], [("qTg", gs)])
            for k in range(2):
                P.tr(ps_tr[1][:, k, :], kr[s2][:, k * 128:(k + 1) * 128], identb[:], [("kr", s2), "identb"], ["ptr1"], mark=(k == 1))
            P.copy("scalar", kTg[gs][:, 0:2, tok], ps_tr[1][:, 0:2, :], ["ptr1"], [("kTg", gs)])
        gsl = slice(g * 512, (g + 1) * 512)
        P.dma("sync", d["qT"][b, :, :, gsl].rearrange("c p t -> p c t"), qTg[gs][:, 0:4, :], r=[("qTg", gs)], key=("qTg", gs), is_out=True)
        P.dma("sync", d["iqT"][b, :, :, gsl].rearrange("c p t -> p c t"), qTg[gs][:, 4:8, :], r=[("qTg", gs)], key=("iqTg", gs), is_out=True)
        P.dma("sync", d["kkT"][b, :, :, gsl].rearrange("c p t -> p c t"), kTg[gs][:, :, :], r=[("kTg", gs)], key=("kTg", gs), is_out=True)
    P.close()


def phase_B(B):
    nc, d, NSEQ = B.nc, B.t, B.NSEQ
    P = Phase(nc, "B")
    identb = P.sb("identb", [128, 128], BF16)
    P.dma("sync", identb[:], d["ident_bf"], w=["identb"], key="identb")
    identf = P.sb("identf", [128, 128], F32)
    P.dma("sync", identf[:], d["ident_f32"], w=["identf"], key="identf")
    ones = P.sb("ones", [128, 128], F32)
    P.memset("vector", ones[:], 1.0, ["ones"])
    eps_t = P.sb("eps", [128, 1], F32)
    P.memset("vector", eps_t[:], EPS, ["eps"])
    cw = P.sb("cw", [31, 512], F32)
    P.dma("sync", cw[:], d["ab_conv_w"], w=["cw"], key="cw")
    prm = P.sb("prm", [128, 12], F32)
    P.dma("sync", prm[:], d["conv_prm"], w=["prm"], key="prm")
    ps_w = P.ps("psw", [128, 4, 32])
    for c in range(4):
        P.tr(ps_w[:, c, 0:31], cw[0:31, c * 128:(c + 1) * 128], identf[0:31, 0:31], ["cw", "identf"], ["psw"], mark=(c == 3))
    wT = P.sb("wT", [128, 4, 32], F32)
    P.copy("vector", wT[:, :, 0:31], ps_w[:, :, 0:31], ["psw"], ["wT"])
    diag = P.sb("diag", [128, 124, 128], BF16)
    for c in range(4):
        for j in range(31):
            i = c * 31 + j
            P.ts("vector" if i % 2 == 0 else "gpsimd", diag[:, i, :], identb[:], wT[:, c, j:j + 1], ALU.mult,
                 ["identb", "wT"], [("diag", c)])
    hbuf = P.sb("hbuf", [128, 4, 30 + SEQ], BF16)
    P.memset("gpsimd", hbuf[:, :, 0:30], 0.0, ["hbuf"])
    ps_c = [P.ps("psc%d" % i, [128, 512]) for i in range(2)]
    ps_s1 = P.ps("ps1", [128, 512])
    ps_s2 = P.ps("ps2", [128, 512])
    hcs = P.sb("hcs", [128, 4, 512], F32)
    sqs = P.sb("sqs", [128, 4, 512], F32)
    m = P.sb("m", [128, 512], F32)
    msq = P.sb("msq", [128, 512], F32)
    var = P.sb("var", [128, 512], F32)
    rstd = P.sb("rstd", [128, 512], F32)
    z = [P.sb("z%d" % i, [128, 512], F32) for i in range(2)]
    yst = [P.sb("yst%d" % i, [128, 4, 512], BF16) for i in range(2)]
    for b in range(NSEQ):
        for c in range(4):
            P.dma("sync", hbuf[:, c, 30:30 + SEQ], d["hconvT"][b, c, :, :], w=["hbuf"], key=("hbuf", c))
        for g in range(4):
            ys = g % 2
            for c in range(4):
                pc = ps_c[c % 2]
                pk = ("psc", c % 2)
                for j in range(31):
                    P.mm(pc[:, :], diag[:, c * 31 + j, :], hbuf[:, c, g * 512 + j:g * 512 + j + 512], j == 0, j == 30,
                         [("diag", c), "hbuf"], [pk])
                P.act(hcs[:, c, :], pc[:, :], AF.Identity, [pk, "prm"], [("hcs", c)], bias=prm[:, c:c + 1], scale=1.0)
                P.act(sqs[:, c, :], pc[:, :], AF.Square, [pk, "prm"], [("sqs", c)], bias=prm[:, c:c + 1], scale=1.0)
            for c in range(4):
                P.mm(ps_s1[:, :], ones[:], hcs[:, c, :], c == 0, c == 3, [("hcs", c), "ones"], ["ps1"])
            for c in range(4):
                P.mm(ps_s2[:, :], ones[:], sqs[:, c, :], c == 0, c == 3, [("sqs", c), "ones"], ["ps2"])
            P.act(m[:], ps_s1[:, :], AF.Copy, ["ps1"], ["m"], scale=1.0 / 512)
            P.tt("gpsimd", msq[:], m[:], m[:], ALU.mult, ["m"], ["msq"])
            P.stt(var[:], ps_s2[:, :], 1.0 / 512, msq[:], ALU.mult, ALU.subtract, ["ps2", "msq"], ["var"])
            P.act(var[:], var[:], AF.Sqrt, ["var", "eps"], ["var"], bias=eps_t[:, 0:1], scale=1.0)
            P.recip(rstd[:], var[:], ["var"], ["rstd"])
            for c in range(4):
                zz = z[c % 2]
                zk = ("z", c % 2)
                P.tt("gpsimd", zz[:], hcs[:, c, :], m[:], ALU.subtract, [("hcs", c), "m"], [zk])
                P.tt("vector", zz[:], zz[:], rstd[:], ALU.mult, [zk, "rstd"], [zk])
                P.act(yst[ys][:, c, :], zz[:], AF.Silu, [zk, "prm"], [("yst", ys)], scale=prm[:, 4 + c:5 + c], bias=prm[:, 8 + c:9 + c])
            P.dma("sync", d["yabT"][b, 0:4, :, g * 512:(g + 1) * 512].rearrange("c p t -> p c t"), yst[ys][:],
                  r=[("yst", ys)], key=("yst", ys), is_out=True)
    P.close()


def phase_C(B):
    nc, d, NSEQ = B.nc, B.t, B.NSEQ
    P = Phase(nc, "C")
    identb = P.sb("identb", [128, 128], BF16)
    P.dma("sync", identb[:], d["ident_bf"], w=["identb"], key="identb")
    negm = P.sb("negm", [128, 128], F32)
    P.dma("sync", negm[:], d["negmask"], w=["negm"], key="negm")
    pow2 = P.sb("pow2", [128, NIT], F32)
    P.dma("sync", pow2[:], d["pow2"], w=["pow2"], key="pow2")
    thr0 = P.sb("thr0", [128, 1], F32)
    P.memset("vector", thr0[:], -1e29, ["thr0"])
    qT = [P.sb("qT%d" % i, [128, 4, SEQ], BF16) for i in range(2)]
    iqT = [P.sb("iqT%d" % i, [128, 4, SEQ], BF16) for i in range(2)]
    kkT = [P.sb("kkT%d" % i, [128, 2, SEQ], BF16) for i in range(2)]
    vaug = [P.sb("vaug%d" % i, [128, NT, 256], BF16) for i in range(2)]
    iw = [P.sb("iw%d" % i, [128, NT, 8], F32) for i in range(2)]
    ybg = [P.sb("ybg%d" % i, [128, 512], BF16) for i in range(2)]
    maskT = [P.sb("maskT%d" % i, [128, NT, 512], BF16) for i in range(2)]
    score = [P.sb("score%d" % i, [128, SEQ], F32) for i in range(2)]
    junk = P.sb("junk", [128, SEQ], BF16)
    rl = [P.sb("rl%d" % i, [128, 512], F32) for i in range(2)]
    mk = [P.sb("mk%d" % i, [128, SEQ], BF16) for i in range(2)]
    st = [P.sb("st%d" % i, [128, 8 + NIT], F32) for i in range(2)]
    pe = [P.sb("pe%d" % i, [128, 512], BF16) for i in range(2)]
    pm = [P.sb("pm%d" % i, [128, 512], BF16) for i in range(2)]
    rc = [P.sb("rc%d" % i, [128, 512], F32) for i in range(2)]
    ps_lg = [P.ps("plg%d" % i, [128, 512]) for i in range(2)]
    ps_tr = [P.ps("ptr%d" % i, [128, 8, 128], BF16) for i in range(2)]
    ps_s = [P.ps("pss%d" % i, [128, 512]) for i in range(2)]
    ps_o = [P.ps("pso%d" % i, [128, 512]) for i in range(2)]
    att_scale = 64 ** -0.5
    ctr = {"lg": 0, "s": 0, "o": 0, "tr": 0, "yb": 0}

    def load_seq(b):
        p = b % 2
        rs = slice(b * SEQ, (b + 1) * SEQ)
        P.dma("sync", iqT[p][:], d["iqT"][b].rearrange("c p t -> p c t"), w=[("iqT", p)], key=("iqT", p))
        P.dma("sync", kkT[p][:], d["kkT"][b].rearrange("c p t -> p c t"), w=[("kkT", p)], key=("kkT", p))
        P.dma("sync", iw[p][:], d["iw"][rs, :].rearrange("(t p) c -> p t c", p=128), w=[("iw", p)], key=("iw", p))
        P.dma("sync", qT[p][:], d["qT"][b].rearrange("c p t -> p c t"), w=[("qT", p)], key=("qT", p))
        P.dma("sync", vaug[p][:], d["vaug"][rs, :].rearrange("(t p) c -> p t c", p=128), w=[("vaug", p)], key=("vaug", p))

    junk2 = [junk, P.sb("junkb", [128, SEQ], BF16)]

    def score_pair(b, G, q4s):
        p = b % 2
        mG = (b * 4 + G) % 2
        info = []
        for q4 in q4s:
            qb = 4 * G + q4
            nk = 128 * (qb + 1)
            nkb = (nk + 511) // 512
            s2 = qb % 2
            sc = score[s2]
            sks = [("score", s2, kb) for kb in range(nkb)]
            qsl = slice(qb * 128, (qb + 1) * 128)
            for h in range(8):
                c, base = h // 2, 64 * (h % 2)
                for kb in range(nkb):
                    n = min(512, nk - kb * 512)
                    r = ctr["lg"] % 2
                    ctr["lg"] += 1
                    P.mm(ps_lg[r][:, 0:n], iqT[p][base:base + 64, c, qsl], kkT[p][base:base + 64, 1, kb * 512:kb * 512 + n],
                         True, True, [("iqT", p), ("kkT", p)], [("plg", r)])
                    P.act(rl[r][:, 0:n], ps_lg[r][:, 0:n], AF.Relu, [("plg", r)], [("rl", r)])
                    dst = sc[:, kb * 512:kb * 512 + n]
                    if h == 0:
                        P.ts("vector", dst, rl[r][:, 0:n], iw[p][:, qb, 0:1], ALU.mult, [("rl", r), ("iw", p)], [sks[kb]])
                    else:
                        P.stt(dst, rl[r][:, 0:n], iw[p][:, qb, h:h + 1], dst, ALU.mult, ALU.add, [("rl", r), ("iw", p), sks[kb]], [sks[kb]])
            info.append((q4, qb, nk, s2, sc, sks, qsl))
        for (q4, qb, nk, s2, sc, sks, qsl) in info:
            stt_ = st[s2]
            stk = ("st", s2)
            if qb >= 2:
                P.reduce(stt_[:, 0:1], sc[:, 0:nk], ALU.max, sks, [stk])
                P.reduce(stt_[:, 1:2], sc[:, 0:nk], ALU.min, sks, [(stk, "lo")])
            P.tt("vector", sc[:, qsl], sc[:, qsl], negm[:], ALU.add, [sks[qb // 4], "negm"], [sks[qb // 4]])
            if qb >= 2:
                P.tt("vector", stt_[:, 2:3], stt_[:, 0:1], stt_[:, 1:2], ALU.subtract, [stk, (stk, "lo")], [stk])
                P.ts("vector", stt_[:, 8:8 + NIT], pow2[:], stt_[:, 2:3], ALU.mult, ["pow2", stk], [(stk, "steps")])
        bis = [x for x in info if x[1] >= 2]
        for i in range(NIT):
            for (q4, qb, nk, s2, sc, sks, qsl) in bis:
                stt_, stk = st[s2], ("st", s2)
                P.tt("vector", stt_[:, 3:4], stt_[:, 1:2], stt_[:, 8 + i:9 + i], ALU.add, [(stk, "lo"), (stk, "steps")], [(stk, "mid")])
            for (q4, qb, nk, s2, sc, sks, qsl) in bis:
                stt_, stk = st[s2], ("st", s2)
                P.ts("vector", junk2[s2][:, 0:nk], sc[:, 0:nk], stt_[:, 3:4], ALU.is_ge, sks + [(stk, "mid")], [("junk", s2), (stk, "cnt")],
                     s2=0.0, op1=ALU.add, accum_out=stt_[:, 4:5])
            for (q4, qb, nk, s2, sc, sks, qsl) in bis:
                stt_, stk = st[s2], ("st", s2)
                P.stt(stt_[:, 5:6], stt_[:, 4:5], TOPK - 0.5, stt_[:, 8 + i:9 + i], ALU.is_ge, ALU.mult, [(stk, "cnt"), (stk, "steps")], [(stk, "sel")])
            for (q4, qb, nk, s2, sc, sks, qsl) in bis:
                stt_, stk = st[s2], ("st", s2)
                P.tt("vector", stt_[:, 1:2], stt_[:, 1:2], stt_[:, 5:6], ALU.add, [(stk, "lo"), (stk, "sel")], [(stk, "lo")])
        for (q4, qb, nk, s2, sc, sks, qsl) in info:
            stt_, stk = st[s2], ("st", s2)
            thr = stt_[:, 1:2] if qb >= 2 else thr0[:, 0:1]
            P.ts("vector", mk[s2][:, 0:nk], sc[:, 0:nk], thr, ALU.is_ge, sks + [(stk, "lo"), "thr0"], [("mk", s2)])
            kt = 0
            while kt <= qb:
                n = min(8, qb + 1 - kt)
                r = ctr["tr"] % 2
                ctr["tr"] += 1
                for j in range(n):
                    P.tr(ps_tr[r][:, j, :], mk[s2][:, (kt + j) * 128:(kt + j + 1) * 128], identb[:], [("mk", s2), "identb"],
                         [("ptr", r)], mark=(j == n - 1))
                P.copy("scalar", maskT[mG][:, kt:kt + n, q4 * 128:(q4 + 1) * 128], ps_tr[r][:, 0:n, :], [("ptr", r)], [("maskT", mG)])
                kt += n

    def attn_chunk(b, G, c):
        p = b % 2
        mG = (b * 4 + G) % 2
        nkt = 4 * G + 4
        gsl0 = G * 512
        ys = ctr["yb"] % 2
        ctr["yb"] += 1
        for e in range(2):
            base = 64 * e
            o = ctr["o"] % 2
            ctr["o"] += 1
            for kt in range(nkt):
                q0 = max(0, kt - 4 * G) * 128
                N = 512 - q0
                r = ctr["s"] % 2
                ctr["s"] += 1
                P.mm(ps_s[r][:, 0:N], kkT[p][base:base + 64, 0, kt * 128:(kt + 1) * 128], qT[p][base:base + 64, c, gsl0 + q0:gsl0 + 512],
                     True, True, [("kkT", p), ("qT", p)], [("pss", r)])
                P.act(pe[r][:, 0:N], ps_s[r][:, 0:N], AF.Exp, [("pss", r)], [("pe", r)], scale=att_scale)
                P.tt("gpsimd", pm[r][:, 0:N], pe[r][:, 0:N], maskT[mG][:, kt, q0:512], ALU.mult, [("pe", r), ("maskT", mG)], [("pm", r)])
                P.mm(ps_o[o][:, q0:512], vaug[p][:, kt, e * 128:(e + 1) * 128], pm[r][:, 0:N], kt == 0, kt == nkt - 1,
                     [("pm", r), ("vaug", p)], [("pso", o)])
            so, ss_ = (slice(0, 64), slice(64, 128)) if e == 0 else (slice(64, 128), slice(0, 64))
            P.act(rc[o][ss_, :], ps_o[o][ss_, :], AF.Ln, [("pso", o)], [("rc", o)])
            P.act(rc[o][ss_, :], rc[o][ss_, :], AF.Exp, [("rc", o)], [("rc", o)], scale=-1.0)
            P.tt("vector", ybg[ys][so, :], ps_o[o][so, :], rc[o][ss_, :], ALU.mult, [("pso", o), ("rc", o)], [("ybg", ys)])
        P.dma("sync", d["yabT"][b, 4 + c, :, gsl0:gsl0 + 512], ybg[ys][:], r=[("ybg", ys)], key=("ybg", ys), is_out=True)

    units = [(b, G, pr) for b in range(NSEQ) for G in range(4) for pr in range(2)]
    load_seq(0)
    for i, (b, G, pr) in enumerate(units):
        if G == 0 and pr == 0 and b + 1 < NSEQ:
            load_seq(b + 1)
        score_pair(b, G, (2 * pr, 2 * pr + 1))
        gprev = i // 2 - 1
        if gprev >= 0:
            attn_chunk(gprev // 4, gprev % 4, 2 * (i % 2))
            attn_chunk(gprev // 4, gprev % 4, 2 * (i % 2) + 1)
    glast = len(units) // 2 - 1
    for c in range(4):
        attn_chunk(glast // 4, glast % 4, c)
    P.close()


def phase_D(B, tag, yT, wname, resid, gname, hmid, hnT_name):
    nc, d, NSEQ = B.nc, B.t, B.NSEQ
    P = Phase(nc, tag)
    Wo = P.sb("Wo", [128, 8, D], BF16)
    P.dma("gpsimd", Wo[:], d[wname].rearrange("(k p) n -> p k n", p=128), w=["Wo"], key="Wo")
    identb = P.sb("identb", [128, 128], BF16)
    P.dma("sync", identb[:], d["ident_bf"], w=["identb"], key="identb")
    gbc = P.sb("gbc", [128, D], F32)
    P.dma("sync", gbc[:], d[gname], w=["gbc"], key="gbc")
    eps_t = P.sb("eps", [128, 1], F32)
    P.memset("vector", eps_t[:], EPS, ["eps"])
    yt = [P.sb("yt%d" % i, [128, 8, 512], BF16) for i in range(2)]
    xt = [P.sb("xt%d" % i, [128, D], F32) for i in range(2)]
    hm = [P.sb("hm%d" % i, [128, D], F32) for i in range(2)]
    junk = P.sb("junk", [128, D], F32)
    stat = [P.sb("stat%d" % i, [128, 4], F32) for i in range(2)]
    hn = [P.sb("hn%d" % i, [128, D], BF16) for i in range(2)]
    hnTg = [P.sb("hnTg%d" % i, [128, 8, 512], BF16) for i in range(2)]
    ps_o = [P.ps("pso%d" % i, [128, 1024]) for i in range(2)]
    ps_tr = [P.ps("ptr%d" % i, [128, 8, 128], BF16) for i in range(2)]
    ngr = NSEQ * 4

    def load_y(gi):
        b, g = gi // 4, gi % 4
        P.dma("sync", yt[gi % 2][:], d[yT][b, :, :, g * 512:(g + 1) * 512].rearrange("c p t -> p c t"), w=[("yt", gi % 2)], key=("yt", gi % 2))

    load_y(0)
    for gi in range(ngr):
        b, g = gi // 4, gi % 4
        gs = gi % 2
        if gi + 1 < ngr:
            load_y(gi + 1)
        for t4 in range(4):
            tt = g * 4 + t4
            r0 = b * SEQ + tt * 128
            tok = slice(t4 * 128, (t4 + 1) * 128)
            s2 = t4 % 2
            P.dma("sync", xt[s2][:], d[resid][r0:r0 + 128, :], w=[("x", s2)], key=("x", s2))
            for half in range(2):
                for c in range(8):
                    P.mm(ps_o[s2][:, half * 512:(half + 1) * 512], yt[gs][:, c, tok], Wo[:, c, half * 512:(half + 1) * 512],
                         c == 0, c == 7, [("yt", gs), "Wo"], [("pso", s2)], mark=(c == 7 and half == 1))
            for half in range(2):
                hs_ = slice(half * 512, (half + 1) * 512)
                P.tt("vector", hm[s2][:, hs_], ps_o[s2][:, hs_], xt[s2][:, hs_], ALU.add, [("pso", s2), ("x", s2)], [("hm", s2)])
            P.dma("sync", d[hmid][r0:r0 + 128, :], hm[s2][:], r=[("hm", s2)], key=("hm", s2), is_out=True)
            rms_to_hnT(P, hm[s2][:], ("hm", s2), gbc[:], eps_t, stat[s2], ("st", s2), junk, hn[s2][:], ("hn", s2),
                       ps_tr[s2], ("ptr", s2), identb, hnTg[gs], ("hnTg", gs), tok)
            if DEBUG_STOP == 77 and gi == 0:
                if t4 == 0:
                    dlog = P.sb("dlog", [128, 16], F32)
                    dhn = P.sb("dhn", [128, 4, D], BF16)
                P.copy("gpsimd", dlog[:, t4 * 4:(t4 + 1) * 4], stat[s2][:, 0:4], [("st", s2), ("hn", s2)], ["dlog"])
                P.copy("gpsimd", dhn[:, t4, :], hn[s2][:], [("hn", s2)], ["dhn"])
                if t4 == 3:
                    P.dma("sync", d["dbg"], dlog[:], r=["dlog"], key="dlog", is_out=True)
                    P.dma("sync", d["dbg2"].rearrange("t p c -> p t c"), dhn[:], r=["dhn"], key="dhn", is_out=True)
        P.dma("sync", d[hnT_name][:, :, gi * 512:(gi + 1) * 512].rearrange("c p t -> p c t"), hnTg[gs][:], r=[("hnTg", gs)],
              key=("hnTg", gs), is_out=True)
    P.close()


def phase_F(B, tag, layer, f0, h_in, h_out, hnT_name, final_g=None):
    nc, d, NSEQ = B.nc, B.t, B.NSEQ
    P = Phase(nc, tag)
    nf = 11
    Wg = P.sb("Wg", [128, 8, nf * 128], BF16)
    Wu = P.sb("Wu", [128, 8, nf * 128], BF16)
    Wd = P.sb("Wd", [128, nf, D], BF16)
    cs = slice(f0 * 128, (f0 + nf) * 128)
    P.dma("gpsimd", Wg[:], d["ffn_w_gate%d" % layer].rearrange("(k p) n -> p k n", p=128)[:, :, cs], w=["Wg"], key="Wg")
    P.dma("gpsimd", Wu[:], d["ffn_w_up%d" % layer].rearrange("(k p) n -> p k n", p=128)[:, :, cs], w=["Wu"], key="Wu")
    P.dma("gpsimd", Wd[:], d["ffn_w_down%d" % layer].rearrange("(f p) n -> p f n", p=128)[:, f0:f0 + nf, :], w=["Wd"], key="Wd")
    if final_g is not None:
        gbc = P.sb("gbc", [128, D], F32)
        P.dma("sync", gbc[:], d[final_g], w=["gbc"], key="gbc")
        eps_t = P.sb("eps", [128, 1], F32)
        P.memset("vector", eps_t[:], EPS, ["eps"])
        junk = P.sb("junk", [128, D], F32)
        stat = [P.sb("stat%d" % i, [128, 4], F32) for i in range(2)]
    hnTg = [P.sb("hnTg%d" % i, [128, 8, 512], BF16) for i in range(2)]
    actT = [P.sb("actT%d" % i, [128, nf, 512], BF16) for i in range(2)]
    sg = [P.sb("sg%d" % i, [128, 512], F32) for i in range(2)]
    hin = [P.sb("hin%d" % i, [128, D], F32) for i in range(2)]
    hout = [P.sb("hout%d" % i, [128, D], F32) for i in range(2)]
    ps_g = [P.ps("psg%d" % i, [128, 512]) for i in range(2)]
    ps_u = [P.ps("psu%d" % i, [128, 512]) for i in range(2)]
    ps_d = [P.ps("psd%d" % i, [128, 1024]) for i in range(2)]
    ngr = NSEQ * 4

    def load_h(gi):
        P.dma("sync", hnTg[gi % 2][:], d[hnT_name][:, :, gi * 512:(gi + 1) * 512].rearrange("c p t -> p c t"),
              w=[("hnTg", gi % 2)], key=("hnTg", gi % 2))

    load_h(0)
    for gi in range(ngr):
        gs = gi % 2
        if gi + 1 < ngr:
            load_h(gi + 1)
        for f in range(nf):
            s2 = f % 2
            for k in range(8):
                P.mm(ps_g[s2][:, :], Wg[:, k, f * 128:(f + 1) * 128], hnTg[gs][:, k, :], k == 0, k == 7, [("hnTg", gs), "Wg"], [("psg", s2)])
            for k in range(8):
                P.mm(ps_u[s2][:, :], Wu[:, k, f * 128:(f + 1) * 128], hnTg[gs][:, k, :], k == 0, k == 7, [("hnTg", gs), "Wu"], [("psu", s2)])
            P.act(sg[s2][:], ps_g[s2][:, :], AF.Silu, [("psg", s2)], [("sg", s2)])
            P.tt("vector", actT[gs][:, f, :], ps_u[s2][:, :], sg[s2][:], ALU.mult, [("psu", s2), ("sg", s2)], [("actT", gs)])
        for t4 in range(4):
            r0 = gi * 512 + t4 * 128
            tok = slice(t4 * 128, (t4 + 1) * 128)
            s2 = t4 % 2
            P.dma("sync", hin[s2][:], d[h_in][r0:r0 + 128, :], w=[("hin", s2)], key=("hin", s2))
            for half in range(2):
                for f in range(nf):
                    P.mm(ps_d[s2][:, half * 512:(half + 1) * 512], actT[gs][:, f, tok], Wd[:, f, half * 512:(half + 1) * 512],
                         f == 0, f == nf - 1, [("actT", gs), "Wd"], [("psd", s2)], mark=(f == nf - 1 and half == 1))
            for half in range(2):
                hs_ = slice(half * 512, (half + 1) * 512)
                P.tt("vector", hout[s2][:, hs_], ps_d[s2][:, hs_], hin[s2][:, hs_], ALU.add, [("psd", s2), ("hin", s2)], [("hout", s2)])
            if final_g is not None:
                sk = ("st", s2)
                P.act(junk[:], hout[s2][:], AF.Square, [("hout", s2)], ["junkA", sk], accum_out=stat[s2][:, 0:1])
                P.act(stat[s2][:, 1:2], stat[s2][:, 0:1], AF.Sqrt, [sk], [sk], scale=1.0 / D, bias=eps_t[:, 0:1])
                P.recip(stat[s2][:, 2:3], stat[s2][:, 1:2], [sk], [sk])
                P.stt(hout[s2][:], hout[s2][:], stat[s2][:, 2:3], gbc[:], ALU.mult, ALU.mult, [("hout", s2), sk, "gbc"], [("hout", s2)])
            P.dma("sync", d[h_out][r0:r0 + 128, :], hout[s2][:], r=[("hout", s2)], key=("hout", s2), is_out=True)
    P.close()


def phase_E(B):
    nc, d, NSEQ = B.nc, B.t, B.NSEQ
    P = Phase(nc, "E")
    Wc = P.sb("Wc", [128, 8, 3104], BF16)
    Wsrc = d["c_w_in"].rearrange("(k p) n -> p k n", p=128)
    WK = []
    P.memset("gpsimd", Wc[:, :, 3088:3104], 0.0, [("W", 2)])
    for i, (c0, c1) in enumerate(((0, 1024), (1024, 2048), (2048, 3088))):
        P.dma("gpsimd", Wc[:, :, c0:c1], Wsrc[:, :, c0:c1], w=[("W", i)], key=("W", i))
        WK.append(("W", i))
    identb = P.sb("identb", [128, 128], BF16)
    P.dma("sync", identb[:], d["ident_bf"], w=["identb"], key="identb")
    gbc = P.sb("gbc", [128, D], F32)
    P.dma("sync", gbc[:], d["g_mix1"], w=["gbc"], key="gbc")
    g2w = P.sb("g2w", [128, 512], F32)
    P.memset("vector", g2w[:], 0.0, ["g2w"])
    P.dma("sync", g2w[0:16, :], d["c_gate_w"], w=["g2w"], key="g2w")
    g2b = P.sb("g2b", [128, 512], F32)
    P.dma("sync", g2b[:], d["c_gate_b_bc"], w=["g2b"], key="g2b")
    eps_t = P.sb("eps", [128, 1], F32)
    P.memset("vector", eps_t[:], EPS, ["eps"])
    one_t = P.sb("one", [128, 1], F32)
    P.memset("vector", one_t[:], 1.0, ["one"])
    xt = [P.sb("xt%d" % i, [128, D], F32) for i in range(2)]
    junk = P.sb("junk", [128, D], F32)
    stat = [P.sb("stat%d" % i, [128, 4], F32) for i in range(2)]
    hn = [P.sb("hn%d" % i, [128, D], BF16) for i in range(2)]
    hnT = [P.sb("hnT%d" % i, [128, 8, 512], BF16) for i in range(2)]
    qkT = [P.sb("qkT%d" % i, [128, 8, 512], BF16) for i in range(2)]
    glrT = [P.sb("glrT%d" % i, [128, 512], F32) for i in range(2)]
    for i in range(2):
        P.memset("vector", glrT[i][:], 0.0, [("glrT", i)])
    kst = [P.sb("kst%d" % i, [128, 512], BF16) for i in range(2)]
    vst = [P.sb("vst%d" % i, [128, 1024], BF16) for i in range(2)]
    rst = [P.sb("rst%d" % i, [128, 1024], F32) for i in range(2)]
    lat = [P.sb("lat%d" % i, [128, 512], F32) for i in range(2)]
    lt1 = P.sb("lt1", [128, 512], F32)
    lt2 = P.sb("lt2", [128, 512], F32)
    ps_tr = P.ps("ptr", [128, 8, 128], BF16)
    ps_f = [P.ps("psf%d" % i, [128, 512]) for i in range(2)]
    ps_t = [P.ps("pst%d" % i, [128, 512]) for i in range(3)]
    ps_l = P.ps("psl", [128, 512])
    n_t = 0
    n_f = 0
    n_tiles = NSEQ * NT

    def load_x(ti):
        P.dma("sync", xt[ti % 2][:], d["h1"][ti * 128:(ti + 1) * 128, :], w=[("x", ti % 2)], key=("x", ti % 2))

    load_x(0)
    for gi in range(NSEQ * 4):
        b, g = gi // 4, gi % 4
        gs = gi % 2
        for t4 in range(4):
            ti = gi * 4 + t4
            if ti + 1 < n_tiles:
                load_x(ti + 1)
            xs = ti % 2
            rms_to_hnT(P, xt[xs][:], ("x", xs), gbc[:], eps_t, stat[xs], ("st", xs), junk, hn[xs][:], ("hn", xs),
                       ps_tr, "ptr", identb, hnT[gs], ("hnT", gs), slice(t4 * 128, (t4 + 1) * 128))
        for oc in range(8):
            r = n_f % 2
            n_f += 1
            for k in range(8):
                P.mm(ps_f[r][:, :], Wc[:, k, oc * 128:(oc + 1) * 128], hnT[gs][:, k, :], k == 0, k == 7, [("hnT", gs)] + WK, [("psf", r)])
            if oc < 4:
                P.act(qkT[gs][:, oc, :], ps_f[r][:, :], AF.Copy, [("psf", r)], [("qkT", gs)], scale=128 ** -0.5)
            else:
                P.copy("vector", qkT[gs][:, oc, :], ps_f[r][:, :], [("psf", r)], [("qkT", gs)])
        r = n_f % 2
        n_f += 1
        for k in range(8):
            P.mm(ps_f[r][0:32, :], Wc[:, k, 3072:3104], hnT[gs][:, k, :], k == 0, k == 7, [("hnT", gs)] + WK, [("psf", r)])
        P.copy("vector", glrT[gs][0:32, :], ps_f[r][0:32, :], [("psf", r)], [("glrT", gs)])
        gsl = slice(g * 512, (g + 1) * 512)
        P.dma("sync", d["c_qT"][b, :, :, gsl].rearrange("c p t -> p c t"), qkT[gs][:, 0:4, :], r=[("qkT", gs)], key=("qTo", gs), is_out=True)
        P.dma("sync", d["c_kT"][b, :, :, gsl].rearrange("c p t -> p c t"), qkT[gs][:, 4:8, :], r=[("qkT", gs)], key=("kTo", gs), is_out=True)
        for t4 in range(4):
            r0 = gi * 512 + t4 * 128
            tok = slice(t4 * 128, (t4 + 1) * 128)
            s2 = t4 % 2
            jobs = [(512, 1024, kst[s2][:, :], ("kst", s2), "k"),
                    (1024, 1536, vst[s2][:, 0:512], ("vst", s2), "v"), (1536, 2048, vst[s2][:, 512:1024], ("vst", s2), "v"),
                    (2048, 2560, rst[s2][:, 0:512], ("rst", s2), "r"), (2560, 3072, rst[s2][:, 512:1024], ("rst", s2), "r")]
            for (c0, c1, dst, dk, kind) in jobs:
                r = n_t % 3
                n_t += 1
                for k in range(8):
                    P.mm(ps_t[r][:, :], hnT[gs][:, k, tok], Wc[:, k, c0:c1], k == 0, k == 7, [("hnT", gs)] + WK, [("pst", r)])
                if kind == "r":
                    P.act(dst, ps_t[r][:, :], AF.Silu, [("pst", r)], [dk])
                elif kind == "v":
                    P.copy("vector", dst, ps_t[r][:, :], [("pst", r)], [dk])
                else:
                    P.copy("scalar", dst, ps_t[r][:, :], [("pst", r)], [dk])
            P.dma("sync", d["c_ktok"][r0:r0 + 128, :], kst[s2][:], r=[("kst", s2)], key=("kst", s2), is_out=True)
            P.dma("sync", d["c_v"][r0:r0 + 128, :], vst[s2][:], r=[("vst", s2)], key=("vst", s2), is_out=True)
            P.dma("sync", d["c_sr"][r0:r0 + 128, :], rst[s2][:], r=[("rst", s2)], key=("rst", s2), is_out=True)
            P.mm(ps_l[:, :], glrT[gs][:, tok], g2w[:, :], True, True, [("glrT", gs), "g2w"], ["psl"])
            P.tt("vector", lt1[:], ps_l[:, :], g2b[:], ALU.add, ["psl", "g2b"], ["lt1"])
            P.act(lt2[:], lt1[:], AF.Exp, ["lt1"], ["lt2"], scale=-1.0)
            P.act(lt1[:], lt2[:], AF.Ln, ["lt2", "one"], ["lt1"], bias=one_t[:, 0:1], scale=1.0)
            P.ts("gpsimd", lat[s2][:], lt1[:], -1.0 / 16.0, ALU.mult, ["lt1"], [("lat", s2)])
            P.dma("sync", d["c_la"][r0:r0 + 128, :], lat[s2][:], r=[("lat", s2)], key=("lat", s2), is_out=True)
    P.close()


def phase_G(B):
    nc, d, NSEQ = B.nc, B.t, B.NSEQ
    P = Phase(nc, "G")
    identb = P.sb("identb", [128, 128], BF16)
    P.dma("sync", identb[:], d["ident_bf"], w=["identb"], key="identb")
    U = P.sb("U", [128, 128], F32)
    SL = P.sb("SL", [128, 128], F32)
    P.dma("sync", U[:], d["tri_u"], w=["U"], key="U")
    P.dma("sync", SL[:], d["tri_sl"], w=["SL"], key="SL")
    gon = P.sb("gon", [128, D], F32)
    P.dma("sync", gon[:], d["c_onorm_g_bc"], w=["gon"], key="gon")
    eps_t = P.sb("eps", [128, 1], F32)
    P.memset("vector", eps_t[:], EPS, ["eps"])
    qT = P.sb("qT", [128, 4, SEQ], BF16)
    kT = P.sb("kT", [128, 4, SEQ], BF16)
    la = [P.sb("la%d" % i, [128, 512], F32) for i in range(2)]
    kt_ = [P.sb("ktk%d" % i, [128, 512], BF16) for i in range(2)]
    vt = [P.sb("vt%d" % i, [128, 1024], BF16) for i in range(2)]
    sr = [P.sb("sr%d" % i, [128, 1024], F32) for i in range(2)]
    eb = [P.sb("eb%d" % i, [128, 512], F32) for i in range(2)]
    enb = P.sb("enb", [128, 512], F32)
    erv = P.sb("erv", [128, 512], F32)
    qtT = [P.sb("qtT%d" % i, [128, 512], BF16) for i in range(2)]
    ktT = [P.sb("ktT%d" % i, [128, 512], BF16) for i in range(2)]
    ks = [P.sb("ks%d" % i, [128, 512], BF16) for i in range(2)]
    attm = [P.sb("attm%d" % i, [128, 128], BF16) for i in range(2)]
    state = P.sb("state", [128, 4, 256], F32)
    stb = P.sb("stb", [128, 4, 256], BF16)
    gr = [P.sb("gr%d" % i, [128, 1024], F32) for i in range(2)]
    stat = [P.sb("stat%d" % i, [128, 12], F32) for i in range(2)]
    junk = P.sb("junk", [128, 256], F32)
    yc = [P.sb("yc%d" % i, [128, 1024], BF16) for i in range(2)]
    ycT = [P.sb("ycT%d" % i, [128, 8, 512], BF16) for i in range(2)]
    ps_b = P.ps("psb", [128, 512])
    ps_r = P.ps("psr", [128, 512])
    ps_a = P.ps("psa", [128, 4, 128])
    ps_o = P.ps("pso", [128, 1024])
    ps_kv = P.ps("pskv", [128, 1024])
    ps_tr = P.ps("ptr", [128, 8, 128], BF16)
    nch = NSEQ * NT

    def load_chunk(ci):
        s2 = ci % 2
        r0 = ci * 128
        P.dma("sync", la[s2][:], d["c_la"][r0:r0 + 128, :], w=[("la", s2)], key=("la", s2))
        P.dma("sync", kt_[s2][:], d["c_ktok"][r0:r0 + 128, :], w=[("ktk", s2)], key=("ktk", s2))
        P.dma("sync", vt[s2][:], d["c_v"][r0:r0 + 128, :], w=[("vt", s2)], key=("vt", s2))
        P.dma("sync", sr[s2][:], d["c_sr"][r0:r0 + 128, :], w=[("sr", s2)], key=("sr", s2))

    load_chunk(0)
    for ci in range(nch):
        b, c = ci // NT, ci % NT
        s2 = ci % 2
        if c == 0:
            P.dma("sync", qT[:], d["c_qT"][b].rearrange("c p t -> p c t"), w=["qT"], key="qT")
            P.dma("sync", kT[:], d["c_kT"][b].rearrange("c p t -> p c t"), w=["kT"], key="kT")
            P.memset("vector", state[:], 0.0, [("state", h) for h in range(4)])
            P.memset("gpsimd", stb[:], 0.0, [("stb", h) for h in range(4)])
        if ci + 1 < nch:
            load_chunk(ci + 1)
        csl = slice(c * 128, (c + 1) * 128)
        for h in range(4):
            P.mm(ps_b[:, h * 128:(h + 1) * 128], la[s2][:, h * 128:(h + 1) * 128], U[:], True, True, [("la", s2), "U"], ["psb"], mark=(h == 3))
        P.mm(ps_r[:, :], SL[:], la[s2][:, :], True, True, [("la", s2), "SL"], ["psr"])
        P.act(eb[s2][:], ps_b[:, :], AF.Exp, ["psb"], [("eb", s2)])
        P.act(enb[:], ps_b[:, :], AF.Exp, ["psb"], ["enb"], scale=-1.0)
        P.act(erv[:], ps_r[:, :], AF.Exp, ["psr"], ["erv"])
        qv = qT[:, :, csl]
        kv_ = kT[:, :, csl]
        P.tt("vector", qtT[s2][:].rearrange("p (h t) -> p h t", t=128), qv, eb[s2][:].rearrange("p (h t) -> p h t", t=128), ALU.mult,
             ["qT", ("eb", s2)], [("qtT", s2)])
        P.tt("gpsimd", ktT[s2][:].rearrange("p (h t) -> p h t", t=128), kv_, enb[:].rearrange("p (h t) -> p h t", t=128), ALU.mult,
             ["kT", "enb"], [("ktT", s2)])
        P.tt("gpsimd", ks[s2][:], kt_[s2][:], erv[:], ALU.mult, [("ktk", s2), "erv"], [("ks", s2)])
        P.tt("gpsimd", gr[s2][:], sr[s2][:], gon[:], ALU.mult, [("sr", s2), "gon"], [("gr", s2)])
        for h in range(4):
            hs = slice(h * 128, (h + 1) * 128)
            vs = slice(h * 256, (h + 1) * 256)
            a2 = h % 2
            P.mm(ps_a[:, h, :], ktT[s2][:, hs], qtT[s2][:, hs], True, True, [("ktT", s2), ("qtT", s2)], [("psa", h)])
            P.tt("vector", attm[a2][:], ps_a[:, h, :], U[:], ALU.mult, [("psa", h), "U"], [("attm", a2)])
            P.mm(ps_o[:, vs], attm[a2][:], vt[s2][:, vs], True, False, [("attm", a2), ("vt", s2)], [("pso", h)], mark=False)
            P.mm(ps_o[:, vs], qtT[s2][:, hs], stb[:, h, :], False, True, [("qtT", s2), ("stb", h)], [("pso", h)])
            P.mm(ps_kv[:, vs], ks[s2][:, hs], vt[s2][:, vs], True, True, [("ks", s2), ("vt", s2)], [("pskv", h)])
            P.stt(state[:, h, :], state[:, h, :], eb[s2][:, h * 128 + 127:h * 128 + 128], ps_kv[:, vs], ALU.mult, ALU.add,
                  [("state", h), ("eb", s2), ("pskv", h)], [("state", h)])
            P.copy("scalar", stb[:, h, :], state[:, h, :], [("state", h)], [("stb", h)])
        sk = ("st", s2)
        for h in range(4):
            vs = slice(h * 256, (h + 1) * 256)
            P.act(junk[:], ps_o[:, vs], AF.Square, [("pso", h)], ["junkA", sk], accum_out=stat[s2][:, h:h + 1])
        P.act(stat[s2][:, 4:8], stat[s2][:, 0:4], AF.Sqrt, [sk], [sk], scale=1.0 / 256, bias=eps_t[:, 0:1])
        P.recip(stat[s2][:, 8:12], stat[s2][:, 4:8], [sk], [sk])
        for h in range(4):
            vs = slice(h * 256, (h + 1) * 256)
            P.stt(yc[s2][:, vs], ps_o[:, vs], stat[s2][:, 8 + h:9 + h], gr[s2][:, vs], ALU.mult, ALU.mult,
                  [("pso", h), sk, ("gr", s2)], [("yc", s2)])
        for k in range(8):
            P.tr(ps_tr[:, k, :], yc[s2][:, k * 128:(k + 1) * 128], identb[:], [("yc", s2), "identb"], ["ptr"], mark=(k == 7))
        g4 = (ci // 4) % 2
        P.copy("scalar", ycT[g4][:, 0:8, (ci % 4) * 128:(ci % 4 + 1) * 128], ps_tr[:, 0:8, :], ["ptr"], [("ycT", g4)])
        if ci % 4 == 3:
            g = c // 4
            P.dma("sync", d["ycT"][b, :, :, g * 512:(g + 1) * 512].rearrange("c p t -> p c t"), ycT[g4][:], r=[("ycT", g4)],
                  key=("ycT", g4), is_out=True)
    P.close()


def declare(B):
    NSEQ = B.NSEQ
    T = NSEQ * SEQ
    X = "ExternalInput"
    B.dram("x", [T, D], F32, X)
    B.dram("ab_w_in", [D, 2248], F32, X)
    B.dram("ab_conv_w", [31, 512], F32, X)
    B.dram("conv_prm", [128, 12], F32, X)
    B.dram("ab_w_out", [D, D], F32, X)
    B.dram("c_w_in", [D, 3088], F32, X)
    B.dram("c_gate_w", [16, 512], F32, X)
    B.dram("c_gate_b_bc", [128, 512], F32, X)
    B.dram("c_onorm_g_bc", [128, D], F32, X)
    B.dram("c_w_out", [D, D], F32, X)
    for l in range(2):
        B.dram("ffn_w_gate%d" % l, [D, DFF], F32, X)
        B.dram("ffn_w_up%d" % l, [D, DFF], F32, X)
        B.dram("ffn_w_down%d" % l, [DFF, D], F32, X)
        B.dram("g_mix%d" % l, [128, D], F32, X)
        B.dram("g_ffn%d" % l, [128, D], F32, X)
    B.dram("g_final", [128, D], F32, X)
    B.dram("ident_bf", [128, 128], BF16, X)
    B.dram("ident_f32", [128, 128], F32, X)
    B.dram("cos128", [SEQ, 128], F32, X)
    B.dram("sin128", [SEQ, 128], F32, X)
    B.dram("negmask", [128, 128], F32, X)
    B.dram("pow2", [128, NIT], F32, X)
    B.dram("tri_u", [128, 128], F32, X)
    B.dram("tri_sl", [128, 128], F32, X)
    B.dram("hconvT", [NSEQ, 4, 128, SEQ], BF16)
    B.dram("qT", [NSEQ, 4, 128, SEQ], BF16)
    B.dram("iqT", [NSEQ, 4, 128, SEQ], BF16)
    B.dram("kkT", [NSEQ, 2, 128, SEQ], BF16)
    B.dram("vaug", [T, 256], BF16)
    B.dram("iw", [T, 8], F32)
    B.dram("yabT", [NSEQ, 8, 128, SEQ], BF16)
    B.dram("h_mid0", [T, D], F32)
    B.dram("hnT", [8, 128, T], BF16)
    B.dram("h_half", [T, D], F32)
    B.dram("h1", [T, D], F32)
    B.dram("c_qT", [NSEQ, 4, 128, SEQ], BF16)
    B.dram("c_kT", [NSEQ, 4, 128, SEQ], BF16)
    B.dram("c_ktok", [T, 512], BF16)
    B.dram("c_v", [T, D], BF16)
    B.dram("c_sr", [T, D], F32)
    B.dram("c_la", [T, 512], F32)
    B.dram("ycT", [NSEQ, 8, 128, SEQ], BF16)
    B.dram("h_mid1", [T, D], F32)
    B.dram("out", [T, D], F32, "ExternalOutput")
    if DEBUG_STOP == 77:
        B.dram("dbg", [128, 16], F32, "ExternalOutput")
        B.dram("dbg2", [4, 128, D], BF16, "ExternalOutput")


PHASES = {
    "A": phase_A,
    "B": phase_B,
    "C": phase_C,
    "D0": lambda B: phase_D(B, "D0", "yabT", "ab_w_out", "x", "g_ffn0", "h_mid0", "hnT"),
    "F0a": lambda B: phase_F(B, "F0a", 0, 0, "h_mid0", "h_half", "hnT"),
    "F0b": lambda B: phase_F(B, "F0b", 0, 11, "h_half", "h1", "hnT"),
    "E": phase_E,
    "G": phase_G,
    "D1": lambda B: phase_D(B, "D1", "ycT", "c_w_out", "h1", "g_ffn1", "h_mid1", "hnT"),
    "F1a": lambda B: phase_F(B, "F1a", 1, 0, "h_mid1", "h_half", "hnT"),
    "F1b": lambda B: phase_F(B, "F1b", 1, 11, "h_half", "out", "hnT", final_g="g_final"),
}
ORDER = ["A", "B", "C", "D0", "F0a", "F0b", "E", "G", "D1", "F1a", "F1b"]


def host_consts():
    bf = ml_dtypes.bfloat16
    c = {}
    c["ident_bf"] = np.eye(128, dtype=np.float32).astype(bf)
    c["ident_f32"] = np.eye(128, dtype=np.float32)
    rot = 16
    inv = (500000.0 ** (-np.arange(0, rot, 2, dtype=np.float32) / np.float32(rot))).astype(np.float32)
    ang = (np.arange(SEQ, dtype=np.float32)[:, None] * inv[None, :]).astype(np.float32)
    c["cos128"] = np.ascontiguousarray(np.tile(np.cos(ang).astype(np.float32), (1, 16)))
    c["sin128"] = np.ascontiguousarray(np.tile(np.sin(ang).astype(np.float32), (1, 16)))
    i = np.arange(128)
    c["negmask"] = np.where(i[None, :] <= i[:, None], 0.0, -1e30).astype(np.float32)
    c["pow2"] = np.ascontiguousarray(np.broadcast_to((0.5 ** np.arange(1, NIT + 1)).astype(np.float32), (128, NIT)))
    c["tri_u"] = (i[:, None] <= i[None, :]).astype(np.float32)
    c["tri_sl"] = (i[:, None] > i[None, :]).astype(np.float32)
    return c


def host_weights(inp):
    f = lambda a: np.ascontiguousarray(np.asarray(a, dtype=np.float32))
    bc = lambda v, n: np.ascontiguousarray(np.broadcast_to(np.asarray(v, np.float32).reshape(1, -1), (128, n)))
    w = {}
    w["ab_w_in"] = f(inp["ab_w_in"][0])
    w["ab_conv_w"] = f(inp["ab_conv_w"][0].reshape(31, 512))
    prm = np.concatenate([np.asarray(inp[k][0], np.float32).reshape(4, 128).T for k in ("ab_conv_b", "ab_ln_g", "ab_ln_b")], axis=1)
    w["conv_prm"] = f(prm)
    w["ab_w_out"] = f(inp["ab_w_out"][0])
    w["c_w_in"] = f(inp["c_w_in"][0])
    w["c_gate_w"] = f(inp["c_gate_w"][0])
    w["c_gate_b_bc"] = bc(inp["c_gate_b"][0], 512)
    w["c_onorm_g_bc"] = bc(inp["c_onorm_g"][0], D)
    w["c_w_out"] = f(inp["c_w_out"][0])
    for l in range(2):
        w["ffn_w_gate%d" % l] = f(inp["ffn_w_gate"][l])
        w["ffn_w_up%d" % l] = f(inp["ffn_w_up"][l])
        w["ffn_w_down%d" % l] = f(inp["ffn_w_down"][l])
        w["g_mix%d" % l] = bc(inp["norm_mix_g"][l], D)
        w["g_ffn%d" % l] = bc(inp["norm_ffn_g"][l], D)
    w["g_final"] = bc(inp["final_norm_g"], D)
    return w


def build_program(nseq, phases, ext=None):
    B = Build(nseq, ext)
    declare(B)
    for p in phases:
        PHASES[p](B)
    return B


def kernel(**inp):
    n = 8
    nseq = 2
    x = np.asarray(inp["x"], dtype=np.float32)
    B = build_program(nseq, ORDER)
    shared = host_consts()
    shared.update(host_weights(inp))
    in_maps = []
    for c in range(n):
        m = dict(shared)
        m["x"] = np.ascontiguousarray(x[c * nseq:(c + 1) * nseq].reshape(nseq * SEQ, D))
        in_maps.append(m)
    res = run_bass_kernel_spmd(B.nc, in_maps, core_ids=list(range(n)))
    out = np.stack([np.asarray(r["out"], dtype=np.float32).reshape(nseq, SEQ, D) for r in res.results], axis=0)
    return out.reshape(16, SEQ, D)
```

```python
import contextlib
import math
import numpy as np
import ml_dtypes
import concourse.bass as bass
import concourse.mybir as mybir
from concourse.bass_utils import run_bass_kernel_spmd

F32 = mybir.dt.float32
BF16 = mybir.dt.bfloat16
ALU = mybir.AluOpType
AF = mybir.ActivationFunctionType
AX = mybir.AxisListType

ENGS = ["sync", "scalar", "vector", "gpsimd", "tensor"]
D = 1024
SEQ = 2048
NT = 16
EPS = 1e-6
DFF = 2816
NFF = 22
NIT = 16
TOPK = 256
DEBUG_STOP = 99
SAME_SYNC = True


class Sched:
    def __init__(self, nc, stack, tag=""):
        self.nc = nc
        self.stack = stack
        self.tag = tag
        self.q = {e: [] for e in ENGS}
        self.sem = {}
        self.cnt = {}
        self.waited = {e: {} for e in ENGS}
        self.lastw = {}
        self.readers = {}
        self.pe_pending = False
        self.out_deps = {}
        for e in ENGS:
            self._mksem("E_" + e)

    def _mksem(self, name):
        if name not in self.sem:
            hw_name = "%s_s%d" % (self.tag, len(self.sem))
            self.sem[name] = self.nc.alloc_semaphore(name=hw_name)
            self.cnt[name] = 0
        return name

    def op(self, eng, fn, reads=(), writes=(), dma=None, mark=True, is_out=False, force_self=False):
        deps = {}

        def add(d):
            if d[1] > deps.get(d[0], 0):
                deps[d[0]] = d[1]

        for k in reads:
            if k in self.lastw:
                add(self.lastw[k])
        for k in writes:
            if k in self.lastw:
                add(self.lastw[k])
            for s_, v_ in self.readers.get(k, {}).items():
                add((s_, v_))
        if dma is not None:
            s = self._mksem("D_" + str(dma))
            if self.cnt[s] > 0:
                add((s, self.cnt[s]))
        own = "E_" + eng
        need = []
        for s_, v_ in deps.items():
            if s_ == own and (eng == "tensor" or not (SAME_SYNC or force_self)):
                continue
            if self.waited[eng].get(s_, 0) >= v_:
                continue
            need.append((s_, v_))
        attach = None
        if eng == "tensor":
            k0 = reads[0] if len(reads) else None
            if k0 is not None and k0 in self.lastw and self.lastw[k0][0] != own:
                attach = self.lastw[k0]
        else:
            selfs = [d_ for d_ in need if d_[0] == own]
            attach = selfs[0] if selfs else (need[-1] if need else None)
        standalone = [d_ for d_ in need if attach is None or d_[0] != attach[0]]
        if attach is not None:
            for d_ in need:
                if d_[0] == attach[0] and d_[1] > attach[1]:
                    attach = d_
        for d_ in need:
            self.waited[eng][d_[0]] = max(self.waited[eng].get(d_[0], 0), d_[1])
        if attach is not None:
            self.waited[eng][attach[0]] = max(self.waited[eng].get(attach[0], 0), attach[1])
        if dma is not None:
            self.cnt[s] += 16
            dep = (s, self.cnt[s])
            self.q[eng].append(("op", fn, s, 16, standalone, attach))
        else:
            s = own
            if mark:
                self.cnt[s] += 1
                dep = (s, self.cnt[s])
                self.q[eng].append(("op", fn, s, 1, standalone, attach))
                if eng == "tensor":
                    self.pe_pending = False
            else:
                assert eng == "tensor"
                dep = (s, self.cnt[s] + 1)
                self.q[eng].append(("op", fn, None, 0, standalone, attach))
                self.pe_pending = True
        for k in writes:
            self.lastw[k] = dep
            self.readers[k] = {}
        for k in reads:
            r = self.readers.setdefault(k, {})
            if dep[1] > r.get(dep[0], 0):
                r[dep[0]] = dep[1]
        if is_out:
            if dep[1] > self.out_deps.get(dep[0], 0):
                self.out_deps[dep[0]] = dep[1]
        return dep

    def finalize(self):
        assert not self.pe_pending, "unmarked PE op at end of phase"
        tail = []
        for s_, v_ in self.out_deps.items():
            if self.waited["sync"].get(s_, 0) < v_:
                tail.append((s_, v_))
        nc = self.nc
        with nc.Block() as block:
            for eng in ENGS:
                items = self.q[eng]

                def body(e, items=items, eng=eng):
                    for it in items:
                        for (ws, wv) in it[4]:
                            e.wait_ge(self.sem[ws], wv)
                        ins = it[1](e)
                        if it[5] is not None:
                            ins._wait_ge(self.sem[it[5][0]], it[5][1])
                        if it[2] is not None:
                            ins.then_inc(self.sem[it[2]], it[3])
                    if eng == "sync":
                        for (ws, wv) in tail:
                            e.wait_ge(self.sem[ws], wv)

                getattr(block, eng)(body)


class Phase:
    def __init__(self, nc, tag):
        self.nc = nc
        self.tag = tag
        self.st = contextlib.ExitStack()
        self.st.enter_context(nc.cleanup_on_exit())
        self.S = Sched(nc, self.st, tag)

    def sb(self, name, shape, dt):
        return self.st.enter_context(self.nc.sbuf_tensor(self.tag + "_" + name, shape, dt))

    def ps(self, name, shape, dt=F32):
        return self.st.enter_context(self.nc.psum_tensor(self.tag + "_" + name, shape, dt))

    def dma(self, eng, out, in_, r=(), w=(), key=None, is_out=False):
        return self.S.op(eng, lambda e: e.dma_start(out=out, in_=in_), r, w, dma=key, is_out=is_out)

    def mm(self, out, lhsT, rhs, start, stop, r, w, mark=None):
        if mark is None:
            mark = stop
        return self.S.op("tensor", lambda e: e.matmul(out, lhsT=lhsT, rhs=rhs, start=start, stop=stop), r, w, mark=mark)

    def tr(self, out, in_, ident, r, w, mark=True):
        return self.S.op("tensor", lambda e: e.transpose(out=out, in_=in_, identity=ident), r, w, mark=mark)

    def act(self, out, in_, func, r, w, **kw):
        return self.S.op("scalar", lambda e: e.activation(out=out, in_=in_, func=func, **kw), r, w)

    def tt(self, eng, out, in0, in1, op, r, w):
        return self.S.op(eng, lambda e: e.tensor_tensor(out=out, in0=in0, in1=in1, op=op), r, w)

    def ts(self, eng, out, in0, s1, op0, r, w, s2=None, op1=None, accum_out=None):
        if op1 is None:
            return self.S.op(eng, lambda e: e.tensor_scalar(out=out, in0=in0, scalar1=s1, scalar2=None, op0=op0), r, w)
        return self.S.op(eng, lambda e: e.tensor_scalar(out=out, in0=in0, scalar1=s1, scalar2=s2, op0=op0, op1=op1,
                                                       accum_out=accum_out), r, w)

    def stt(self, out, in0, scalar, in1, op0, op1, r, w):
        return self.S.op("vector", lambda e: e.scalar_tensor_tensor(out=out, in0=in0, scalar=scalar, in1=in1,
                                                                     op0=op0, op1=op1), r, w)

    def copy(self, eng, out, in_, r, w):
        if eng == "scalar":
            return self.S.op(eng, lambda e: e.copy(out=out, in_=in_), r, w)
        return self.S.op(eng, lambda e: e.tensor_copy(out=out, in_=in_), r, w)

    def recip(self, out, in_, r, w):
        return self.S.op("vector", lambda e: e.reciprocal(out=out, in_=in_), r, w)

    def memset(self, eng, ap, val, w):
        return self.S.op(eng, lambda e: e.memset(ap, val), (), w)

    def reduce(self, out, in_, op, r, w):
        return self.S.op("vector", lambda e: e.tensor_reduce(out=out, in_=in_, axis=AX.X, op=op), r, w)

    def close(self):
        self.S.finalize()
        self.st.close()


class Build:
    def __init__(self, nseq, ext=None):
        self.nc = bass.Bass("TRN2", target_bir_lowering=False)
        self.NSEQ = nseq
        self.t = {}
        self.ext = ext or {}
        self.kinds = {}

    def dram(self, name, shape, dt, kind="Internal"):
        kind = self.ext.get(name, kind)
        self.kinds[name] = (kind, list(shape), dt)
        self.t[name] = self.nc.dram_tensor(name, list(shape), dt, kind=kind).ap()
        return self.t[name]


def rms_to_hnT(P, xt, xkey, gbc, eps_t, stat, skey, junk, hn, hkey, ps_tr, pkey, identb, dst, dkey, sl):
    P.act(junk[:], xt, AF.Square, [xkey], ["junkA", skey], accum_out=stat[:, 0:1])
    P.act(stat[:, 1:2], stat[:, 0:1], AF.Sqrt, [skey, "eps"], [skey], scale=1.0 / D, bias=eps_t[:, 0:1])
    P.recip(stat[:, 2:3], stat[:, 1:2], [skey], [skey])
    P.stt(hn, xt, stat[:, 2:3], gbc, ALU.mult, ALU.mult, [xkey, skey, "gbc"], [hkey])
    for k in range(8):
        P.tr(ps_tr[:, k, :], hn[:, k * 128:(k + 1) * 128], identb[:], [hkey, "identb"], [pkey], mark=(k == 7))
    P.copy("scalar", dst[:, 0:8, sl], ps_tr[:, 0:8, :], [pkey], [dkey])


def phase_A(B):
    nc, d, NSEQ = B.nc, B.t, B.NSEQ
    P = Phase(nc, "A")
    Wt = P.sb("Wt", [128, 8, 2376], BF16)
    Wsrc = d["ab_w_in"].rearrange("(k p) n -> p k n", p=128)
    segs = [(0, 1024, 0), (1024, 1536, 1024), (1664, 2176, 1536), (1536, 1600, 2048), (1536, 1600, 2112),
            (2176, 2240, 2176), (2176, 2240, 2240), (1600, 1664, 2304), (2240, 2248, 2368)]
    WK = []
    for i, (c0, c1, d0) in enumerate(segs):
        P.dma("gpsimd", Wt[:, :, d0:d0 + c1 - c0], Wsrc[:, :, c0:c1], w=[("W", i)], key=("W", i))
        WK.append(("W", i))
    identb = P.sb("identb", [128, 128], BF16)
    P.dma("sync", identb[:], d["ident_bf"], w=["identb"], key="identb")
    gbc = P.sb("gbc", [128, D], F32)
    P.dma("sync", gbc[:], d["g_mix0"], w=["gbc"], key="gbc")
    cos = P.sb("cos", [128, NT, 128], F32)
    sin = P.sb("sin", [128, NT, 128], F32)
    P.dma("sync", cos[:], d["cos128"].rearrange("(t p) c -> p t c", p=128), w=["cos"], key="cos")
    P.dma("sync", sin[:], d["sin128"].rearrange("(t p) c -> p t c", p=128), w=["sin"], key="sin")
    eps_t = P.sb("eps", [128, 1], F32)
    P.memset("vector", eps_t[:], EPS, ["eps"])

    xt = [P.sb("xt%d" % i, [128, D], F32) for i in range(2)]
    junk = P.sb("junk", [128, D], F32)
    stat = [P.sb("stat%d" % i, [128, 4], F32) for i in range(2)]
    hn = [P.sb("hn%d" % i, [128, D], BF16) for i in range(2)]
    hnT = [P.sb("hnT%d" % i, [128, 8, 512], BF16) for i in range(2)]
    ps_tr = [P.ps("ptr%d" % i, [128, 8, 128], BF16) for i in range(2)]
    ps_a = P.ps("psa", [128, 512])
    ps_g = P.ps("psg", [128, 512])
    ps_q = P.ps("psq", [128, 1024])
    ps_c = P.ps("psc", [128, 512])
    sig = [P.sb("sig%d" % i, [128, 512], F32) for i in range(2)]
    hc = [P.sb("hc%d" % i, [128, 512], BF16) for i in range(2)]
    tmp = [P.sb("tmp%d" % i, [128, 128], F32) for i in range(4)]
    qr = [P.sb("qr%d" % i, [128, 1024], BF16) for i in range(2)]
    kr = [P.sb("kr%d" % i, [128, 256], BF16) for i in range(2)]
    vaug = [P.sb("vaug%d" % i, [128, 256], BF16) for i in range(2)]
    iwt = [P.sb("iw%d" % i, [128, 8], F32) for i in range(2)]
    for i in range(2):
        P.memset("gpsimd", vaug[i][:, 64:192], 1.0, [("vaug", i)])
    qTg = [P.sb("qTg%d" % i, [128, 8, 512], BF16) for i in range(2)]
    kTg = [P.sb("kTg%d" % i, [128, 2, 512], BF16) for i in range(2)]
    w_scale = (8 ** -0.5) * (64 ** -0.5)

    def load_x(b, tt):
        xs = tt % 2
        r0 = b * SEQ + tt * 128
        P.dma("sync", xt[xs][:], d["x"][r0:r0 + 128, :], w=[("x", xs)], key=("x", xs))

    n_tiles = NSEQ * NT
    if DEBUG_STOP <= 1:
        P.dma("sync", d["iw"][0:128, :], iwt[0][:], r=[("iw", 0), "cos", "sin", "gbc", "identb"] + WK, key=("iw", 0), is_out=True)
        P.close()
        return
    load_x(0, 0)
    for gi in range(NSEQ * 4):
        b, g = gi // 4, gi % 4
        gs = gi % 2
        if DEBUG_STOP <= 6 and gi >= max(1, DEBUG_STOP - 3):
            break
        for t4 in range(4):
            tt = g * 4 + t4
            ti = gi * 4 + t4
            if ti + 1 < n_tiles and not (DEBUG_STOP <= 6 and ti + 1 >= 4 * max(1, DEBUG_STOP - 3)):
                load_x((ti + 1) // NT, (ti + 1) % NT)
            xs = tt % 2
            rms_to_hnT(P, xt[xs][:], ("x", xs), gbc[:], eps_t, stat[xs], ("st", xs), junk, hn[xs][:], ("hn", xs),
                       ps_tr[0], "ptr0", identb, hnT[gs], ("hnT", gs), slice(t4 * 128, (t4 + 1) * 128))
        if DEBUG_STOP <= 2:
            P.dma("sync", d["qT"][b, :, :, 0:512].rearrange("c p t -> p c t"), hnT[gs][:, 0:4, :], r=[("hnT", gs)], key=("qTg", gs), is_out=True)
            break
        for c in range(4):
            for k in range(8):
                P.mm(ps_a[:, :], Wt[:, k, c * 128:(c + 1) * 128], hnT[gs][:, k, :], k == 0, k == 7,
                     [("hnT", gs)] + WK, ["psa"])
            for k in range(8):
                P.mm(ps_g[:, :], Wt[:, k, 512 + c * 128:512 + (c + 1) * 128], hnT[gs][:, k, :], k == 0, k == 7,
                     [("hnT", gs)] + WK, ["psg"])
            s2 = c % 2
            P.act(sig[s2][:], ps_g[:, :], AF.Sigmoid, ["psg"], [("sig", s2)])
            P.tt("vector", hc[s2][:], ps_a[:, :], sig[s2][:], ALU.mult, ["psa", ("sig", s2)], [("hc", s2)])
            P.dma("sync", d["hconvT"][b, c, :, g * 512:(g + 1) * 512], hc[s2][:], r=[("hc", s2)], key=("hc", s2), is_out=True)
        if DEBUG_STOP <= 3:
            break
        for t4 in range(4):
            tt = g * 4 + t4
            r0 = b * SEQ + tt * 128
            tok = slice(t4 * 128, (t4 + 1) * 128)
            s2 = t4 % 2
            for k in range(8):
                P.mm(ps_q[:, 0:512], hnT[gs][:, k, tok], Wt[:, k, 1024:1536], k == 0, k == 7, [("hnT", gs)] + WK, ["psq"], mark=False)
            for k in range(8):
                P.mm(ps_q[:, 512:1024], hnT[gs][:, k, tok], Wt[:, k, 1536:2048], k == 0, k == 7, [("hnT", gs)] + WK, ["psq"], mark=False)
            for k in range(8):
                P.mm(ps_c[:, 0:328], hnT[gs][:, k, tok], Wt[:, k, 2048:2376], k == 0, k == 7, [("hnT", gs)] + WK, ["psq"])
            for (src, nh, dst, dkey) in ((ps_q[:, 0:512], 8, qr[s2][:, 0:512], ("qr", s2)), (ps_q[:, 512:1024], 8, qr[s2][:, 512:1024], ("qr", s2)),
                                         (ps_c[:, 0:256], 4, kr[s2][:, 0:256], ("kr", s2))):
                sv = src.rearrange("p (h e) -> p h e", e=64)
                dv = dst.rearrange("p (h e) -> p h e", e=64)
                x1, x2 = sv[:, :, 0:8], sv[:, :, 8:16]
                cv = cos[:, tt, 0:nh * 8].rearrange("p (h e) -> p h e", e=8)
                sn = sin[:, tt, 0:nh * 8].rearrange("p (h e) -> p h e", e=8)
                tv = [tmp[i][:, 0:nh * 8].rearrange("p (h e) -> p h e", e=8) for i in range(4)]
                P.tt("vector", tv[0], x1, cv, ALU.mult, ["psq", "cos"], ["tmp0"])
                P.tt("vector", tv[1], x2, sn, ALU.mult, ["psq", "sin"], ["tmp1"])
                P.tt("vector", dv[:, :, 0:8], tv[0], tv[1], ALU.subtract, ["tmp0", "tmp1"], [dkey])
                P.tt("vector", tv[2], x2, cv, ALU.mult, ["psq", "cos"], ["tmp2"])
                P.tt("vector", tv[3], x1, sn, ALU.mult, ["psq", "sin"], ["tmp3"])
                P.tt("vector", dv[:, :, 8:16], tv[2], tv[3], ALU.add, ["tmp2", "tmp3"], [dkey])
                P.copy("scalar", dv[:, :, 16:64], sv[:, :, 16:64], ["psq"], [dkey])
            P.copy("scalar", vaug[s2][:, 0:64], ps_c[:, 256:320], ["psq"], [("vaug", s2)])
            P.copy("scalar", vaug[s2][:, 192:256], ps_c[:, 256:320], ["psq"], [("vaug", s2)])
            P.ts("vector", iwt[s2][:], ps_c[:, 320:328], w_scale, ALU.mult, ["psq"], [("iw", s2)])
            P.dma("sync", d["vaug"][r0:r0 + 128, :], vaug[s2][:], r=[("vaug", s2)], key=("vaug", s2), is_out=True)
            P.dma("sync", d["iw"][r0:r0 + 128, :], iwt[s2][:], r=[("iw", s2)], key=("iw", s2), is_out=True)
            for k in range(8):
                P.tr(ps_tr[1][:, k, :], qr[s2][:, k * 128:(k + 1) * 128], identb[:], [("qr", s2), "identb"], ["ptr1"], mark=(k == 7))
            P.copy("scalar", qTg[gs][:, 0:8, tok], ps_tr[1][:, 0:8, :], ["ptr1"], [("qTg", gs)])
            for k in range(2):
                P.tr(ps_tr[1][:, k, :], kr[s2][:, k * 128:(k + 1) * 128], identb[:], [("kr", s2), "identb"], ["ptr1"], mark=(k == 1))
            P.copy("scalar", kTg[gs][:, 0:2, tok], ps_tr[1][:, 0:2, :], ["ptr1"], [("kTg", gs)])
        gsl = slice(g * 512, (g + 1) * 512)
        P.dma("sync", d["qT"][b, :, :, gsl].rearrange("c p t -> p c t"), qTg[gs][:, 0:4, :], r=[("qTg", gs)], key=("qTg", gs), is_out=True)
        P.dma("sync", d["iqT"][b, :, :, gsl].rearrange("c p t -> p c t"), qTg[gs][:, 4:8, :], r=[("qTg", gs)], key=("iqTg", gs), is_out=True)
        P.dma("sync", d["kkT"][b, :, :, gsl].rearrange("c p t -> p c t"), kTg[gs][:, :, :], r=[("kTg", gs)], key=("kTg", gs), is_out=True)
    P.close()


def phase_B(B):
    nc, d, NSEQ = B.nc, B.t, B.NSEQ
    P = Phase(nc, "B")
    identb = P.sb("identb", [128, 128], BF16)
    P.dma("sync", identb[:], d["ident_bf"], w=["identb"], key="identb")
    identf = P.sb("identf", [128, 128], F32)
    P.dma("sync", identf[:], d["ident_f32"], w=["identf"], key="identf")
    ones = P.sb("ones", [128, 128], F32)
    P.memset("vector", ones[:], 1.0, ["ones"])
    eps_t = P.sb("eps", [128, 1], F32)
    P.memset("vector", eps_t[:], EPS, ["eps"])
    cw = P.sb("cw", [31, 512], F32)
    P.dma("sync", cw[:], d["ab_conv_w"], w=["cw"], key="cw")
    prm = P.sb("prm", [128, 12], F32)
    P.dma("sync", prm[:], d["conv_prm"], w=["prm"], key="prm")
    ps_w = P.ps("psw", [128, 4, 32])
    for c in range(4):
        P.tr(ps_w[:, c, 0:31], cw[0:31, c * 128:(c + 1) * 128], identf[0:31, 0:31], ["cw", "identf"], ["psw"], mark=(c == 3))
    wT = P.sb("wT", [128, 4, 32], F32)
    P.copy("vector", wT[:, :, 0:31], ps_w[:, :, 0:31], ["psw"], ["wT"])
    diag = P.sb("diag", [128, 124, 128], BF16)
    for c in range(4):
        for j in range(31):
            i = c * 31 + j
            P.ts("vector" if i % 2 == 0 else "gpsimd", diag[:, i, :], identb[:], wT[:, c, j:j + 1], ALU.mult,
                 ["identb", "wT"], [("diag", c)])
    hbuf = P.sb("hbuf", [128, 4, 30 + SEQ], BF16)
    P.memset("gpsimd", hbuf[:, :, 0:30], 0.0, ["hbuf"])
    ps_c = [P.ps("psc%d" % i, [128, 512]) for i in range(2)]
    ps_s1 = P.ps("ps1", [128, 512])
    ps_s2 = P.ps("ps2", [128, 512])
    hcs = P.sb("hcs", [128, 4, 512], F32)
    sqs = P.sb("sqs", [128, 4, 512], F32)
    m = P.sb("m", [128, 512], F32)
    msq = P.sb("msq", [128, 512], F32)
    var = P.sb("var", [128, 512], F32)
    rstd = P.sb("rstd", [128, 512], F32)
    z = [P.sb("z%d" % i, [128, 512], F32) for i in range(2)]
    yst = [P.sb("yst%d" % i, [128, 4, 512], BF16) for i in range(2)]
    for b in range(NSEQ):
        for c in range(4):
            P.dma("sync", hbuf[:, c, 30:30 + SEQ], d["hconvT"][b, c, :, :], w=["hbuf"], key=("hbuf", c))
        for g in range(4):
            ys = g % 2
            for c in range(4):
                pc = ps_c[c % 2]
                pk = ("psc", c % 2)
                for j in range(31):
                    P.mm(pc[:, :], diag[:, c * 31 + j, :], hbuf[:, c, g * 512 + j:g * 512 + j + 512], j == 0, j == 30,
                         [("diag", c), "hbuf"], [pk])
                P.act(hcs[:, c, :], pc[:, :], AF.Identity, [pk, "prm"], [("hcs", c)], bias=prm[:, c:c + 1], scale=1.0)
                P.act(sqs[:, c, :], pc[:, :], AF.Square, [pk, "prm"], [("sqs", c)], bias=prm[:, c:c + 1], scale=1.0)
            for c in range(4):
                P.mm(ps_s1[:, :], ones[:], hcs[:, c, :], c == 0, c == 3, [("hcs", c), "ones"], ["ps1"])
            for c in range(4):
                P.mm(ps_s2[:, :], ones[:], sqs[:, c, :], c == 0, c == 3, [("sqs", c), "ones"], ["ps2"])
            P.act(m[:], ps_s1[:, :], AF.Copy, ["ps1"], ["m"], scale=1.0 / 512)
            P.tt("gpsimd", msq[:], m[:], m[:], ALU.mult, ["m"], ["msq"])
            P.stt(var[:], ps_s2[:, :], 1.0 / 512, msq[:], ALU.mult, ALU.subtract, ["ps2", "msq"], ["var"])
            P.act(var[:], var[:], AF.Sqrt, ["var", "eps"], ["var"], bias=eps_t[:, 0:1], scale=1.0)
            P.recip(rstd[:], var[:], ["var"], ["rstd"])
            for c in range(4):
                zz = z[c % 2]
                zk = ("z", c % 2)
                P.tt("gpsimd", zz[:], hcs[:, c, :], m[:], ALU.subtract, [("hcs", c), "m"], [zk])
                P.tt("vector", zz[:], zz[:], rstd[:], ALU.mult, [zk, "rstd"], [zk])
                P.act(yst[ys][:, c, :], zz[:], AF.Silu, [zk, "prm"], [("yst", ys)], scale=prm[:, 4 + c:5 + c], bias=prm[:, 8 + c:9 + c])
            P.dma("sync", d["yabT"][b, 0:4, :, g * 512:(g + 1) * 512].rearrange("c p t -> p c t"), yst[ys][:],
                  r=[("yst", ys)], key=("yst", ys), is_out=True)
    P.close()


def phase_C(B):
    nc, d, NSEQ = B.nc, B.t, B.NSEQ
    P = Phase(nc, "C")
    identb = P.sb("identb", [128, 128], BF16)
    P.dma("sync", identb[:], d["ident_bf"], w=["identb"], key="identb")
    negm = P.sb("negm", [128, 128], F32)
    P.dma("sync", negm[:], d["negmask"], w=["negm"], key="negm")
    pow2 = P.sb("pow2", [128, NIT], F32)
    P.dma("sync", pow2[:], d["pow2"], w=["pow2"], key="pow2")
    thr0 = P.sb("thr0", [128, 1], F32)
    P.memset("vector", thr0[:], -1e29, ["thr0"])
    bigI = P.sb("bigI", [128, 128], BF16)
    P.ts("vector", bigI[:], identb[:], 30000.0, ALU.mult, ["identb"], ["bigI"])
    qT = [P.sb("qT%d" % i, [128, 4, SEQ], BF16) for i in range(2)]
    iqT = [P.sb("iqT%d" % i, [128, 4, SEQ], BF16) for i in range(2)]
    kkT = [P.sb("kkT%d" % i, [128, 2, SEQ], BF16) for i in range(2)]
    vaug = [P.sb("vaug%d" % i, [128, NT, 256], BF16) for i in range(2)]
    iw = [P.sb("iw%d" % i, [128, NT, 8], F32) for i in range(2)]
    ybg = [P.sb("ybg%d" % i, [128, 512], BF16) for i in range(2)]
    maskT = [P.sb("maskT%d" % i, [128, NT, 512], BF16) for i in range(2)]
    score = [P.sb("score%d" % i, [128, SEQ], F32) for i in range(2)]
    junk = P.sb("junk", [128, SEQ], BF16)
    rl = [P.sb("rl%d" % i, [128, 512], F32) for i in range(2)]
    mk = [P.sb("mk%d" % i, [128, SEQ], BF16) for i in range(2)]
    st = [P.sb("st%d" % i, [128, 8 + NIT], F32) for i in range(2)]
    pe = [P.sb("pe%d" % i, [128, 512], BF16) for i in range(3)]
    rc = [P.sb("rc%d" % i, [128, 512], F32) for i in range(2)]
    ps_lg = [P.ps("plg%d" % i, [128, 512]) for i in range(2)]
    ps_tr = [P.ps("ptr%d" % i, [128, 8, 128], BF16) for i in range(2)]
    ps_s = [P.ps("pss%d" % i, [128, 512]) for i in range(2)]
    ps_o = [P.ps("pso%d" % i, [128, 512]) for i in range(2)]
    att_scale = 64 ** -0.5
    ctr = {"lg": 0, "s": 0, "o": 0, "tr": 0, "yb": 0, "g": 0}

    mslot = {}

    def load_seq(b):
        p = b % 2
        rs = slice(b * SEQ, (b + 1) * SEQ)
        P.dma("sync", iqT[p][:], d["iqT"][b].rearrange("c p t -> p c t"), w=[("iqT", p)], key=("iqT", p))
        P.dma("sync", kkT[p][:], d["kkT"][b].rearrange("c p t -> p c t"), w=[("kkT", p)], key=("kkT", p))
        P.dma("sync", iw[p][:], d["iw"][rs, :].rearrange("(t p) c -> p t c", p=128), w=[("iw", p)], key=("iw", p))
        P.dma("sync", qT[p][:], d["qT"][b].rearrange("c p t -> p c t"), w=[("qT", p)], key=("qT", p))
        P.dma("sync", vaug[p][:], d["vaug"][rs, :].rearrange("(t p) c -> p t c", p=128), w=[("vaug", p)], key=("vaug", p))

    junk2 = [junk, P.sb("junkb", [128, SEQ], BF16)]

    def score_pair(b, G, q4s):
        p = b % 2
        mG = mslot[(b, G)]
        info = []
        for q4 in q4s:
            qb = 4 * G + q4
            nk = 128 * (qb + 1)
            nkb = (nk + 511) // 512
            s2 = qb % 2
            sc = score[s2]
            sks = [("score", s2, kb) for kb in range(nkb)]
            qsl = slice(qb * 128, (qb + 1) * 128)
            for h in range(8):
                c, base = h // 2, 64 * (h % 2)
                for kb in range(nkb):
                    n = min(512, nk - kb * 512)
                    r = ctr["lg"] % 2
                    ctr["lg"] += 1
                    P.mm(ps_lg[r][:, 0:n], iqT[p][base:base + 64, c, qsl], kkT[p][base:base + 64, 1, kb * 512:kb * 512 + n],
                         True, True, [("iqT", p), ("kkT", p)], [("plg", r)])
                    P.act(rl[r][:, 0:n], ps_lg[r][:, 0:n], AF.Relu, [("plg", r)], [("rl", r)])
                    dst = sc[:, kb * 512:kb * 512 + n]
                    if h == 0:
                        P.ts("vector", dst, rl[r][:, 0:n], iw[p][:, qb, 0:1], ALU.mult, [("rl", r), ("iw", p)], [sks[kb]])
                    else:
                        P.stt(dst, rl[r][:, 0:n], iw[p][:, qb, h:h + 1], dst, ALU.mult, ALU.add, [("rl", r), ("iw", p), sks[kb]], [sks[kb]])
            info.append((q4, qb, nk, s2, sc, sks, qsl))
        for (q4, qb, nk, s2, sc, sks, qsl) in info:
            stt_ = st[s2]
            stk = ("st", s2)
            if qb >= 2:
                P.reduce(stt_[:, 0:1], sc[:, 0:nk], ALU.max, sks, [stk])
                P.reduce(stt_[:, 1:2], sc[:, 0:nk], ALU.min, sks, [(stk, "lo")])
            P.tt("vector", sc[:, qsl], sc[:, qsl], negm[:], ALU.add, [sks[qb // 4], "negm"], [sks[qb // 4]])
            if qb >= 2:
                P.tt("vector", stt_[:, 2:3], stt_[:, 0:1], stt_[:, 1:2], ALU.subtract, [stk, (stk, "lo")], [stk])
                P.ts("vector", stt_[:, 8:8 + NIT], pow2[:], stt_[:, 2:3], ALU.mult, ["pow2", stk], [(stk, "steps")])
        bis = [x for x in info if x[1] >= 2]
        for i in range(NIT):
            for (q4, qb, nk, s2, sc, sks, qsl) in bis:
                stt_, stk = st[s2], ("st", s2)
                P.tt("vector", stt_[:, 3:4], stt_[:, 1:2], stt_[:, 8 + i:9 + i], ALU.add, [(stk, "lo"), (stk, "steps")], [(stk, "mid")])
            for (q4, qb, nk, s2, sc, sks, qsl) in bis:
                stt_, stk = st[s2], ("st", s2)
                P.ts("vector", junk2[s2][:, 0:nk], sc[:, 0:nk], stt_[:, 3:4], ALU.is_ge, sks + [(stk, "mid")], [("junk", s2), (stk, "cnt")],
                     s2=0.0, op1=ALU.add, accum_out=stt_[:, 4:5])
            for (q4, qb, nk, s2, sc, sks, qsl) in bis:
                stt_, stk = st[s2], ("st", s2)
                P.stt(stt_[:, 5:6], stt_[:, 4:5], TOPK - 0.5, stt_[:, 8 + i:9 + i], ALU.is_ge, ALU.mult, [(stk, "cnt"), (stk, "steps")], [(stk, "sel")])
            for (q4, qb, nk, s2, sc, sks, qsl) in bis:
                stt_, stk = st[s2], ("st", s2)
                P.tt("vector", stt_[:, 1:2], stt_[:, 1:2], stt_[:, 5:6], ALU.add, [(stk, "lo"), (stk, "sel")], [(stk, "lo")])
        for (q4, qb, nk, s2, sc, sks, qsl) in info:
            stt_, stk = st[s2], ("st", s2)
            thr = stt_[:, 1:2] if qb >= 2 else thr0[:, 0:1]
            P.ts("vector", mk[s2][:, 0:nk], sc[:, 0:nk], thr, ALU.is_ge, sks + [(stk, "lo"), "thr0"], [("mk", s2)],
                 s2=-1.0, op1=ALU.add)
            kt = 0
            while kt <= qb:
                n = min(8, qb + 1 - kt)
                r = ctr["tr"] % 2
                ctr["tr"] += 1
                for j in range(n):
                    P.tr(ps_tr[r][:, j, :], mk[s2][:, (kt + j) * 128:(kt + j + 1) * 128], identb[:], [("mk", s2), "identb"],
                         [("ptr", r)], mark=(j == n - 1))
                P.copy("scalar", maskT[mG][:, kt:kt + n, q4 * 128:(q4 + 1) * 128], ps_tr[r][:, 0:n, :], [("ptr", r)], [("maskT", mG)])
                kt += n

    def attn_chunk(b, G, c):
        p = b % 2
        mG = mslot[(b, G)]
        nkt = 4 * G + 4
        gsl0 = G * 512
        ys = ctr["yb"] % 2
        ctr["yb"] += 1
        for e in range(2):
            base = 64 * e
            o = ctr["o"] % 2
            ctr["o"] += 1
            for kt in range(nkt):
                q0 = max(0, kt - 4 * G) * 128
                N = 512 - q0
                r = ctr["s"] % 2
                ctr["s"] += 1
                P.mm(ps_s[r][:, 0:N], kkT[p][base:base + 64, 0, kt * 128:(kt + 1) * 128], qT[p][base:base + 64, c, gsl0 + q0:gsl0 + 512],
                     True, False, [("kkT", p), ("qT", p)], [("pss", r)], mark=False)
                P.mm(ps_s[r][:, 0:N], bigI[:], maskT[mG][:, kt, q0:512], False, True, ["bigI", ("maskT", mG)], [("pss", r)])
                r3 = ctr["s"] % 3
                P.act(pe[r3][:, 0:N], ps_s[r][:, 0:N], AF.Exp, [("pss", r)], [("pe", r3)], scale=att_scale)
                P.mm(ps_o[o][:, q0:512], vaug[p][:, kt, e * 128:(e + 1) * 128], pe[r3][:, 0:N], kt == 0, kt == nkt - 1,
                     [("pe", r3), ("vaug", p)], [("pso", o)])
            so, ss_ = (slice(0, 64), slice(64, 128)) if e == 0 else (slice(64, 128), slice(0, 64))
            P.act(rc[o][ss_, :], ps_o[o][ss_, :], AF.Ln, [("pso", o)], [("rc", o)])
            P.act(rc[o][ss_, :], rc[o][ss_, :], AF.Exp, [("rc", o)], [("rc", o)], scale=-1.0)
            P.tt("vector", ybg[ys][so, :], ps_o[o][so, :], rc[o][ss_, :], ALU.mult, [("pso", o), ("rc", o)], [("ybg", ys)])
        P.dma("sync", d["yabT"][b, 4 + c, :, gsl0:gsl0 + 512], ybg[ys][:], r=[("ybg", ys)], key=("ybg", ys), is_out=True)

    groups = [(b, G) for G in range(4) for b in range(NSEQ)]
    for b in range(NSEQ):
        load_seq(b)
    for gi_, (b, G) in enumerate(groups):
        mslot[(b, G)] = gi_ % 2
        for pr in range(2):
            score_pair(b, G, (2 * pr, 2 * pr + 1))
            if gi_ >= 1:
                pb, pG = groups[gi_ - 1]
                attn_chunk(pb, pG, 2 * pr)
                attn_chunk(pb, pG, 2 * pr + 1)
    pb, pG = groups[-1]
    for c in range(4):
        attn_chunk(pb, pG, c)
    P.close()


def phase_D(B, tag, yT, wname, resid, gname, hmid, hnT_name):
    nc, d, NSEQ = B.nc, B.t, B.NSEQ
    P = Phase(nc, tag)
    Wo = P.sb("Wo", [128, 8, D], BF16)
    P.dma("gpsimd", Wo[:], d[wname].rearrange("(k p) n -> p k n", p=128), w=["Wo"], key="Wo")
    identb = P.sb("identb", [128, 128], BF16)
    P.dma("sync", identb[:], d["ident_bf"], w=["identb"], key="identb")
    gbc = P.sb("gbc", [128, D], F32)
    P.dma("sync", gbc[:], d[gname], w=["gbc"], key="gbc")
    eps_t = P.sb("eps", [128, 1], F32)
    P.memset("vector", eps_t[:], EPS, ["eps"])
    yt = [P.sb("yt%d" % i, [128, 8, 512], BF16) for i in range(2)]
    xt = [P.sb("xt%d" % i, [128, D], F32) for i in range(2)]
    hm = [P.sb("hm%d" % i, [128, D], F32) for i in range(2)]
    junk = P.sb("junk", [128, D], F32)
    stat = [P.sb("stat%d" % i, [128, 4], F32) for i in range(2)]
    hn = [P.sb("hn%d" % i, [128, D], BF16) for i in range(2)]
    hnTg = [P.sb("hnTg%d" % i, [128, 8, 512], BF16) for i in range(2)]
    ps_o = [P.ps("pso%d" % i, [128, 1024]) for i in range(2)]
    ps_tr = [P.ps("ptr%d" % i, [128, 8, 128], BF16) for i in range(2)]
    ngr = NSEQ * 4

    def load_y(gi):
        b, g = gi // 4, gi % 4
        P.dma("sync", yt[gi % 2][:], d[yT][b, :, :, g * 512:(g + 1) * 512].rearrange("c p t -> p c t"), w=[("yt", gi % 2)], key=("yt", gi % 2))

    load_y(0)
    for gi in range(ngr):
        b, g = gi // 4, gi % 4
        gs = gi % 2
        if gi + 1 < ngr:
            load_y(gi + 1)
        for t4 in range(4):
            tt = g * 4 + t4
            r0 = b * SEQ + tt * 128
            tok = slice(t4 * 128, (t4 + 1) * 128)
            s2 = t4 % 2
            P.dma("sync", xt[s2][:], d[resid][r0:r0 + 128, :], w=[("x", s2)], key=("x", s2))
            for half in range(2):
                for c in range(8):
                    P.mm(ps_o[s2][:, half * 512:(half + 1) * 512], yt[gs][:, c, tok], Wo[:, c, half * 512:(half + 1) * 512],
                         c == 0, c == 7, [("yt", gs), "Wo"], [("pso", s2)], mark=(c == 7 and half == 1))
            for half in range(2):
                hs_ = slice(half * 512, (half + 1) * 512)
                P.tt("vector", hm[s2][:, hs_], ps_o[s2][:, hs_], xt[s2][:, hs_], ALU.add, [("pso", s2), ("x", s2)], [("hm", s2)])
            P.dma("sync", d[hmid][r0:r0 + 128, :], hm[s2][:], r=[("hm", s2)], key=("hm", s2), is_out=True)
            rms_to_hnT(P, hm[s2][:], ("hm", s2), gbc[:], eps_t, stat[s2], ("st", s2), junk, hn[s2][:], ("hn", s2),
                       ps_tr[s2], ("ptr", s2), identb, hnTg[gs], ("hnTg", gs), tok)
            if DEBUG_STOP == 77 and gi == 0:
                if t4 == 0:
                    dlog = P.sb("dlog", [128, 16], F32)
                    dhn = P.sb("dhn", [128, 4, D], BF16)
                P.copy("gpsimd", dlog[:, t4 * 4:(t4 + 1) * 4], stat[s2][:, 0:4], [("st", s2), ("hn", s2)], ["dlog"])
                P.copy("gpsimd", dhn[:, t4, :], hn[s2][:], [("hn", s2)], ["dhn"])
                if t4 == 3:
                    P.dma("sync", d["dbg"], dlog[:], r=["dlog"], key="dlog", is_out=True)
                    P.dma("sync", d["dbg2"].rearrange("t p c -> p t c"), dhn[:], r=["dhn"], key="dhn", is_out=True)
        P.dma("sync", d[hnT_name][:, :, gi * 512:(gi + 1) * 512].rearrange("c p t -> p c t"), hnTg[gs][:], r=[("hnTg", gs)],
              key=("hnTg", gs), is_out=True)
    P.close()


def phase_F(B, tag, layer, f0, h_in, h_out, hnT_name, final_g=None):
    nc, d, NSEQ = B.nc, B.t, B.NSEQ
    P = Phase(nc, tag)
    nf = 11
    Wg = P.sb("Wg", [128, 8, nf * 128], BF16)
    Wu = P.sb("Wu", [128, 8, nf * 128], BF16)
    Wd = P.sb("Wd", [128, nf, D], BF16)
    cs = slice(f0 * 128, (f0 + nf) * 128)
    P.dma("gpsimd", Wg[:], d["ffn_w_gate%d" % layer].rearrange("(k p) n -> p k n", p=128)[:, :, cs], w=["Wg"], key="Wg")
    P.dma("gpsimd", Wu[:], d["ffn_w_up%d" % layer].rearrange("(k p) n -> p k n", p=128)[:, :, cs], w=["Wu"], key="Wu")
    P.dma("gpsimd", Wd[:], d["ffn_w_down%d" % layer].rearrange("(f p) n -> p f n", p=128)[:, f0:f0 + nf, :], w=["Wd"], key="Wd")
    if final_g is not None:
        gbc = P.sb("gbc", [128, D], F32)
        P.dma("sync", gbc[:], d[final_g], w=["gbc"], key="gbc")
        eps_t = P.sb("eps", [128, 1], F32)
        P.memset("vector", eps_t[:], EPS, ["eps"])
        junk = P.sb("junk", [128, D], F32)
        stat = [P.sb("stat%d" % i, [128, 4], F32) for i in range(2)]
    hnTg = [P.sb("hnTg%d" % i, [128, 8, 512], BF16) for i in range(2)]
    actT = [P.sb("actT%d" % i, [128, nf, 512], BF16) for i in range(2)]
    sg = [P.sb("sg%d" % i, [128, 512], F32) for i in range(2)]
    hin = [P.sb("hin%d" % i, [128, D], F32) for i in range(2)]
    hout = [P.sb("hout%d" % i, [128, D], F32) for i in range(2)]
    ps_g = [P.ps("psg%d" % i, [128, 512]) for i in range(2)]
    ps_u = [P.ps("psu%d" % i, [128, 512]) for i in range(2)]
    ps_d = [P.ps("psd%d" % i, [128, 1024]) for i in range(2)]
    ngr = NSEQ * 4

    def load_h(gi):
        P.dma("sync", hnTg[gi % 2][:], d[hnT_name][:, :, gi * 512:(gi + 1) * 512].rearrange("c p t -> p c t"),
              w=[("hnTg", gi % 2)], key=("hnTg", gi % 2))

    load_h(0)
    for gi in range(ngr):
        gs = gi % 2
        if gi + 1 < ngr:
            load_h(gi + 1)
        for f in range(nf):
            s2 = f % 2
            for k in range(8):
                P.mm(ps_g[s2][:, :], Wg[:, k, f * 128:(f + 1) * 128], hnTg[gs][:, k, :], k == 0, k == 7, [("hnTg", gs), "Wg"], [("psg", s2)])
            for k in range(8):
                P.mm(ps_u[s2][:, :], Wu[:, k, f * 128:(f + 1) * 128], hnTg[gs][:, k, :], k == 0, k == 7, [("hnTg", gs), "Wu"], [("psu", s2)])
            P.act(sg[s2][:], ps_g[s2][:, :], AF.Silu, [("psg", s2)], [("sg", s2)])
            P.tt("vector", actT[gs][:, f, :], ps_u[s2][:, :], sg[s2][:], ALU.mult, [("psu", s2), ("sg", s2)], [("actT", gs)])
        for t4 in range(4):
            r0 = gi * 512 + t4 * 128
            tok = slice(t4 * 128, (t4 + 1) * 128)
            s2 = t4 % 2
            P.dma("sync", hin[s2][:], d[h_in][r0:r0 + 128, :], w=[("hin", s2)], key=("hin", s2))
            for half in range(2):
                for f in range(nf):
                    P.mm(ps_d[s2][:, half * 512:(half + 1) * 512], actT[gs][:, f, tok], Wd[:, f, half * 512:(half + 1) * 512],
                         f == 0, f == nf - 1, [("actT", gs), "Wd"], [("psd", s2)], mark=(f == nf - 1 and half == 1))
            for half in range(2):
                hs_ = slice(half * 512, (half + 1) * 512)
                P.tt("vector", hout[s2][:, hs_], ps_d[s2][:, hs_], hin[s2][:, hs_], ALU.add, [("psd", s2), ("hin", s2)], [("hout", s2)])
            if final_g is not None:
                sk = ("st", s2)
                P.act(junk[:], hout[s2][:], AF.Square, [("hout", s2)], ["junkA", sk], accum_out=stat[s2][:, 0:1])
                P.act(stat[s2][:, 1:2], stat[s2][:, 0:1], AF.Sqrt, [sk], [sk], scale=1.0 / D, bias=eps_t[:, 0:1])
                P.recip(stat[s2][:, 2:3], stat[s2][:, 1:2], [sk], [sk])
                P.stt(hout[s2][:], hout[s2][:], stat[s2][:, 2:3], gbc[:], ALU.mult, ALU.mult, [("hout", s2), sk, "gbc"], [("hout", s2)])
            P.dma("sync", d[h_out][r0:r0 + 128, :], hout[s2][:], r=[("hout", s2)], key=("hout", s2), is_out=True)
    P.close()


def phase_E(B):
    nc, d, NSEQ = B.nc, B.t, B.NSEQ
    P = Phase(nc, "E")
    Wc = P.sb("Wc", [128, 8, 3104], BF16)
    Wsrc = d["c_w_in"].rearrange("(k p) n -> p k n", p=128)
    WK = []
    P.memset("gpsimd", Wc[:, :, 3088:3104], 0.0, [("W", 2)])
    for i, (c0, c1) in enumerate(((0, 1024), (1024, 2048), (2048, 3088))):
        P.dma("gpsimd", Wc[:, :, c0:c1], Wsrc[:, :, c0:c1], w=[("W", i)], key=("W", i))
        WK.append(("W", i))
    identb = P.sb("identb", [128, 128], BF16)
    P.dma("sync", identb[:], d["ident_bf"], w=["identb"], key="identb")
    gbc = P.sb("gbc", [128, D], F32)
    P.dma("sync", gbc[:], d["g_mix1"], w=["gbc"], key="gbc")
    g2w = P.sb("g2w", [128, 512], F32)
    P.memset("vector", g2w[:], 0.0, ["g2w"])
    P.dma("sync", g2w[0:16, :], d["c_gate_w"], w=["g2w"], key="g2w")
    g2b = P.sb("g2b", [128, 512], F32)
    P.dma("sync", g2b[:], d["c_gate_b_bc"], w=["g2b"], key="g2b")
    eps_t = P.sb("eps", [128, 1], F32)
    P.memset("vector", eps_t[:], EPS, ["eps"])
    one_t = P.sb("one", [128, 1], F32)
    P.memset("vector", one_t[:], 1.0, ["one"])
    xt = [P.sb("xt%d" % i, [128, D], F32) for i in range(2)]
    junk = P.sb("junk", [128, D], F32)
    stat = [P.sb("stat%d" % i, [128, 4], F32) for i in range(2)]
    hn = [P.sb("hn%d" % i, [128, D], BF16) for i in range(2)]
    hnT = [P.sb("hnT%d" % i, [128, 8, 512], BF16) for i in range(2)]
    qkT = [P.sb("qkT%d" % i, [128, 8, 512], BF16) for i in range(2)]
    glrT = [P.sb("glrT%d" % i, [128, 512], F32) for i in range(2)]
    for i in range(2):
        P.memset("vector", glrT[i][:], 0.0, [("glrT", i)])
    kst = [P.sb("kst%d" % i, [128, 512], BF16) for i in range(2)]
    vst = [P.sb("vst%d" % i, [128, 1024], BF16) for i in range(2)]
    rst = [P.sb("rst%d" % i, [128, 1024], F32) for i in range(2)]
    lat = [P.sb("lat%d" % i, [128, 512], F32) for i in range(2)]
    lt1 = P.sb("lt1", [128, 512], F32)
    lt2 = P.sb("lt2", [128, 512], F32)
    ps_tr = P.ps("ptr", [128, 8, 128], BF16)
    ps_f = [P.ps("psf%d" % i, [128, 512]) for i in range(2)]
    ps_t = [P.ps("pst%d" % i, [128, 512]) for i in range(3)]
    ps_l = P.ps("psl", [128, 512])
    n_t = 0
    n_f = 0
    n_tiles = NSEQ * NT

    def load_x(ti):
        P.dma("sync", xt[ti % 2][:], d["h1"][ti * 128:(ti + 1) * 128, :], w=[("x", ti % 2)], key=("x", ti % 2))

    load_x(0)
    for gi in range(NSEQ * 4):
        b, g = gi // 4, gi % 4
        gs = gi % 2
        for t4 in range(4):
            ti = gi * 4 + t4
            if ti + 1 < n_tiles:
                load_x(ti + 1)
            xs = ti % 2
            rms_to_hnT(P, xt[xs][:], ("x", xs), gbc[:], eps_t, stat[xs], ("st", xs), junk, hn[xs][:], ("hn", xs),
                       ps_tr, "ptr", identb, hnT[gs], ("hnT", gs), slice(t4 * 128, (t4 + 1) * 128))
        for oc in range(8):
            r = n_f % 2
            n_f += 1
            for k in range(8):
                P.mm(ps_f[r][:, :], Wc[:, k, oc * 128:(oc + 1) * 128], hnT[gs][:, k, :], k == 0, k == 7, [("hnT", gs)] + WK, [("psf", r)])
            if oc < 4:
                P.act(qkT[gs][:, oc, :], ps_f[r][:, :], AF.Copy, [("psf", r)], [("qkT", gs)], scale=128 ** -0.5)
            else:
                P.copy("vector", qkT[gs][:, oc, :], ps_f[r][:, :], [("psf", r)], [("qkT", gs)])
        r = n_f % 2
        n_f += 1
        for k in range(8):
            P.mm(ps_f[r][0:32, :], Wc[:, k, 3072:3104], hnT[gs][:, k, :], k == 0, k == 7, [("hnT", gs)] + WK, [("psf", r)])
        P.copy("vector", glrT[gs][0:32, :], ps_f[r][0:32, :], [("psf", r)], [("glrT", gs)])
        gsl = slice(g * 512, (g + 1) * 512)
        P.dma("sync", d["c_qT"][b, :, :, gsl].rearrange("c p t -> p c t"), qkT[gs][:, 0:4, :], r=[("qkT", gs)], key=("qTo", gs), is_out=True)
        P.dma("sync", d["c_kT"][b, :, :, gsl].rearrange("c p t -> p c t"), qkT[gs][:, 4:8, :], r=[("qkT", gs)], key=("kTo", gs), is_out=True)
        for t4 in range(4):
            r0 = gi * 512 + t4 * 128
            tok = slice(t4 * 128, (t4 + 1) * 128)
            s2 = t4 % 2
            jobs = [(512, 1024, kst[s2][:, :], ("kst", s2), "k"),
                    (1024, 1536, vst[s2][:, 0:512], ("vst", s2), "v"), (1536, 2048, vst[s2][:, 512:1024], ("vst", s2), "v"),
                    (2048, 2560, rst[s2][:, 0:512], ("rst", s2), "r"), (2560, 3072, rst[s2][:, 512:1024], ("rst", s2), "r")]
            for (c0, c1, dst, dk, kind) in jobs:
                r = n_t % 3
                n_t += 1
                for k in range(8):
                    P.mm(ps_t[r][:, :], hnT[gs][:, k, tok], Wc[:, k, c0:c1], k == 0, k == 7, [("hnT", gs)] + WK, [("pst", r)])
                if kind == "r":
                    P.act(dst, ps_t[r][:, :], AF.Silu, [("pst", r)], [dk])
                elif kind == "v":
                    P.copy("vector", dst, ps_t[r][:, :], [("pst", r)], [dk])
                else:
                    P.copy("scalar", dst, ps_t[r][:, :], [("pst", r)], [dk])
            P.dma("sync", d["c_ktok"][r0:r0 + 128, :], kst[s2][:], r=[("kst", s2)], key=("kst", s2), is_out=True)
            P.dma("sync", d["c_v"][r0:r0 + 128, :], vst[s2][:], r=[("vst", s2)], key=("vst", s2), is_out=True)
            P.dma("sync", d["c_sr"][r0:r0 + 128, :], rst[s2][:], r=[("rst", s2)], key=("rst", s2), is_out=True)
            P.mm(ps_l[:, :], glrT[gs][:, tok], g2w[:, :], True, True, [("glrT", gs), "g2w"], ["psl"])
            P.tt("vector", lt1[:], ps_l[:, :], g2b[:], ALU.add, ["psl", "g2b"], ["lt1"])
            P.act(lt2[:], lt1[:], AF.Exp, ["lt1"], ["lt2"], scale=-1.0)
            P.act(lt1[:], lt2[:], AF.Ln, ["lt2", "one"], ["lt1"], bias=one_t[:, 0:1], scale=1.0)
            P.ts("gpsimd", lat[s2][:], lt1[:], -1.0 / 16.0, ALU.mult, ["lt1"], [("lat", s2)])
            P.dma("sync", d["c_la"][r0:r0 + 128, :], lat[s2][:], r=[("lat", s2)], key=("lat", s2), is_out=True)
    P.close()


def phase_G(B):
    nc, d, NSEQ = B.nc, B.t, B.NSEQ
    P = Phase(nc, "G")
    identb = P.sb("identb", [128, 128], BF16)
    P.dma("sync", identb[:], d["ident_bf"], w=["identb"], key="identb")
    U = P.sb("U", [128, 128], F32)
    SL = P.sb("SL", [128, 128], F32)
    P.dma("sync", U[:], d["tri_u"], w=["U"], key="U")
    P.dma("sync", SL[:], d["tri_sl"], w=["SL"], key="SL")
    gon = P.sb("gon", [128, D], F32)
    P.dma("sync", gon[:], d["c_onorm_g_bc"], w=["gon"], key="gon")
    eps_t = P.sb("eps", [128, 1], F32)
    P.memset("vector", eps_t[:], EPS, ["eps"])
    qT = P.sb("qT", [128, 4, SEQ], BF16)
    kT = P.sb("kT", [128, 4, SEQ], BF16)
    la = [P.sb("la%d" % i, [128, 512], F32) for i in range(2)]
    kt_ = [P.sb("ktk%d" % i, [128, 512], BF16) for i in range(2)]
    vt = [P.sb("vt%d" % i, [128, 1024], BF16) for i in range(2)]
    sr = [P.sb("sr%d" % i, [128, 1024], F32) for i in range(2)]
    eb = [P.sb("eb%d" % i, [128, 512], F32) for i in range(2)]
    enb = P.sb("enb", [128, 512], F32)
    erv = P.sb("erv", [128, 512], F32)
    qtT = [P.sb("qtT%d" % i, [128, 512], BF16) for i in range(2)]
    ktT = [P.sb("ktT%d" % i, [128, 512], BF16) for i in range(2)]
    ks = [P.sb("ks%d" % i, [128, 512], BF16) for i in range(2)]
    attm = [P.sb("attm%d" % i, [128, 128], BF16) for i in range(2)]
    state = P.sb("state", [128, 4, 256], F32)
    stb = P.sb("stb", [128, 4, 256], BF16)
    gr = [P.sb("gr%d" % i, [128, 1024], F32) for i in range(2)]
    stat = [P.sb("stat%d" % i, [128, 12], F32) for i in range(2)]
    junk = P.sb("junk", [128, 256], F32)
    yc = [P.sb("yc%d" % i, [128, 1024], BF16) for i in range(2)]
    ycT = [P.sb("ycT%d" % i, [128, 8, 512], BF16) for i in range(2)]
    ps_b = P.ps("psb", [128, 512])
    ps_r = P.ps("psr", [128, 512])
    ps_a = P.ps("psa", [128, 4, 128])
    ps_o = P.ps("pso", [128, 1024])
    ps_kv = P.ps("pskv", [128, 1024])
    ps_tr = P.ps("ptr", [128, 8, 128], BF16)
    nch = NSEQ * NT

    def load_chunk(ci):
        s2 = ci % 2
        r0 = ci * 128
        P.dma("sync", la[s2][:], d["c_la"][r0:r0 + 128, :], w=[("la", s2)], key=("la", s2))
        P.dma("sync", kt_[s2][:], d["c_ktok"][r0:r0 + 128, :], w=[("ktk", s2)], key=("ktk", s2))
        P.dma("sync", vt[s2][:], d["c_v"][r0:r0 + 128, :], w=[("vt", s2)], key=("vt", s2))
        P.dma("sync", sr[s2][:], d["c_sr"][r0:r0 + 128, :], w=[("sr", s2)], key=("sr", s2))

    load_chunk(0)
    for ci in range(nch):
        b, c = ci // NT, ci % NT
        s2 = ci % 2
        if c == 0:
            P.dma("sync", qT[:], d["c_qT"][b].rearrange("c p t -> p c t"), w=["qT"], key="qT")
            P.dma("sync", kT[:], d["c_kT"][b].rearrange("c p t -> p c t"), w=["kT"], key="kT")
            P.memset("vector", state[:], 0.0, [("state", h) for h in range(4)])
            P.memset("gpsimd", stb[:], 0.0, [("stb", h) for h in range(4)])
        if ci + 1 < nch:
            load_chunk(ci + 1)
        csl = slice(c * 128, (c + 1) * 128)
        for h in range(4):
            P.mm(ps_b[:, h * 128:(h + 1) * 128], la[s2][:, h * 128:(h + 1) * 128], U[:], True, True, [("la", s2), "U"], ["psb"], mark=(h == 3))
        P.mm(ps_r[:, :], SL[:], la[s2][:, :], True, True, [("la", s2), "SL"], ["psr"])
        P.act(eb[s2][:], ps_b[:, :], AF.Exp, ["psb"], [("eb", s2)])
        P.act(enb[:], ps_b[:, :], AF.Exp, ["psb"], ["enb"], scale=-1.0)
        P.act(erv[:], ps_r[:, :], AF.Exp, ["psr"], ["erv"])
        qv = qT[:, :, csl]
        kv_ = kT[:, :, csl]
        P.tt("vector", qtT[s2][:].rearrange("p (h t) -> p h t", t=128), qv, eb[s2][:].rearrange("p (h t) -> p h t", t=128), ALU.mult,
             ["qT", ("eb", s2)], [("qtT", s2)])
        P.tt("gpsimd", ktT[s2][:].rearrange("p (h t) -> p h t", t=128), kv_, enb[:].rearrange("p (h t) -> p h t", t=128), ALU.mult,
             ["kT", "enb"], [("ktT", s2)])
        P.tt("gpsimd", ks[s2][:], kt_[s2][:], erv[:], ALU.mult, [("ktk", s2), "erv"], [("ks", s2)])
        P.tt("gpsimd", gr[s2][:], sr[s2][:], gon[:], ALU.mult, [("sr", s2), "gon"], [("gr", s2)])
        for h in range(4):
            hs = slice(h * 128, (h + 1) * 128)
            vs = slice(h * 256, (h + 1) * 256)
            a2 = h % 2
            P.mm(ps_a[:, h, :], ktT[s2][:, hs], qtT[s2][:, hs], True, True, [("ktT", s2), ("qtT", s2)], [("psa", h)])
            P.tt("vector", attm[a2][:], ps_a[:, h, :], U[:], ALU.mult, [("psa", h), "U"], [("attm", a2)])
            P.mm(ps_o[:, vs], attm[a2][:], vt[s2][:, vs], True, False, [("attm", a2), ("vt", s2)], [("pso", h)], mark=False)
            P.mm(ps_o[:, vs], qtT[s2][:, hs], stb[:, h, :], False, True, [("qtT", s2), ("stb", h)], [("pso", h)])
            P.mm(ps_kv[:, vs], ks[s2][:, hs], vt[s2][:, vs], True, True, [("ks", s2), ("vt", s2)], [("pskv", h)])
            P.stt(state[:, h, :], state[:, h, :], eb[s2][:, h * 128 + 127:h * 128 + 128], ps_kv[:, vs], ALU.mult, ALU.add,
                  [("state", h), ("eb", s2), ("pskv", h)], [("state", h)])
            P.copy("scalar", stb[:, h, :], state[:, h, :], [("state", h)], [("stb", h)])
        sk = ("st", s2)
        for h in range(4):
            vs = slice(h * 256, (h + 1) * 256)
            P.act(junk[:], ps_o[:, vs], AF.Square, [("pso", h)], ["junkA", sk], accum_out=stat[s2][:, h:h + 1])
        P.act(stat[s2][:, 4:8], stat[s2][:, 0:4], AF.Sqrt, [sk], [sk], scale=1.0 / 256, bias=eps_t[:, 0:1])
        P.recip(stat[s2][:, 8:12], stat[s2][:, 4:8], [sk], [sk])
        for h in range(4):
            vs = slice(h * 256, (h + 1) * 256)
            P.stt(yc[s2][:, vs], ps_o[:, vs], stat[s2][:, 8 + h:9 + h], gr[s2][:, vs], ALU.mult, ALU.mult,
                  [("pso", h), sk, ("gr", s2)], [("yc", s2)])
        for k in range(8):
            P.tr(ps_tr[:, k, :], yc[s2][:, k * 128:(k + 1) * 128], identb[:], [("yc", s2), "identb"], ["ptr"], mark=(k == 7))
        g4 = (ci // 4) % 2
        P.copy("scalar", ycT[g4][:, 0:8, (ci % 4) * 128:(ci % 4 + 1) * 128], ps_tr[:, 0:8, :], ["ptr"], [("ycT", g4)])
        if ci % 4 == 3:
            g = c // 4
            P.dma("sync", d["ycT"][b, :, :, g * 512:(g + 1) * 512].rearrange("c p t -> p c t"), ycT[g4][:], r=[("ycT", g4)],
                  key=("ycT", g4), is_out=True)
    P.close()


def declare(B):
    NSEQ = B.NSEQ
    T = NSEQ * SEQ
    X = "ExternalInput"
    B.dram("x", [T, D], F32, X)
    B.dram("ab_w_in", [D, 2248], F32, X)
    B.dram("ab_conv_w", [31, 512], F32, X)
    B.dram("conv_prm", [128, 12], F32, X)
    B.dram("ab_w_out", [D, D], F32, X)
    B.dram("c_w_in", [D, 3088], F32, X)
    B.dram("c_gate_w", [16, 512], F32, X)
    B.dram("c_gate_b_bc", [128, 512], F32, X)
    B.dram("c_onorm_g_bc", [128, D], F32, X)
    B.dram("c_w_out", [D, D], F32, X)
    for l in range(2):
        B.dram("ffn_w_gate%d" % l, [D, DFF], F32, X)
        B.dram("ffn_w_up%d" % l, [D, DFF], F32, X)
        B.dram("ffn_w_down%d" % l, [DFF, D], F32, X)
        B.dram("g_mix%d" % l, [128, D], F32, X)
        B.dram("g_ffn%d" % l, [128, D], F32, X)
    B.dram("g_final", [128, D], F32, X)
    B.dram("ident_bf", [128, 128], BF16, X)
    B.dram("ident_f32", [128, 128], F32, X)
    B.dram("cos128", [SEQ, 128], F32, X)
    B.dram("sin128", [SEQ, 128], F32, X)
    B.dram("negmask", [128, 128], F32, X)
    B.dram("pow2", [128, NIT], F32, X)
    B.dram("tri_u", [128, 128], F32, X)
    B.dram("tri_sl", [128, 128], F32, X)
    B.dram("hconvT", [NSEQ, 4, 128, SEQ], BF16)
    B.dram("qT", [NSEQ, 4, 128, SEQ], BF16)
    B.dram("iqT", [NSEQ, 4, 128, SEQ], BF16)
    B.dram("kkT", [NSEQ, 2, 128, SEQ], BF16)
    B.dram("vaug", [T, 256], BF16)
    B.dram("iw", [T, 8], F32)
    B.dram("yabT", [NSEQ, 8, 128, SEQ], BF16)
    B.dram("h_mid0", [T, D], F32)
    B.dram("hnT", [8, 128, T], BF16)
    B.dram("h_half", [T, D], F32)
    B.dram("h1", [T, D], F32)
    B.dram("c_qT", [NSEQ, 4, 128, SEQ], BF16)
    B.dram("c_kT", [NSEQ, 4, 128, SEQ], BF16)
    B.dram("c_ktok", [T, 512], BF16)
    B.dram("c_v", [T, D], BF16)
    B.dram("c_sr", [T, D], F32)
    B.dram("c_la", [T, 512], F32)
    B.dram("ycT", [NSEQ, 8, 128, SEQ], BF16)
    B.dram("h_mid1", [T, D], F32)
    B.dram("out", [T, D], F32, "ExternalOutput")
    if DEBUG_STOP == 77:
        B.dram("dbg", [128, 16], F32, "ExternalOutput")
        B.dram("dbg2", [4, 128, D], BF16, "ExternalOutput")


PHASES = {
    "A": phase_A,
    "B": phase_B,
    "C": phase_C,
    "D0": lambda B: phase_D(B, "D0", "yabT", "ab_w_out", "x", "g_ffn0", "h_mid0", "hnT"),
    "F0a": lambda B: phase_F(B, "F0a", 0, 0, "h_mid0", "h_half", "hnT"),
    "F0b": lambda B: phase_F(B, "F0b", 0, 11, "h_half", "h1", "hnT"),
    "E": phase_E,
    "G": phase_G,
    "D1": lambda B: phase_D(B, "D1", "ycT", "c_w_out", "h1", "g_ffn1", "h_mid1", "hnT"),
    "F1a": lambda B: phase_F(B, "F1a", 1, 0, "h_mid1", "h_half", "hnT"),
    "F1b": lambda B: phase_F(B, "F1b", 1, 11, "h_half", "out", "hnT", final_g="g_final"),
}
ORDER = ["A", "B", "C", "D0", "F0a", "F0b", "E", "G", "D1", "F1a", "F1b"]


def host_consts():
    bf = ml_dtypes.bfloat16
    c = {}
    c["ident_bf"] = np.eye(128, dtype=np.float32).astype(bf)
    c["ident_f32"] = np.eye(128, dtype=np.float32)
    rot = 16
    inv = (500000.0 ** (-np.arange(0, rot, 2, dtype=np.float32) / np.float32(rot))).astype(np.float32)
    ang = (np.arange(SEQ, dtype=np.float32)[:, None] * inv[None, :]).astype(np.float32)
    c["cos128"] = np.ascontiguousarray(np.tile(np.cos(ang).astype(np.float32), (1, 16)))
    c["sin128"] = np.ascontiguousarray(np.tile(np.sin(ang).astype(np.float32), (1, 16)))
    i = np.arange(128)
    c["negmask"] = np.where(i[None, :] <= i[:, None], 0.0, -1e30).astype(np.float32)
    c["pow2"] = np.ascontiguousarray(np.broadcast_to((0.5 ** np.arange(1, NIT + 1)).astype(np.float32), (128, NIT)))
    c["tri_u"] = (i[:, None] <= i[None, :]).astype(np.float32)
    c["tri_sl"] = (i[:, None] > i[None, :]).astype(np.float32)
    return c


def host_weights(inp):
    f = lambda a: np.ascontiguousarray(np.asarray(a, dtype=np.float32))
    bc = lambda v, n: np.ascontiguousarray(np.broadcast_to(np.asarray(v, np.float32).reshape(1, -1), (128, n)))
    w = {}
    w["ab_w_in"] = f(inp["ab_w_in"][0])
    w["ab_conv_w"] = f(inp["ab_conv_w"][0].reshape(31, 512))
    prm = np.concatenate([np.asarray(inp[k][0], np.float32).reshape(4, 128).T for k in ("ab_conv_b", "ab_ln_g", "ab_ln_b")], axis=1)
    w["conv_prm"] = f(prm)
    w["ab_w_out"] = f(inp["ab_w_out"][0])
    w["c_w_in"] = f(inp["c_w_in"][0])
    w["c_gate_w"] = f(inp["c_gate_w"][0])
    w["c_gate_b_bc"] = bc(inp["c_gate_b"][0], 512)
    w["c_onorm_g_bc"] = bc(inp["c_onorm_g"][0], D)
    w["c_w_out"] = f(inp["c_w_out"][0])
    for l in range(2):
        w["ffn_w_gate%d" % l] = f(inp["ffn_w_gate"][l])
        w["ffn_w_up%d" % l] = f(inp["ffn_w_up"][l])
        w["ffn_w_down%d" % l] = f(inp["ffn_w_down"][l])
        w["g_mix%d" % l] = bc(inp["norm_mix_g"][l], D)
        w["g_ffn%d" % l] = bc(inp["norm_ffn_g"][l], D)
    w["g_final"] = bc(inp["final_norm_g"], D)
    return w


def build_program(nseq, phases, ext=None):
    B = Build(nseq, ext)
    declare(B)
    for p in phases:
        PHASES[p](B)
    return B


def kernel(**inp):
    n = 8
    nseq = 2
    x = np.asarray(inp["x"], dtype=np.float32)
    B = build_program(nseq, ORDER)
    shared = host_consts()
    shared.update(host_weights(inp))
    in_maps = []
    for c in range(n):
        m = dict(shared)
        m["x"] = np.ascontiguousarray(x[c * nseq:(c + 1) * nseq].reshape(nseq * SEQ, D))
        in_maps.append(m)
    res = run_bass_kernel_spmd(B.nc, in_maps, core_ids=list(range(n)))
    out = np.stack([np.asarray(r["out"], dtype=np.float32).reshape(nseq, SEQ, D) for r in res.results], axis=0)
    return out.reshape(16, SEQ, D)
```

```python
import contextlib
import math
import numpy as np
import ml_dtypes
import concourse.bass as bass
import concourse.mybir as mybir
from concourse.bass_utils import run_bass_kernel_spmd

F32 = mybir.dt.float32
BF16 = mybir.dt.bfloat16
ALU = mybir.AluOpType
AF = mybir.ActivationFunctionType
AX = mybir.AxisListType

ENGS = ["sync", "scalar", "vector", "gpsimd", "tensor"]
D = 1024
SEQ = 2048
NT = 16
EPS = 1e-6
DFF = 2816
NFF = 22
NIT = 16
TOPK = 256
DEBUG_STOP = 99
SAME_SYNC = True


class Sched:
    def __init__(self, nc, stack, tag=""):
        self.nc = nc
        self.stack = stack
        self.tag = tag
        self.q = {e: [] for e in ENGS}
        self.sem = {}
        self.cnt = {}
        self.waited = {e: {} for e in ENGS}
        self.lastw = {}
        self.readers = {}
        self.pe_pending = False
        self.out_deps = {}
        for e in ENGS:
            self._mksem("E_" + e)

    def _mksem(self, name):
        if name not in self.sem:
            hw_name = "%s_s%d" % (self.tag, len(self.sem))
            self.sem[name] = self.nc.alloc_semaphore(name=hw_name)
            self.cnt[name] = 0
        return name

    def op(self, eng, fn, reads=(), writes=(), dma=None, mark=True, is_out=False, force_self=False):
        deps = {}

        def add(d):
            if d[1] > deps.get(d[0], 0):
                deps[d[0]] = d[1]

        for k in reads:
            if k in self.lastw:
                add(self.lastw[k])
        for k in writes:
            if k in self.lastw:
                add(self.lastw[k])
            for s_, v_ in self.readers.get(k, {}).items():
                add((s_, v_))
        if dma is not None:
            s = self._mksem("D_" + str(dma))
            if self.cnt[s] > 0:
                add((s, self.cnt[s]))
        own = "E_" + eng
        need = []
        for s_, v_ in deps.items():
            if s_ == own and (eng == "tensor" or not (SAME_SYNC or force_self)):
                continue
            if self.waited[eng].get(s_, 0) >= v_:
                continue
            need.append((s_, v_))
        attach = None
        if eng == "tensor":
            k0 = reads[0] if len(reads) else None
            if k0 is not None and k0 in self.lastw and self.lastw[k0][0] != own:
                attach = self.lastw[k0]
        else:
            selfs = [d_ for d_ in need if d_[0] == own]
            attach = selfs[0] if selfs else (need[-1] if need else None)
        standalone = [d_ for d_ in need if attach is None or d_[0] != attach[0]]
        if attach is not None:
            for d_ in need:
                if d_[0] == attach[0] and d_[1] > attach[1]:
                    attach = d_
        for d_ in need:
            self.waited[eng][d_[0]] = max(self.waited[eng].get(d_[0], 0), d_[1])
        if attach is not None:
            self.waited[eng][attach[0]] = max(self.waited[eng].get(attach[0], 0), attach[1])
        if dma is not None:
            self.cnt[s] += 16
            dep = (s, self.cnt[s])
            self.q[eng].append(("op", fn, s, 16, standalone, attach))
        else:
            s = own
            if mark:
                self.cnt[s] += 1
                dep = (s, self.cnt[s])
                self.q[eng].append(("op", fn, s, 1, standalone, attach))
                if eng == "tensor":
                    self.pe_pending = False
            else:
                assert eng == "tensor"
                dep = (s, self.cnt[s] + 1)
                self.q[eng].append(("op", fn, None, 0, standalone, attach))
                self.pe_pending = True
        for k in writes:
            self.lastw[k] = dep
            self.readers[k] = {}
        for k in reads:
            r = self.readers.setdefault(k, {})
            if dep[1] > r.get(dep[0], 0):
                r[dep[0]] = dep[1]
        if is_out:
            if dep[1] > self.out_deps.get(dep[0], 0):
                self.out_deps[dep[0]] = dep[1]
        return dep

    def finalize(self):
        assert not self.pe_pending, "unmarked PE op at end of phase"
        tail = []
        for s_, v_ in self.out_deps.items():
            if self.waited["sync"].get(s_, 0) < v_:
                tail.append((s_, v_))
        nc = self.nc
        with nc.Block() as block:
            for eng in ENGS:
                items = self.q[eng]

                def body(e, items=items, eng=eng):
                    for it in items:
                        for (ws, wv) in it[4]:
                            e.wait_ge(self.sem[ws], wv)
                        ins = it[1](e)
                        if it[5] is not None:
                            ins._wait_ge(self.sem[it[5][0]], it[5][1])
                        if it[2] is not None:
                            ins.then_inc(self.sem[it[2]], it[3])
                    if eng == "sync":
                        for (ws, wv) in tail:
                            e.wait_ge(self.sem[ws], wv)

                getattr(block, eng)(body)


class Phase:
    def __init__(self, nc, tag):
        self.nc = nc
        self.tag = tag
        self.st = contextlib.ExitStack()
        self.st.enter_context(nc.cleanup_on_exit())
        self.S = Sched(nc, self.st, tag)

    def sb(self, name, shape, dt):
        return self.st.enter_context(self.nc.sbuf_tensor(self.tag + "_" + name, shape, dt))

    def ps(self, name, shape, dt=F32):
        return self.st.enter_context(self.nc.psum_tensor(self.tag + "_" + name, shape, dt))

    def dma(self, eng, out, in_, r=(), w=(), key=None, is_out=False):
        return self.S.op(eng, lambda e: e.dma_start(out=out, in_=in_), r, w, dma=key, is_out=is_out)

    def mm(self, out, lhsT, rhs, start, stop, r, w, mark=None):
        if mark is None:
            mark = stop
        return self.S.op("tensor", lambda e: e.matmul(out, lhsT=lhsT, rhs=rhs, start=start, stop=stop), r, w, mark=mark)

    def tr(self, out, in_, ident, r, w, mark=True):
        return self.S.op("tensor", lambda e: e.transpose(out=out, in_=in_, identity=ident), r, w, mark=mark)

    def act(self, out, in_, func, r, w, **kw):
        return self.S.op("scalar", lambda e: e.activation(out=out, in_=in_, func=func, **kw), r, w)

    def tt(self, eng, out, in0, in1, op, r, w):
        return self.S.op(eng, lambda e: e.tensor_tensor(out=out, in0=in0, in1=in1, op=op), r, w)

    def ts(self, eng, out, in0, s1, op0, r, w, s2=None, op1=None, accum_out=None):
        if op1 is None:
            return self.S.op(eng, lambda e: e.tensor_scalar(out=out, in0=in0, scalar1=s1, scalar2=None, op0=op0), r, w)
        return self.S.op(eng, lambda e: e.tensor_scalar(out=out, in0=in0, scalar1=s1, scalar2=s2, op0=op0, op1=op1,
                                                       accum_out=accum_out), r, w)

    def stt(self, out, in0, scalar, in1, op0, op1, r, w):
        return self.S.op("vector", lambda e: e.scalar_tensor_tensor(out=out, in0=in0, scalar=scalar, in1=in1,
                                                                     op0=op0, op1=op1), r, w)

    def copy(self, eng, out, in_, r, w):
        if eng == "scalar":
            return self.S.op(eng, lambda e: e.copy(out=out, in_=in_), r, w)
        return self.S.op(eng, lambda e: e.tensor_copy(out=out, in_=in_), r, w)

    def recip(self, out, in_, r, w):
        return self.S.op("vector", lambda e: e.reciprocal(out=out, in_=in_), r, w)

    def memset(self, eng, ap, val, w):
        return self.S.op(eng, lambda e: e.memset(ap, val), (), w)

    def reduce(self, out, in_, op, r, w):
        return self.S.op("vector", lambda e: e.tensor_reduce(out=out, in_=in_, axis=AX.X, op=op), r, w)

    def close(self):
        self.S.finalize()
        self.st.close()


class Build:
    def __init__(self, nseq, ext=None):
        self.nc = bass.Bass("TRN2", target_bir_lowering=False)
        self.NSEQ = nseq
        self.t = {}
        self.ext = ext or {}
        self.kinds = {}

    def dram(self, name, shape, dt, kind="Internal"):
        kind = self.ext.get(name, kind)
        self.kinds[name] = (kind, list(shape), dt)
        self.t[name] = self.nc.dram_tensor(name, list(shape), dt, kind=kind).ap()
        return self.t[name]


def rms_to_hnT(P, xt, xkey, gbc, eps_t, stat, skey, junk, hn, hkey, ps_tr, pkey, identb, dst, dkey, sl):
    P.act(junk[:], xt, AF.Square, [xkey], ["junkA", skey], accum_out=stat[:, 0:1])
    P.act(stat[:, 1:2], stat[:, 0:1], AF.Sqrt, [skey, "eps"], [skey], scale=1.0 / D, bias=eps_t[:, 0:1])
    P.recip(stat[:, 2:3], stat[:, 1:2], [skey], [skey])
    P.stt(hn, xt, stat[:, 2:3], gbc, ALU.mult, ALU.mult, [xkey, skey, "gbc"], [hkey])
    for k in range(8):
        P.tr(ps_tr[:, k, :], hn[:, k * 128:(k + 1) * 128], identb[:], [hkey, "identb"], [pkey], mark=(k == 7))
    P.copy("scalar", dst[:, 0:8, sl], ps_tr[:, 0:8, :], [pkey], [dkey])


def phase_A(B):
    nc, d, NSEQ = B.nc, B.t, B.NSEQ
    P = Phase(nc, "A")
    Wt = P.sb("Wt", [128, 8, 2376], BF16)
    Wsrc = d["ab_w_in"].rearrange("(k p) n -> p k n", p=128)
    segs = [(0, 1024, 0), (1024, 1536, 1024), (1664, 2176, 1536), (1536, 1600, 2048), (1536, 1600, 2112),
            (2176, 2240, 2176), (2176, 2240, 2240), (1600, 1664, 2304), (2240, 2248, 2368)]
    WK = []
    for i, (c0, c1, d0) in enumerate(segs):
        P.dma("gpsimd", Wt[:, :, d0:d0 + c1 - c0], Wsrc[:, :, c0:c1], w=[("W", i)], key=("W", i))
        WK.append(("W", i))
    identb = P.sb("identb", [128, 128], BF16)
    P.dma("sync", identb[:], d["ident_bf"], w=["identb"], key="identb")
    gbc = P.sb("gbc", [128, D], F32)
    P.dma("sync", gbc[:], d["g_mix0"], w=["gbc"], key="gbc")
    cos = P.sb("cos", [128, NT, 128], F32)
    sin = P.sb("sin", [128, NT, 128], F32)
    P.dma("sync", cos[:], d["cos128"].rearrange("(t p) c -> p t c", p=128), w=["cos"], key="cos")
    P.dma("sync", sin[:], d["sin128"].rearrange("(t p) c -> p t c", p=128), w=["sin"], key="sin")
    eps_t = P.sb("eps", [128, 1], F32)
    P.memset("vector", eps_t[:], EPS, ["eps"])

    xt = [P.sb("xt%d" % i, [128, D], F32) for i in range(2)]
    junk = P.sb("junk", [128, D], F32)
    stat = [P.sb("stat%d" % i, [128, 4], F32) for i in range(2)]
    hn = [P.sb("hn%d" % i, [128, D], BF16) for i in range(2)]
    hnT = [P.sb("hnT%d" % i, [128, 8, 512], BF16) for i in range(2)]
    ps_tr = [P.ps("ptr%d" % i, [128, 8, 128], BF16) for i in range(2)]
    ps_a = P.ps("psa", [128, 512])
    ps_g = P.ps("psg", [128, 512])
    ps_q = P.ps("psq", [128, 1024])
    ps_c = P.ps("psc", [128, 512])
    sig = [P.sb("sig%d" % i, [128, 512], F32) for i in range(2)]
    hc = [P.sb("hc%d" % i, [128, 512], BF16) for i in range(2)]
    tmp = [P.sb("tmp%d" % i, [128, 128], F32) for i in range(4)]
    qr = [P.sb("qr%d" % i, [128, 1024], BF16) for i in range(2)]
    kr = [P.sb("kr%d" % i, [128, 256], BF16) for i in range(2)]
    vaug = [P.sb("vaug%d" % i, [128, 256], BF16) for i in range(2)]
    iwt = [P.sb("iw%d" % i, [128, 8], F32) for i in range(2)]
    for i in range(2):
        P.memset("gpsimd", vaug[i][:, 64:192], 1.0, [("vaug", i)])
    qTg = [P.sb("qTg%d" % i, [128, 8, 512], BF16) for i in range(2)]
    kTg = [P.sb("kTg%d" % i, [128, 2, 512], BF16) for i in range(2)]
    w_scale = (8 ** -0.5) * (64 ** -0.5)

    def load_x(b, tt):
        xs = tt % 2
        r0 = b * SEQ + tt * 128
        P.dma("sync", xt[xs][:], d["x"][r0:r0 + 128, :], w=[("x", xs)], key=("x", xs))

    n_tiles = NSEQ * NT
    if DEBUG_STOP <= 1:
        P.dma("sync", d["iw"][0:128, :], iwt[0][:], r=[("iw", 0), "cos", "sin", "gbc", "identb"] + WK, key=("iw", 0), is_out=True)
        P.close()
        return
    load_x(0, 0)

    def norm_group(gi):
        b, g = gi // 4, gi % 4
        gs = gi % 2
        for t4 in range(4):
            tt = g * 4 + t4
            ti = gi * 4 + t4
            if ti + 1 < n_tiles:
                load_x((ti + 1) // NT, (ti + 1) % NT)
            xs = tt % 2
            rms_to_hnT(P, xt[xs][:], ("x", xs), gbc[:], eps_t, stat[xs], ("st", xs), junk, hn[xs][:], ("hn", xs),
                       ps_tr[0], "ptr0", identb, hnT[gs], ("hnT", gs), slice(t4 * 128, (t4 + 1) * 128))

    def mm_group(gi):
        b, g = gi // 4, gi % 4
        gs = gi % 2
        for c in range(4):
            for k in range(8):
                P.mm(ps_a[:, :], Wt[:, k, c * 128:(c + 1) * 128], hnT[gs][:, k, :], k == 0, k == 7,
                     [("hnT", gs)] + WK, ["psa"])
            for k in range(8):
                P.mm(ps_g[:, :], Wt[:, k, 512 + c * 128:512 + (c + 1) * 128], hnT[gs][:, k, :], k == 0, k == 7,
                     [("hnT", gs)] + WK, ["psg"])
            s2 = c % 2
            P.act(sig[s2][:], ps_g[:, :], AF.Sigmoid, ["psg"], [("sig", s2)])
            P.tt("vector", hc[s2][:], ps_a[:, :], sig[s2][:], ALU.mult, ["psa", ("sig", s2)], [("hc", s2)])
            P.dma("sync", d["hconvT"][b, c, :, g * 512:(g + 1) * 512], hc[s2][:], r=[("hc", s2)], key=("hc", s2), is_out=True)
        for t4 in range(4):
            tt = g * 4 + t4
            r0 = b * SEQ + tt * 128
            tok = slice(t4 * 128, (t4 + 1) * 128)
            s2 = t4 % 2
            for k in range(8):
                P.mm(ps_q[:, 0:512], hnT[gs][:, k, tok], Wt[:, k, 1024:1536], k == 0, k == 7, [("hnT", gs)] + WK, ["psq"], mark=False)
            for k in range(8):
                P.mm(ps_q[:, 512:1024], hnT[gs][:, k, tok], Wt[:, k, 1536:2048], k == 0, k == 7, [("hnT", gs)] + WK, ["psq"], mark=False)
            for k in range(8):
                P.mm(ps_c[:, 0:328], hnT[gs][:, k, tok], Wt[:, k, 2048:2376], k == 0, k == 7, [("hnT", gs)] + WK, ["psq"])
            for (src, nh, dst, dkey) in ((ps_q[:, 0:512], 8, qr[s2][:, 0:512], ("qr", s2)), (ps_q[:, 512:1024], 8, qr[s2][:, 512:1024], ("qr", s2)),
                                         (ps_c[:, 0:256], 4, kr[s2][:, 0:256], ("kr", s2))):
                sv = src.rearrange("p (h e) -> p h e", e=64)
                dv = dst.rearrange("p (h e) -> p h e", e=64)
                x1, x2 = sv[:, :, 0:8], sv[:, :, 8:16]
                cv = cos[:, tt, 0:nh * 8].rearrange("p (h e) -> p h e", e=8)
                sn = sin[:, tt, 0:nh * 8].rearrange("p (h e) -> p h e", e=8)
                tv = [tmp[i][:, 0:nh * 8].rearrange("p (h e) -> p h e", e=8) for i in range(4)]
                P.tt("vector", tv[0], x1, cv, ALU.mult, ["psq", "cos"], ["tmp0"])
                P.tt("vector", tv[1], x2, sn, ALU.mult, ["psq", "sin"], ["tmp1"])
                P.tt("vector", dv[:, :, 0:8], tv[0], tv[1], ALU.subtract, ["tmp0", "tmp1"], [dkey])
                P.tt("vector", tv[2], x2, cv, ALU.mult, ["psq", "cos"], ["tmp2"])
                P.tt("vector", tv[3], x1, sn, ALU.mult, ["psq", "sin"], ["tmp3"])
                P.tt("vector", dv[:, :, 8:16], tv[2], tv[3], ALU.add, ["tmp2", "tmp3"], [dkey])
                P.copy("scalar", dv[:, :, 16:64], sv[:, :, 16:64], ["psq"], [dkey])
            P.copy("scalar", vaug[s2][:, 0:64], ps_c[:, 256:320], ["psq"], [("vaug", s2)])
            P.copy("scalar", vaug[s2][:, 192:256], ps_c[:, 256:320], ["psq"], [("vaug", s2)])
            P.ts("vector", iwt[s2][:], ps_c[:, 320:328], w_scale, ALU.mult, ["psq"], [("iw", s2)])
            P.dma("sync", d["vaug"][r0:r0 + 128, :], vaug[s2][:], r=[("vaug", s2)], key=("vaug", s2), is_out=True)
            P.dma("sync", d["iw"][r0:r0 + 128, :], iwt[s2][:], r=[("iw", s2)], key=("iw", s2), is_out=True)
            for k in range(8):
                P.tr(ps_tr[1][:, k, :], qr[s2][:, k * 128:(k + 1) * 128], identb[:], [("qr", s2), "identb"], ["ptr1"], mark=(k == 7))
            P.copy("scalar", qTg[gs][:, 0:8, tok], ps_tr[1][:, 0:8, :], ["ptr1"], [("qTg", gs)])
            for k in range(2):
                P.tr(ps_tr[1][:, k, :], kr[s2][:, k * 128:(k + 1) * 128], identb[:], [("kr", s2), "identb"], ["ptr1"], mark=(k == 1))
            P.copy("scalar", kTg[gs][:, 0:2, tok], ps_tr[1][:, 0:2, :], ["ptr1"], [("kTg", gs)])
        gsl = slice(g * 512, (g + 1) * 512)
        P.dma("sync", d["qT"][b, :, :, gsl].rearrange("c p t -> p c t"), qTg[gs][:, 0:4, :], r=[("qTg", gs)], key=("qTg", gs), is_out=True)
        P.dma("sync", d["iqT"][b, :, :, gsl].rearrange("c p t -> p c t"), qTg[gs][:, 4:8, :], r=[("qTg", gs)], key=("iqTg", gs), is_out=True)
        P.dma("sync", d["kkT"][b, :, :, gsl].rearrange("c p t -> p c t"), kTg[gs][:, :, :], r=[("kTg", gs)], key=("kTg", gs), is_out=True)

    ngr = NSEQ * 4
    norm_group(0)
    for gi in range(ngr):
        if gi + 1 < ngr:
            norm_group(gi + 1)
        mm_group(gi)
    P.close()


def phase_B(B):
    nc, d, NSEQ = B.nc, B.t, B.NSEQ
    P = Phase(nc, "B")
    identb = P.sb("identb", [128, 128], BF16)
    P.dma("sync", identb[:], d["ident_bf"], w=["identb"], key="identb")
    identf = P.sb("identf", [128, 128], F32)
    P.dma("sync", identf[:], d["ident_f32"], w=["identf"], key="identf")
    ones = P.sb("ones", [128, 128], F32)
    P.memset("vector", ones[:], 1.0, ["ones"])
    eps_t = P.sb("eps", [128, 1], F32)
    P.memset("vector", eps_t[:], EPS, ["eps"])
    cw = P.sb("cw", [31, 512], F32)
    P.dma("sync", cw[:], d["ab_conv_w"], w=["cw"], key="cw")
    prm = P.sb("prm", [128, 12], F32)
    P.dma("sync", prm[:], d["conv_prm"], w=["prm"], key="prm")
    ps_w = P.ps("psw", [128, 4, 32])
    for c in range(4):
        P.tr(ps_w[:, c, 0:31], cw[0:31, c * 128:(c + 1) * 128], identf[0:31, 0:31], ["cw", "identf"], ["psw"], mark=(c == 3))
    wT = P.sb("wT", [128, 4, 32], F32)
    P.copy("vector", wT[:, :, 0:31], ps_w[:, :, 0:31], ["psw"], ["wT"])
    diag = P.sb("diag", [128, 124, 128], BF16)
    for c in range(4):
        for j in range(31):
            i = c * 31 + j
            P.ts("vector" if i % 2 == 0 else "gpsimd", diag[:, i, :], identb[:], wT[:, c, j:j + 1], ALU.mult,
                 ["identb", "wT"], [("diag", c)])
    hbuf = P.sb("hbuf", [128, 4, 30 + SEQ], BF16)
    P.memset("gpsimd", hbuf[:, :, 0:30], 0.0, ["hbuf"])
    ps_c = [P.ps("psc%d" % i, [128, 512]) for i in range(2)]
    ps_s1 = P.ps("ps1", [128, 512])
    ps_s2 = P.ps("ps2", [128, 512])
    hcs = P.sb("hcs", [128, 4, 512], F32)
    sqs = P.sb("sqs", [128, 4, 512], F32)
    m = P.sb("m", [128, 512], F32)
    msq = P.sb("msq", [128, 512], F32)
    var = P.sb("var", [128, 512], F32)
    rstd = P.sb("rstd", [128, 512], F32)
    z = [P.sb("z%d" % i, [128, 512], F32) for i in range(2)]
    yst = [P.sb("yst%d" % i, [128, 4, 512], BF16) for i in range(2)]
    for b in range(NSEQ):
        for c in range(4):
            P.dma("sync", hbuf[:, c, 30:30 + SEQ], d["hconvT"][b, c, :, :], w=["hbuf"], key=("hbuf", c))
        for g in range(4):
            ys = g % 2
            for c in range(4):
                pc = ps_c[c % 2]
                pk = ("psc", c % 2)
                for j in range(31):
                    P.mm(pc[:, :], diag[:, c * 31 + j, :], hbuf[:, c, g * 512 + j:g * 512 + j + 512], j == 0, j == 30,
                         [("diag", c), "hbuf"], [pk])
                P.act(hcs[:, c, :], pc[:, :], AF.Identity, [pk, "prm"], [("hcs", c)], bias=prm[:, c:c + 1], scale=1.0)
                P.act(sqs[:, c, :], pc[:, :], AF.Square, [pk, "prm"], [("sqs", c)], bias=prm[:, c:c + 1], scale=1.0)
            for c in range(4):
                P.mm(ps_s1[:, :], ones[:], hcs[:, c, :], c == 0, c == 3, [("hcs", c), "ones"], ["ps1"])
            for c in range(4):
                P.mm(ps_s2[:, :], ones[:], sqs[:, c, :], c == 0, c == 3, [("sqs", c), "ones"], ["ps2"])
            P.act(m[:], ps_s1[:, :], AF.Copy, ["ps1"], ["m"], scale=1.0 / 512)
            P.tt("gpsimd", msq[:], m[:], m[:], ALU.mult, ["m"], ["msq"])
            P.stt(var[:], ps_s2[:, :], 1.0 / 512, msq[:], ALU.mult, ALU.subtract, ["ps2", "msq"], ["var"])
            P.act(var[:], var[:], AF.Sqrt, ["var", "eps"], ["var"], bias=eps_t[:, 0:1], scale=1.0)
            P.recip(rstd[:], var[:], ["var"], ["rstd"])
            for c in range(4):
                zz = z[c % 2]
                zk = ("z", c % 2)
                P.tt("gpsimd", zz[:], hcs[:, c, :], m[:], ALU.subtract, [("hcs", c), "m"], [zk])
                P.tt("vector", zz[:], zz[:], rstd[:], ALU.mult, [zk, "rstd"], [zk])
                P.act(yst[ys][:, c, :], zz[:], AF.Silu, [zk, "prm"], [("yst", ys)], scale=prm[:, 4 + c:5 + c], bias=prm[:, 8 + c:9 + c])
            P.dma("sync", d["yabT"][b, 0:4, :, g * 512:(g + 1) * 512].rearrange("c p t -> p c t"), yst[ys][:],
                  r=[("yst", ys)], key=("yst", ys), is_out=True)
    P.close()


def phase_C(B):
    nc, d, NSEQ = B.nc, B.t, B.NSEQ
    P = Phase(nc, "C")
    identb = P.sb("identb", [128, 128], BF16)
    P.dma("sync", identb[:], d["ident_bf"], w=["identb"], key="identb")
    negm = P.sb("negm", [128, 128], F32)
    P.dma("sync", negm[:], d["negmask"], w=["negm"], key="negm")
    pow2 = P.sb("pow2", [128, NIT], F32)
    P.dma("sync", pow2[:], d["pow2"], w=["pow2"], key="pow2")
    thr0 = P.sb("thr0", [128, 1], F32)
    P.memset("vector", thr0[:], -1e29, ["thr0"])
    bigI = P.sb("bigI", [128, 128], BF16)
    P.ts("vector", bigI[:], identb[:], 30000.0, ALU.mult, ["identb"], ["bigI"])
    qT = [P.sb("qT%d" % i, [128, 4, SEQ], BF16) for i in range(2)]
    iqT = [P.sb("iqT%d" % i, [128, 4, SEQ], BF16) for i in range(2)]
    kkT = [P.sb("kkT%d" % i, [128, 2, SEQ], BF16) for i in range(2)]
    vaug = [P.sb("vaug%d" % i, [128, NT, 256], BF16) for i in range(2)]
    iw = [P.sb("iw%d" % i, [128, NT, 8], F32) for i in range(2)]
    ybg = [P.sb("ybg%d" % i, [128, 512], BF16) for i in range(2)]
    maskT = [P.sb("maskT%d" % i, [128, NT, 512], BF16) for i in range(2)]
    score = [P.sb("score%d" % i, [128, SEQ], F32) for i in range(2)]
    junk = P.sb("junk", [128, SEQ], BF16)
    rl = [P.sb("rl%d" % i, [128, 512], F32) for i in range(2)]
    mk = [P.sb("mk%d" % i, [128, SEQ], BF16) for i in range(2)]
    st = [P.sb("st%d" % i, [128, 8 + NIT], F32) for i in range(2)]
    pe = [P.sb("pe%d" % i, [128, 512], BF16) for i in range(3)]
    rc = [P.sb("rc%d" % i, [128, 512], F32) for i in range(2)]
    ps_lg = [P.ps("plg%d" % i, [128, 512]) for i in range(2)]
    ps_tr = [P.ps("ptr%d" % i, [128, 8, 128], BF16) for i in range(2)]
    ps_s = [P.ps("pss%d" % i, [128, 512]) for i in range(2)]
    ps_o = [P.ps("pso%d" % i, [128, 512]) for i in range(2)]
    att_scale = 64 ** -0.5
    ctr = {"lg": 0, "s": 0, "o": 0, "tr": 0, "yb": 0, "g": 0}

    mslot = {}

    def load_seq(b):
        p = b % 2
        rs = slice(b * SEQ, (b + 1) * SEQ)
        P.dma("sync", iqT[p][:], d["iqT"][b].rearrange("c p t -> p c t"), w=[("iqT", p)], key=("iqT", p))
        P.dma("sync", kkT[p][:], d["kkT"][b].rearrange("c p t -> p c t"), w=[("kkT", p)], key=("kkT", p))
        P.dma("sync", iw[p][:], d["iw"][rs, :].rearrange("(t p) c -> p t c", p=128), w=[("iw", p)], key=("iw", p))
        P.dma("sync", qT[p][:], d["qT"][b].rearrange("c p t -> p c t"), w=[("qT", p)], key=("qT", p))
        P.dma("sync", vaug[p][:], d["vaug"][rs, :].rearrange("(t p) c -> p t c", p=128), w=[("vaug", p)], key=("vaug", p))

    junk2 = [junk, P.sb("junkb", [128, SEQ], BF16)]

    def score_pair(b, G, q4s):
        p = b % 2
        mG = mslot[(b, G)]
        info = []
        for q4 in q4s:
            qb = 4 * G + q4
            nk = 128 * (qb + 1)
            nkb = (nk + 511) // 512
            s2 = qb % 2
            sc = score[s2]
            sks = [("score", s2, kb) for kb in range(nkb)]
            qsl = slice(qb * 128, (qb + 1) * 128)
            for h in range(8):
                c, base = h // 2, 64 * (h % 2)
                for kb in range(nkb):
                    n = min(512, nk - kb * 512)
                    r = ctr["lg"] % 2
                    ctr["lg"] += 1
                    P.mm(ps_lg[r][:, 0:n], iqT[p][base:base + 64, c, qsl], kkT[p][base:base + 64, 1, kb * 512:kb * 512 + n],
                         True, True, [("iqT", p), ("kkT", p)], [("plg", r)])
                    P.act(rl[r][:, 0:n], ps_lg[r][:, 0:n], AF.Relu, [("plg", r)], [("rl", r)])
                    dst = sc[:, kb * 512:kb * 512 + n]
                    if h == 0:
                        P.ts("vector", dst, rl[r][:, 0:n], iw[p][:, qb, 0:1], ALU.mult, [("rl", r), ("iw", p)], [sks[kb]])
                    else:
                        P.stt(dst, rl[r][:, 0:n], iw[p][:, qb, h:h + 1], dst, ALU.mult, ALU.add, [("rl", r), ("iw", p), sks[kb]], [sks[kb]])
            info.append((q4, qb, nk, s2, sc, sks, qsl))
        for (q4, qb, nk, s2, sc, sks, qsl) in info:
            stt_ = st[s2]
            stk = ("st", s2)
            if qb >= 2:
                P.reduce(stt_[:, 0:1], sc[:, 0:nk], ALU.max, sks, [stk])
                P.reduce(stt_[:, 1:2], sc[:, 0:nk], ALU.min, sks, [(stk, "lo")])
            P.tt("vector", sc[:, qsl], sc[:, qsl], negm[:], ALU.add, [sks[qb // 4], "negm"], [sks[qb // 4]])
            if qb >= 2:
                P.tt("vector", stt_[:, 2:3], stt_[:, 0:1], stt_[:, 1:2], ALU.subtract, [stk, (stk, "lo")], [stk])
                P.ts("vector", stt_[:, 8:8 + NIT], pow2[:], stt_[:, 2:3], ALU.mult, ["pow2", stk], [(stk, "steps")])
        bis = [x for x in info if x[1] >= 2]
        for i in range(NIT):
            for (q4, qb, nk, s2, sc, sks, qsl) in bis:
                stt_, stk = st[s2], ("st", s2)
                P.tt("vector", stt_[:, 3:4], stt_[:, 1:2], stt_[:, 8 + i:9 + i], ALU.add, [(stk, "lo"), (stk, "steps")], [(stk, "mid")])
            for (q4, qb, nk, s2, sc, sks, qsl) in bis:
                stt_, stk = st[s2], ("st", s2)
                P.ts("vector", junk2[s2][:, 0:nk], sc[:, 0:nk], stt_[:, 3:4], ALU.is_ge, sks + [(stk, "mid")], [("junk", s2), (stk, "cnt")],
                     s2=0.0, op1=ALU.add, accum_out=stt_[:, 4:5])
            for (q4, qb, nk, s2, sc, sks, qsl) in bis:
                stt_, stk = st[s2], ("st", s2)
                P.stt(stt_[:, 5:6], stt_[:, 4:5], TOPK - 0.5, stt_[:, 8 + i:9 + i], ALU.is_ge, ALU.mult, [(stk, "cnt"), (stk, "steps")], [(stk, "sel")])
            for (q4, qb, nk, s2, sc, sks, qsl) in bis:
                stt_, stk = st[s2], ("st", s2)
                P.tt("vector", stt_[:, 1:2], stt_[:, 1:2], stt_[:, 5:6], ALU.add, [(stk, "lo"), (stk, "sel")], [(stk, "lo")])
        for (q4, qb, nk, s2, sc, sks, qsl) in info:
            stt_, stk = st[s2], ("st", s2)
            thr = stt_[:, 1:2] if qb >= 2 else thr0[:, 0:1]
            P.ts("vector", mk[s2][:, 0:nk], sc[:, 0:nk], thr, ALU.is_ge, sks + [(stk, "lo"), "thr0"], [("mk", s2)],
                 s2=-1.0, op1=ALU.add)
            kt = 0
            while kt <= qb:
                n = min(8, qb + 1 - kt)
                r = ctr["tr"] % 2
                ctr["tr"] += 1
                for j in range(n):
                    P.tr(ps_tr[r][:, j, :], mk[s2][:, (kt + j) * 128:(kt + j + 1) * 128], identb[:], [("mk", s2), "identb"],
                         [("ptr", r)], mark=(j == n - 1))
                P.copy("scalar", maskT[mG][:, kt:kt + n, q4 * 128:(q4 + 1) * 128], ps_tr[r][:, 0:n, :], [("ptr", r)], [("maskT", mG)])
                kt += n

    def attn_chunk(b, G, c):
        p = b % 2
        mG = mslot[(b, G)]
        nkt = 4 * G + 4
        gsl0 = G * 512
        ys = ctr["yb"] % 2
        ctr["yb"] += 1
        for e in range(2):
            base = 64 * e
            o = ctr["o"] % 2
            ctr["o"] += 1
            for kt in range(nkt):
                q0 = max(0, kt - 4 * G) * 128
                N = 512 - q0
                r = ctr["s"] % 2
                ctr["s"] += 1
                P.mm(ps_s[r][:, 0:N], kkT[p][base:base + 64, 0, kt * 128:(kt + 1) * 128], qT[p][base:base + 64, c, gsl0 + q0:gsl0 + 512],
                     True, False, [("kkT", p), ("qT", p)], [("pss", r)], mark=False)
                P.mm(ps_s[r][:, 0:N], bigI[:], maskT[mG][:, kt, q0:512], False, True, ["bigI", ("maskT", mG)], [("pss", r)])
                r3 = ctr["s"] % 3
                P.act(pe[r3][:, 0:N], ps_s[r][:, 0:N], AF.Exp, [("pss", r)], [("pe", r3)], scale=att_scale)
                P.mm(ps_o[o][:, q0:512], vaug[p][:, kt, e * 128:(e + 1) * 128], pe[r3][:, 0:N], kt == 0, kt == nkt - 1,
                     [("pe", r3), ("vaug", p)], [("pso", o)])
            so, ss_ = (slice(0, 64), slice(64, 128)) if e == 0 else (slice(64, 128), slice(0, 64))
            P.act(rc[o][ss_, :], ps_o[o][ss_, :], AF.Ln, [("pso", o)], [("rc", o)])
            P.act(rc[o][ss_, :], rc[o][ss_, :], AF.Exp, [("rc", o)], [("rc", o)], scale=-1.0)
            P.tt("vector", ybg[ys][so, :], ps_o[o][so, :], rc[o][ss_, :], ALU.mult, [("pso", o), ("rc", o)], [("ybg", ys)])
        P.dma("sync", d["yabT"][b, 4 + c, :, gsl0:gsl0 + 512], ybg[ys][:], r=[("ybg", ys)], key=("ybg", ys), is_out=True)

    groups = [(b, G) for G in range(4) for b in range(NSEQ)]
    for b in range(NSEQ):
        load_seq(b)
    for gi_, (b, G) in enumerate(groups):
        mslot[(b, G)] = gi_ % 2
        for pr in range(2):
            score_pair(b, G, (2 * pr, 2 * pr + 1))
            if gi_ >= 1:
                pb, pG = groups[gi_ - 1]
                attn_chunk(pb, pG, 2 * pr)
                attn_chunk(pb, pG, 2 * pr + 1)
    pb, pG = groups[-1]
    for c in range(4):
        attn_chunk(pb, pG, c)
    P.close()


def phase_D(B, tag, yT, wname, resid, gname, hmid, hnT_name):
    nc, d, NSEQ = B.nc, B.t, B.NSEQ
    P = Phase(nc, tag)
    Wo = P.sb("Wo", [128, 8, D], BF16)
    P.dma("gpsimd", Wo[:], d[wname].rearrange("(k p) n -> p k n", p=128), w=["Wo"], key="Wo")
    identb = P.sb("identb", [128, 128], BF16)
    P.dma("sync", identb[:], d["ident_bf"], w=["identb"], key="identb")
    gbc = P.sb("gbc", [128, D], F32)
    P.dma("sync", gbc[:], d[gname], w=["gbc"], key="gbc")
    eps_t = P.sb("eps", [128, 1], F32)
    P.memset("vector", eps_t[:], EPS, ["eps"])
    yt = [P.sb("yt%d" % i, [128, 8, 512], BF16) for i in range(2)]
    xt = [P.sb("xt%d" % i, [128, D], F32) for i in range(2)]
    hm = [P.sb("hm%d" % i, [128, D], F32) for i in range(2)]
    junk = P.sb("junk", [128, D], F32)
    stat = [P.sb("stat%d" % i, [128, 4], F32) for i in range(2)]
    hn = [P.sb("hn%d" % i, [128, D], BF16) for i in range(2)]
    hnTg = [P.sb("hnTg%d" % i, [128, 8, 512], BF16) for i in range(2)]
    ps_o = [P.ps("pso%d" % i, [128, 1024]) for i in range(2)]
    ps_tr = [P.ps("ptr%d" % i, [128, 8, 128], BF16) for i in range(2)]
    ngr = NSEQ * 4

    def load_y(gi):
        b, g = gi // 4, gi % 4
        P.dma("sync", yt[gi % 2][:], d[yT][b, :, :, g * 512:(g + 1) * 512].rearrange("c p t -> p c t"), w=[("yt", gi % 2)], key=("yt", gi % 2))

    load_y(0)

    def outproj(ti):
        gi, t4 = ti // 4, ti % 4
        b, g = gi // 4, gi % 4
        gs = gi % 2
        if t4 == 0 and gi + 1 < ngr:
            load_y(gi + 1)
        tt = g * 4 + t4
        r0 = b * SEQ + tt * 128
        tok = slice(t4 * 128, (t4 + 1) * 128)
        s2 = ti % 2
        P.dma("sync", xt[s2][:], d[resid][r0:r0 + 128, :], w=[("x", s2)], key=("x", s2))
        for half in range(2):
            for c in range(8):
                P.mm(ps_o[s2][:, half * 512:(half + 1) * 512], yt[gs][:, c, tok], Wo[:, c, half * 512:(half + 1) * 512],
                     c == 0, c == 7, [("yt", gs), "Wo"], [("pso", s2)], mark=(c == 7 and half == 1))

    def post(ti):
        gi, t4 = ti // 4, ti % 4
        b, g = gi // 4, gi % 4
        gs = gi % 2
        tt = g * 4 + t4
        r0 = b * SEQ + tt * 128
        tok = slice(t4 * 128, (t4 + 1) * 128)
        s2 = ti % 2
        for half in range(2):
            hs_ = slice(half * 512, (half + 1) * 512)
            P.tt("vector", hm[s2][:, hs_], ps_o[s2][:, hs_], xt[s2][:, hs_], ALU.add, [("pso", s2), ("x", s2)], [("hm", s2)])
        P.dma("sync", d[hmid][r0:r0 + 128, :], hm[s2][:], r=[("hm", s2)], key=("hm", s2), is_out=True)
        rms_to_hnT(P, hm[s2][:], ("hm", s2), gbc[:], eps_t, stat[s2], ("st", s2), junk, hn[s2][:], ("hn", s2),
                   ps_tr[s2], ("ptr", s2), identb, hnTg[gs], ("hnTg", gs), tok)
        if t4 == 3:
            P.dma("sync", d[hnT_name][:, :, gi * 512:(gi + 1) * 512].rearrange("c p t -> p c t"), hnTg[gs][:], r=[("hnTg", gs)],
                  key=("hnTg", gs), is_out=True)

    ntl = ngr * 4
    outproj(0)
    for ti in range(ntl):
        if ti + 1 < ntl:
            outproj(ti + 1)
        post(ti)
    P.close()


def phase_F(B, tag, layer, f0, h_in, h_out, hnT_name, final_g=None):
    nc, d, NSEQ = B.nc, B.t, B.NSEQ
    P = Phase(nc, tag)
    nf = 11
    Wg = P.sb("Wg", [128, 8, nf * 128], BF16)
    Wu = P.sb("Wu", [128, 8, nf * 128], BF16)
    Wd = P.sb("Wd", [128, nf, D], BF16)
    cs = slice(f0 * 128, (f0 + nf) * 128)
    P.dma("gpsimd", Wg[:], d["ffn_w_gate%d" % layer].rearrange("(k p) n -> p k n", p=128)[:, :, cs], w=["Wg"], key="Wg")
    P.dma("gpsimd", Wu[:], d["ffn_w_up%d" % layer].rearrange("(k p) n -> p k n", p=128)[:, :, cs], w=["Wu"], key="Wu")
    P.dma("gpsimd", Wd[:], d["ffn_w_down%d" % layer].rearrange("(f p) n -> p f n", p=128)[:, f0:f0 + nf, :], w=["Wd"], key="Wd")
    if final_g is not None:
        gbc = P.sb("gbc", [128, D], F32)
        P.dma("sync", gbc[:], d[final_g], w=["gbc"], key="gbc")
        eps_t = P.sb("eps", [128, 1], F32)
        P.memset("vector", eps_t[:], EPS, ["eps"])
        junk = P.sb("junk", [128, D], F32)
        stat = [P.sb("stat%d" % i, [128, 4], F32) for i in range(2)]
    hnTg = [P.sb("hnTg%d" % i, [128, 8, 512], BF16) for i in range(2)]
    actT = [P.sb("actT%d" % i, [128, nf, 512], BF16) for i in range(2)]
    sg = [P.sb("sg%d" % i, [128, 512], F32) for i in range(2)]
    hin = [P.sb("hin%d" % i, [128, D], F32) for i in range(2)]
    hout = [P.sb("hout%d" % i, [128, D], F32) for i in range(2)]
    ps_g = [P.ps("psg%d" % i, [128, 512]) for i in range(2)]
    ps_u = [P.ps("psu%d" % i, [128, 512]) for i in range(2)]
    ps_d = [P.ps("psd%d" % i, [128, 1024]) for i in range(2)]
    ngr = NSEQ * 4

    def load_h(gi):
        P.dma("sync", hnTg[gi % 2][:], d[hnT_name][:, :, gi * 512:(gi + 1) * 512].rearrange("c p t -> p c t"),
              w=[("hnTg", gi % 2)], key=("hnTg", gi % 2))

    load_h(0)
    for gi in range(ngr):
        gs = gi % 2
        if gi + 1 < ngr:
            load_h(gi + 1)
        for f in range(nf):
            s2 = f % 2
            for k in range(8):
                P.mm(ps_g[s2][:, :], Wg[:, k, f * 128:(f + 1) * 128], hnTg[gs][:, k, :], k == 0, k == 7, [("hnTg", gs), "Wg"], [("psg", s2)])
            for k in range(8):
                P.mm(ps_u[s2][:, :], Wu[:, k, f * 128:(f + 1) * 128], hnTg[gs][:, k, :], k == 0, k == 7, [("hnTg", gs), "Wu"], [("psu", s2)])
            P.act(sg[s2][:], ps_g[s2][:, :], AF.Silu, [("psg", s2)], [("sg", s2)])
            P.tt("vector", actT[gs][:, f, :], ps_u[s2][:, :], sg[s2][:], ALU.mult, [("psu", s2), ("sg", s2)], [("actT", gs)])
        for t4 in range(4):
            r0 = gi * 512 + t4 * 128
            tok = slice(t4 * 128, (t4 + 1) * 128)
            s2 = t4 % 2
            P.dma("sync", hin[s2][:], d[h_in][r0:r0 + 128, :], w=[("hin", s2)], key=("hin", s2))
            for half in range(2):
                for f in range(nf):
                    P.mm(ps_d[s2][:, half * 512:(half + 1) * 512], actT[gs][:, f, tok], Wd[:, f, half * 512:(half + 1) * 512],
                         f == 0, f == nf - 1, [("actT", gs), "Wd"], [("psd", s2)], mark=(f == nf - 1 and half == 1))
            for half in range(2):
                hs_ = slice(half * 512, (half + 1) * 512)
                P.tt("vector", hout[s2][:, hs_], ps_d[s2][:, hs_], hin[s2][:, hs_], ALU.add, [("psd", s2), ("hin", s2)], [("hout", s2)])
            if final_g is not None:
                sk = ("st", s2)
                P.act(junk[:], hout[s2][:], AF.Square, [("hout", s2)], ["junkA", sk], accum_out=stat[s2][:, 0:1])
                P.act(stat[s2][:, 1:2], stat[s2][:, 0:1], AF.Sqrt, [sk], [sk], scale=1.0 / D, bias=eps_t[:, 0:1])
                P.recip(stat[s2][:, 2:3], stat[s2][:, 1:2], [sk], [sk])
                P.stt(hout[s2][:], hout[s2][:], stat[s2][:, 2:3], gbc[:], ALU.mult, ALU.mult, [("hout", s2), sk, "gbc"], [("hout", s2)])
            P.dma("sync", d[h_out][r0:r0 + 128, :], hout[s2][:], r=[("hout", s2)], key=("hout", s2), is_out=True)
    P.close()


def phase_E(B):
    nc, d, NSEQ = B.nc, B.t, B.NSEQ
    P = Phase(nc, "E")
    Wc = P.sb("Wc", [128, 8, 3104], BF16)
    Wsrc = d["c_w_in"].rearrange("(k p) n -> p k n", p=128)
    WK = []
    P.memset("gpsimd", Wc[:, :, 3088:3104], 0.0, [("W", 2)])
    for i, (c0, c1) in enumerate(((0, 1024), (1024, 2048), (2048, 3088))):
        P.dma("gpsimd", Wc[:, :, c0:c1], Wsrc[:, :, c0:c1], w=[("W", i)], key=("W", i))
        WK.append(("W", i))
    identb = P.sb("identb", [128, 128], BF16)
    P.dma("sync", identb[:], d["ident_bf"], w=["identb"], key="identb")
    gbc = P.sb("gbc", [128, D], F32)
    P.dma("sync", gbc[:], d["g_mix1"], w=["gbc"], key="gbc")
    g2w = P.sb("g2w", [128, 512], F32)
    P.memset("vector", g2w[:], 0.0, ["g2w"])
    P.dma("sync", g2w[0:16, :], d["c_gate_w"], w=["g2w"], key="g2w")
    g2b = P.sb("g2b", [128, 512], F32)
    P.dma("sync", g2b[:], d["c_gate_b_bc"], w=["g2b"], key="g2b")
    eps_t = P.sb("eps", [128, 1], F32)
    P.memset("vector", eps_t[:], EPS, ["eps"])
    one_t = P.sb("one", [128, 1], F32)
    P.memset("vector", one_t[:], 1.0, ["one"])
    xt = [P.sb("xt%d" % i, [128, D], F32) for i in range(2)]
    junk = P.sb("junk", [128, D], F32)
    stat = [P.sb("stat%d" % i, [128, 4], F32) for i in range(2)]
    hn = [P.sb("hn%d" % i, [128, D], BF16) for i in range(2)]
    hnT = [P.sb("hnT%d" % i, [128, 8, 512], BF16) for i in range(2)]
    qkT = [P.sb("qkT%d" % i, [128, 8, 512], BF16) for i in range(2)]
    glrT = [P.sb("glrT%d" % i, [128, 512], F32) for i in range(2)]
    for i in range(2):
        P.memset("vector", glrT[i][:], 0.0, [("glrT", i)])
    kst = [P.sb("kst%d" % i, [128, 512], BF16) for i in range(2)]
    vst = [P.sb("vst%d" % i, [128, 1024], BF16) for i in range(2)]
    rst = [P.sb("rst%d" % i, [128, 1024], F32) for i in range(2)]
    lat = [P.sb("lat%d" % i, [128, 512], F32) for i in range(2)]
    lt1 = P.sb("lt1", [128, 512], F32)
    lt2 = P.sb("lt2", [128, 512], F32)
    ps_tr = P.ps("ptr", [128, 8, 128], BF16)
    ps_f = [P.ps("psf%d" % i, [128, 512]) for i in range(2)]
    ps_t = [P.ps("pst%d" % i, [128, 512]) for i in range(3)]
    ps_l = P.ps("psl", [128, 512])
    n_t = 0
    n_f = 0
    n_tiles = NSEQ * NT

    def load_x(ti):
        P.dma("sync", xt[ti % 2][:], d["h1"][ti * 128:(ti + 1) * 128, :], w=[("x", ti % 2)], key=("x", ti % 2))

    load_x(0)

    def norm_group(gi):
        gs = gi % 2
        for t4 in range(4):
            ti = gi * 4 + t4
            if ti + 1 < n_tiles:
                load_x(ti + 1)
            xs = ti % 2
            rms_to_hnT(P, xt[xs][:], ("x", xs), gbc[:], eps_t, stat[xs], ("st", xs), junk, hn[xs][:], ("hn", xs),
                       ps_tr, "ptr", identb, hnT[gs], ("hnT", gs), slice(t4 * 128, (t4 + 1) * 128))

    def mm_group(gi):
        nonlocal n_t, n_f
        b, g = gi // 4, gi % 4
        gs = gi % 2
        for oc in range(8):
            r = n_f % 2
            n_f += 1
            for k in range(8):
                P.mm(ps_f[r][:, :], Wc[:, k, oc * 128:(oc + 1) * 128], hnT[gs][:, k, :], k == 0, k == 7, [("hnT", gs)] + WK, [("psf", r)])
            if oc < 4:
                P.act(qkT[gs][:, oc, :], ps_f[r][:, :], AF.Copy, [("psf", r)], [("qkT", gs)], scale=128 ** -0.5)
            else:
                P.copy("vector", qkT[gs][:, oc, :], ps_f[r][:, :], [("psf", r)], [("qkT", gs)])
        r = n_f % 2
        n_f += 1
        for k in range(8):
            P.mm(ps_f[r][0:32, :], Wc[:, k, 3072:3104], hnT[gs][:, k, :], k == 0, k == 7, [("hnT", gs)] + WK, [("psf", r)])
        P.copy("vector", glrT[gs][0:32, :], ps_f[r][0:32, :], [("psf", r)], [("glrT", gs)])
        gsl = slice(g * 512, (g + 1) * 512)
        P.dma("sync", d["c_qT"][b, :, :, gsl].rearrange("c p t -> p c t"), qkT[gs][:, 0:4, :], r=[("qkT", gs)], key=("qTo", gs), is_out=True)
        P.dma("sync", d["c_kT"][b, :, :, gsl].rearrange("c p t -> p c t"), qkT[gs][:, 4:8, :], r=[("qkT", gs)], key=("kTo", gs), is_out=True)
        for t4 in range(4):
            r0 = gi * 512 + t4 * 128
            tok = slice(t4 * 128, (t4 + 1) * 128)
            s2 = t4 % 2
            jobs = [(512, 1024, kst[s2][:, :], ("kst", s2), "k"),
                    (1024, 1536, vst[s2][:, 0:512], ("vst", s2), "v"), (1536, 2048, vst[s2][:, 512:1024], ("vst", s2), "v"),
                    (2048, 2560, rst[s2][:, 0:512], ("rst", s2), "r"), (2560, 3072, rst[s2][:, 512:1024], ("rst", s2), "r")]
            for (c0, c1, dst, dk, kind) in jobs:
                r = n_t % 3
                n_t += 1
                for k in range(8):
                    P.mm(ps_t[r][:, :], hnT[gs][:, k, tok], Wc[:, k, c0:c1], k == 0, k == 7, [("hnT", gs)] + WK, [("pst", r)])
                if kind == "r":
                    P.act(dst, ps_t[r][:, :], AF.Silu, [("pst", r)], [dk])
                elif kind == "v":
                    P.copy("vector", dst, ps_t[r][:, :], [("pst", r)], [dk])
                else:
                    P.copy("scalar", dst, ps_t[r][:, :], [("pst", r)], [dk])
            P.dma("sync", d["c_ktok"][r0:r0 + 128, :], kst[s2][:], r=[("kst", s2)], key=("kst", s2), is_out=True)
            P.dma("sync", d["c_v"][r0:r0 + 128, :], vst[s2][:], r=[("vst", s2)], key=("vst", s2), is_out=True)
            P.dma("sync", d["c_sr"][r0:r0 + 128, :], rst[s2][:], r=[("rst", s2)], key=("rst", s2), is_out=True)
            P.mm(ps_l[:, :], glrT[gs][:, tok], g2w[:, :], True, True, [("glrT", gs), "g2w"], ["psl"])
            P.tt("vector", lt1[:], ps_l[:, :], g2b[:], ALU.add, ["psl", "g2b"], ["lt1"])
            P.act(lt2[:], lt1[:], AF.Exp, ["lt1"], ["lt2"], scale=-1.0)
            P.act(lt1[:], lt2[:], AF.Ln, ["lt2", "one"], ["lt1"], bias=one_t[:, 0:1], scale=1.0)
            P.ts("gpsimd", lat[s2][:], lt1[:], -1.0 / 16.0, ALU.mult, ["lt1"], [("lat", s2)])
            P.dma("sync", d["c_la"][r0:r0 + 128, :], lat[s2][:], r=[("lat", s2)], key=("lat", s2), is_out=True)

    ngr = NSEQ * 4
    norm_group(0)
    for gi in range(ngr):
        if gi + 1 < ngr:
            norm_group(gi + 1)
        mm_group(gi)
    P.close()


def phase_G(B):
    nc, d, NSEQ = B.nc, B.t, B.NSEQ
    P = Phase(nc, "G")
    identb = P.sb("identb", [128, 128], BF16)
    P.dma("sync", identb[:], d["ident_bf"], w=["identb"], key="identb")
    U = P.sb("U", [128, 128], F32)
    SL = P.sb("SL", [128, 128], F32)
    P.dma("sync", U[:], d["tri_u"], w=["U"], key="U")
    P.dma("sync", SL[:], d["tri_sl"], w=["SL"], key="SL")
    gon = P.sb("gon", [128, D], F32)
    P.dma("sync", gon[:], d["c_onorm_g_bc"], w=["gon"], key="gon")
    eps_t = P.sb("eps", [128, 1], F32)
    P.memset("vector", eps_t[:], EPS, ["eps"])
    qT = P.sb("qT", [128, 4, SEQ], BF16)
    kT = P.sb("kT", [128, 4, SEQ], BF16)
    la = [P.sb("la%d" % i, [128, 512], F32) for i in range(2)]
    kt_ = [P.sb("ktk%d" % i, [128, 512], BF16) for i in range(2)]
    vt = [P.sb("vt%d" % i, [128, 1024], BF16) for i in range(2)]
    sr = [P.sb("sr%d" % i, [128, 1024], F32) for i in range(2)]
    eb = [P.sb("eb%d" % i, [128, 512], F32) for i in range(2)]
    enb = P.sb("enb", [128, 512], F32)
    erv = P.sb("erv", [128, 512], F32)
    qtT = [P.sb("qtT%d" % i, [128, 512], BF16) for i in range(2)]
    ktT = [P.sb("ktT%d" % i, [128, 512], BF16) for i in range(2)]
    ks = [P.sb("ks%d" % i, [128, 512], BF16) for i in range(2)]
    attm = [P.sb("attm%d" % i, [128, 128], BF16) for i in range(2)]
    state = P.sb("state", [128, 4, 256], F32)
    stb = P.sb("stb", [128, 4, 256], BF16)
    gr = [P.sb("gr%d" % i, [128, 1024], F32) for i in range(2)]
    stat = [P.sb("stat%d" % i, [128, 12], F32) for i in range(2)]
    junk = P.sb("junk", [128, 256], F32)
    yc = [P.sb("yc%d" % i, [128, 1024], BF16) for i in range(2)]
    ycT = [P.sb("ycT%d" % i, [128, 8, 512], BF16) for i in range(2)]
    ps_b = P.ps("psb", [128, 512])
    ps_r = P.ps("psr", [128, 512])
    ps_a = P.ps("psa", [128, 4, 128])
    ps_o = P.ps("pso", [128, 1024])
    ps_kv = P.ps("pskv", [128, 1024])
    ps_tr = P.ps("ptr", [128, 8, 128], BF16)
    nch = NSEQ * NT

    def load_chunk(ci):
        s2 = ci % 2
        r0 = ci * 128
        P.dma("sync", la[s2][:], d["c_la"][r0:r0 + 128, :], w=[("la", s2)], key=("la", s2))
        P.dma("sync", kt_[s2][:], d["c_ktok"][r0:r0 + 128, :], w=[("ktk", s2)], key=("ktk", s2))
        P.dma("sync", vt[s2][:], d["c_v"][r0:r0 + 128, :], w=[("vt", s2)], key=("vt", s2))
        P.dma("sync", sr[s2][:], d["c_sr"][r0:r0 + 128, :], w=[("sr", s2)], key=("sr", s2))

    load_chunk(0)
    for ci in range(nch):
        b, c = ci // NT, ci % NT
        s2 = ci % 2
        if c == 0:
            P.dma("sync", qT[:], d["c_qT"][b].rearrange("c p t -> p c t"), w=["qT"], key="qT")
            P.dma("sync", kT[:], d["c_kT"][b].rearrange("c p t -> p c t"), w=["kT"], key="kT")
            P.memset("vector", state[:], 0.0, [("state", h) for h in range(4)])
            P.memset("gpsimd", stb[:], 0.0, [("stb", h) for h in range(4)])
        if ci + 1 < nch:
            load_chunk(ci + 1)
        csl = slice(c * 128, (c + 1) * 128)
        for h in range(4):
            P.mm(ps_b[:, h * 128:(h + 1) * 128], la[s2][:, h * 128:(h + 1) * 128], U[:], True, True, [("la", s2), "U"], ["psb"], mark=(h == 3))
        P.mm(ps_r[:, :], SL[:], la[s2][:, :], True, True, [("la", s2), "SL"], ["psr"])
        P.act(eb[s2][:], ps_b[:, :], AF.Exp, ["psb"], [("eb", s2)])
        P.act(enb[:], ps_b[:, :], AF.Exp, ["psb"], ["enb"], scale=-1.0)
        P.act(erv[:], ps_r[:, :], AF.Exp, ["psr"], ["erv"])
        qv = qT[:, :, csl]
        kv_ = kT[:, :, csl]
        P.tt("vector", qtT[s2][:].rearrange("p (h t) -> p h t", t=128), qv, eb[s2][:].rearrange("p (h t) -> p h t", t=128), ALU.mult,
             ["qT", ("eb", s2)], [("qtT", s2)])
        P.tt("gpsimd", ktT[s2][:].rearrange("p (h t) -> p h t", t=128), kv_, enb[:].rearrange("p (h t) -> p h t", t=128), ALU.mult,
             ["kT", "enb"], [("ktT", s2)])
        P.tt("gpsimd", ks[s2][:], kt_[s2][:], erv[:], ALU.mult, [("ktk", s2), "erv"], [("ks", s2)])
        P.tt("gpsimd", gr[s2][:], sr[s2][:], gon[:], ALU.mult, [("sr", s2), "gon"], [("gr", s2)])
        for h in range(4):
            hs = slice(h * 128, (h + 1) * 128)
            vs = slice(h * 256, (h + 1) * 256)
            a2 = h % 2
            P.mm(ps_a[:, h, :], ktT[s2][:, hs], qtT[s2][:, hs], True, True, [("ktT", s2), ("qtT", s2)], [("psa", h)])
            P.tt("vector", attm[a2][:], ps_a[:, h, :], U[:], ALU.mult, [("psa", h), "U"], [("attm", a2)])
            P.mm(ps_o[:, vs], attm[a2][:], vt[s2][:, vs], True, False, [("attm", a2), ("vt", s2)], [("pso", h)], mark=False)
            P.mm(ps_o[:, vs], qtT[s2][:, hs], stb[:, h, :], False, True, [("qtT", s2), ("stb", h)], [("pso", h)])
            P.mm(ps_kv[:, vs], ks[s2][:, hs], vt[s2][:, vs], True, True, [("ks", s2), ("vt", s2)], [("pskv", h)])
            P.stt(state[:, h, :], state[:, h, :], eb[s2][:, h * 128 + 127:h * 128 + 128], ps_kv[:, vs], ALU.mult, ALU.add,
                  [("state", h), ("eb", s2), ("pskv", h)], [("state", h)])
            P.copy("scalar", stb[:, h, :], state[:, h, :], [("state", h)], [("stb", h)])
        sk = ("st", s2)
        for h in range(4):
            vs = slice(h * 256, (h + 1) * 256)
            P.act(junk[:], ps_o[:, vs], AF.Square, [("pso", h)], ["junkA", sk], accum_out=stat[s2][:, h:h + 1])
        P.act(stat[s2][:, 4:8], stat[s2][:, 0:4], AF.Sqrt, [sk], [sk], scale=1.0 / 256, bias=eps_t[:, 0:1])
        P.recip(stat[s2][:, 8:12], stat[s2][:, 4:8], [sk], [sk])
        for h in range(4):
            vs = slice(h * 256, (h + 1) * 256)
            P.stt(yc[s2][:, vs], ps_o[:, vs], stat[s2][:, 8 + h:9 + h], gr[s2][:, vs], ALU.mult, ALU.mult,
                  [("pso", h), sk, ("gr", s2)], [("yc", s2)])
        for k in range(8):
            P.tr(ps_tr[:, k, :], yc[s2][:, k * 128:(k + 1) * 128], identb[:], [("yc", s2), "identb"], ["ptr"], mark=(k == 7))
        g4 = (ci // 4) % 2
        P.copy("scalar", ycT[g4][:, 0:8, (ci % 4) * 128:(ci % 4 + 1) * 128], ps_tr[:, 0:8, :], ["ptr"], [("ycT", g4)])
        if ci % 4 == 3:
            g = c // 4
            P.dma("sync", d["ycT"][b, :, :, g * 512:(g + 1) * 512].rearrange("c p t -> p c t"), ycT[g4][:], r=[("ycT", g4)],
                  key=("ycT", g4), is_out=True)
    P.close()


def declare(B):
    NSEQ = B.NSEQ
    T = NSEQ * SEQ
    X = "ExternalInput"
    B.dram("x", [T, D], F32, X)
    B.dram("ab_w_in", [D, 2248], F32, X)
    B.dram("ab_conv_w", [31, 512], F32, X)
    B.dram("conv_prm", [128, 12], F32, X)
    B.dram("ab_w_out", [D, D], F32, X)
    B.dram("c_w_in", [D, 3088], F32, X)
    B.dram("c_gate_w", [16, 512], F32, X)
    B.dram("c_gate_b_bc", [128, 512], F32, X)
    B.dram("c_onorm_g_bc", [128, D], F32, X)
    B.dram("c_w_out", [D, D], F32, X)
    for l in range(2):
        B.dram("ffn_w_gate%d" % l, [D, DFF], F32, X)
        B.dram("ffn_w_up%d" % l, [D, DFF], F32, X)
        B.dram("ffn_w_down%d" % l, [DFF, D], F32, X)
        B.dram("g_mix%d" % l, [128, D], F32, X)
        B.dram("g_ffn%d" % l, [128, D], F32, X)
    B.dram("g_final", [128, D], F32, X)
    B.dram("ident_bf", [128, 128], BF16, X)
    B.dram("ident_f32", [128, 128], F32, X)
    B.dram("cos128", [SEQ, 128], F32, X)
    B.dram("sin128", [SEQ, 128], F32, X)
    B.dram("negmask", [128, 128], F32, X)
    B.dram("pow2", [128, NIT], F32, X)
    B.dram("tri_u", [128, 128], F32, X)
    B.dram("tri_sl", [128, 128], F32, X)
    B.dram("hconvT", [NSEQ, 4, 128, SEQ], BF16)
    B.dram("qT", [NSEQ, 4, 128, SEQ], BF16)
    B.dram("iqT", [NSEQ, 4, 128, SEQ], BF16)
    B.dram("kkT", [NSEQ, 2, 128, SEQ], BF16)
    B.dram("vaug", [T, 256], BF16)
    B.dram("iw", [T, 8], F32)
    B.dram("yabT", [NSEQ, 8, 128, SEQ], BF16)
    B.dram("h_mid0", [T, D], F32)
    B.dram("hnT", [8, 128, T], BF16)
    B.dram("h_half", [T, D], F32)
    B.dram("h1", [T, D], F32)
    B.dram("c_qT", [NSEQ, 4, 128, SEQ], BF16)
    B.dram("c_kT", [NSEQ, 4, 128, SEQ], BF16)
    B.dram("c_ktok", [T, 512], BF16)
    B.dram("c_v", [T, D], BF16)
    B.dram("c_sr", [T, D], F32)
    B.dram("c_la", [T, 512], F32)
    B.dram("ycT", [NSEQ, 8, 128, SEQ], BF16)
    B.dram("h_mid1", [T, D], F32)
    B.dram("out", [T, D], F32, "ExternalOutput")
    if DEBUG_STOP == 77:
        B.dram("dbg", [128, 16], F32, "ExternalOutput")
        B.dram("dbg2", [4, 128, D], BF16, "ExternalOutput")


PHASES = {
    "A": phase_A,
    "B": phase_B,
    "C": phase_C,
    "D0": lambda B: phase_D(B, "D0", "yabT", "ab_w_out", "x", "g_ffn0", "h_mid0", "hnT"),
    "F0a": lambda B: phase_F(B, "F0a", 0, 0, "h_mid0", "h_half", "hnT"),
    "F0b": lambda B: phase_F(B, "F0b", 0, 11, "h_half", "h1", "hnT"),
    "E": phase_E,
    "G": phase_G,
    "D1": lambda B: phase_D(B, "D1", "ycT", "c_w_out", "h1", "g_ffn1", "h_mid1", "hnT"),
    "F1a": lambda B: phase_F(B, "F1a", 1, 0, "h_mid1", "h_half", "hnT"),
    "F1b": lambda B: phase_F(B, "F1b", 1, 11, "h_half", "out", "hnT", final_g="g_final"),
}
ORDER = ["A", "B", "C", "D0", "F0a", "F0b", "E", "G", "D1", "F1a", "F1b"]


def host_consts():
    bf = ml_dtypes.bfloat16
    c = {}
    c["ident_bf"] = np.eye(128, dtype=np.float32).astype(bf)
    c["ident_f32"] = np.eye(128, dtype=np.float32)
    rot = 16
    inv = (500000.0 ** (-np.arange(0, rot, 2, dtype=np.float32) / np.float32(rot))).astype(np.float32)
    ang = (np.arange(SEQ, dtype=np.float32)[:, None] * inv[None, :]).astype(np.float32)
    c["cos128"] = np.ascontiguousarray(np.tile(np.cos(ang).astype(np.float32), (1, 16)))
    c["sin128"] = np.ascontiguousarray(np.tile(np.sin(ang).astype(np.float32), (1, 16)))
    i = np.arange(128)
    c["negmask"] = np.where(i[None, :] <= i[:, None], 0.0, -1e30).astype(np.float32)
    c["pow2"] = np.ascontiguousarray(np.broadcast_to((0.5 ** np.arange(1, NIT + 1)).astype(np.float32), (128, NIT)))
    c["tri_u"] = (i[:, None] <= i[None, :]).astype(np.float32)
    c["tri_sl"] = (i[:, None] > i[None, :]).astype(np.float32)
    return c


def host_weights(inp):
    f = lambda a: np.ascontiguousarray(np.asarray(a, dtype=np.float32))
    bc = lambda v, n: np.ascontiguousarray(np.broadcast_to(np.asarray(v, np.float32).reshape(1, -1), (128, n)))
    w = {}
    w["ab_w_in"] = f(inp["ab_w_in"][0])
    w["ab_conv_w"] = f(inp["ab_conv_w"][0].reshape(31, 512))
    prm = np.concatenate([np.asarray(inp[k][0], np.float32).reshape(4, 128).T for k in ("ab_conv_b", "ab_ln_g", "ab_ln_b")], axis=1)
    w["conv_prm"] = f(prm)
    w["ab_w_out"] = f(inp["ab_w_out"][0])
    w["c_w_in"] = f(inp["c_w_in"][0])
    w["c_gate_w"] = f(inp["c_gate_w"][0])
    w["c_gate_b_bc"] = bc(inp["c_gate_b"][0], 512)
    w["c_onorm_g_bc"] = bc(inp["c_onorm_g"][0], D)
    w["c_w_out"] = f(inp["c_w_out"][0])
    for l in range(2):
        w["ffn_w_gate%d" % l] = f(inp["ffn_w_gate"][l])
        w["ffn_w_up%d" % l] = f(inp["ffn_w_up"][l])
        w["ffn_w_down%d" % l] = f(inp["ffn_w_down"][l])
        w["g_mix%d" % l] = bc(inp["norm_mix_g"][l], D)
        w["g_ffn%d" % l] = bc(inp["norm_ffn_g"][l], D)
    w["g_final"] = bc(inp["final_norm_g"], D)
    return w


def build_program(nseq, phases, ext=None):
    B = Build(nseq, ext)
    declare(B)
    for p in phases:
        PHASES[p](B)
    return B


def kernel(**inp):
    n = 8
    nseq = 2
    x = np.asarray(inp["x"], dtype=np.float32)
    B = build_program(nseq, ORDER)
    shared = host_consts()
    shared.update(host_weights(inp))
    in_maps = []
    for c in range(n):
        m = dict(shared)
        m["x"] = np.ascontiguousarray(x[c * nseq:(c + 1) * nseq].reshape(nseq * SEQ, D))
        in_maps.append(m)
    res = run_bass_kernel_spmd(B.nc, in_maps, core_ids=list(range(n)))
    out = np.stack([np.asarray(r["out"], dtype=np.float32).reshape(nseq, SEQ, D) for r in res.results], axis=0)
    return out.reshape(16, SEQ, D)
```

```python
import contextlib
import math
import numpy as np
import ml_dtypes
import concourse.bass as bass
import concourse.mybir as mybir
from concourse.bass_utils import run_bass_kernel_spmd

F32 = mybir.dt.float32
BF16 = mybir.dt.bfloat16
ALU = mybir.AluOpType
AF = mybir.ActivationFunctionType
AX = mybir.AxisListType

ENGS = ["sync", "scalar", "vector", "gpsimd", "tensor"]
D = 1024
SEQ = 2048
NT = 16
EPS = 1e-6
DFF = 2816
NFF = 22
NIT = 16
TOPK = 256
DEBUG_STOP = 99
SAME_SYNC = True


class Sched:
    def __init__(self, nc, stack, tag=""):
        self.nc = nc
        self.stack = stack
        self.tag = tag
        self.q = {e: [] for e in ENGS}
        self.sem = {}
        self.cnt = {}
        self.waited = {e: {} for e in ENGS}
        self.lastw = {}
        self.readers = {}
        self.pe_pending = False
        self.out_deps = {}
        for e in ENGS:
            self._mksem("E_" + e)

    def _mksem(self, name):
        if name not in self.sem:
            hw_name = "%s_s%d" % (self.tag, len(self.sem))
            self.sem[name] = self.nc.alloc_semaphore(name=hw_name)
            self.cnt[name] = 0
        return name

    def op(self, eng, fn, reads=(), writes=(), dma=None, mark=True, is_out=False, force_self=False):
        deps = {}

        def add(d):
            if d[1] > deps.get(d[0], 0):
                deps[d[0]] = d[1]

        for k in reads:
            if k in self.lastw:
                add(self.lastw[k])
        for k in writes:
            if k in self.lastw:
                add(self.lastw[k])
            for s_, v_ in self.readers.get(k, {}).items():
                add((s_, v_))
        if dma is not None:
            s = self._mksem("D_" + str(dma))
            if self.cnt[s] > 0:
                add((s, self.cnt[s]))
        own = "E_" + eng
        need = []
        for s_, v_ in deps.items():
            if s_ == own and (eng == "tensor" or not (SAME_SYNC or force_self)):
                continue
            if self.waited[eng].get(s_, 0) >= v_:
                continue
            need.append((s_, v_))
        attach = None
        if eng == "tensor":
            k0 = reads[0] if len(reads) else None
            if k0 is not None and k0 in self.lastw and self.lastw[k0][0] != own:
                attach = self.lastw[k0]
        else:
            selfs = [d_ for d_ in need if d_[0] == own]
            attach = selfs[0] if selfs else (need[-1] if need else None)
        standalone = [d_ for d_ in need if attach is None or d_[0] != attach[0]]
        if attach is not None:
            for d_ in need:
                if d_[0] == attach[0] and d_[1] > attach[1]:
                    attach = d_
        for d_ in need:
            self.waited[eng][d_[0]] = max(self.waited[eng].get(d_[0], 0), d_[1])
        if attach is not None:
            self.waited[eng][attach[0]] = max(self.waited[eng].get(attach[0], 0), attach[1])
        if dma is not None:
            self.cnt[s] += 16
            dep = (s, self.cnt[s])
            self.q[eng].append(("op", fn, s, 16, standalone, attach))
        else:
            s = own
            if mark:
                self.cnt[s] += 1
                dep = (s, self.cnt[s])
                self.q[eng].append(("op", fn, s, 1, standalone, attach))
                if eng == "tensor":
                    self.pe_pending = False
            else:
                assert eng == "tensor"
                dep = (s, self.cnt[s] + 1)
                self.q[eng].append(("op", fn, None, 0, standalone, attach))
                self.pe_pending = True
        for k in writes:
            self.lastw[k] = dep
            self.readers[k] = {}
        for k in reads:
            r = self.readers.setdefault(k, {})
            if dep[1] > r.get(dep[0], 0):
                r[dep[0]] = dep[1]
        if is_out:
            if dep[1] > self.out_deps.get(dep[0], 0):
                self.out_deps[dep[0]] = dep[1]
        return dep

    def finalize(self):
        assert not self.pe_pending, "unmarked PE op at end of phase"
        tail = []
        for s_, v_ in self.out_deps.items():
            if self.waited["sync"].get(s_, 0) < v_:
                tail.append((s_, v_))
        nc = self.nc
        with nc.Block() as block:
            for eng in ENGS:
                items = self.q[eng]

                def body(e, items=items, eng=eng):
                    for it in items:
                        for (ws, wv) in it[4]:
                            e.wait_ge(self.sem[ws], wv)
                        ins = it[1](e)
                        if it[5] is not None:
                            ins._wait_ge(self.sem[it[5][0]], it[5][1])
                        if it[2] is not None:
                            ins.then_inc(self.sem[it[2]], it[3])
                    if eng == "sync":
                        for (ws, wv) in tail:
                            e.wait_ge(self.sem[ws], wv)

                getattr(block, eng)(body)


class Phase:
    def __init__(self, nc, tag):
        self.nc = nc
        self.tag = tag
        self.st = contextlib.ExitStack()
        self.st.enter_context(nc.cleanup_on_exit())
        self.S = Sched(nc, self.st, tag)

    def sb(self, name, shape, dt):
        return self.st.enter_context(self.nc.sbuf_tensor(self.tag + "_" + name, shape, dt))

    def ps(self, name, shape, dt=F32):
        return self.st.enter_context(self.nc.psum_tensor(self.tag + "_" + name, shape, dt))

    def dma(self, eng, out, in_, r=(), w=(), key=None, is_out=False):
        return self.S.op(eng, lambda e: e.dma_start(out=out, in_=in_), r, w, dma=key, is_out=is_out)

    def mm(self, out, lhsT, rhs, start, stop, r, w, mark=None):
        if mark is None:
            mark = stop
        return self.S.op("tensor", lambda e: e.matmul(out, lhsT=lhsT, rhs=rhs, start=start, stop=stop), r, w, mark=mark)

    def tr(self, out, in_, ident, r, w, mark=True):
        return self.S.op("tensor", lambda e: e.transpose(out=out, in_=in_, identity=ident), r, w, mark=mark)

    def act(self, out, in_, func, r, w, **kw):
        return self.S.op("scalar", lambda e: e.activation(out=out, in_=in_, func=func, **kw), r, w)

    def tt(self, eng, out, in0, in1, op, r, w):
        return self.S.op(eng, lambda e: e.tensor_tensor(out=out, in0=in0, in1=in1, op=op), r, w)

    def ts(self, eng, out, in0, s1, op0, r, w, s2=None, op1=None, accum_out=None):
        if op1 is None:
            return self.S.op(eng, lambda e: e.tensor_scalar(out=out, in0=in0, scalar1=s1, scalar2=None, op0=op0), r, w)
        return self.S.op(eng, lambda e: e.tensor_scalar(out=out, in0=in0, scalar1=s1, scalar2=s2, op0=op0, op1=op1,
                                                       accum_out=accum_out), r, w)

    def stt(self, out, in0, scalar, in1, op0, op1, r, w):
        return self.S.op("vector", lambda e: e.scalar_tensor_tensor(out=out, in0=in0, scalar=scalar, in1=in1,
                                                                     op0=op0, op1=op1), r, w)

    def copy(self, eng, out, in_, r, w):
        if eng == "scalar":
            return self.S.op(eng, lambda e: e.copy(out=out, in_=in_), r, w)
        return self.S.op(eng, lambda e: e.tensor_copy(out=out, in_=in_), r, w)

    def recip(self, out, in_, r, w):
        return self.S.op("vector", lambda e: e.reciprocal(out=out, in_=in_), r, w)

    def memset(self, eng, ap, val, w):
        return self.S.op(eng, lambda e: e.memset(ap, val), (), w)

    def reduce(self, out, in_, op, r, w):
        return self.S.op("vector", lambda e: e.tensor_reduce(out=out, in_=in_, axis=AX.X, op=op), r, w)

    def close(self):
        self.S.finalize()
        self.st.close()


class Build:
    def __init__(self, nseq, ext=None):
        self.nc = bass.Bass("TRN2", target_bir_lowering=False)
        self.NSEQ = nseq
        self.t = {}
        self.ext = ext or {}
        self.kinds = {}

    def dram(self, name, shape, dt, kind="Internal"):
        kind = self.ext.get(name, kind)
        self.kinds[name] = (kind, list(shape), dt)
        self.t[name] = self.nc.dram_tensor(name, list(shape), dt, kind=kind).ap()
        return self.t[name]


def rms_to_hnT(P, xt, xkey, gbc, eps_t, stat, skey, junk, hn, hkey, ps_tr, pkey, identb, dst, dkey, sl):
    P.act(junk[:], xt, AF.Square, [xkey], ["junkA", skey], accum_out=stat[:, 0:1])
    P.act(stat[:, 1:2], stat[:, 0:1], AF.Sqrt, [skey, "eps"], [skey], scale=1.0 / D, bias=eps_t[:, 0:1])
    P.recip(stat[:, 2:3], stat[:, 1:2], [skey], [skey])
    P.stt(hn, xt, stat[:, 2:3], gbc, ALU.mult, ALU.mult, [xkey, skey, "gbc"], [hkey])
    for k in range(8):
        P.tr(ps_tr[:, k, :], hn[:, k * 128:(k + 1) * 128], identb[:], [hkey, "identb"], [pkey], mark=(k == 7))
    P.copy("scalar", dst[:, 0:8, sl], ps_tr[:, 0:8, :], [pkey], [dkey])


def phase_A(B):
    nc, d, NSEQ = B.nc, B.t, B.NSEQ
    P = Phase(nc, "A")
    Wt = P.sb("Wt", [128, 8, 2376], BF16)
    Wsrc = d["ab_w_in"].rearrange("(k p) n -> p k n", p=128)
    segs = [(0, 1024, 0), (1024, 1536, 1024), (1664, 2176, 1536), (1536, 1600, 2048), (1536, 1600, 2112),
            (2176, 2240, 2176), (2176, 2240, 2240), (1600, 1664, 2304), (2240, 2248, 2368)]
    WK = []
    for i, (c0, c1, d0) in enumerate(segs):
        P.dma("gpsimd", Wt[:, :, d0:d0 + c1 - c0], Wsrc[:, :, c0:c1], w=[("W", i)], key=("W", i))
        WK.append(("W", i))
    identb = P.sb("identb", [128, 128], BF16)
    P.dma("sync", identb[:], d["ident_bf"], w=["identb"], key="identb")
    gbc = P.sb("gbc", [128, D], F32)
    P.dma("sync", gbc[:], d["g_mix0"], w=["gbc"], key="gbc")
    cos = P.sb("cos", [128, NT, 128], F32)
    sin = P.sb("sin", [128, NT, 128], F32)
    P.dma("sync", cos[:], d["cos128"].rearrange("(t p) c -> p t c", p=128), w=["cos"], key="cos")
    P.dma("sync", sin[:], d["sin128"].rearrange("(t p) c -> p t c", p=128), w=["sin"], key="sin")
    eps_t = P.sb("eps", [128, 1], F32)
    P.memset("vector", eps_t[:], EPS, ["eps"])

    xt = [P.sb("xt%d" % i, [128, D], F32) for i in range(2)]
    junk = P.sb("junk", [128, D], F32)
    stat = [P.sb("stat%d" % i, [128, 4], F32) for i in range(2)]
    hn = [P.sb("hn%d" % i, [128, D], BF16) for i in range(2)]
    hnT = [P.sb("hnT%d" % i, [128, 8, 512], BF16) for i in range(2)]
    ps_tr = [P.ps("ptr%d" % i, [128, 8, 128], BF16) for i in range(2)]
    ps_a = P.ps("psa", [128, 512])
    ps_g = P.ps("psg", [128, 512])
    ps_q = P.ps("psq", [128, 1024])
    ps_c = P.ps("psc", [128, 512])
    sig = [P.sb("sig%d" % i, [128, 512], F32) for i in range(2)]
    hc = [P.sb("hc%d" % i, [128, 512], BF16) for i in range(2)]
    tmp = [P.sb("tmp%d" % i, [128, 128], F32) for i in range(4)]
    qr = [P.sb("qr%d" % i, [128, 1024], BF16) for i in range(2)]
    kr = [P.sb("kr%d" % i, [128, 256], BF16) for i in range(2)]
    vaug = [P.sb("vaug%d" % i, [128, 256], BF16) for i in range(2)]
    iwt = [P.sb("iw%d" % i, [128, 8], F32) for i in range(2)]
    for i in range(2):
        P.memset("gpsimd", vaug[i][:, 64:192], 1.0, [("vaug", i)])
    qTg = [P.sb("qTg%d" % i, [128, 8, 512], BF16) for i in range(2)]
    kTg = [P.sb("kTg%d" % i, [128, 2, 512], BF16) for i in range(2)]
    w_scale = (8 ** -0.5) * (64 ** -0.5)

    def load_x(b, tt):
        xs = tt % 2
        r0 = b * SEQ + tt * 128
        P.dma("sync", xt[xs][:], d["x"][r0:r0 + 128, :], w=[("x", xs)], key=("x", xs))

    n_tiles = NSEQ * NT
    if DEBUG_STOP <= 1:
        P.dma("sync", d["iw"][0:128, :], iwt[0][:], r=[("iw", 0), "cos", "sin", "gbc", "identb"] + WK, key=("iw", 0), is_out=True)
        P.close()
        return
    load_x(0, 0)

    def norm_group(gi):
        b, g = gi // 4, gi % 4
        gs = gi % 2
        for t4 in range(4):
            tt = g * 4 + t4
            ti = gi * 4 + t4
            if ti + 1 < n_tiles:
                load_x((ti + 1) // NT, (ti + 1) % NT)
            xs = tt % 2
            rms_to_hnT(P, xt[xs][:], ("x", xs), gbc[:], eps_t, stat[xs], ("st", xs), junk, hn[xs][:], ("hn", xs),
                       ps_tr[0], "ptr0", identb, hnT[gs], ("hnT", gs), slice(t4 * 128, (t4 + 1) * 128))

    def mm_group(gi):
        b, g = gi // 4, gi % 4
        gs = gi % 2
        for c in range(4):
            for k in range(8):
                P.mm(ps_a[:, :], Wt[:, k, c * 128:(c + 1) * 128], hnT[gs][:, k, :], k == 0, k == 7,
                     [("hnT", gs)] + WK, ["psa"])
            for k in range(8):
                P.mm(ps_g[:, :], Wt[:, k, 512 + c * 128:512 + (c + 1) * 128], hnT[gs][:, k, :], k == 0, k == 7,
                     [("hnT", gs)] + WK, ["psg"])
            s2 = c % 2
            P.act(sig[s2][:], ps_g[:, :], AF.Sigmoid, ["psg"], [("sig", s2)])
            P.tt("vector", hc[s2][:], ps_a[:, :], sig[s2][:], ALU.mult, ["psa", ("sig", s2)], [("hc", s2)])
            P.dma("sync", d["hconvT"][b, c, :, g * 512:(g + 1) * 512], hc[s2][:], r=[("hc", s2)], key=("hc", s2), is_out=True)
        for t4 in range(4):
            tt = g * 4 + t4
            r0 = b * SEQ + tt * 128
            tok = slice(t4 * 128, (t4 + 1) * 128)
            s2 = t4 % 2
            for k in range(8):
                P.mm(ps_q[:, 0:512], hnT[gs][:, k, tok], Wt[:, k, 1024:1536], k == 0, k == 7, [("hnT", gs)] + WK, ["psq"], mark=False)
            for k in range(8):
                P.mm(ps_q[:, 512:1024], hnT[gs][:, k, tok], Wt[:, k, 1536:2048], k == 0, k == 7, [("hnT", gs)] + WK, ["psq"], mark=False)
            for k in range(8):
                P.mm(ps_c[:, 0:328], hnT[gs][:, k, tok], Wt[:, k, 2048:2376], k == 0, k == 7, [("hnT", gs)] + WK, ["psq"])
            for (src, nh, dst, dkey) in ((ps_q[:, 0:512], 8, qr[s2][:, 0:512], ("qr", s2)), (ps_q[:, 512:1024], 8, qr[s2][:, 512:1024], ("qr", s2)),
                                         (ps_c[:, 0:256], 4, kr[s2][:, 0:256], ("kr", s2))):
                sv = src.rearrange("p (h e) -> p h e", e=64)
                dv = dst.rearrange("p (h e) -> p h e", e=64)
                x1, x2 = sv[:, :, 0:8], sv[:, :, 8:16]
                cv = cos[:, tt, 0:nh * 8].rearrange("p (h e) -> p h e", e=8)
                sn = sin[:, tt, 0:nh * 8].rearrange("p (h e) -> p h e", e=8)
                tv = [tmp[i][:, 0:nh * 8].rearrange("p (h e) -> p h e", e=8) for i in range(4)]
                P.tt("vector", tv[0], x1, cv, ALU.mult, ["psq", "cos"], ["tmp0"])
                P.tt("vector", tv[1], x2, sn, ALU.mult, ["psq", "sin"], ["tmp1"])
                P.tt("vector", dv[:, :, 0:8], tv[0], tv[1], ALU.subtract, ["tmp0", "tmp1"], [dkey])
                P.tt("vector", tv[2], x2, cv, ALU.mult, ["psq", "cos"], ["tmp2"])
                P.tt("vector", tv[3], x1, sn, ALU.mult, ["psq", "sin"], ["tmp3"])
                P.tt("vector", dv[:, :, 8:16], tv[2], tv[3], ALU.add, ["tmp2", "tmp3"], [dkey])
                P.copy("scalar", dv[:, :, 16:64], sv[:, :, 16:64], ["psq"], [dkey])
            P.copy("scalar", vaug[s2][:, 0:64], ps_c[:, 256:320], ["psq"], [("vaug", s2)])
            P.copy("scalar", vaug[s2][:, 192:256], ps_c[:, 256:320], ["psq"], [("vaug", s2)])
            P.ts("vector", iwt[s2][:], ps_c[:, 320:328], w_scale, ALU.mult, ["psq"], [("iw", s2)])
            P.dma("sync", d["vaug"][r0:r0 + 128, :], vaug[s2][:], r=[("vaug", s2)], key=("vaug", s2), is_out=True)
            P.dma("sync", d["iw"][r0:r0 + 128, :], iwt[s2][:], r=[("iw", s2)], key=("iw", s2), is_out=True)
            for k in range(8):
                P.tr(ps_tr[1][:, k, :], qr[s2][:, k * 128:(k + 1) * 128], identb[:], [("qr", s2), "identb"], ["ptr1"], mark=(k == 7))
            P.copy("scalar", qTg[gs][:, 0:8, tok], ps_tr[1][:, 0:8, :], ["ptr1"], [("qTg", gs)])
            for k in range(2):
                P.tr(ps_tr[1][:, k, :], kr[s2][:, k * 128:(k + 1) * 128], identb[:], [("kr", s2), "identb"], ["ptr1"], mark=(k == 1))
            P.copy("scalar", kTg[gs][:, 0:2, tok], ps_tr[1][:, 0:2, :], ["ptr1"], [("kTg", gs)])
        gsl = slice(g * 512, (g + 1) * 512)
        P.dma("sync", d["qT"][b, :, :, gsl].rearrange("c p t -> p c t"), qTg[gs][:, 0:4, :], r=[("qTg", gs)], key=("qTg", gs), is_out=True)
        P.dma("sync", d["iqT"][b, :, :, gsl].rearrange("c p t -> p c t"), qTg[gs][:, 4:8, :], r=[("qTg", gs)], key=("iqTg", gs), is_out=True)
        P.dma("sync", d["kkT"][b, :, :, gsl].rearrange("c p t -> p c t"), kTg[gs][:, :, :], r=[("kTg", gs)], key=("kTg", gs), is_out=True)

    ngr = NSEQ * 4
    norm_group(0)
    for gi in range(ngr):
        if gi + 1 < ngr:
            norm_group(gi + 1)
        mm_group(gi)
    P.close()


def phase_B(B):
    nc, d, NSEQ = B.nc, B.t, B.NSEQ
    P = Phase(nc, "B")
    identb = P.sb("identb", [128, 128], BF16)
    P.dma("sync", identb[:], d["ident_bf"], w=["identb"], key="identb")
    identf = P.sb("identf", [128, 128], F32)
    P.dma("sync", identf[:], d["ident_f32"], w=["identf"], key="identf")
    ones = P.sb("ones", [128, 128], F32)
    P.memset("vector", ones[:], 1.0, ["ones"])
    eps_t = P.sb("eps", [128, 1], F32)
    P.memset("vector", eps_t[:], EPS, ["eps"])
    cw = P.sb("cw", [31, 512], F32)
    P.dma("sync", cw[:], d["ab_conv_w"], w=["cw"], key="cw")
    prm = P.sb("prm", [128, 12], F32)
    P.dma("sync", prm[:], d["conv_prm"], w=["prm"], key="prm")
    ps_w = P.ps("psw", [128, 4, 32])
    for c in range(4):
        P.tr(ps_w[:, c, 0:31], cw[0:31, c * 128:(c + 1) * 128], identf[0:31, 0:31], ["cw", "identf"], ["psw"], mark=(c == 3))
    wT = P.sb("wT", [128, 4, 32], F32)
    P.copy("vector", wT[:, :, 0:31], ps_w[:, :, 0:31], ["psw"], ["wT"])
    diag = P.sb("diag", [128, 124, 128], BF16)
    for c in range(4):
        for j in range(31):
            i = c * 31 + j
            P.ts("vector" if i % 2 == 0 else "gpsimd", diag[:, i, :], identb[:], wT[:, c, j:j + 1], ALU.mult,
                 ["identb", "wT"], [("diag", c)])
    hbuf = P.sb("hbuf", [128, 4, 30 + SEQ], BF16)
    P.memset("gpsimd", hbuf[:, :, 0:30], 0.0, ["hbuf"])
    ps_c = [P.ps("psc%d" % i, [128, 512]) for i in range(2)]
    ps_s1 = P.ps("ps1", [128, 512])
    ps_s2 = P.ps("ps2", [128, 512])
    hcs = P.sb("hcs", [128, 4, 512], F32)
    sqs = P.sb("sqs", [128, 4, 512], F32)
    m = P.sb("m", [128, 512], F32)
    msq = P.sb("msq", [128, 512], F32)
    var = P.sb("var", [128, 512], F32)
    rstd = P.sb("rstd", [128, 512], F32)
    z = [P.sb("z%d" % i, [128, 512], F32) for i in range(2)]
    yst = [P.sb("yst%d" % i, [128, 4, 512], BF16) for i in range(2)]
    for b in range(NSEQ):
        for c in range(4):
            P.dma("sync", hbuf[:, c, 30:30 + SEQ], d["hconvT"][b, c, :, :], w=["hbuf"], key=("hbuf", c))
        for g in range(4):
            ys = g % 2
            for c in range(4):
                pc = ps_c[c % 2]
                pk = ("psc", c % 2)
                for j in range(31):
                    P.mm(pc[:, :], diag[:, c * 31 + j, :], hbuf[:, c, g * 512 + j:g * 512 + j + 512], j == 0, j == 30,
                         [("diag", c), "hbuf"], [pk])
                P.act(hcs[:, c, :], pc[:, :], AF.Identity, [pk, "prm"], [("hcs", c)], bias=prm[:, c:c + 1], scale=1.0)
                P.act(sqs[:, c, :], pc[:, :], AF.Square, [pk, "prm"], [("sqs", c)], bias=prm[:, c:c + 1], scale=1.0)
            for c in range(4):
                P.mm(ps_s1[:, :], ones[:], hcs[:, c, :], c == 0, c == 3, [("hcs", c), "ones"], ["ps1"])
            for c in range(4):
                P.mm(ps_s2[:, :], ones[:], sqs[:, c, :], c == 0, c == 3, [("sqs", c), "ones"], ["ps2"])
            P.act(m[:], ps_s1[:, :], AF.Copy, ["ps1"], ["m"], scale=1.0 / 512)
            P.tt("gpsimd", msq[:], m[:], m[:], ALU.mult, ["m"], ["msq"])
            P.stt(var[:], ps_s2[:, :], 1.0 / 512, msq[:], ALU.mult, ALU.subtract, ["ps2", "msq"], ["var"])
            P.act(var[:], var[:], AF.Sqrt, ["var", "eps"], ["var"], bias=eps_t[:, 0:1], scale=1.0)
            P.recip(rstd[:], var[:], ["var"], ["rstd"])
            for c in range(4):
                zz = z[c % 2]
                zk = ("z", c % 2)
                P.tt("gpsimd", zz[:], hcs[:, c, :], m[:], ALU.subtract, [("hcs", c), "m"], [zk])
                P.tt("vector", zz[:], zz[:], rstd[:], ALU.mult, [zk, "rstd"], [zk])
                P.act(yst[ys][:, c, :], zz[:], AF.Silu, [zk, "prm"], [("yst", ys)], scale=prm[:, 4 + c:5 + c], bias=prm[:, 8 + c:9 + c])
            P.dma("sync", d["yabT"][b, 0:4, :, g * 512:(g + 1) * 512].rearrange("c p t -> p c t"), yst[ys][:],
                  r=[("yst", ys)], key=("yst", ys), is_out=True)
    P.close()


def phase_C(B):
    nc, d, NSEQ = B.nc, B.t, B.NSEQ
    P = Phase(nc, "C")
    identb = P.sb("identb", [128, 128], BF16)
    P.dma("sync", identb[:], d["ident_bf"], w=["identb"], key="identb")
    negm = P.sb("negm", [128, 128], F32)
    P.dma("sync", negm[:], d["negmask"], w=["negm"], key="negm")
    pow2 = P.sb("pow2", [128, NIT], F32)
    P.dma("sync", pow2[:], d["pow2"], w=["pow2"], key="pow2")
    thr0 = P.sb("thr0", [128, 1], F32)
    P.memset("vector", thr0[:], -1e29, ["thr0"])
    bigI = P.sb("bigI", [128, 128], BF16)
    P.ts("vector", bigI[:], identb[:], 30000.0, ALU.mult, ["identb"], ["bigI"])
    qT = [P.sb("qT%d" % i, [128, 4, SEQ], BF16) for i in range(2)]
    iqT = [P.sb("iqT%d" % i, [128, 4, SEQ], BF16) for i in range(2)]
    kkT = [P.sb("kkT%d" % i, [128, 2, SEQ], BF16) for i in range(2)]
    vaug = [P.sb("vaug%d" % i, [128, NT, 256], BF16) for i in range(2)]
    iw = [P.sb("iw%d" % i, [128, NT, 8], F32) for i in range(2)]
    ybg = [P.sb("ybg%d" % i, [128, 512], BF16) for i in range(2)]
    maskT = [P.sb("maskT%d" % i, [128, NT, 512], BF16) for i in range(2)]
    score = [P.sb("score%d" % i, [128, SEQ], F32) for i in range(2)]
    junk = P.sb("junk", [128, SEQ], BF16)
    rl = [P.sb("rl%d" % i, [128, 512], F32) for i in range(4)]
    mk = [P.sb("mk%d" % i, [128, SEQ], BF16) for i in range(2)]
    st = [P.sb("st%d" % i, [128, 8 + NIT], F32) for i in range(2)]
    pe = [P.sb("pe%d" % i, [128, 512], BF16) for i in range(3)]
    rc = [P.sb("rc%d" % i, [128, 512], F32) for i in range(2)]
    ps_lg = [P.ps("plg%d" % i, [128, 512]) for i in range(3)]
    ps_tr = [P.ps("ptr%d" % i, [128, 8, 128], BF16) for i in range(1)]
    ps_s = [P.ps("pss%d" % i, [128, 512]) for i in range(2)]
    ps_o = [P.ps("pso%d" % i, [128, 512]) for i in range(2)]
    att_scale = 64 ** -0.5
    ctr = {"lg": 0, "s": 0, "o": 0, "tr": 0, "yb": 0, "g": 0}

    mslot = {}

    def load_seq(b):
        p = b % 2
        rs = slice(b * SEQ, (b + 1) * SEQ)
        P.dma("sync", iqT[p][:], d["iqT"][b].rearrange("c p t -> p c t"), w=[("iqT", p)], key=("iqT", p))
        P.dma("sync", kkT[p][:], d["kkT"][b].rearrange("c p t -> p c t"), w=[("kkT", p)], key=("kkT", p))
        P.dma("sync", iw[p][:], d["iw"][rs, :].rearrange("(t p) c -> p t c", p=128), w=[("iw", p)], key=("iw", p))
        P.dma("sync", qT[p][:], d["qT"][b].rearrange("c p t -> p c t"), w=[("qT", p)], key=("qT", p))
        P.dma("sync", vaug[p][:], d["vaug"][rs, :].rearrange("(t p) c -> p t c", p=128), w=[("vaug", p)], key=("vaug", p))

    junk2 = [junk, P.sb("junkb", [128, SEQ], BF16)]

    def score_pair(b, G, q4s):
        p = b % 2
        mG = mslot[(b, G)]
        info = []
        for q4 in q4s:
            qb = 4 * G + q4
            nk = 128 * (qb + 1)
            nkb = (nk + 511) // 512
            s2 = qb % 2
            sc = score[s2]
            sks = [("score", s2, kb) for kb in range(nkb)]
            qsl = slice(qb * 128, (qb + 1) * 128)
            for h in range(8):
                c, base = h // 2, 64 * (h % 2)
                for kb in range(nkb):
                    n = min(512, nk - kb * 512)
                    r = ctr["lg"] % 3
                    r4 = ctr["lg"] % 4
                    ctr["lg"] += 1
                    P.mm(ps_lg[r][:, 0:n], iqT[p][base:base + 64, c, qsl], kkT[p][base:base + 64, 1, kb * 512:kb * 512 + n],
                         True, True, [("iqT", p), ("kkT", p)], [("plg", r)])
                    P.act(rl[r4][:, 0:n], ps_lg[r][:, 0:n], AF.Relu, [("plg", r)], [("rl", r4)])
                    dst = sc[:, kb * 512:kb * 512 + n]
                    if h == 0:
                        P.ts("vector", dst, rl[r4][:, 0:n], iw[p][:, qb, 0:1], ALU.mult, [("rl", r4), ("iw", p)], [sks[kb]])
                    else:
                        P.stt(dst, rl[r4][:, 0:n], iw[p][:, qb, h:h + 1], dst, ALU.mult, ALU.add, [("rl", r4), ("iw", p), sks[kb]], [sks[kb]])
            info.append((q4, qb, nk, s2, sc, sks, qsl))
        for (q4, qb, nk, s2, sc, sks, qsl) in info:
            stt_ = st[s2]
            stk = ("st", s2)
            if qb >= 2:
                P.reduce(stt_[:, 0:1], sc[:, 0:nk], ALU.max, sks, [stk])
                P.reduce(stt_[:, 1:2], sc[:, 0:nk], ALU.min, sks, [(stk, "lo")])
            P.tt("vector", sc[:, qsl], sc[:, qsl], negm[:], ALU.add, [sks[qb // 4], "negm"], [sks[qb // 4]])
            if qb >= 2:
                P.tt("vector", stt_[:, 2:3], stt_[:, 0:1], stt_[:, 1:2], ALU.subtract, [stk, (stk, "lo")], [stk])
                P.ts("vector", stt_[:, 8:8 + NIT], pow2[:], stt_[:, 2:3], ALU.mult, ["pow2", stk], [(stk, "steps")])
        bis = [x for x in info if x[1] >= 2]
        for i in range(NIT):
            for (q4, qb, nk, s2, sc, sks, qsl) in bis:
                stt_, stk = st[s2], ("st", s2)
                P.tt("vector", stt_[:, 3:4], stt_[:, 1:2], stt_[:, 8 + i:9 + i], ALU.add, [(stk, "lo"), (stk, "steps")], [(stk, "mid")])
            for (q4, qb, nk, s2, sc, sks, qsl) in bis:
                stt_, stk = st[s2], ("st", s2)
                P.ts("vector", junk2[s2][:, 0:nk], sc[:, 0:nk], stt_[:, 3:4], ALU.is_ge, sks + [(stk, "mid")], [("junk", s2), (stk, "cnt")],
                     s2=0.0, op1=ALU.add, accum_out=stt_[:, 4:5])
            for (q4, qb, nk, s2, sc, sks, qsl) in bis:
                stt_, stk = st[s2], ("st", s2)
                P.stt(stt_[:, 5:6], stt_[:, 4:5], TOPK - 0.5, stt_[:, 8 + i:9 + i], ALU.is_ge, ALU.mult, [(stk, "cnt"), (stk, "steps")], [(stk, "sel")])
            for (q4, qb, nk, s2, sc, sks, qsl) in bis:
                stt_, stk = st[s2], ("st", s2)
                P.tt("vector", stt_[:, 1:2], stt_[:, 1:2], stt_[:, 5:6], ALU.add, [(stk, "lo"), (stk, "sel")], [(stk, "lo")])
        for (q4, qb, nk, s2, sc, sks, qsl) in info:
            stt_, stk = st[s2], ("st", s2)
            thr = stt_[:, 1:2] if qb >= 2 else thr0[:, 0:1]
            P.ts("vector", mk[s2][:, 0:nk], sc[:, 0:nk], thr, ALU.is_ge, sks + [(stk, "lo"), "thr0"], [("mk", s2)],
                 s2=-1.0, op1=ALU.add)
            kt = 0
            while kt <= qb:
                n = min(8, qb + 1 - kt)
                r = 0
                for j in range(n):
                    P.tr(ps_tr[r][:, j, :], mk[s2][:, (kt + j) * 128:(kt + j + 1) * 128], identb[:], [("mk", s2), "identb"],
                         [("ptr", r)], mark=(j == n - 1))
                P.copy("scalar", maskT[mG][:, kt:kt + n, q4 * 128:(q4 + 1) * 128], ps_tr[r][:, 0:n, :], [("ptr", r)], [("maskT", mG)])
                kt += n

    def attn_chunk(b, G, c):
        p = b % 2
        mG = mslot[(b, G)]
        nkt = 4 * G + 4
        gsl0 = G * 512
        ys = ctr["yb"] % 2
        ctr["yb"] += 1
        for e in range(2):
            base = 64 * e
            o = ctr["o"] % 2
            ctr["o"] += 1
            for kt in range(nkt):
                q0 = max(0, kt - 4 * G) * 128
                N = 512 - q0
                r = ctr["s"] % 2
                ctr["s"] += 1
                P.mm(ps_s[r][:, 0:N], kkT[p][base:base + 64, 0, kt * 128:(kt + 1) * 128], qT[p][base:base + 64, c, gsl0 + q0:gsl0 + 512],
                     True, False, [("kkT", p), ("qT", p)], [("pss", r)], mark=False)
                P.mm(ps_s[r][:, 0:N], bigI[:], maskT[mG][:, kt, q0:512], False, True, ["bigI", ("maskT", mG)], [("pss", r)])
                r3 = ctr["s"] % 3
                P.act(pe[r3][:, 0:N], ps_s[r][:, 0:N], AF.Exp, [("pss", r)], [("pe", r3)], scale=att_scale)
                P.mm(ps_o[o][:, q0:512], vaug[p][:, kt, e * 128:(e + 1) * 128], pe[r3][:, 0:N], kt == 0, kt == nkt - 1,
                     [("pe", r3), ("vaug", p)], [("pso", o)])
            so, ss_ = (slice(0, 64), slice(64, 128)) if e == 0 else (slice(64, 128), slice(0, 64))
            P.act(rc[o][ss_, :], ps_o[o][ss_, :], AF.Ln, [("pso", o)], [("rc", o)])
            P.act(rc[o][ss_, :], rc[o][ss_, :], AF.Exp, [("rc", o)], [("rc", o)], scale=-1.0)
            P.tt("vector", ybg[ys][so, :], ps_o[o][so, :], rc[o][ss_, :], ALU.mult, [("pso", o), ("rc", o)], [("ybg", ys)])
        P.dma("sync", d["yabT"][b, 4 + c, :, gsl0:gsl0 + 512], ybg[ys][:], r=[("ybg", ys)], key=("ybg", ys), is_out=True)

    groups = [(b, G) for G in range(4) for b in range(NSEQ)]
    for b in range(NSEQ):
        load_seq(b)
    for gi_, (b, G) in enumerate(groups):
        mslot[(b, G)] = gi_ % 2
        for pr in range(2):
            score_pair(b, G, (2 * pr, 2 * pr + 1))
            if gi_ >= 1:
                pb, pG = groups[gi_ - 1]
                attn_chunk(pb, pG, 2 * pr)
                attn_chunk(pb, pG, 2 * pr + 1)
    pb, pG = groups[-1]
    for c in range(4):
        attn_chunk(pb, pG, c)
    P.close()


def phase_D(B, tag, yT, wname, resid, gname, hmid, hnT_name):
    nc, d, NSEQ = B.nc, B.t, B.NSEQ
    P = Phase(nc, tag)
    Wo = P.sb("Wo", [128, 8, D], BF16)
    P.dma("gpsimd", Wo[:], d[wname].rearrange("(k p) n -> p k n", p=128), w=["Wo"], key="Wo")
    identb = P.sb("identb", [128, 128], BF16)
    P.dma("sync", identb[:], d["ident_bf"], w=["identb"], key="identb")
    gbc = P.sb("gbc", [128, D], F32)
    P.dma("sync", gbc[:], d[gname], w=["gbc"], key="gbc")
    eps_t = P.sb("eps", [128, 1], F32)
    P.memset("vector", eps_t[:], EPS, ["eps"])
    yt = [P.sb("yt%d" % i, [128, 8, 512], BF16) for i in range(2)]
    xt = [P.sb("xt%d" % i, [128, D], F32) for i in range(2)]
    hm = [P.sb("hm%d" % i, [128, D], F32) for i in range(2)]
    junk = P.sb("junk", [128, D], F32)
    stat = [P.sb("stat%d" % i, [128, 4], F32) for i in range(2)]
    hn = [P.sb("hn%d" % i, [128, D], BF16) for i in range(2)]
    hnTg = [P.sb("hnTg%d" % i, [128, 8, 512], BF16) for i in range(2)]
    ps_o = [P.ps("pso%d" % i, [128, 1024]) for i in range(2)]
    ps_tr = [P.ps("ptr%d" % i, [128, 8, 128], BF16) for i in range(2)]
    ngr = NSEQ * 4

    def load_y(gi):
        b, g = gi // 4, gi % 4
        P.dma("sync", yt[gi % 2][:], d[yT][b, :, :, g * 512:(g + 1) * 512].rearrange("c p t -> p c t"), w=[("yt", gi % 2)], key=("yt", gi % 2))

    load_y(0)

    def outproj(ti):
        gi, t4 = ti // 4, ti % 4
        b, g = gi // 4, gi % 4
        gs = gi % 2
        if t4 == 0 and gi + 1 < ngr:
            load_y(gi + 1)
        tt = g * 4 + t4
        r0 = b * SEQ + tt * 128
        tok = slice(t4 * 128, (t4 + 1) * 128)
        s2 = ti % 2
        P.dma("sync", xt[s2][:], d[resid][r0:r0 + 128, :], w=[("x", s2)], key=("x", s2))
        for half in range(2):
            for c in range(8):
                P.mm(ps_o[s2][:, half * 512:(half + 1) * 512], yt[gs][:, c, tok], Wo[:, c, half * 512:(half + 1) * 512],
                     c == 0, c == 7, [("yt", gs), "Wo"], [("pso", s2)], mark=(c == 7 and half == 1))

    def post(ti):
        gi, t4 = ti // 4, ti % 4
        b, g = gi // 4, gi % 4
        gs = gi % 2
        tt = g * 4 + t4
        r0 = b * SEQ + tt * 128
        tok = slice(t4 * 128, (t4 + 1) * 128)
        s2 = ti % 2
        for half in range(2):
            hs_ = slice(half * 512, (half + 1) * 512)
            P.tt("vector", hm[s2][:, hs_], ps_o[s2][:, hs_], xt[s2][:, hs_], ALU.add, [("pso", s2), ("x", s2)], [("hm", s2)])
        P.dma("sync", d[hmid][r0:r0 + 128, :], hm[s2][:], r=[("hm", s2)], key=("hm", s2), is_out=True)
        rms_to_hnT(P, hm[s2][:], ("hm", s2), gbc[:], eps_t, stat[s2], ("st", s2), junk, hn[s2][:], ("hn", s2),
                   ps_tr[s2], ("ptr", s2), identb, hnTg[gs], ("hnTg", gs), tok)
        if t4 == 3:
            P.dma("sync", d[hnT_name][:, :, gi * 512:(gi + 1) * 512].rearrange("c p t -> p c t"), hnTg[gs][:], r=[("hnTg", gs)],
                  key=("hnTg", gs), is_out=True)

    ntl = ngr * 4
    outproj(0)
    for ti in range(ntl):
        if ti + 1 < ntl:
            outproj(ti + 1)
        post(ti)
    P.close()


def phase_F(B, tag, layer, f0, h_in, h_out, hnT_name, final_g=None):
    nc, d, NSEQ = B.nc, B.t, B.NSEQ
    P = Phase(nc, tag)
    nf = 11
    Wg = P.sb("Wg", [128, 8, nf * 128], BF16)
    Wu = P.sb("Wu", [128, 8, nf * 128], BF16)
    Wd = P.sb("Wd", [128, nf, D], BF16)
    cs = slice(f0 * 128, (f0 + nf) * 128)
    P.dma("gpsimd", Wg[:], d["ffn_w_gate%d" % layer].rearrange("(k p) n -> p k n", p=128)[:, :, cs], w=["Wg"], key="Wg")
    P.dma("gpsimd", Wu[:], d["ffn_w_up%d" % layer].rearrange("(k p) n -> p k n", p=128)[:, :, cs], w=["Wu"], key="Wu")
    P.dma("gpsimd", Wd[:], d["ffn_w_down%d" % layer].rearrange("(f p) n -> p f n", p=128)[:, f0:f0 + nf, :], w=["Wd"], key="Wd")
    if final_g is not None:
        gbc = P.sb("gbc", [128, D], F32)
        P.dma("sync", gbc[:], d[final_g], w=["gbc"], key="gbc")
        eps_t = P.sb("eps", [128, 1], F32)
        P.memset("vector", eps_t[:], EPS, ["eps"])
        junk = P.sb("junk", [128, D], F32)
        stat = [P.sb("stat%d" % i, [128, 4], F32) for i in range(2)]
    hnTg = [P.sb("hnTg%d" % i, [128, 8, 512], BF16) for i in range(2)]
    actT = [P.sb("actT%d" % i, [128, nf, 512], BF16) for i in range(2)]
    sg = [P.sb("sg%d" % i, [128, 512], F32) for i in range(2)]
    hin = [P.sb("hin%d" % i, [128, D], F32) for i in range(2)]
    hout = [P.sb("hout%d" % i, [128, D], F32) for i in range(2)]
    ps_g = [P.ps("psg%d" % i, [128, 512]) for i in range(2)]
    ps_u = [P.ps("psu%d" % i, [128, 512]) for i in range(2)]
    ps_d = [P.ps("psd%d" % i, [128, 1024]) for i in range(2)]
    ngr = NSEQ * 4

    def load_h(gi):
        P.dma("sync", hnTg[gi % 2][:], d[hnT_name][:, :, gi * 512:(gi + 1) * 512].rearrange("c p t -> p c t"),
              w=[("hnTg", gi % 2)], key=("hnTg", gi % 2))

    load_h(0)
    for gi in range(ngr):
        gs = gi % 2
        if gi + 1 < ngr:
            load_h(gi + 1)
        for f in range(nf):
            s2 = f % 2
            for k in range(8):
                P.mm(ps_g[s2][:, :], Wg[:, k, f * 128:(f + 1) * 128], hnTg[gs][:, k, :], k == 0, k == 7, [("hnTg", gs), "Wg"], [("psg", s2)])
            for k in range(8):
                P.mm(ps_u[s2][:, :], Wu[:, k, f * 128:(f + 1) * 128], hnTg[gs][:, k, :], k == 0, k == 7, [("hnTg", gs), "Wu"], [("psu", s2)])
            P.act(sg[s2][:], ps_g[s2][:, :], AF.Silu, [("psg", s2)], [("sg", s2)])
            P.tt("vector", actT[gs][:, f, :], ps_u[s2][:, :], sg[s2][:], ALU.mult, [("psu", s2), ("sg", s2)], [("actT", gs)])
        for t4 in range(4):
            r0 = gi * 512 + t4 * 128
            tok = slice(t4 * 128, (t4 + 1) * 128)
            s2 = t4 % 2
            P.dma("sync", hin[s2][:], d[h_in][r0:r0 + 128, :], w=[("hin", s2)], key=("hin", s2))
            for half in range(2):
                for f in range(nf):
                    P.mm(ps_d[s2][:, half * 512:(half + 1) * 512], actT[gs][:, f, tok], Wd[:, f, half * 512:(half + 1) * 512],
                         f == 0, f == nf - 1, [("actT", gs), "Wd"], [("psd", s2)], mark=(f == nf - 1 and half == 1))
            for half in range(2):
                hs_ = slice(half * 512, (half + 1) * 512)
                P.tt("vector", hout[s2][:, hs_], ps_d[s2][:, hs_], hin[s2][:, hs_], ALU.add, [("psd", s2), ("hin", s2)], [("hout", s2)])
            if final_g is not None:
                sk = ("st", s2)
                P.act(junk[:], hout[s2][:], AF.Square, [("hout", s2)], ["junkA", sk], accum_out=stat[s2][:, 0:1])
                P.act(stat[s2][:, 1:2], stat[s2][:, 0:1], AF.Sqrt, [sk], [sk], scale=1.0 / D, bias=eps_t[:, 0:1])
                P.recip(stat[s2][:, 2:3], stat[s2][:, 1:2], [sk], [sk])
                P.stt(hout[s2][:], hout[s2][:], stat[s2][:, 2:3], gbc[:], ALU.mult, ALU.mult, [("hout", s2), sk, "gbc"], [("hout", s2)])
            P.dma("sync", d[h_out][r0:r0 + 128, :], hout[s2][:], r=[("hout", s2)], key=("hout", s2), is_out=True)
    P.close()


def phase_E(B):
    nc, d, NSEQ = B.nc, B.t, B.NSEQ
    P = Phase(nc, "E")
    Wc = P.sb("Wc", [128, 8, 3104], BF16)
    Wsrc = d["c_w_in"].rearrange("(k p) n -> p k n", p=128)
    WK = []
    P.memset("gpsimd", Wc[:, :, 3088:3104], 0.0, [("W", 2)])
    for i, (c0, c1) in enumerate(((0, 1024), (1024, 2048), (2048, 3088))):
        P.dma("gpsimd", Wc[:, :, c0:c1], Wsrc[:, :, c0:c1], w=[("W", i)], key=("W", i))
        WK.append(("W", i))
    identb = P.sb("identb", [128, 128], BF16)
    P.dma("sync", identb[:], d["ident_bf"], w=["identb"], key="identb")
    gbc = P.sb("gbc", [128, D], F32)
    P.dma("sync", gbc[:], d["g_mix1"], w=["gbc"], key="gbc")
    g2w = P.sb("g2w", [128, 512], F32)
    P.memset("vector", g2w[:], 0.0, ["g2w"])
    P.dma("sync", g2w[0:16, :], d["c_gate_w"], w=["g2w"], key="g2w")
    g2b = P.sb("g2b", [128, 512], F32)
    P.dma("sync", g2b[:], d["c_gate_b_bc"], w=["g2b"], key="g2b")
    eps_t = P.sb("eps", [128, 1], F32)
    P.memset("vector", eps_t[:], EPS, ["eps"])
    one_t = P.sb("one", [128, 1], F32)
    P.memset("vector", one_t[:], 1.0, ["one"])
    xt = [P.sb("xt%d" % i, [128, D], F32) for i in range(2)]
    junk = P.sb("junk", [128, D], F32)
    stat = [P.sb("stat%d" % i, [128, 4], F32) for i in range(2)]
    hn = [P.sb("hn%d" % i, [128, D], BF16) for i in range(2)]
    hnT = [P.sb("hnT%d" % i, [128, 8, 512], BF16) for i in range(2)]
    qkT = [P.sb("qkT%d" % i, [128, 8, 512], BF16) for i in range(2)]
    glrT = [P.sb("glrT%d" % i, [128, 512], F32) for i in range(2)]
    for i in range(2):
        P.memset("vector", glrT[i][:], 0.0, [("glrT", i)])
    kst = [P.sb("kst%d" % i, [128, 512], BF16) for i in range(2)]
    vst = [P.sb("vst%d" % i, [128, 1024], BF16) for i in range(2)]
    rst = [P.sb("rst%d" % i, [128, 1024], F32) for i in range(2)]
    lat = [P.sb("lat%d" % i, [128, 512], F32) for i in range(2)]
    lt1 = P.sb("lt1", [128, 512], F32)
    lt2 = P.sb("lt2", [128, 512], F32)
    ps_tr = P.ps("ptr", [128, 8, 128], BF16)
    ps_f = [P.ps("psf%d" % i, [128, 512]) for i in range(2)]
    ps_t = [P.ps("pst%d" % i, [128, 512]) for i in range(3)]
    ps_l = P.ps("psl", [128, 512])
    n_t = 0
    n_f = 0
    n_tiles = NSEQ * NT

    def load_x(ti):
        P.dma("sync", xt[ti % 2][:], d["h1"][ti * 128:(ti + 1) * 128, :], w=[("x", ti % 2)], key=("x", ti % 2))

    load_x(0)

    def norm_group(gi):
        gs = gi % 2
        for t4 in range(4):
            ti = gi * 4 + t4
            if ti + 1 < n_tiles:
                load_x(ti + 1)
            xs = ti % 2
            rms_to_hnT(P, xt[xs][:], ("x", xs), gbc[:], eps_t, stat[xs], ("st", xs), junk, hn[xs][:], ("hn", xs),
                       ps_tr, "ptr", identb, hnT[gs], ("hnT", gs), slice(t4 * 128, (t4 + 1) * 128))

    def mm_group(gi):
        nonlocal n_t, n_f
        b, g = gi // 4, gi % 4
        gs = gi % 2
        for oc in range(8):
            r = n_f % 2
            n_f += 1
            for k in range(8):
                P.mm(ps_f[r][:, :], Wc[:, k, oc * 128:(oc + 1) * 128], hnT[gs][:, k, :], k == 0, k == 7, [("hnT", gs)] + WK, [("psf", r)])
            if oc < 4:
                P.act(qkT[gs][:, oc, :], ps_f[r][:, :], AF.Copy, [("psf", r)], [("qkT", gs)], scale=128 ** -0.5)
            else:
                P.copy("vector", qkT[gs][:, oc, :], ps_f[r][:, :], [("psf", r)], [("qkT", gs)])
        r = n_f % 2
        n_f += 1
        for k in range(8):
            P.mm(ps_f[r][0:32, :], Wc[:, k, 3072:3104], hnT[gs][:, k, :], k == 0, k == 7, [("hnT", gs)] + WK, [("psf", r)])
        P.copy("vector", glrT[gs][0:32, :], ps_f[r][0:32, :], [("psf", r)], [("glrT", gs)])
        gsl = slice(g * 512, (g + 1) * 512)
        P.dma("sync", d["c_qT"][b, :, :, gsl].rearrange("c p t -> p c t"), qkT[gs][:, 0:4, :], r=[("qkT", gs)], key=("qTo", gs), is_out=True)
        P.dma("sync", d["c_kT"][b, :, :, gsl].rearrange("c p t -> p c t"), qkT[gs][:, 4:8, :], r=[("qkT", gs)], key=("kTo", gs), is_out=True)
        for t4 in range(4):
            r0 = gi * 512 + t4 * 128
            tok = slice(t4 * 128, (t4 + 1) * 128)
            s2 = t4 % 2
            jobs = [(512, 1024, kst[s2][:, :], ("kst", s2), "k"),
                    (1024, 1536, vst[s2][:, 0:512], ("vst", s2), "v"), (1536, 2048, vst[s2][:, 512:1024], ("vst", s2), "v"),
                    (2048, 2560, rst[s2][:, 0:512], ("rst", s2), "r"), (2560, 3072, rst[s2][:, 512:1024], ("rst", s2), "r")]
            for (c0, c1, dst, dk, kind) in jobs:
                r = n_t % 3
                n_t += 1
                for k in range(8):
                    P.mm(ps_t[r][:, :], hnT[gs][:, k, tok], Wc[:, k, c0:c1], k == 0, k == 7, [("hnT", gs)] + WK, [("pst", r)])
                if kind == "r":
                    P.act(dst, ps_t[r][:, :], AF.Silu, [("pst", r)], [dk])
                elif kind == "v":
                    P.copy("vector", dst, ps_t[r][:, :], [("pst", r)], [dk])
                else:
                    P.copy("scalar", dst, ps_t[r][:, :], [("pst", r)], [dk])
            P.dma("sync", d["c_ktok"][r0:r0 + 128, :], kst[s2][:], r=[("kst", s2)], key=("kst", s2), is_out=True)
            P.dma("sync", d["c_v"][r0:r0 + 128, :], vst[s2][:], r=[("vst", s2)], key=("vst", s2), is_out=True)
            P.dma("sync", d["c_sr"][r0:r0 + 128, :], rst[s2][:], r=[("rst", s2)], key=("rst", s2), is_out=True)
            P.mm(ps_l[:, :], glrT[gs][:, tok], g2w[:, :], True, True, [("glrT", gs), "g2w"], ["psl"])
            P.tt("vector", lt1[:], ps_l[:, :], g2b[:], ALU.add, ["psl", "g2b"], ["lt1"])
            P.act(lt2[:], lt1[:], AF.Exp, ["lt1"], ["lt2"], scale=-1.0)
            P.act(lt1[:], lt2[:], AF.Ln, ["lt2", "one"], ["lt1"], bias=one_t[:, 0:1], scale=1.0)
            P.ts("gpsimd", lat[s2][:], lt1[:], -1.0 / 16.0, ALU.mult, ["lt1"], [("lat", s2)])
            P.dma("sync", d["c_la"][r0:r0 + 128, :], lat[s2][:], r=[("lat", s2)], key=("lat", s2), is_out=True)

    ngr = NSEQ * 4
    norm_group(0)
    for gi in range(ngr):
        if gi + 1 < ngr:
            norm_group(gi + 1)
        mm_group(gi)
    P.close()


def phase_G(B):
    nc, d, NSEQ = B.nc, B.t, B.NSEQ
    P = Phase(nc, "G")
    identb = P.sb("identb", [128, 128], BF16)
    P.dma("sync", identb[:], d["ident_bf"], w=["identb"], key="identb")
    U = P.sb("U", [128, 128], F32)
    SL = P.sb("SL", [128, 128], F32)
    P.dma("sync", U[:], d["tri_u"], w=["U"], key="U")
    P.dma("sync", SL[:], d["tri_sl"], w=["SL"], key="SL")
    gon = P.sb("gon", [128, D], F32)
    P.dma("sync", gon[:], d["c_onorm_g_bc"], w=["gon"], key="gon")
    eps_t = P.sb("eps", [128, 1], F32)
    P.memset("vector", eps_t[:], EPS, ["eps"])
    qT = P.sb("qT", [128, 4, SEQ], BF16)
    kT = P.sb("kT", [128, 4, SEQ], BF16)
    la = [P.sb("la%d" % i, [128, 512], F32) for i in range(2)]
    kt_ = [P.sb("ktk%d" % i, [128, 512], BF16) for i in range(2)]
    vt = [P.sb("vt%d" % i, [128, 1024], BF16) for i in range(3)]
    sr = [P.sb("sr%d" % i, [128, 1024], F32) for i in range(2)]
    eb = [P.sb("eb%d" % i, [128, 512], F32) for i in range(2)]
    enb = P.sb("enb", [128, 512], F32)
    erv = P.sb("erv", [128, 512], F32)
    qtT = [P.sb("qtT%d" % i, [128, 512], BF16) for i in range(2)]
    ktT = [P.sb("ktT%d" % i, [128, 512], BF16) for i in range(2)]
    ks = [P.sb("ks%d" % i, [128, 512], BF16) for i in range(2)]
    attm = [P.sb("attm%d" % i, [128, 128], BF16) for i in range(2)]
    state = P.sb("state", [128, 4, 256], F32)
    stb = P.sb("stb", [128, 4, 256], BF16)
    gr = [P.sb("gr%d" % i, [128, 1024], F32) for i in range(2)]
    stat = [P.sb("stat%d" % i, [128, 12], F32) for i in range(2)]
    junk = P.sb("junk", [128, 256], F32)
    yc = [P.sb("yc%d" % i, [128, 1024], BF16) for i in range(2)]
    ycT = [P.sb("ycT%d" % i, [128, 8, 512], BF16) for i in range(2)]
    ps_b = P.ps("psb", [128, 512])
    ps_r = P.ps("psr", [128, 512])
    ps_a = P.ps("psa", [128, 4, 128])
    ps_o = P.ps("pso", [128, 1024])
    ps_kv = P.ps("pskv", [128, 1024])
    ps_tr = P.ps("ptr", [128, 8, 128], BF16)
    nch = NSEQ * NT

    def load_chunk(ci):
        s2 = ci % 2
        r0 = ci * 128
        P.dma("sync", la[s2][:], d["c_la"][r0:r0 + 128, :], w=[("la", s2)], key=("la", s2))
        P.dma("sync", kt_[s2][:], d["c_ktok"][r0:r0 + 128, :], w=[("ktk", s2)], key=("ktk", s2))
        P.dma("sync", vt[ci % 3][:], d["c_v"][r0:r0 + 128, :], w=[("vt", ci % 3)], key=("vt", ci % 3))
        P.dma("sync", sr[s2][:], d["c_sr"][r0:r0 + 128, :], w=[("sr", s2)], key=("sr", s2))

    enb2 = [enb, P.sb("enbb", [128, 512], F32)]
    erv2 = [erv, P.sb("ervb", [128, 512], F32)]
    attm4 = attm + [P.sb("attm%d" % i, [128, 128], BF16) for i in range(2, 4)]

    def prep(ci):
        b, c = ci // NT, ci % NT
        s2 = ci % 2
        if c == 0:
            P.dma("sync", qT[:], d["c_qT"][b].rearrange("c p t -> p c t"), w=["qT"], key="qT")
            P.dma("sync", kT[:], d["c_kT"][b].rearrange("c p t -> p c t"), w=["kT"], key="kT")
        if ci + 1 < nch:
            load_chunk(ci + 1)
        csl = slice(c * 128, (c + 1) * 128)
        for h in range(4):
            P.mm(ps_b[:, h * 128:(h + 1) * 128], la[s2][:, h * 128:(h + 1) * 128], U[:], True, True, [("la", s2), "U"], ["psb"], mark=(h == 3))
        P.mm(ps_r[:, :], SL[:], la[s2][:, :], True, True, ["SL", ("la", s2)], ["psr"])
        P.act(eb[s2][:], ps_b[:, :], AF.Exp, ["psb"], [("eb", s2)])
        P.act(enb2[s2][:], ps_b[:, :], AF.Exp, ["psb"], [("enb", s2)], scale=-1.0)
        P.act(erv2[s2][:], ps_r[:, :], AF.Exp, ["psr"], [("erv", s2)])
        qv = qT[:, :, csl]
        kv_ = kT[:, :, csl]
        P.tt("vector", qtT[s2][:].rearrange("p (h t) -> p h t", t=128), qv, eb[s2][:].rearrange("p (h t) -> p h t", t=128), ALU.mult,
             ["qT", ("eb", s2)], [("qtT", s2)])
        P.tt("gpsimd", ktT[s2][:].rearrange("p (h t) -> p h t", t=128), kv_, enb2[s2][:].rearrange("p (h t) -> p h t", t=128), ALU.mult,
             ["kT", ("enb", s2)], [("ktT", s2)])
        P.tt("gpsimd", ks[s2][:], kt_[s2][:], erv2[s2][:], ALU.mult, [("ktk", s2), ("erv", s2)], [("ks", s2)])
        P.tt("gpsimd", gr[s2][:], sr[s2][:], gon[:], ALU.mult, [("sr", s2), "gon"], [("gr", s2)])

    def rec(ci):
        b, c = ci // NT, ci % NT
        s2 = ci % 2
        if c == 0:
            P.memset("vector", state[:], 0.0, [("state", h) for h in range(4)])
            P.memset("gpsimd", stb[:], 0.0, [("stb", h) for h in range(4)])
        HS = [slice(h * 128, (h + 1) * 128) for h in range(4)]
        VS = [slice(h * 256, (h + 1) * 256) for h in range(4)]
        for h in range(4):
            P.mm(ps_a[:, h, :], ktT[s2][:, HS[h]], qtT[s2][:, HS[h]], True, True, [("ktT", s2), ("qtT", s2)], ["psa"], mark=(h == 3))
        for h in range(4):
            P.tt("vector", attm4[h][:], ps_a[:, h, :], U[:], ALU.mult, ["psa", "U"], [("attm", h)])
        for h in range(4):
            P.mm(ps_o[:, VS[h]], attm4[h][:], vt[ci % 3][:, VS[h]], True, False, [("attm", h), ("vt", ci % 3)], ["pso"], mark=False)
            P.mm(ps_o[:, VS[h]], qtT[s2][:, HS[h]], stb[:, h, :], False, True, [("qtT", s2), ("stb", h)], ["pso"], mark=(h == 3))
        for h in range(4):
            P.mm(ps_kv[:, VS[h]], ks[s2][:, HS[h]], vt[ci % 3][:, VS[h]], True, True, [("ks", s2), ("vt", ci % 3)], ["pskv"], mark=(h == 3))
        for h in range(4):
            P.stt(state[:, h, :], state[:, h, :], eb[s2][:, h * 128 + 127:h * 128 + 128], ps_kv[:, VS[h]], ALU.mult, ALU.add,
                  [("state", h), ("eb", s2), "pskv"], [("state", h)])
        for h in range(4):
            P.copy("scalar", stb[:, h, :], state[:, h, :], [("state", h)], [("stb", h)])
        sk = ("st", s2)
        for h in range(4):
            P.act(junk[:], ps_o[:, VS[h]], AF.Square, ["pso"], ["junkA", sk], accum_out=stat[s2][:, h:h + 1])
        P.act(stat[s2][:, 4:8], stat[s2][:, 0:4], AF.Sqrt, [sk, "eps"], [sk], scale=1.0 / 256, bias=eps_t[:, 0:1])
        P.recip(stat[s2][:, 8:12], stat[s2][:, 4:8], [sk], [sk])
        for h in range(4):
            P.stt(yc[s2][:, VS[h]], ps_o[:, VS[h]], stat[s2][:, 8 + h:9 + h], gr[s2][:, VS[h]], ALU.mult, ALU.mult,
                  ["pso", sk, ("gr", s2)], [("yc", s2)])
        for k in range(8):
            P.tr(ps_tr[:, k, :], yc[s2][:, k * 128:(k + 1) * 128], identb[:], [("yc", s2), "identb"], ["ptr"], mark=(k == 7))
        g4 = (ci // 4) % 2
        P.copy("scalar", ycT[g4][:, 0:8, (ci % 4) * 128:(ci % 4 + 1) * 128], ps_tr[:, 0:8, :], ["ptr"], [("ycT", g4)])
        if ci % 4 == 3:
            g = c // 4
            P.dma("sync", d["ycT"][b, :, :, g * 512:(g + 1) * 512].rearrange("c p t -> p c t"), ycT[g4][:], r=[("ycT", g4)],
                  key=("ycT", g4), is_out=True)

    load_chunk(0)
    prep(0)
    for ci in range(nch):
        if ci + 1 < nch:
            prep(ci + 1)
        rec(ci)
    P.close()


def declare(B):
    NSEQ = B.NSEQ
    T = NSEQ * SEQ
    X = "ExternalInput"
    B.dram("x", [T, D], F32, X)
    B.dram("ab_w_in", [D, 2248], F32, X)
    B.dram("ab_conv_w", [31, 512], F32, X)
    B.dram("conv_prm", [128, 12], F32, X)
    B.dram("ab_w_out", [D, D], F32, X)
    B.dram("c_w_in", [D, 3088], F32, X)
    B.dram("c_gate_w", [16, 512], F32, X)
    B.dram("c_gate_b_bc", [128, 512], F32, X)
    B.dram("c_onorm_g_bc", [128, D], F32, X)
    B.dram("c_w_out", [D, D], F32, X)
    for l in range(2):
        B.dram("ffn_w_gate%d" % l, [D, DFF], F32, X)
        B.dram("ffn_w_up%d" % l, [D, DFF], F32, X)
        B.dram("ffn_w_down%d" % l, [DFF, D], F32, X)
        B.dram("g_mix%d" % l, [128, D], F32, X)
        B.dram("g_ffn%d" % l, [128, D], F32, X)
    B.dram("g_final", [128, D], F32, X)
    B.dram("ident_bf", [128, 128], BF16, X)
    B.dram("ident_f32", [128, 128], F32, X)
    B.dram("cos128", [SEQ, 128], F32, X)
    B.dram("sin128", [SEQ, 128], F32, X)
    B.dram("negmask", [128, 128], F32, X)
    B.dram("pow2", [128, NIT], F32, X)
    B.dram("tri_u", [128, 128], F32, X)
    B.dram("tri_sl", [128, 128], F32, X)
    B.dram("hconvT", [NSEQ, 4, 128, SEQ], BF16)
    B.dram("qT", [NSEQ, 4, 128, SEQ], BF16)
    B.dram("iqT", [NSEQ, 4, 128, SEQ], BF16)
    B.dram("kkT", [NSEQ, 2, 128, SEQ], BF16)
    B.dram("vaug", [T, 256], BF16)
    B.dram("iw", [T, 8], F32)
    B.dram("yabT", [NSEQ, 8, 128, SEQ], BF16)
    B.dram("h_mid0", [T, D], F32)
    B.dram("hnT", [8, 128, T], BF16)
    B.dram("h_half", [T, D], F32)
    B.dram("h1", [T, D], F32)
    B.dram("c_qT", [NSEQ, 4, 128, SEQ], BF16)
    B.dram("c_kT", [NSEQ, 4, 128, SEQ], BF16)
    B.dram("c_ktok", [T, 512], BF16)
    B.dram("c_v", [T, D], BF16)
    B.dram("c_sr", [T, D], F32)
    B.dram("c_la", [T, 512], F32)
    B.dram("ycT", [NSEQ, 8, 128, SEQ], BF16)
    B.dram("h_mid1", [T, D], F32)
    B.dram("out", [T, D], F32, "ExternalOutput")
    if DEBUG_STOP == 77:
        B.dram("dbg", [128, 16], F32, "ExternalOutput")
        B.dram("dbg2", [4, 128, D], BF16, "ExternalOutput")


PHASES = {
    "A": phase_A,
    "B": phase_B,
    "C": phase_C,
    "D0": lambda B: phase_D(B, "D0", "yabT", "ab_w_out", "x", "g_ffn0", "h_mid0", "hnT"),
    "F0a": lambda B: phase_F(B, "F0a", 0, 0, "h_mid0", "h_half", "hnT"),
    "F0b": lambda B: phase_F(B, "F0b", 0, 11, "h_half", "h1", "hnT"),
    "E": phase_E,
    "G": phase_G,
    "D1": lambda B: phase_D(B, "D1", "ycT", "c_w_out", "h1", "g_ffn1", "h_mid1", "hnT"),
    "F1a": lambda B: phase_F(B, "F1a", 1, 0, "h_mid1", "h_half", "hnT"),
    "F1b": lambda B: phase_F(B, "F1b", 1, 11, "h_half", "out", "hnT", final_g="g_final"),
}
ORDER = ["A", "B", "C", "D0", "F0a", "F0b", "E", "G", "D1", "F1a", "F1b"]


def host_consts():
    bf = ml_dtypes.bfloat16
    c = {}
    c["ident_bf"] = np.eye(128, dtype=np.float32).astype(bf)
    c["ident_f32"] = np.eye(128, dtype=np.float32)
    rot = 16
    inv = (500000.0 ** (-np.arange(0, rot, 2, dtype=np.float32) / np.float32(rot))).astype(np.float32)
    ang = (np.arange(SEQ, dtype=np.float32)[:, None] * inv[None, :]).astype(np.float32)
    c["cos128"] = np.ascontiguousarray(np.tile(np.cos(ang).astype(np.float32), (1, 16)))
    c["sin128"] = np.ascontiguousarray(np.tile(np.sin(ang).astype(np.float32), (1, 16)))
    i = np.arange(128)
    c["negmask"] = np.where(i[None, :] <= i[:, None], 0.0, -1e30).astype(np.float32)
    c["pow2"] = np.ascontiguousarray(np.broadcast_to((0.5 ** np.arange(1, NIT + 1)).astype(np.float32), (128, NIT)))
    c["tri_u"] = (i[:, None] <= i[None, :]).astype(np.float32)
    c["tri_sl"] = (i[:, None] > i[None, :]).astype(np.float32)
    return c


def host_weights(inp):
    f = lambda a: np.ascontiguousarray(np.asarray(a, dtype=np.float32))
    bc = lambda v, n: np.ascontiguousarray(np.broadcast_to(np.asarray(v, np.float32).reshape(1, -1), (128, n)))
    w = {}
    w["ab_w_in"] = f(inp["ab_w_in"][0])
    w["ab_conv_w"] = f(inp["ab_conv_w"][0].reshape(31, 512))
    prm = np.concatenate([np.asarray(inp[k][0], np.float32).reshape(4, 128).T for k in ("ab_conv_b", "ab_ln_g", "ab_ln_b")], axis=1)
    w["conv_prm"] = f(prm)
    w["ab_w_out"] = f(inp["ab_w_out"][0])
    w["c_w_in"] = f(inp["c_w_in"][0])
    w["c_gate_w"] = f(inp["c_gate_w"][0])
    w["c_gate_b_bc"] = bc(inp["c_gate_b"][0], 512)
    w["c_onorm_g_bc"] = bc(inp["c_onorm_g"][0], D)
    w["c_w_out"] = f(inp["c_w_out"][0])
    for l in range(2):
        w["ffn_w_gate%d" % l] = f(inp["ffn_w_gate"][l])
        w["ffn_w_up%d" % l] = f(inp["ffn_w_up"][l])
        w["ffn_w_down%d" % l] = f(inp["ffn_w_down"][l])
        w["g_mix%d" % l] = bc(inp["norm_mix_g"][l], D)
        w["g_ffn%d" % l] = bc(inp["norm_ffn_g"][l], D)
    w["g_final"] = bc(inp["final_norm_g"], D)
    return w


def build_program(nseq, phases, ext=None):
    B = Build(nseq, ext)
    declare(B)
    for p in phases:
        PHASES[p](B)
    return B


def kernel(**inp):
    n = 8
    nseq = 2
    x = np.asarray(inp["x"], dtype=np.float32)
    B = build_program(nseq, ORDER)
    shared = host_consts()
    shared.update(host_weights(inp))
    in_maps = []
    for c in range(n):
        m = dict(shared)
        m["x"] = np.ascontiguousarray(x[c * nseq:(c + 1) * nseq].reshape(nseq * SEQ, D))
        in_maps.append(m)
    res = run_bass_kernel_spmd(B.nc, in_maps, core_ids=list(range(n)))
    out = np.stack([np.asarray(r["out"], dtype=np.float32).reshape(nseq, SEQ, D) for r in res.results], axis=0)
    return out.reshape(16, SEQ, D)
```

```python
import contextlib
import math
import numpy as np
import ml_dtypes
import concourse.bass as bass
import concourse.mybir as mybir
from concourse.bass_utils import run_bass_kernel_spmd

F32 = mybir.dt.float32
BF16 = mybir.dt.bfloat16
ALU = mybir.AluOpType
AF = mybir.ActivationFunctionType
AX = mybir.AxisListType

ENGS = ["sync", "scalar", "vector", "gpsimd", "tensor"]
D = 1024
SEQ = 2048
NT = 16
EPS = 1e-6
DFF = 2816
NFF = 22
NIT = 16
TOPK = 256
DEBUG_STOP = 99
SAME_SYNC = True


class Sched:
    def __init__(self, nc, stack, tag=""):
        self.nc = nc
        self.stack = stack
        self.tag = tag
        self.q = {e: [] for e in ENGS}
        self.sem = {}
        self.cnt = {}
        self.waited = {e: {} for e in ENGS}
        self.lastw = {}
        self.readers = {}
        self.pe_pending = False
        self.out_deps = {}
        for e in ENGS:
            self._mksem("E_" + e)

    def _mksem(self, name):
        if name not in self.sem:
            hw_name = "%s_s%d" % (self.tag, len(self.sem))
            self.sem[name] = self.nc.alloc_semaphore(name=hw_name)
            self.cnt[name] = 0
        return name

    def op(self, eng, fn, reads=(), writes=(), dma=None, mark=True, is_out=False, force_self=False):
        deps = {}

        def add(d):
            if d[1] > deps.get(d[0], 0):
                deps[d[0]] = d[1]

        for k in reads:
            if k in self.lastw:
                add(self.lastw[k])
        for k in writes:
            if k in self.lastw:
                add(self.lastw[k])
            for s_, v_ in self.readers.get(k, {}).items():
                add((s_, v_))
        if dma is not None:
            s = self._mksem("D_" + str(dma))
            if self.cnt[s] > 0:
                add((s, self.cnt[s]))
        own = "E_" + eng
        need = []
        for s_, v_ in deps.items():
            if s_ == own and (eng == "tensor" or not (SAME_SYNC or force_self)):
                continue
            if self.waited[eng].get(s_, 0) >= v_:
                continue
            need.append((s_, v_))
        attach = None
        if eng == "tensor":
            k0 = reads[0] if len(reads) else None
            if k0 is not None and k0 in self.lastw and self.lastw[k0][0] != own:
                attach = self.lastw[k0]
        else:
            selfs = [d_ for d_ in need if d_[0] == own]
            attach = selfs[0] if selfs else (need[-1] if need else None)
        standalone = [d_ for d_ in need if attach is None or d_[0] != attach[0]]
        if attach is not None:
            for d_ in need:
                if d_[0] == attach[0] and d_[1] > attach[1]:
                    attach = d_
        for d_ in need:
            self.waited[eng][d_[0]] = max(self.waited[eng].get(d_[0], 0), d_[1])
        if attach is not None:
            self.waited[eng][attach[0]] = max(self.waited[eng].get(attach[0], 0), attach[1])
        if dma is not None:
            self.cnt[s] += 16
            dep = (s, self.cnt[s])
            self.q[eng].append(("op", fn, s, 16, standalone, attach))
        else:
            s = own
            if mark:
                self.cnt[s] += 1
                dep = (s, self.cnt[s])
                self.q[eng].append(("op", fn, s, 1, standalone, attach))
                if eng == "tensor":
                    self.pe_pending = False
            else:
                assert eng == "tensor"
                dep = (s, self.cnt[s] + 1)
                self.q[eng].append(("op", fn, None, 0, standalone, attach))
                self.pe_pending = True
        for k in writes:
            self.lastw[k] = dep
            self.readers[k] = {}
        for k in reads:
            r = self.readers.setdefault(k, {})
            if dep[1] > r.get(dep[0], 0):
                r[dep[0]] = dep[1]
        if is_out:
            if dep[1] > self.out_deps.get(dep[0], 0):
                self.out_deps[dep[0]] = dep[1]
        return dep

    def finalize(self):
        assert not self.pe_pending, "unmarked PE op at end of phase"
        tail = []
        for s_, v_ in self.out_deps.items():
            if self.waited["sync"].get(s_, 0) < v_:
                tail.append((s_, v_))
        nc = self.nc
        with nc.Block() as block:
            for eng in ENGS:
                items = self.q[eng]

                def body(e, items=items, eng=eng):
                    for it in items:
                        for (ws, wv) in it[4]:
                            e.wait_ge(self.sem[ws], wv)
                        ins = it[1](e)
                        if it[5] is not None:
                            ins._wait_ge(self.sem[it[5][0]], it[5][1])
                        if it[2] is not None:
                            ins.then_inc(self.sem[it[2]], it[3])
                    if eng == "sync":
                        for (ws, wv) in tail:
                            e.wait_ge(self.sem[ws], wv)

                getattr(block, eng)(body)


class Phase:
    def __init__(self, nc, tag):
        self.nc = nc
        self.tag = tag
        self.st = contextlib.ExitStack()
        self.st.enter_context(nc.cleanup_on_exit())
        self.S = Sched(nc, self.st, tag)

    def sb(self, name, shape, dt):
        return self.st.enter_context(self.nc.sbuf_tensor(self.tag + "_" + name, shape, dt))

    def ps(self, name, shape, dt=F32):
        return self.st.enter_context(self.nc.psum_tensor(self.tag + "_" + name, shape, dt))

    def dma(self, eng, out, in_, r=(), w=(), key=None, is_out=False):
        return self.S.op(eng, lambda e: e.dma_start(out=out, in_=in_), r, w, dma=key, is_out=is_out)

    def mm(self, out, lhsT, rhs, start, stop, r, w, mark=None):
        if mark is None:
            mark = stop
        return self.S.op("tensor", lambda e: e.matmul(out, lhsT=lhsT, rhs=rhs, start=start, stop=stop), r, w, mark=mark)

    def tr(self, out, in_, ident, r, w, mark=True):
        return self.S.op("tensor", lambda e: e.transpose(out=out, in_=in_, identity=ident), r, w, mark=mark)

    def act(self, out, in_, func, r, w, **kw):
        return self.S.op("scalar", lambda e: e.activation(out=out, in_=in_, func=func, **kw), r, w)

    def tt(self, eng, out, in0, in1, op, r, w):
        return self.S.op(eng, lambda e: e.tensor_tensor(out=out, in0=in0, in1=in1, op=op), r, w)

    def ts(self, eng, out, in0, s1, op0, r, w, s2=None, op1=None, accum_out=None):
        if op1 is None:
            return self.S.op(eng, lambda e: e.tensor_scalar(out=out, in0=in0, scalar1=s1, scalar2=None, op0=op0), r, w)
        return self.S.op(eng, lambda e: e.tensor_scalar(out=out, in0=in0, scalar1=s1, scalar2=s2, op0=op0, op1=op1,
                                                       accum_out=accum_out), r, w)

    def stt(self, out, in0, scalar, in1, op0, op1, r, w):
        return self.S.op("vector", lambda e: e.scalar_tensor_tensor(out=out, in0=in0, scalar=scalar, in1=in1,
                                                                     op0=op0, op1=op1), r, w)

    def copy(self, eng, out, in_, r, w):
        if eng == "scalar":
            return self.S.op(eng, lambda e: e.copy(out=out, in_=in_), r, w)
        return self.S.op(eng, lambda e: e.tensor_copy(out=out, in_=in_), r, w)

    def recip(self, out, in_, r, w):
        return self.S.op("vector", lambda e: e.reciprocal(out=out, in_=in_), r, w)

    def memset(self, eng, ap, val, w):
        return self.S.op(eng, lambda e: e.memset(ap, val), (), w)

    def reduce(self, out, in_, op, r, w):
        return self.S.op("vector", lambda e: e.tensor_reduce(out=out, in_=in_, axis=AX.X, op=op), r, w)

    def close(self):
        self.S.finalize()
        self.st.close()


class Build:
    def __init__(self, nseq, ext=None):
        self.nc = bass.Bass("TRN2", target_bir_lowering=False)
        self.NSEQ = nseq
        self.t = {}
        self.ext = ext or {}
        self.kinds = {}

    def dram(self, name, shape, dt, kind="Internal"):
        kind = self.ext.get(name, kind)
        self.kinds[name] = (kind, list(shape), dt)
        self.t[name] = self.nc.dram_tensor(name, list(shape), dt, kind=kind).ap()
        return self.t[name]


def rms_to_hnT(P, xt, xkey, gbc, eps_t, stat, skey, junk, hn, hkey, ps_tr, pkey, identb, dst, dkey, sl):
    P.act(junk[:], xt, AF.Square, [xkey], ["junkA", skey], accum_out=stat[:, 0:1])
    P.act(stat[:, 1:2], stat[:, 0:1], AF.Sqrt, [skey, "eps"], [skey], scale=1.0 / D, bias=eps_t[:, 0:1])
    P.recip(stat[:, 2:3], stat[:, 1:2], [skey], [skey])
    P.stt(hn, xt, stat[:, 2:3], gbc, ALU.mult, ALU.mult, [xkey, skey, "gbc"], [hkey])
    for k in range(8):
        P.tr(ps_tr[:, k, :], hn[:, k * 128:(k + 1) * 128], identb[:], [hkey, "identb"], [pkey], mark=(k == 7))
    P.copy("scalar", dst[:, 0:8, sl], ps_tr[:, 0:8, :], [pkey], [dkey])


def phase_A(B):
    nc, d, NSEQ = B.nc, B.t, B.NSEQ
    P = Phase(nc, "A")
    Wt = P.sb("Wt", [128, 8, 2376], BF16)
    Wsrc = d["ab_w_in"].rearrange("(k p) n -> p k n", p=128)
    segs = [(0, 1024, 0), (1024, 1536, 1024), (1664, 2176, 1536), (1536, 1600, 2048), (1536, 1600, 2112),
            (2176, 2240, 2176), (2176, 2240, 2240), (1600, 1664, 2304), (2240, 2248, 2368)]
    WK = []
    for i, (c0, c1, d0) in enumerate(segs):
        P.dma("gpsimd", Wt[:, :, d0:d0 + c1 - c0], Wsrc[:, :, c0:c1], w=[("W", i)], key=("W", i))
        WK.append(("W", i))
    identb = P.sb("identb", [128, 128], BF16)
    P.dma("sync", identb[:], d["ident_bf"], w=["identb"], key="identb")
    gbc = P.sb("gbc", [128, D], F32)
    P.dma("sync", gbc[:], d["g_mix0"], w=["gbc"], key="gbc")
    cos = P.sb("cos", [128, NT, 128], F32)
    sin = P.sb("sin", [128, NT, 128], F32)
    P.dma("sync", cos[:], d["cos128"].rearrange("(t p) c -> p t c", p=128), w=["cos"], key="cos")
    P.dma("sync", sin[:], d["sin128"].rearrange("(t p) c -> p t c", p=128), w=["sin"], key="sin")
    eps_t = P.sb("eps", [128, 1], F32)
    P.memset("vector", eps_t[:], EPS, ["eps"])

    xt = [P.sb("xt%d" % i, [128, D], F32) for i in range(2)]
    junk = P.sb("junk", [128, D], F32)
    stat = [P.sb("stat%d" % i, [128, 4], F32) for i in range(2)]
    hn = [P.sb("hn%d" % i, [128, D], BF16) for i in range(2)]
    hnT = [P.sb("hnT%d" % i, [128, 8, 512], BF16) for i in range(2)]
    ps_tr = [P.ps("ptr%d" % i, [128, 8, 128], BF16) for i in range(2)]
    ps_a = P.ps("psa", [128, 512])
    ps_g = P.ps("psg", [128, 512])
    ps_q = P.ps("psq", [128, 1024])
    ps_c = P.ps("psc", [128, 512])
    sig = [P.sb("sig%d" % i, [128, 512], F32) for i in range(2)]
    hc = [P.sb("hc%d" % i, [128, 512], BF16) for i in range(2)]
    tmp = [P.sb("tmp%d" % i, [128, 128], F32) for i in range(4)]
    qr = [P.sb("qr%d" % i, [128, 1024], BF16) for i in range(2)]
    kr = [P.sb("kr%d" % i, [128, 256], BF16) for i in range(2)]
    vaug = [P.sb("vaug%d" % i, [128, 256], BF16) for i in range(2)]
    iwt = [P.sb("iw%d" % i, [128, 8], F32) for i in range(2)]
    for i in range(2):
        P.memset("gpsimd", vaug[i][:, 64:192], 1.0, [("vaug", i)])
    qTg = [P.sb("qTg%d" % i, [128, 8, 512], BF16) for i in range(2)]
    kTg = [P.sb("kTg%d" % i, [128, 2, 512], BF16) for i in range(2)]
    w_scale = (8 ** -0.5) * (64 ** -0.5)

    def load_x(b, tt):
        xs = tt % 2
        r0 = b * SEQ + tt * 128
        P.dma("sync", xt[xs][:], d["x"][r0:r0 + 128, :], w=[("x", xs)], key=("x", xs))

    n_tiles = NSEQ * NT
    if DEBUG_STOP <= 1:
        P.dma("sync", d["iw"][0:128, :], iwt[0][:], r=[("iw", 0), "cos", "sin", "gbc", "identb"] + WK, key=("iw", 0), is_out=True)
        P.close()
        return
    load_x(0, 0)

    def norm_group(gi):
        b, g = gi // 4, gi % 4
        gs = gi % 2
        for t4 in range(4):
            tt = g * 4 + t4
            ti = gi * 4 + t4
            if ti + 1 < n_tiles:
                load_x((ti + 1) // NT, (ti + 1) % NT)
            xs = tt % 2
            rms_to_hnT(P, xt[xs][:], ("x", xs), gbc[:], eps_t, stat[xs], ("st", xs), junk, hn[xs][:], ("hn", xs),
                       ps_tr[0], "ptr0", identb, hnT[gs], ("hnT", gs), slice(t4 * 128, (t4 + 1) * 128))

    def mm_group(gi):
        b, g = gi // 4, gi % 4
        gs = gi % 2
        for c in range(4):
            for k in range(8):
                P.mm(ps_a[:, :], Wt[:, k, c * 128:(c + 1) * 128], hnT[gs][:, k, :], k == 0, k == 7,
                     [("hnT", gs)] + WK, ["psa"])
            for k in range(8):
                P.mm(ps_g[:, :], Wt[:, k, 512 + c * 128:512 + (c + 1) * 128], hnT[gs][:, k, :], k == 0, k == 7,
                     [("hnT", gs)] + WK, ["psg"])
            s2 = c % 2
            P.act(sig[s2][:], ps_g[:, :], AF.Sigmoid, ["psg"], [("sig", s2)])
            P.tt("vector", hc[s2][:], ps_a[:, :], sig[s2][:], ALU.mult, ["psa", ("sig", s2)], [("hc", s2)])
            P.dma("sync", d["hconvT"][b, c, :, g * 512:(g + 1) * 512], hc[s2][:], r=[("hc", s2)], key=("hc", s2), is_out=True)
        for t4 in range(4):
            tt = g * 4 + t4
            r0 = b * SEQ + tt * 128
            tok = slice(t4 * 128, (t4 + 1) * 128)
            s2 = t4 % 2
            for k in range(8):
                P.mm(ps_q[:, 0:512], hnT[gs][:, k, tok], Wt[:, k, 1024:1536], k == 0, k == 7, [("hnT", gs)] + WK, ["psq"], mark=False)
            for k in range(8):
                P.mm(ps_q[:, 512:1024], hnT[gs][:, k, tok], Wt[:, k, 1536:2048], k == 0, k == 7, [("hnT", gs)] + WK, ["psq"], mark=False)
            for k in range(8):
                P.mm(ps_c[:, 0:328], hnT[gs][:, k, tok], Wt[:, k, 2048:2376], k == 0, k == 7, [("hnT", gs)] + WK, ["psq"])
            for (src, nh, dst, dkey) in ((ps_q[:, 0:512], 8, qr[s2][:, 0:512], ("qr", s2)), (ps_q[:, 512:1024], 8, qr[s2][:, 512:1024], ("qr", s2)),
                                         (ps_c[:, 0:256], 4, kr[s2][:, 0:256], ("kr", s2))):
                sv = src.rearrange("p (h e) -> p h e", e=64)
                dv = dst.rearrange("p (h e) -> p h e", e=64)
                x1, x2 = sv[:, :, 0:8], sv[:, :, 8:16]
                cv = cos[:, tt, 0:nh * 8].rearrange("p (h e) -> p h e", e=8)
                sn = sin[:, tt, 0:nh * 8].rearrange("p (h e) -> p h e", e=8)
                tv = [tmp[i][:, 0:nh * 8].rearrange("p (h e) -> p h e", e=8) for i in range(4)]
                P.tt("vector", tv[0], x1, cv, ALU.mult, ["psq", "cos"], ["tmp0"])
                P.tt("vector", tv[1], x2, sn, ALU.mult, ["psq", "sin"], ["tmp1"])
                P.tt("vector", dv[:, :, 0:8], tv[0], tv[1], ALU.subtract, ["tmp0", "tmp1"], [dkey])
                P.tt("vector", tv[2], x2, cv, ALU.mult, ["psq", "cos"], ["tmp2"])
                P.tt("vector", tv[3], x1, sn, ALU.mult, ["psq", "sin"], ["tmp3"])
                P.tt("vector", dv[:, :, 8:16], tv[2], tv[3], ALU.add, ["tmp2", "tmp3"], [dkey])
                P.copy("scalar", dv[:, :, 16:64], sv[:, :, 16:64], ["psq"], [dkey])
            P.copy("scalar", vaug[s2][:, 0:64], ps_c[:, 256:320], ["psq"], [("vaug", s2)])
            P.copy("scalar", vaug[s2][:, 192:256], ps_c[:, 256:320], ["psq"], [("vaug", s2)])
            P.ts("vector", iwt[s2][:], ps_c[:, 320:328], w_scale, ALU.mult, ["psq"], [("iw", s2)])
            P.dma("sync", d["vaug"][r0:r0 + 128, :], vaug[s2][:], r=[("vaug", s2)], key=("vaug", s2), is_out=True)
            P.dma("sync", d["iw"][r0:r0 + 128, :], iwt[s2][:], r=[("iw", s2)], key=("iw", s2), is_out=True)
            for k in range(8):
                P.tr(ps_tr[1][:, k, :], qr[s2][:, k * 128:(k + 1) * 128], identb[:], [("qr", s2), "identb"], ["ptr1"], mark=(k == 7))
            P.copy("scalar", qTg[gs][:, 0:8, tok], ps_tr[1][:, 0:8, :], ["ptr1"], [("qTg", gs)])
            for k in range(2):
                P.tr(ps_tr[1][:, k, :], kr[s2][:, k * 128:(k + 1) * 128], identb[:], [("kr", s2), "identb"], ["ptr1"], mark=(k == 1))
            P.copy("scalar", kTg[gs][:, 0:2, tok], ps_tr[1][:, 0:2, :], ["ptr1"], [("kTg", gs)])
        gsl = slice(g * 512, (g + 1) * 512)
        P.dma("sync", d["qT"][b, :, :, gsl].rearrange("c p t -> p c t"), qTg[gs][:, 0:4, :], r=[("qTg", gs)], key=("qTg", gs), is_out=True)
        P.dma("sync", d["iqT"][b, :, :, gsl].rearrange("c p t -> p c t"), qTg[gs][:, 4:8, :], r=[("qTg", gs)], key=("iqTg", gs), is_out=True)
        P.dma("sync", d["kkT"][b, :, :, gsl].rearrange("c p t -> p c t"), kTg[gs][:, :, :], r=[("kTg", gs)], key=("kTg", gs), is_out=True)

    ngr = NSEQ * 4
    norm_group(0)
    for gi in range(ngr):
        if gi + 1 < ngr:
            norm_group(gi + 1)
        mm_group(gi)
    P.close()


def phase_B(B):
    nc, d, NSEQ = B.nc, B.t, B.NSEQ
    P = Phase(nc, "B")
    identb = P.sb("identb", [128, 128], BF16)
    P.dma("sync", identb[:], d["ident_bf"], w=["identb"], key="identb")
    identf = P.sb("identf", [128, 128], F32)
    P.dma("sync", identf[:], d["ident_f32"], w=["identf"], key="identf")
    ones = P.sb("ones", [128, 128], F32)
    P.memset("vector", ones[:], 1.0, ["ones"])
    eps_t = P.sb("eps", [128, 1], F32)
    P.memset("vector", eps_t[:], EPS, ["eps"])
    cw = P.sb("cw", [31, 512], F32)
    P.dma("sync", cw[:], d["ab_conv_w"], w=["cw"], key="cw")
    prm = P.sb("prm", [128, 12], F32)
    P.dma("sync", prm[:], d["conv_prm"], w=["prm"], key="prm")
    ps_w = P.ps("psw", [128, 4, 32])
    for c in range(4):
        P.tr(ps_w[:, c, 0:31], cw[0:31, c * 128:(c + 1) * 128], identf[0:31, 0:31], ["cw", "identf"], ["psw"], mark=(c == 3))
    wT = P.sb("wT", [128, 4, 32], F32)
    P.copy("vector", wT[:, :, 0:31], ps_w[:, :, 0:31], ["psw"], ["wT"])
    diag = P.sb("diag", [128, 124, 128], BF16)
    for c in range(4):
        for j in range(31):
            i = c * 31 + j
            P.ts("vector" if i % 2 == 0 else "gpsimd", diag[:, i, :], identb[:], wT[:, c, j:j + 1], ALU.mult,
                 ["identb", "wT"], [("diag", c)])
    hbuf = P.sb("hbuf", [128, 4, 30 + SEQ], BF16)
    P.memset("gpsimd", hbuf[:, :, 0:30], 0.0, ["hbuf"])
    ps_c = [P.ps("psc%d" % i, [128, 512]) for i in range(2)]
    ps_s1 = P.ps("ps1", [128, 512])
    ps_s2 = P.ps("ps2", [128, 512])
    hcs = P.sb("hcs", [128, 4, 512], F32)
    sqs = P.sb("sqs", [128, 4, 512], F32)
    m = P.sb("m", [128, 512], F32)
    msq = P.sb("msq", [128, 512], F32)
    var = P.sb("var", [128, 512], F32)
    rstd = P.sb("rstd", [128, 512], F32)
    z = [P.sb("z%d" % i, [128, 512], F32) for i in range(2)]
    yst = [P.sb("yst%d" % i, [128, 4, 512], BF16) for i in range(2)]
    for b in range(NSEQ):
        for c in range(4):
            P.dma("sync", hbuf[:, c, 30:30 + SEQ], d["hconvT"][b, c, :, :], w=["hbuf"], key=("hbuf", c))
        for g in range(4):
            ys = g % 2
            for c in range(4):
                pc = ps_c[c % 2]
                pk = ("psc", c % 2)
                for j in range(31):
                    P.mm(pc[:, :], diag[:, c * 31 + j, :], hbuf[:, c, g * 512 + j:g * 512 + j + 512], j == 0, j == 30,
                         [("diag", c), "hbuf"], [pk])
                P.act(hcs[:, c, :], pc[:, :], AF.Identity, [pk, "prm"], [("hcs", c)], bias=prm[:, c:c + 1], scale=1.0)
                P.act(sqs[:, c, :], pc[:, :], AF.Square, [pk, "prm"], [("sqs", c)], bias=prm[:, c:c + 1], scale=1.0)
            for c in range(4):
                P.mm(ps_s1[:, :], ones[:], hcs[:, c, :], c == 0, c == 3, [("hcs", c), "ones"], ["ps1"])
            for c in range(4):
                P.mm(ps_s2[:, :], ones[:], sqs[:, c, :], c == 0, c == 3, [("sqs", c), "ones"], ["ps2"])
            P.act(m[:], ps_s1[:, :], AF.Copy, ["ps1"], ["m"], scale=1.0 / 512)
            P.tt("gpsimd", msq[:], m[:], m[:], ALU.mult, ["m"], ["msq"])
            P.stt(var[:], ps_s2[:, :], 1.0 / 512, msq[:], ALU.mult, ALU.subtract, ["ps2", "msq"], ["var"])
            P.act(var[:], var[:], AF.Sqrt, ["var", "eps"], ["var"], bias=eps_t[:, 0:1], scale=1.0)
            P.recip(rstd[:], var[:], ["var"], ["rstd"])
            for c in range(4):
                zz = z[c % 2]
                zk = ("z", c % 2)
                P.tt("gpsimd", zz[:], hcs[:, c, :], m[:], ALU.subtract, [("hcs", c), "m"], [zk])
                P.tt("vector", zz[:], zz[:], rstd[:], ALU.mult, [zk, "rstd"], [zk])
                P.act(yst[ys][:, c, :], zz[:], AF.Silu, [zk, "prm"], [("yst", ys)], scale=prm[:, 4 + c:5 + c], bias=prm[:, 8 + c:9 + c])
            P.dma("sync", d["yabT"][b, 0:4, :, g * 512:(g + 1) * 512].rearrange("c p t -> p c t"), yst[ys][:],
                  r=[("yst", ys)], key=("yst", ys), is_out=True)
    P.close()


def phase_C(B):
    nc, d, NSEQ = B.nc, B.t, B.NSEQ
    P = Phase(nc, "C")
    identb = P.sb("identb", [128, 128], BF16)
    P.dma("sync", identb[:], d["ident_bf"], w=["identb"], key="identb")
    negm = P.sb("negm", [128, 128], F32)
    P.dma("sync", negm[:], d["negmask"], w=["negm"], key="negm")
    pow2 = P.sb("pow2", [128, NIT], F32)
    P.dma("sync", pow2[:], d["pow2"], w=["pow2"], key="pow2")
    thr0 = P.sb("thr0", [128, 1], F32)
    P.memset("vector", thr0[:], -1e29, ["thr0"])
    bigI = P.sb("bigI", [128, 128], BF16)
    P.ts("vector", bigI[:], identb[:], 30000.0, ALU.mult, ["identb"], ["bigI"])
    qT = [P.sb("qT%d" % i, [128, 4, SEQ], BF16) for i in range(2)]
    iqT = [P.sb("iqT%d" % i, [128, 4, SEQ], BF16) for i in range(2)]
    kkT = [P.sb("kkT%d" % i, [128, 2, SEQ], BF16) for i in range(2)]
    vaug = [P.sb("vaug%d" % i, [128, NT, 256], BF16) for i in range(2)]
    iw = [P.sb("iw%d" % i, [128, NT, 8], F32) for i in range(2)]
    ybg = [P.sb("ybg%d" % i, [128, 512], BF16) for i in range(2)]
    maskT = [P.sb("maskT%d" % i, [128, NT, 512], BF16) for i in range(2)]
    score = [P.sb("score%d" % i, [128, SEQ], F32) for i in range(2)]
    junk = P.sb("junk", [128, SEQ], BF16)
    rl = [P.sb("rl%d" % i, [128, 512], F32) for i in range(4)]
    mk = [P.sb("mk%d" % i, [128, SEQ], BF16) for i in range(2)]
    st = [P.sb("st%d" % i, [128, 8 + NIT], F32) for i in range(2)]
    pe = [P.sb("pe%d" % i, [128, 512], BF16) for i in range(3)]
    rc = [P.sb("rc%d" % i, [128, 512], F32) for i in range(2)]
    ps_lg = [P.ps("plg%d" % i, [128, 512]) for i in range(3)]
    ps_tr = [P.ps("ptr%d" % i, [128, 8, 128], BF16) for i in range(1)]
    ps_s = [P.ps("pss%d" % i, [128, 512]) for i in range(2)]
    ps_o = [P.ps("pso%d" % i, [128, 512]) for i in range(2)]
    att_scale = 64 ** -0.5
    ctr = {"lg": 0, "s": 0, "o": 0, "tr": 0, "yb": 0, "g": 0}

    mslot = {}

    def load_seq(b):
        p = b % 2
        rs = slice(b * SEQ, (b + 1) * SEQ)
        P.dma("sync", iqT[p][:], d["iqT"][b].rearrange("c p t -> p c t"), w=[("iqT", p)], key=("iqT", p))
        P.dma("sync", kkT[p][:], d["kkT"][b].rearrange("c p t -> p c t"), w=[("kkT", p)], key=("kkT", p))
        P.dma("sync", iw[p][:], d["iw"][rs, :].rearrange("(t p) c -> p t c", p=128), w=[("iw", p)], key=("iw", p))
        P.dma("sync", qT[p][:], d["qT"][b].rearrange("c p t -> p c t"), w=[("qT", p)], key=("qT", p))
        P.dma("sync", vaug[p][:], d["vaug"][rs, :].rearrange("(t p) c -> p t c", p=128), w=[("vaug", p)], key=("vaug", p))

    junk2 = [junk, P.sb("junkb", [128, SEQ], BF16)]

    def score_pair(b, G, q4s):
        p = b % 2
        mG = mslot[(b, G)]
        info = []
        for q4 in q4s:
            qb = 4 * G + q4
            nk = 128 * (qb + 1)
            nkb = (nk + 511) // 512
            s2 = qb % 2
            sc = score[s2]
            sks = [("score", s2, kb) for kb in range(nkb)]
            qsl = slice(qb * 128, (qb + 1) * 128)
            for h in range(8):
                c, base = h // 2, 64 * (h % 2)
                for kb in range(nkb):
                    n = min(512, nk - kb * 512)
                    r = ctr["lg"] % 3
                    r4 = ctr["lg"] % 4
                    ctr["lg"] += 1
                    P.mm(ps_lg[r][:, 0:n], iqT[p][base:base + 64, c, qsl], kkT[p][base:base + 64, 1, kb * 512:kb * 512 + n],
                         True, True, [("iqT", p), ("kkT", p)], [("plg", r)])
                    P.act(rl[r4][:, 0:n], ps_lg[r][:, 0:n], AF.Relu, [("plg", r)], [("rl", r4)])
                    dst = sc[:, kb * 512:kb * 512 + n]
                    if h == 0:
                        P.ts("vector", dst, rl[r4][:, 0:n], iw[p][:, qb, 0:1], ALU.mult, [("rl", r4), ("iw", p)], [sks[kb]])
                    else:
                        P.stt(dst, rl[r4][:, 0:n], iw[p][:, qb, h:h + 1], dst, ALU.mult, ALU.add, [("rl", r4), ("iw", p), sks[kb]], [sks[kb]])
            info.append((q4, qb, nk, s2, sc, sks, qsl))
        for (q4, qb, nk, s2, sc, sks, qsl) in info:
            stt_ = st[s2]
            stk = ("st", s2)
            if qb >= 2:
                P.reduce(stt_[:, 0:1], sc[:, 0:nk], ALU.max, sks, [stk])
                P.reduce(stt_[:, 1:2], sc[:, 0:nk], ALU.min, sks, [(stk, "lo")])
            P.tt("vector", sc[:, qsl], sc[:, qsl], negm[:], ALU.add, [sks[qb // 4], "negm"], [sks[qb // 4]])
            if qb >= 2:
                P.tt("vector", stt_[:, 2:3], stt_[:, 0:1], stt_[:, 1:2], ALU.subtract, [stk, (stk, "lo")], [stk])
                P.ts("vector", stt_[:, 8:8 + NIT], pow2[:], stt_[:, 2:3], ALU.mult, ["pow2", stk], [(stk, "steps")])
        bis = [x for x in info if x[1] >= 2]
        for i in range(NIT):
            for (q4, qb, nk, s2, sc, sks, qsl) in bis:
                stt_, stk = st[s2], ("st", s2)
                P.tt("vector", stt_[:, 3:4], stt_[:, 1:2], stt_[:, 8 + i:9 + i], ALU.add, [(stk, "lo"), (stk, "steps")], [(stk, "mid")])
            for (q4, qb, nk, s2, sc, sks, qsl) in bis:
                stt_, stk = st[s2], ("st", s2)
                P.ts("vector", junk2[s2][:, 0:nk], sc[:, 0:nk], stt_[:, 3:4], ALU.is_ge, sks + [(stk, "mid")], [("junk", s2), (stk, "cnt")],
                     s2=0.0, op1=ALU.add, accum_out=stt_[:, 4:5])
            for (q4, qb, nk, s2, sc, sks, qsl) in bis:
                stt_, stk = st[s2], ("st", s2)
                P.stt(stt_[:, 5:6], stt_[:, 4:5], TOPK - 0.5, stt_[:, 8 + i:9 + i], ALU.is_ge, ALU.mult, [(stk, "cnt"), (stk, "steps")], [(stk, "sel")])
            for (q4, qb, nk, s2, sc, sks, qsl) in bis:
                stt_, stk = st[s2], ("st", s2)
                P.tt("vector", stt_[:, 1:2], stt_[:, 1:2], stt_[:, 5:6], ALU.add, [(stk, "lo"), (stk, "sel")], [(stk, "lo")])
        for (q4, qb, nk, s2, sc, sks, qsl) in info:
            stt_, stk = st[s2], ("st", s2)
            thr = stt_[:, 1:2] if qb >= 2 else thr0[:, 0:1]
            P.ts("vector", mk[s2][:, 0:nk], sc[:, 0:nk], thr, ALU.is_ge, sks + [(stk, "lo"), "thr0"], [("mk", s2)],
                 s2=-1.0, op1=ALU.add)
            kt = 0
            while kt <= qb:
                n = min(8, qb + 1 - kt)
                r = 0
                for j in range(n):
                    P.tr(ps_tr[r][:, j, :], mk[s2][:, (kt + j) * 128:(kt + j + 1) * 128], identb[:], [("mk", s2), "identb"],
                         [("ptr", r)], mark=(j == n - 1))
                P.copy("scalar", maskT[mG][:, kt:kt + n, q4 * 128:(q4 + 1) * 128], ps_tr[r][:, 0:n, :], [("ptr", r)], [("maskT", mG)])
                kt += n

    def attn_chunk(b, G, c):
        p = b % 2
        mG = mslot[(b, G)]
        nkt = 4 * G + 4
        gsl0 = G * 512
        ys = ctr["yb"] % 2
        ctr["yb"] += 1
        for e in range(2):
            base = 64 * e
            o = ctr["o"] % 2
            ctr["o"] += 1
            for kt in range(nkt):
                q0 = max(0, kt - 4 * G) * 128
                N = 512 - q0
                r = ctr["s"] % 2
                ctr["s"] += 1
                P.mm(ps_s[r][:, 0:N], kkT[p][base:base + 64, 0, kt * 128:(kt + 1) * 128], qT[p][base:base + 64, c, gsl0 + q0:gsl0 + 512],
                     True, False, [("kkT", p), ("qT", p)], [("pss", r)], mark=False)
                P.mm(ps_s[r][:, 0:N], bigI[:], maskT[mG][:, kt, q0:512], False, True, ["bigI", ("maskT", mG)], [("pss", r)])
                r3 = ctr["s"] % 3
                P.act(pe[r3][:, 0:N], ps_s[r][:, 0:N], AF.Exp, [("pss", r)], [("pe", r3)], scale=att_scale)
                P.mm(ps_o[o][:, q0:512], vaug[p][:, kt, e * 128:(e + 1) * 128], pe[r3][:, 0:N], kt == 0, kt == nkt - 1,
                     [("pe", r3), ("vaug", p)], [("pso", o)])
            so, ss_ = (slice(0, 64), slice(64, 128)) if e == 0 else (slice(64, 128), slice(0, 64))
            P.act(rc[o][ss_, :], ps_o[o][ss_, :], AF.Ln, [("pso", o)], [("rc", o)])
            P.act(rc[o][ss_, :], rc[o][ss_, :], AF.Exp, [("rc", o)], [("rc", o)], scale=-1.0)
            P.tt("vector", ybg[ys][so, :], ps_o[o][so, :], rc[o][ss_, :], ALU.mult, [("pso", o), ("rc", o)], [("ybg", ys)])
        P.dma("sync", d["yabT"][b, 4 + c, :, gsl0:gsl0 + 512], ybg[ys][:], r=[("ybg", ys)], key=("ybg", ys), is_out=True)

    groups = [(b, G) for G in range(4) for b in range(NSEQ)]
    for b in range(NSEQ):
        load_seq(b)
    for gi_, (b, G) in enumerate(groups):
        mslot[(b, G)] = gi_ % 2
        for pr in range(2):
            score_pair(b, G, (2 * pr, 2 * pr + 1))
            if gi_ >= 1:
                pb, pG = groups[gi_ - 1]
                attn_chunk(pb, pG, 2 * pr)
                attn_chunk(pb, pG, 2 * pr + 1)
    pb, pG = groups[-1]
    for c in range(4):
        attn_chunk(pb, pG, c)
    P.close()


def phase_D(B, tag, yT, wname, resid, gname, hmid, hnT_name):
    nc, d, NSEQ = B.nc, B.t, B.NSEQ
    P = Phase(nc, tag)
    Wo = P.sb("Wo", [128, 8, D], BF16)
    P.dma("gpsimd", Wo[:], d[wname].rearrange("(k p) n -> p k n", p=128), w=["Wo"], key="Wo")
    identb = P.sb("identb", [128, 128], BF16)
    P.dma("sync", identb[:], d["ident_bf"], w=["identb"], key="identb")
    gbc = P.sb("gbc", [128, D], F32)
    P.dma("sync", gbc[:], d[gname], w=["gbc"], key="gbc")
    eps_t = P.sb("eps", [128, 1], F32)
    P.memset("vector", eps_t[:], EPS, ["eps"])
    yt = [P.sb("yt%d" % i, [128, 8, 512], BF16) for i in range(2)]
    xt = [P.sb("xt%d" % i, [128, D], F32) for i in range(2)]
    hm = [P.sb("hm%d" % i, [128, D], F32) for i in range(2)]
    junk = P.sb("junk", [128, D], F32)
    stat = [P.sb("stat%d" % i, [128, 4], F32) for i in range(2)]
    hn = [P.sb("hn%d" % i, [128, D], BF16) for i in range(2)]
    hnTg = [P.sb("hnTg%d" % i, [128, 8, 512], BF16) for i in range(2)]
    ps_o = [P.ps("pso%d" % i, [128, 1024]) for i in range(2)]
    ps_tr = [P.ps("ptr%d" % i, [128, 8, 128], BF16) for i in range(2)]
    ngr = NSEQ * 4

    def load_y(gi):
        b, g = gi // 4, gi % 4
        P.dma("sync", yt[gi % 2][:], d[yT][b, :, :, g * 512:(g + 1) * 512].rearrange("c p t -> p c t"), w=[("yt", gi % 2)], key=("yt", gi % 2))

    load_y(0)

    def outproj(ti):
        gi, t4 = ti // 4, ti % 4
        b, g = gi // 4, gi % 4
        gs = gi % 2
        if t4 == 0 and gi + 1 < ngr:
            load_y(gi + 1)
        tt = g * 4 + t4
        r0 = b * SEQ + tt * 128
        tok = slice(t4 * 128, (t4 + 1) * 128)
        s2 = ti % 2
        P.dma("sync", xt[s2][:], d[resid][r0:r0 + 128, :], w=[("x", s2)], key=("x", s2))
        for half in range(2):
            for c in range(8):
                P.mm(ps_o[s2][:, half * 512:(half + 1) * 512], yt[gs][:, c, tok], Wo[:, c, half * 512:(half + 1) * 512],
                     c == 0, c == 7, [("yt", gs), "Wo"], [("pso", s2)], mark=(c == 7 and half == 1))

    def post(ti):
        gi, t4 = ti // 4, ti % 4
        b, g = gi // 4, gi % 4
        gs = gi % 2
        tt = g * 4 + t4
        r0 = b * SEQ + tt * 128
        tok = slice(t4 * 128, (t4 + 1) * 128)
        s2 = ti % 2
        for half in range(2):
            hs_ = slice(half * 512, (half + 1) * 512)
            P.tt("vector", hm[s2][:, hs_], ps_o[s2][:, hs_], xt[s2][:, hs_], ALU.add, [("pso", s2), ("x", s2)], [("hm", s2)])
        P.dma("sync", d[hmid][r0:r0 + 128, :], hm[s2][:], r=[("hm", s2)], key=("hm", s2), is_out=True)
        rms_to_hnT(P, hm[s2][:], ("hm", s2), gbc[:], eps_t, stat[s2], ("st", s2), junk, hn[s2][:], ("hn", s2),
                   ps_tr[s2], ("ptr", s2), identb, hnTg[gs], ("hnTg", gs), tok)
        if t4 == 3:
            P.dma("sync", d[hnT_name][:, :, gi * 512:(gi + 1) * 512].rearrange("c p t -> p c t"), hnTg[gs][:], r=[("hnTg", gs)],
                  key=("hnTg", gs), is_out=True)

    ntl = ngr * 4
    outproj(0)
    for ti in range(ntl):
        if ti + 1 < ntl:
            outproj(ti + 1)
        post(ti)
    P.close()


def phase_F(B, tag, layer, f0, h_in, h_out, hnT_name, final_g=None):
    nc, d, NSEQ = B.nc, B.t, B.NSEQ
    P = Phase(nc, tag)
    nf = 11
    Wg = P.sb("Wg", [128, 8, nf * 128], BF16)
    Wu = P.sb("Wu", [128, 8, nf * 128], BF16)
    Wd = P.sb("Wd", [128, nf, D], BF16)
    pieces = [(0, 3), (3, 6), (6, 9), (9, 11)]
    pf = {f: pi for pi, (fa, fb) in enumerate(pieces) for f in range(fa, fb)}
    srcg = d["ffn_w_gate%d" % layer].rearrange("(k p) n -> p k n", p=128)
    srcu = d["ffn_w_up%d" % layer].rearrange("(k p) n -> p k n", p=128)
    srcd = d["ffn_w_down%d" % layer].rearrange("(f p) n -> p f n", p=128)
    for pi, (fa, fb) in enumerate(pieces):
        P.dma("gpsimd", Wg[:, :, fa * 128:fb * 128], srcg[:, :, (f0 + fa) * 128:(f0 + fb) * 128], w=[("Wg", pi)], key=("Wg", pi))
        P.dma("gpsimd", Wu[:, :, fa * 128:fb * 128], srcu[:, :, (f0 + fa) * 128:(f0 + fb) * 128], w=[("Wu", pi)], key=("Wu", pi))
    for pi, (fa, fb) in enumerate(pieces):
        P.dma("gpsimd", Wd[:, fa:fb, :], srcd[:, f0 + fa:f0 + fb, :], w=[("Wd", pi)], key=("Wd", pi))
    if final_g is not None:
        gbc = P.sb("gbc", [128, D], F32)
        P.dma("sync", gbc[:], d[final_g], w=["gbc"], key="gbc")
        eps_t = P.sb("eps", [128, 1], F32)
        P.memset("vector", eps_t[:], EPS, ["eps"])
        junk = P.sb("junk", [128, D], F32)
        stat = [P.sb("stat%d" % i, [128, 4], F32) for i in range(2)]
    hnTg = [P.sb("hnTg%d" % i, [128, 8, 512], BF16) for i in range(2)]
    actT = [P.sb("actT%d" % i, [128, nf, 512], BF16) for i in range(2)]
    sg = [P.sb("sg%d" % i, [128, 512], F32) for i in range(2)]
    hin = [P.sb("hin%d" % i, [128, D], F32) for i in range(2)]
    hout = [P.sb("hout%d" % i, [128, D], F32) for i in range(2)]
    ps_g = [P.ps("psg%d" % i, [128, 512]) for i in range(2)]
    ps_u = [P.ps("psu%d" % i, [128, 512]) for i in range(2)]
    ps_d = [P.ps("psd%d" % i, [128, 1024]) for i in range(2)]
    ngr = NSEQ * 4

    def load_h(gi):
        P.dma("sync", hnTg[gi % 2][:], d[hnT_name][:, :, gi * 512:(gi + 1) * 512].rearrange("c p t -> p c t"),
              w=[("hnTg", gi % 2)], key=("hnTg", gi % 2))

    load_h(0)
    for gi in range(ngr):
        gs = gi % 2
        if gi + 1 < ngr:
            load_h(gi + 1)
        for f in range(nf):
            s2 = f % 2
            for k in range(8):
                P.mm(ps_g[s2][:, :], Wg[:, k, f * 128:(f + 1) * 128], hnTg[gs][:, k, :], k == 0, k == 7, [("hnTg", gs), ("Wg", pf[f])], [("psg", s2)])
            for k in range(8):
                P.mm(ps_u[s2][:, :], Wu[:, k, f * 128:(f + 1) * 128], hnTg[gs][:, k, :], k == 0, k == 7, [("hnTg", gs), ("Wu", pf[f])], [("psu", s2)])
            P.act(sg[s2][:], ps_g[s2][:, :], AF.Silu, [("psg", s2)], [("sg", s2)])
            P.tt("vector", actT[gs][:, f, :], ps_u[s2][:, :], sg[s2][:], ALU.mult, [("psu", s2), ("sg", s2)], [("actT", gs)])
        for t4 in range(4):
            r0 = gi * 512 + t4 * 128
            tok = slice(t4 * 128, (t4 + 1) * 128)
            s2 = t4 % 2
            P.dma("sync", hin[s2][:], d[h_in][r0:r0 + 128, :], w=[("hin", s2)], key=("hin", s2))
            for half in range(2):
                for f in range(nf):
                    P.mm(ps_d[s2][:, half * 512:(half + 1) * 512], actT[gs][:, f, tok], Wd[:, f, half * 512:(half + 1) * 512],
                         f == 0, f == nf - 1, [("actT", gs), ("Wd", pf[f])], [("psd", s2)], mark=(f == nf - 1 and half == 1))
            for half in range(2):
                hs_ = slice(half * 512, (half + 1) * 512)
                P.tt("vector", hout[s2][:, hs_], ps_d[s2][:, hs_], hin[s2][:, hs_], ALU.add, [("psd", s2), ("hin", s2)], [("hout", s2)])
            if final_g is not None:
                sk = ("st", s2)
                P.act(junk[:], hout[s2][:], AF.Square, [("hout", s2)], ["junkA", sk], accum_out=stat[s2][:, 0:1])
                P.act(stat[s2][:, 1:2], stat[s2][:, 0:1], AF.Sqrt, [sk], [sk], scale=1.0 / D, bias=eps_t[:, 0:1])
                P.recip(stat[s2][:, 2:3], stat[s2][:, 1:2], [sk], [sk])
                P.stt(hout[s2][:], hout[s2][:], stat[s2][:, 2:3], gbc[:], ALU.mult, ALU.mult, [("hout", s2), sk, "gbc"], [("hout", s2)])
            P.dma("sync", d[h_out][r0:r0 + 128, :], hout[s2][:], r=[("hout", s2)], key=("hout", s2), is_out=True)
    P.close()


def phase_E(B):
    nc, d, NSEQ = B.nc, B.t, B.NSEQ
    P = Phase(nc, "E")
    Wc = P.sb("Wc", [128, 8, 3104], BF16)
    Wsrc = d["c_w_in"].rearrange("(k p) n -> p k n", p=128)
    WK = []
    P.memset("gpsimd", Wc[:, :, 3088:3104], 0.0, [("W", 2)])
    for i, (c0, c1) in enumerate(((0, 1024), (1024, 2048), (2048, 3088))):
        P.dma("gpsimd", Wc[:, :, c0:c1], Wsrc[:, :, c0:c1], w=[("W", i)], key=("W", i))
        WK.append(("W", i))
    identb = P.sb("identb", [128, 128], BF16)
    P.dma("sync", identb[:], d["ident_bf"], w=["identb"], key="identb")
    gbc = P.sb("gbc", [128, D], F32)
    P.dma("sync", gbc[:], d["g_mix1"], w=["gbc"], key="gbc")
    g2w = P.sb("g2w", [128, 512], F32)
    P.memset("vector", g2w[:], 0.0, ["g2w"])
    P.dma("sync", g2w[0:16, :], d["c_gate_w"], w=["g2w"], key="g2w")
    g2b = P.sb("g2b", [128, 512], F32)
    P.dma("sync", g2b[:], d["c_gate_b_bc"], w=["g2b"], key="g2b")
    eps_t = P.sb("eps", [128, 1], F32)
    P.memset("vector", eps_t[:], EPS, ["eps"])
    one_t = P.sb("one", [128, 1], F32)
    P.memset("vector", one_t[:], 1.0, ["one"])
    xt = [P.sb("xt%d" % i, [128, D], F32) for i in range(2)]
    junk = P.sb("junk", [128, D], F32)
    stat = [P.sb("stat%d" % i, [128, 4], F32) for i in range(2)]
    hn = [P.sb("hn%d" % i, [128, D], BF16) for i in range(2)]
    hnT = [P.sb("hnT%d" % i, [128, 8, 512], BF16) for i in range(2)]
    qkT = [P.sb("qkT%d" % i, [128, 8, 512], BF16) for i in range(2)]
    glrT = [P.sb("glrT%d" % i, [128, 512], F32) for i in range(2)]
    for i in range(2):
        P.memset("vector", glrT[i][:], 0.0, [("glrT", i)])
    kst = [P.sb("kst%d" % i, [128, 512], BF16) for i in range(2)]
    vst = [P.sb("vst%d" % i, [128, 1024], BF16) for i in range(2)]
    rst = [P.sb("rst%d" % i, [128, 1024], F32) for i in range(2)]
    lat = [P.sb("lat%d" % i, [128, 512], F32) for i in range(2)]
    lt1 = P.sb("lt1", [128, 512], F32)
    lt2 = P.sb("lt2", [128, 512], F32)
    ps_tr = P.ps("ptr", [128, 8, 128], BF16)
    ps_f = [P.ps("psf%d" % i, [128, 512]) for i in range(2)]
    ps_t = [P.ps("pst%d" % i, [128, 512]) for i in range(3)]
    ps_l = P.ps("psl", [128, 512])
    n_t = 0
    n_f = 0
    n_tiles = NSEQ * NT

    def load_x(ti):
        P.dma("sync", xt[ti % 2][:], d["h1"][ti * 128:(ti + 1) * 128, :], w=[("x", ti % 2)], key=("x", ti % 2))

    load_x(0)

    def norm_group(gi):
        gs = gi % 2
        for t4 in range(4):
            ti = gi * 4 + t4
            if ti + 1 < n_tiles:
                load_x(ti + 1)
            xs = ti % 2
            rms_to_hnT(P, xt[xs][:], ("x", xs), gbc[:], eps_t, stat[xs], ("st", xs), junk, hn[xs][:], ("hn", xs),
                       ps_tr, "ptr", identb, hnT[gs], ("hnT", gs), slice(t4 * 128, (t4 + 1) * 128))

    def mm_group(gi):
        nonlocal n_t, n_f
        b, g = gi // 4, gi % 4
        gs = gi % 2
        for oc in range(8):
            r = n_f % 2
            n_f += 1
            for k in range(8):
                P.mm(ps_f[r][:, :], Wc[:, k, oc * 128:(oc + 1) * 128], hnT[gs][:, k, :], k == 0, k == 7, [("hnT", gs), ("W", 0)], [("psf", r)])
            if oc < 4:
                P.act(qkT[gs][:, oc, :], ps_f[r][:, :], AF.Copy, [("psf", r)], [("qkT", gs)], scale=128 ** -0.5)
            else:
                P.copy("vector", qkT[gs][:, oc, :], ps_f[r][:, :], [("psf", r)], [("qkT", gs)])
        r = n_f % 2
        n_f += 1
        for k in range(8):
            P.mm(ps_f[r][0:32, :], Wc[:, k, 3072:3104], hnT[gs][:, k, :], k == 0, k == 7, [("hnT", gs), ("W", 2)], [("psf", r)])
        P.copy("vector", glrT[gs][0:32, :], ps_f[r][0:32, :], [("psf", r)], [("glrT", gs)])
        gsl = slice(g * 512, (g + 1) * 512)
        P.dma("sync", d["c_qT"][b, :, :, gsl].rearrange("c p t -> p c t"), qkT[gs][:, 0:4, :], r=[("qkT", gs)], key=("qTo", gs), is_out=True)
        P.dma("sync", d["c_kT"][b, :, :, gsl].rearrange("c p t -> p c t"), qkT[gs][:, 4:8, :], r=[("qkT", gs)], key=("kTo", gs), is_out=True)
        for t4 in range(4):
            r0 = gi * 512 + t4 * 128
            tok = slice(t4 * 128, (t4 + 1) * 128)
            s2 = t4 % 2
            jobs = [(512, 1024, kst[s2][:, :], ("kst", s2), "k"),
                    (1024, 1536, vst[s2][:, 0:512], ("vst", s2), "v"), (1536, 2048, vst[s2][:, 512:1024], ("vst", s2), "v"),
                    (2048, 2560, rst[s2][:, 0:512], ("rst", s2), "r"), (2560, 3072, rst[s2][:, 512:1024], ("rst", s2), "r")]
            for (c0, c1, dst, dk, kind) in jobs:
                r = n_t % 3
                n_t += 1
                for k in range(8):
                    P.mm(ps_t[r][:, :], hnT[gs][:, k, tok], Wc[:, k, c0:c1], k == 0, k == 7, [("hnT", gs), ("W", c0 // 1024)], [("pst", r)])
                if kind == "r":
                    P.act(dst, ps_t[r][:, :], AF.Silu, [("pst", r)], [dk])
                elif kind == "v":
                    P.copy("vector", dst, ps_t[r][:, :], [("pst", r)], [dk])
                else:
                    P.copy("scalar", dst, ps_t[r][:, :], [("pst", r)], [dk])
            P.dma("sync", d["c_ktok"][r0:r0 + 128, :], kst[s2][:], r=[("kst", s2)], key=("kst", s2), is_out=True)
            P.dma("sync", d["c_v"][r0:r0 + 128, :], vst[s2][:], r=[("vst", s2)], key=("vst", s2), is_out=True)
            P.dma("sync", d["c_sr"][r0:r0 + 128, :], rst[s2][:], r=[("rst", s2)], key=("rst", s2), is_out=True)
            P.mm(ps_l[:, :], glrT[gs][:, tok], g2w[:, :], True, True, [("glrT", gs), "g2w"], ["psl"])
            P.tt("vector", lt1[:], ps_l[:, :], g2b[:], ALU.add, ["psl", "g2b"], ["lt1"])
            P.act(lt2[:], lt1[:], AF.Exp, ["lt1"], ["lt2"], scale=-1.0)
            P.act(lt1[:], lt2[:], AF.Ln, ["lt2", "one"], ["lt1"], bias=one_t[:, 0:1], scale=1.0)
            P.ts("gpsimd", lat[s2][:], lt1[:], -1.0 / 16.0, ALU.mult, ["lt1"], [("lat", s2)])
            P.dma("sync", d["c_la"][r0:r0 + 128, :], lat[s2][:], r=[("lat", s2)], key=("lat", s2), is_out=True)

    ngr = NSEQ * 4
    norm_group(0)
    for gi in range(ngr):
        if gi + 1 < ngr:
            norm_group(gi + 1)
        mm_group(gi)
    P.close()


def phase_G(B):
    nc, d, NSEQ = B.nc, B.t, B.NSEQ
    P = Phase(nc, "G")
    identb = P.sb("identb", [128, 128], BF16)
    P.dma("sync", identb[:], d["ident_bf"], w=["identb"], key="identb")
    U = P.sb("U", [128, 128], F32)
    SL = P.sb("SL", [128, 128], F32)
    P.dma("sync", U[:], d["tri_u"], w=["U"], key="U")
    P.dma("sync", SL[:], d["tri_sl"], w=["SL"], key="SL")
    gon = P.sb("gon", [128, D], F32)
    P.dma("sync", gon[:], d["c_onorm_g_bc"], w=["gon"], key="gon")
    eps_t = P.sb("eps", [128, 1], F32)
    P.memset("vector", eps_t[:], EPS, ["eps"])
    qT = P.sb("qT", [128, 4, SEQ], BF16)
    kT = P.sb("kT", [128, 4, SEQ], BF16)
    la = [P.sb("la%d" % i, [128, 512], F32) for i in range(2)]
    kt_ = [P.sb("ktk%d" % i, [128, 512], BF16) for i in range(2)]
    vt = [P.sb("vt%d" % i, [128, 1024], BF16) for i in range(3)]
    sr = [P.sb("sr%d" % i, [128, 1024], F32) for i in range(2)]
    eb = [P.sb("eb%d" % i, [128, 512], F32) for i in range(2)]
    enb = P.sb("enb", [128, 512], F32)
    erv = P.sb("erv", [128, 512], F32)
    qtT = [P.sb("qtT%d" % i, [128, 512], BF16) for i in range(2)]
    ktT = [P.sb("ktT%d" % i, [128, 512], BF16) for i in range(2)]
    ks = [P.sb("ks%d" % i, [128, 512], BF16) for i in range(2)]
    attm = [P.sb("attm%d" % i, [128, 128], BF16) for i in range(2)]
    state = P.sb("state", [128, 4, 256], F32)
    stb = P.sb("stb", [128, 4, 256], BF16)
    gr = [P.sb("gr%d" % i, [128, 1024], F32) for i in range(2)]
    stat = [P.sb("stat%d" % i, [128, 12], F32) for i in range(2)]
    junk = P.sb("junk", [128, 256], F32)
    yc = [P.sb("yc%d" % i, [128, 1024], BF16) for i in range(2)]
    ycT = [P.sb("ycT%d" % i, [128, 8, 512], BF16) for i in range(2)]
    ps_b = P.ps("psb", [128, 512])
    ps_r = P.ps("psr", [128, 512])
    ps_a = P.ps("psa", [128, 4, 128])
    ps_o = P.ps("pso", [128, 1024])
    ps_kv = P.ps("pskv", [128, 1024])
    ps_tr = P.ps("ptr", [128, 8, 128], BF16)
    nch = NSEQ * NT

    def load_chunk(ci):
        s2 = ci % 2
        r0 = ci * 128
        P.dma("sync", la[s2][:], d["c_la"][r0:r0 + 128, :], w=[("la", s2)], key=("la", s2))
        P.dma("sync", kt_[s2][:], d["c_ktok"][r0:r0 + 128, :], w=[("ktk", s2)], key=("ktk", s2))
        P.dma("sync", vt[ci % 3][:], d["c_v"][r0:r0 + 128, :], w=[("vt", ci % 3)], key=("vt", ci % 3))
        P.dma("sync", sr[s2][:], d["c_sr"][r0:r0 + 128, :], w=[("sr", s2)], key=("sr", s2))

    enb2 = [enb, P.sb("enbb", [128, 512], F32)]
    erv2 = [erv, P.sb("ervb", [128, 512], F32)]
    attm4 = attm + [P.sb("attm%d" % i, [128, 128], BF16) for i in range(2, 4)]

    def prep(ci):
        b, c = ci // NT, ci % NT
        s2 = ci % 2
        if c == 0:
            P.dma("sync", qT[:], d["c_qT"][b].rearrange("c p t -> p c t"), w=["qT"], key="qT")
            P.dma("sync", kT[:], d["c_kT"][b].rearrange("c p t -> p c t"), w=["kT"], key="kT")
        if ci + 1 < nch:
            load_chunk(ci + 1)
        csl = slice(c * 128, (c + 1) * 128)
        for h in range(4):
            P.mm(ps_b[:, h * 128:(h + 1) * 128], la[s2][:, h * 128:(h + 1) * 128], U[:], True, True, [("la", s2), "U"], ["psb"], mark=(h == 3))
        P.mm(ps_r[:, :], SL[:], la[s2][:, :], True, True, ["SL", ("la", s2)], ["psr"])
        P.act(eb[s2][:], ps_b[:, :], AF.Exp, ["psb"], [("eb", s2)])
        P.act(enb2[s2][:], ps_b[:, :], AF.Exp, ["psb"], [("enb", s2)], scale=-1.0)
        P.act(erv2[s2][:], ps_r[:, :], AF.Exp, ["psr"], [("erv", s2)])
        qv = qT[:, :, csl]
        kv_ = kT[:, :, csl]
        P.tt("vector", qtT[s2][:].rearrange("p (h t) -> p h t", t=128), qv, eb[s2][:].rearrange("p (h t) -> p h t", t=128), ALU.mult,
             ["qT", ("eb", s2)], [("qtT", s2)])
        P.tt("gpsimd", ktT[s2][:].rearrange("p (h t) -> p h t", t=128), kv_, enb2[s2][:].rearrange("p (h t) -> p h t", t=128), ALU.mult,
             ["kT", ("enb", s2)], [("ktT", s2)])
        P.tt("gpsimd", ks[s2][:], kt_[s2][:], erv2[s2][:], ALU.mult, [("ktk", s2), ("erv", s2)], [("ks", s2)])
        P.tt("gpsimd", gr[s2][:], sr[s2][:], gon[:], ALU.mult, [("sr", s2), "gon"], [("gr", s2)])

    def rec(ci):
        b, c = ci // NT, ci % NT
        s2 = ci % 2
        if c == 0:
            P.memset("vector", state[:], 0.0, [("state", h) for h in range(4)])
            P.memset("gpsimd", stb[:], 0.0, [("stb", h) for h in range(4)])
        HS = [slice(h * 128, (h + 1) * 128) for h in range(4)]
        VS = [slice(h * 256, (h + 1) * 256) for h in range(4)]
        for h in range(4):
            P.mm(ps_a[:, h, :], ktT[s2][:, HS[h]], qtT[s2][:, HS[h]], True, True, [("ktT", s2), ("qtT", s2)], ["psa"], mark=(h == 3))
        for h in range(4):
            P.tt("vector", attm4[h][:], ps_a[:, h, :], U[:], ALU.mult, ["psa", "U"], [("attm", h)])
        for h in range(4):
            P.mm(ps_o[:, VS[h]], attm4[h][:], vt[ci % 3][:, VS[h]], True, False, [("attm", h), ("vt", ci % 3)], ["pso"], mark=False)
            P.mm(ps_o[:, VS[h]], qtT[s2][:, HS[h]], stb[:, h, :], False, True, [("qtT", s2), ("stb", h)], ["pso"], mark=(h == 3))
        for h in range(4):
            P.mm(ps_kv[:, VS[h]], ks[s2][:, HS[h]], vt[ci % 3][:, VS[h]], True, True, [("ks", s2), ("vt", ci % 3)], ["pskv"], mark=(h == 3))
        for h in range(4):
            P.stt(state[:, h, :], state[:, h, :], eb[s2][:, h * 128 + 127:h * 128 + 128], ps_kv[:, VS[h]], ALU.mult, ALU.add,
                  [("state", h), ("eb", s2), "pskv"], [("state", h)])
        for h in range(4):
            P.copy("scalar", stb[:, h, :], state[:, h, :], [("state", h)], [("stb", h)])
        sk = ("st", s2)
        for h in range(4):
            P.act(junk[:], ps_o[:, VS[h]], AF.Square, ["pso"], ["junkA", sk], accum_out=stat[s2][:, h:h + 1])
        P.act(stat[s2][:, 4:8], stat[s2][:, 0:4], AF.Sqrt, [sk, "eps"], [sk], scale=1.0 / 256, bias=eps_t[:, 0:1])
        P.recip(stat[s2][:, 8:12], stat[s2][:, 4:8], [sk], [sk])
        for h in range(4):
            P.stt(yc[s2][:, VS[h]], ps_o[:, VS[h]], stat[s2][:, 8 + h:9 + h], gr[s2][:, VS[h]], ALU.mult, ALU.mult,
                  ["pso", sk, ("gr", s2)], [("yc", s2)])
        for k in range(8):
            P.tr(ps_tr[:, k, :], yc[s2][:, k * 128:(k + 1) * 128], identb[:], [("yc", s2), "identb"], ["ptr"], mark=(k == 7))
        g4 = (ci // 4) % 2
        P.copy("scalar", ycT[g4][:, 0:8, (ci % 4) * 128:(ci % 4 + 1) * 128], ps_tr[:, 0:8, :], ["ptr"], [("ycT", g4)])
        if ci % 4 == 3:
            g = c // 4
            P.dma("sync", d["ycT"][b, :, :, g * 512:(g + 1) * 512].rearrange("c p t -> p c t"), ycT[g4][:], r=[("ycT", g4)],
                  key=("ycT", g4), is_out=True)

    load_chunk(0)
    prep(0)
    for ci in range(nch):
        if ci + 1 < nch:
            prep(ci + 1)
        rec(ci)
    P.close()


def declare(B):
    NSEQ = B.NSEQ
    T = NSEQ * SEQ
    X = "ExternalInput"
    B.dram("x", [T, D], F32, X)
    B.dram("ab_w_in", [D, 2248], F32, X)
    B.dram("ab_conv_w", [31, 512], F32, X)
    B.dram("conv_prm", [128, 12], F32, X)
    B.dram("ab_w_out", [D, D], F32, X)
    B.dram("c_w_in", [D, 3088], F32, X)
    B.dram("c_gate_w", [16, 512], F32, X)
    B.dram("c_gate_b_bc", [128, 512], F32, X)
    B.dram("c_onorm_g_bc", [128, D], F32, X)
    B.dram("c_w_out", [D, D], F32, X)
    for l in range(2):
        B.dram("ffn_w_gate%d" % l, [D, DFF], F32, X)
        B.dram("ffn_w_up%d" % l, [D, DFF], F32, X)
        B.dram("ffn_w_down%d" % l, [DFF, D], F32, X)
        B.dram("g_mix%d" % l, [128, D], F32, X)
        B.dram("g_ffn%d" % l, [128, D], F32, X)
    B.dram("g_final", [128, D], F32, X)
    B.dram("ident_bf", [128, 128], BF16, X)
    B.dram("ident_f32", [128, 128], F32, X)
    B.dram("cos128", [SEQ, 128], F32, X)
    B.dram("sin128", [SEQ, 128], F32, X)
    B.dram("negmask", [128, 128], F32, X)
    B.dram("pow2", [128, NIT], F32, X)
    B.dram("tri_u", [128, 128], F32, X)
    B.dram("tri_sl", [128, 128], F32, X)
    B.dram("hconvT", [NSEQ, 4, 128, SEQ], BF16)
    B.dram("qT", [NSEQ, 4, 128, SEQ], BF16)
    B.dram("iqT", [NSEQ, 4, 128, SEQ], BF16)
    B.dram("kkT", [NSEQ, 2, 128, SEQ], BF16)
    B.dram("vaug", [T, 256], BF16)
    B.dram("iw", [T, 8], F32)
    B.dram("yabT", [NSEQ, 8, 128, SEQ], BF16)
    B.dram("h_mid0", [T, D], F32)
    B.dram("hnT", [8, 128, T], BF16)
    B.dram("h_half", [T, D], F32)
    B.dram("h1", [T, D], F32)
    B.dram("c_qT", [NSEQ, 4, 128, SEQ], BF16)
    B.dram("c_kT", [NSEQ, 4, 128, SEQ], BF16)
    B.dram("c_ktok", [T, 512], BF16)
    B.dram("c_v", [T, D], BF16)
    B.dram("c_sr", [T, D], F32)
    B.dram("c_la", [T, 512], F32)
    B.dram("ycT", [NSEQ, 8, 128, SEQ], BF16)
    B.dram("h_mid1", [T, D], F32)
    B.dram("out", [T, D], F32, "ExternalOutput")
    if DEBUG_STOP == 77:
        B.dram("dbg", [128, 16], F32, "ExternalOutput")
        B.dram("dbg2", [4, 128, D], BF16, "ExternalOutput")


PHASES = {
    "A": phase_A,
    "B": phase_B,
    "C": phase_C,
    "D0": lambda B: phase_D(B, "D0", "yabT", "ab_w_out", "x", "g_ffn0", "h_mid0", "hnT"),
    "F0a": lambda B: phase_F(B, "F0a", 0, 0, "h_mid0", "h_half", "hnT"),
    "F0b": lambda B: phase_F(B, "F0b", 0, 11, "h_half", "h1", "hnT"),
    "E": phase_E,
    "G": phase_G,
    "D1": lambda B: phase_D(B, "D1", "ycT", "c_w_out", "h1", "g_ffn1", "h_mid1", "hnT"),
    "F1a": lambda B: phase_F(B, "F1a", 1, 0, "h_mid1", "h_half", "hnT"),
    "F1b": lambda B: phase_F(B, "F1b", 1, 11, "h_half", "out", "hnT", final_g="g_final"),
}
ORDER = ["A", "B", "C", "D0", "F0a", "F0b", "E", "G", "D1", "F1a", "F1b"]


def host_consts():
    bf = ml_dtypes.bfloat16
    c = {}
    c["ident_bf"] = np.eye(128, dtype=np.float32).astype(bf)
    c["ident_f32"] = np.eye(128, dtype=np.float32)
    rot = 16
    inv = (500000.0 ** (-np.arange(0, rot, 2, dtype=np.float32) / np.float32(rot))).astype(np.float32)
    ang = (np.arange(SEQ, dtype=np.float32)[:, None] * inv[None, :]).astype(np.float32)
    c["cos128"] = np.ascontiguousarray(np.tile(np.cos(ang).astype(np.float32), (1, 16)))
    c["sin128"] = np.ascontiguousarray(np.tile(np.sin(ang).astype(np.float32), (1, 16)))
    i = np.arange(128)
    c["negmask"] = np.where(i[None, :] <= i[:, None], 0.0, -1e30).astype(np.float32)
    c["pow2"] = np.ascontiguousarray(np.broadcast_to((0.5 ** np.arange(1, NIT + 1)).astype(np.float32), (128, NIT)))
    c["tri_u"] = (i[:, None] <= i[None, :]).astype(np.float32)
    c["tri_sl"] = (i[:, None] > i[None, :]).astype(np.float32)
    return c


def host_weights(inp):
    f = lambda a: np.ascontiguousarray(np.asarray(a, dtype=np.float32))
    bc = lambda v, n: np.ascontiguousarray(np.broadcast_to(np.asarray(v, np.float32).reshape(1, -1), (128, n)))
    w = {}
    w["ab_w_in"] = f(inp["ab_w_in"][0])
    w["ab_conv_w"] = f(inp["ab_conv_w"][0].reshape(31, 512))
    prm = np.concatenate([np.asarray(inp[k][0], np.float32).reshape(4, 128).T for k in ("ab_conv_b", "ab_ln_g", "ab_ln_b")], axis=1)
    w["conv_prm"] = f(prm)
    w["ab_w_out"] = f(inp["ab_w_out"][0])
    w["c_w_in"] = f(inp["c_w_in"][0])
    w["c_gate_w"] = f(inp["c_gate_w"][0])
    w["c_gate_b_bc"] = bc(inp["c_gate_b"][0], 512)
    w["c_onorm_g_bc"] = bc(inp["c_onorm_g"][0], D)
    w["c_w_out"] = f(inp["c_w_out"][0])
    for l in range(2):
        w["ffn_w_gate%d" % l] = f(inp["ffn_w_gate"][l])
        w["ffn_w_up%d" % l] = f(inp["ffn_w_up"][l])
        w["ffn_w_down%d" % l] = f(inp["ffn_w_down"][l])
        w["g_mix%d" % l] = bc(inp["norm_mix_g"][l], D)
        w["g_ffn%d" % l] = bc(inp["norm_ffn_g"][l], D)
    w["g_final"] = bc(inp["final_norm_g"], D)
    return w


def build_program(nseq, phases, ext=None):
    B = Build(nseq, ext)
    declare(B)
    for p in phases:
        PHASES[p](B)
    return B


def kernel(**inp):
    n = 8
    nseq = 2
    x = np.asarray(inp["x"], dtype=np.float32)
    B = build_program(nseq, ORDER)
    shared = host_consts()
    shared.update(host_weights(inp))
    in_maps = []
    for c in range(n):
        m = dict(shared)
        m["x"] = np.ascontiguousarray(x[c * nseq:(c + 1) * nseq].reshape(nseq * SEQ, D))
        in_maps.append(m)
    res = run_bass_kernel_spmd(B.nc, in_maps, core_ids=list(range(n)))
    out = np.stack([np.asarray(r["out"], dtype=np.float32).reshape(nseq, SEQ, D) for r in res.results], axis=0)
    return out.reshape(16, SEQ, D)
```

```python
import contextlib
import math
import numpy as np
import ml_dtypes
import concourse.bass as bass
import concourse.mybir as mybir
from concourse.bass_utils import run_bass_kernel_spmd

F32 = mybir.dt.float32
BF16 = mybir.dt.bfloat16
ALU = mybir.AluOpType
AF = mybir.ActivationFunctionType
AX = mybir.AxisListType

ENGS = ["sync", "scalar", "vector", "gpsimd", "tensor"]
D = 1024
SEQ = 2048
NT = 16
EPS = 1e-6
DFF = 2816
NFF = 22
NIT = 16
TOPK = 256
DEBUG_STOP = 99
SAME_SYNC = True


class Sched:
    def __init__(self, nc, stack, tag=""):
        self.nc = nc
        self.stack = stack
        self.tag = tag
        self.q = {e: [] for e in ENGS}
        self.sem = {}
        self.cnt = {}
        self.waited = {e: {} for e in ENGS}
        self.lastw = {}
        self.readers = {}
        self.pe_pending = False
        self.out_deps = {}
        for e in ENGS:
            self._mksem("E_" + e)

    def _mksem(self, name):
        if name not in self.sem:
            hw_name = "%s_s%d" % (self.tag, len(self.sem))
            self.sem[name] = self.nc.alloc_semaphore(name=hw_name)
            self.cnt[name] = 0
        return name

    def op(self, eng, fn, reads=(), writes=(), dma=None, mark=True, is_out=False, force_self=False):
        deps = {}

        def add(d):
            if d[1] > deps.get(d[0], 0):
                deps[d[0]] = d[1]

        for k in reads:
            if k in self.lastw:
                add(self.lastw[k])
        for k in writes:
            if k in self.lastw:
                add(self.lastw[k])
            for s_, v_ in self.readers.get(k, {}).items():
                add((s_, v_))
        if dma is not None:
            s = self._mksem("D_" + str(dma))
            if self.cnt[s] > 0:
                add((s, self.cnt[s]))
        own = "E_" + eng
        need = []
        for s_, v_ in deps.items():
            if s_ == own and (eng == "tensor" or not (SAME_SYNC or force_self)):
                continue
            if self.waited[eng].get(s_, 0) >= v_:
                continue
            need.append((s_, v_))
        attach = None
        if eng == "tensor":
            k0 = reads[0] if len(reads) else None
            if k0 is not None and k0 in self.lastw and self.lastw[k0][0] != own:
                attach = self.lastw[k0]
        else:
            selfs = [d_ for d_ in need if d_[0] == own]
            attach = selfs[0] if selfs else (need[-1] if need else None)
        standalone = [d_ for d_ in need if attach is None or d_[0] != attach[0]]
        if attach is not None:
            for d_ in need:
                if d_[0] == attach[0] and d_[1] > attach[1]:
                    attach = d_
        for d_ in need:
            self.waited[eng][d_[0]] = max(self.waited[eng].get(d_[0], 0), d_[1])
        if attach is not None:
            self.waited[eng][attach[0]] = max(self.waited[eng].get(attach[0], 0), attach[1])
        if dma is not None:
            self.cnt[s] += 16
            dep = (s, self.cnt[s])
            self.q[eng].append(("op", fn, s, 16, standalone, attach))
        else:
            s = own
            if mark:
                self.cnt[s] += 1
                dep = (s, self.cnt[s])
                self.q[eng].append(("op", fn, s, 1, standalone, attach))
                if eng == "tensor":
                    self.pe_pending = False
            else:
                assert eng == "tensor"
                dep = (s, self.cnt[s] + 1)
                self.q[eng].append(("op", fn, None, 0, standalone, attach))
                self.pe_pending = True
        for k in writes:
            self.lastw[k] = dep
            self.readers[k] = {}
        for k in reads:
            r = self.readers.setdefault(k, {})
            if dep[1] > r.get(dep[0], 0):
                r[dep[0]] = dep[1]
        if is_out:
            if dep[1] > self.out_deps.get(dep[0], 0):
                self.out_deps[dep[0]] = dep[1]
        return dep

    def finalize(self):
        assert not self.pe_pending, "unmarked PE op at end of phase"
        tail = []
        for s_, v_ in self.out_deps.items():
            if self.waited["sync"].get(s_, 0) < v_:
                tail.append((s_, v_))
        nc = self.nc
        with nc.Block() as block:
            for eng in ENGS:
                items = self.q[eng]

                def body(e, items=items, eng=eng):
                    for it in items:
                        for (ws, wv) in it[4]:
                            e.wait_ge(self.sem[ws], wv)
                        ins = it[1](e)
                        if it[5] is not None:
                            ins._wait_ge(self.sem[it[5][0]], it[5][1])
                        if it[2] is not None:
                            ins.then_inc(self.sem[it[2]], it[3])
                    if eng == "sync":
                        for (ws, wv) in tail:
                            e.wait_ge(self.sem[ws], wv)

                getattr(block, eng)(body)


class Phase:
    def __init__(self, nc, tag):
        self.nc = nc
        self.tag = tag
        self.st = contextlib.ExitStack()
        self.st.enter_context(nc.cleanup_on_exit())
        self.S = Sched(nc, self.st, tag)

    def sb(self, name, shape, dt):
        return self.st.enter_context(self.nc.sbuf_tensor(self.tag + "_" + name, shape, dt))

    def ps(self, name, shape, dt=F32):
        return self.st.enter_context(self.nc.psum_tensor(self.tag + "_" + name, shape, dt))

    def dma(self, eng, out, in_, r=(), w=(), key=None, is_out=False):
        return self.S.op(eng, lambda e: e.dma_start(out=out, in_=in_), r, w, dma=key, is_out=is_out)

    def mm(self, out, lhsT, rhs, start, stop, r, w, mark=None):
        if mark is None:
            mark = stop
        return self.S.op("tensor", lambda e: e.matmul(out, lhsT=lhsT, rhs=rhs, start=start, stop=stop), r, w, mark=mark)

    def tr(self, out, in_, ident, r, w, mark=True):
        return self.S.op("tensor", lambda e: e.transpose(out=out, in_=in_, identity=ident), r, w, mark=mark)

    def act(self, out, in_, func, r, w, **kw):
        return self.S.op("scalar", lambda e: e.activation(out=out, in_=in_, func=func, **kw), r, w)

    def tt(self, eng, out, in0, in1, op, r, w):
        return self.S.op(eng, lambda e: e.tensor_tensor(out=out, in0=in0, in1=in1, op=op), r, w)

    def ts(self, eng, out, in0, s1, op0, r, w, s2=None, op1=None, accum_out=None):
        if op1 is None:
            return self.S.op(eng, lambda e: e.tensor_scalar(out=out, in0=in0, scalar1=s1, scalar2=None, op0=op0), r, w)
        return self.S.op(eng, lambda e: e.tensor_scalar(out=out, in0=in0, scalar1=s1, scalar2=s2, op0=op0, op1=op1,
                                                       accum_out=accum_out), r, w)

    def stt(self, out, in0, scalar, in1, op0, op1, r, w):
        return self.S.op("vector", lambda e: e.scalar_tensor_tensor(out=out, in0=in0, scalar=scalar, in1=in1,
                                                                     op0=op0, op1=op1), r, w)

    def copy(self, eng, out, in_, r, w):
        if eng == "scalar":
            return self.S.op(eng, lambda e: e.copy(out=out, in_=in_), r, w)
        return self.S.op(eng, lambda e: e.tensor_copy(out=out, in_=in_), r, w)

    def recip(self, out, in_, r, w):
        return self.S.op("vector", lambda e: e.reciprocal(out=out, in_=in_), r, w)

    def memset(self, eng, ap, val, w):
        return self.S.op(eng, lambda e: e.memset(ap, val), (), w)

    def reduce(self, out, in_, op, r, w):
        return self.S.op("vector", lambda e: e.tensor_reduce(out=out, in_=in_, axis=AX.X, op=op), r, w)

    def close(self):
        self.S.finalize()
        self.st.close()


class Build:
    def __init__(self, nseq, ext=None):
        self.nc = bass.Bass("TRN2", target_bir_lowering=False)
        self.NSEQ = nseq
        self.t = {}
        self.ext = ext or {}
        self.kinds = {}

    def dram(self, name, shape, dt, kind="Internal"):
        kind = self.ext.get(name, kind)
        self.kinds[name] = (kind, list(shape), dt)
        self.t[name] = self.nc.dram_tensor(name, list(shape), dt, kind=kind).ap()
        return self.t[name]


def rms_to_hnT(P, xt, xkey, gbc, eps_t, stat, skey, junk, hn, hkey, ps_tr, pkey, identb, dst, dkey, sl):
    P.act(junk[:], xt, AF.Square, [xkey], ["junkA", skey], accum_out=stat[:, 0:1])
    P.act(stat[:, 1:2], stat[:, 0:1], AF.Sqrt, [skey, "eps"], [skey], scale=1.0 / D, bias=eps_t[:, 0:1])
    P.recip(stat[:, 2:3], stat[:, 1:2], [skey], [skey])
    P.stt(hn, xt, stat[:, 2:3], gbc, ALU.mult, ALU.mult, [xkey, skey, "gbc"], [hkey])
    for k in range(8):
        P.tr(ps_tr[:, k, :], hn[:, k * 128:(k + 1) * 128], identb[:], [hkey, "identb"], [pkey], mark=(k == 7))
    P.copy("scalar", dst[:, 0:8, sl], ps_tr[:, 0:8, :], [pkey], [dkey])


def phase_A(B):
    nc, d, NSEQ = B.nc, B.t, B.NSEQ
    P = Phase(nc, "A")
    Wt = P.sb("Wt", [128, 8, 2376], BF16)
    Wsrc = d["ab_w_in"].rearrange("(k p) n -> p k n", p=128)
    segs = [(0, 1024, 0), (1024, 1536, 1024), (1664, 2176, 1536), (1536, 1600, 2048), (1536, 1600, 2112),
            (2176, 2240, 2176), (2176, 2240, 2240), (1600, 1664, 2304), (2240, 2248, 2368)]
    WK = []
    for i, (c0, c1, d0) in enumerate(segs):
        P.dma("gpsimd", Wt[:, :, d0:d0 + c1 - c0], Wsrc[:, :, c0:c1], w=[("W", i)], key=("W", i))
        WK.append(("W", i))
    identb = P.sb("identb", [128, 128], BF16)
    P.dma("sync", identb[:], d["ident_bf"], w=["identb"], key="identb")
    gbc = P.sb("gbc", [128, D], F32)
    P.dma("sync", gbc[:], d["g_mix0"], w=["gbc"], key="gbc")
    cos = P.sb("cos", [128, NT, 128], F32)
    sin = P.sb("sin", [128, NT, 128], F32)
    P.dma("sync", cos[:], d["cos128"].rearrange("(t p) c -> p t c", p=128), w=["cos"], key="cos")
    P.dma("sync", sin[:], d["sin128"].rearrange("(t p) c -> p t c", p=128), w=["sin"], key="sin")
    eps_t = P.sb("eps", [128, 1], F32)
    P.memset("vector", eps_t[:], EPS, ["eps"])

    xt = [P.sb("xt%d" % i, [128, D], F32) for i in range(2)]
    junk = P.sb("junk", [128, D], F32)
    stat = [P.sb("stat%d" % i, [128, 4], F32) for i in range(2)]
    hn = [P.sb("hn%d" % i, [128, D], BF16) for i in range(2)]
    hnT = [P.sb("hnT%d" % i, [128, 8, 512], BF16) for i in range(2)]
    ps_tr = [P.ps("ptr%d" % i, [128, 8, 128], BF16) for i in range(2)]
    ps_a = P.ps("psa", [128, 512])
    ps_g = P.ps("psg", [128, 512])
    ps_q = P.ps("psq", [128, 1024])
    ps_c = P.ps("psc", [128, 512])
    sig = [P.sb("sig%d" % i, [128, 512], F32) for i in range(2)]
    hc = [P.sb("hc%d" % i, [128, 512], BF16) for i in range(2)]
    tmp = [P.sb("tmp%d" % i, [128, 128], F32) for i in range(4)]
    qr = [P.sb("qr%d" % i, [128, 1024], BF16) for i in range(2)]
    kr = [P.sb("kr%d" % i, [128, 256], BF16) for i in range(2)]
    vaug = [P.sb("vaug%d" % i, [128, 256], BF16) for i in range(2)]
    iwt = [P.sb("iw%d" % i, [128, 8], F32) for i in range(2)]
    for i in range(2):
        P.memset("gpsimd", vaug[i][:, 64:192], 1.0, [("vaug", i)])
    qTg = [P.sb("qTg%d" % i, [128, 8, 512], BF16) for i in range(2)]
    kTg = [P.sb("kTg%d" % i, [128, 2, 512], BF16) for i in range(2)]
    w_scale = (8 ** -0.5) * (64 ** -0.5)

    def load_x(b, tt):
        xs = tt % 2
        r0 = b * SEQ + tt * 128
        P.dma("sync", xt[xs][:], d["x"][r0:r0 + 128, :], w=[("x", xs)], key=("x", xs))

    n_tiles = NSEQ * NT
    if DEBUG_STOP <= 1:
        P.dma("sync", d["iw"][0:128, :], iwt[0][:], r=[("iw", 0), "cos", "sin", "gbc", "identb"] + WK, key=("iw", 0), is_out=True)
        P.close()
        return
    load_x(0, 0)

    def norm_group(gi):
        b, g = gi // 4, gi % 4
        gs = gi % 2
        for t4 in range(4):
            tt = g * 4 + t4
            ti = gi * 4 + t4
            if ti + 1 < n_tiles:
                load_x((ti + 1) // NT, (ti + 1) % NT)
            xs = tt % 2
            rms_to_hnT(P, xt[xs][:], ("x", xs), gbc[:], eps_t, stat[xs], ("st", xs), junk, hn[xs][:], ("hn", xs),
                       ps_tr[0], "ptr0", identb, hnT[gs], ("hnT", gs), slice(t4 * 128, (t4 + 1) * 128))

    def mm_group(gi):
        b, g = gi // 4, gi % 4
        gs = gi % 2
        for c in range(4):
            t4 = c
            for k in range(8):
                P.mm(ps_a[:, :], Wt[:, k, c * 128:(c + 1) * 128], hnT[gs][:, k, :], k == 0, k == 7,
                     [("hnT", gs)] + WK, ["psa"])
            for k in range(8):
                P.mm(ps_g[:, :], Wt[:, k, 512 + c * 128:512 + (c + 1) * 128], hnT[gs][:, k, :], k == 0, k == 7,
                     [("hnT", gs)] + WK, ["psg"])
            s2 = c % 2
            P.act(sig[s2][:], ps_g[:, :], AF.Sigmoid, ["psg"], [("sig", s2)])
            P.tt("vector", hc[s2][:], ps_a[:, :], sig[s2][:], ALU.mult, ["psa", ("sig", s2)], [("hc", s2)])
            P.dma("sync", d["hconvT"][b, c, :, g * 512:(g + 1) * 512], hc[s2][:], r=[("hc", s2)], key=("hc", s2), is_out=True)
            tt = g * 4 + t4
            r0 = b * SEQ + tt * 128
            tok = slice(t4 * 128, (t4 + 1) * 128)
            s2 = t4 % 2
            for k in range(8):
                P.mm(ps_q[:, 0:512], hnT[gs][:, k, tok], Wt[:, k, 1024:1536], k == 0, k == 7, [("hnT", gs)] + WK, ["psq"], mark=False)
            for k in range(8):
                P.mm(ps_q[:, 512:1024], hnT[gs][:, k, tok], Wt[:, k, 1536:2048], k == 0, k == 7, [("hnT", gs)] + WK, ["psq"], mark=False)
            for k in range(8):
                P.mm(ps_c[:, 0:328], hnT[gs][:, k, tok], Wt[:, k, 2048:2376], k == 0, k == 7, [("hnT", gs)] + WK, ["psq"])
            for (src, nh, dst, dkey) in ((ps_q[:, 0:512], 8, qr[s2][:, 0:512], ("qr", s2)), (ps_q[:, 512:1024], 8, qr[s2][:, 512:1024], ("qr", s2)),
                                         (ps_c[:, 0:256], 4, kr[s2][:, 0:256], ("kr", s2))):
                sv = src.rearrange("p (h e) -> p h e", e=64)
                dv = dst.rearrange("p (h e) -> p h e", e=64)
                x1, x2 = sv[:, :, 0:8], sv[:, :, 8:16]
                cv = cos[:, tt, 0:nh * 8].rearrange("p (h e) -> p h e", e=8)
                sn = sin[:, tt, 0:nh * 8].rearrange("p (h e) -> p h e", e=8)
                tv = [tmp[i][:, 0:nh * 8].rearrange("p (h e) -> p h e", e=8) for i in range(4)]
                P.tt("vector", tv[0], x1, cv, ALU.mult, ["psq", "cos"], ["tmp0"])
                P.tt("vector", tv[1], x2, sn, ALU.mult, ["psq", "sin"], ["tmp1"])
                P.tt("vector", dv[:, :, 0:8], tv[0], tv[1], ALU.subtract, ["tmp0", "tmp1"], [dkey])
                P.tt("vector", tv[2], x2, cv, ALU.mult, ["psq", "cos"], ["tmp2"])
                P.tt("vector", tv[3], x1, sn, ALU.mult, ["psq", "sin"], ["tmp3"])
                P.tt("vector", dv[:, :, 8:16], tv[2], tv[3], ALU.add, ["tmp2", "tmp3"], [dkey])
                P.copy("scalar", dv[:, :, 16:64], sv[:, :, 16:64], ["psq"], [dkey])
            P.copy("scalar", vaug[s2][:, 0:64], ps_c[:, 256:320], ["psq"], [("vaug", s2)])
            P.copy("scalar", vaug[s2][:, 192:256], ps_c[:, 256:320], ["psq"], [("vaug", s2)])
            P.ts("vector", iwt[s2][:], ps_c[:, 320:328], w_scale, ALU.mult, ["psq"], [("iw", s2)])
            P.dma("sync", d["vaug"][r0:r0 + 128, :], vaug[s2][:], r=[("vaug", s2)], key=("vaug", s2), is_out=True)
            P.dma("sync", d["iw"][r0:r0 + 128, :], iwt[s2][:], r=[("iw", s2)], key=("iw", s2), is_out=True)
            for k in range(8):
                P.tr(ps_tr[1][:, k, :], qr[s2][:, k * 128:(k + 1) * 128], identb[:], [("qr", s2), "identb"], ["ptr1"], mark=(k == 7))
            P.copy("scalar", qTg[gs][:, 0:8, tok], ps_tr[1][:, 0:8, :], ["ptr1"], [("qTg", gs)])
            for k in range(2):
                P.tr(ps_tr[1][:, k, :], kr[s2][:, k * 128:(k + 1) * 128], identb[:], [("kr", s2), "identb"], ["ptr1"], mark=(k == 1))
            P.copy("scalar", kTg[gs][:, 0:2, tok], ps_tr[1][:, 0:2, :], ["ptr1"], [("kTg", gs)])
        gsl = slice(g * 512, (g + 1) * 512)
        P.dma("sync", d["qT"][b, :, :, gsl].rearrange("c p t -> p c t"), qTg[gs][:, 0:4, :], r=[("qTg", gs)], key=("qTg", gs), is_out=True)
        P.dma("sync", d["iqT"][b, :, :, gsl].rearrange("c p t -> p c t"), qTg[gs][:, 4:8, :], r=[("qTg", gs)], key=("iqTg", gs), is_out=True)
        P.dma("sync", d["kkT"][b, :, :, gsl].rearrange("c p t -> p c t"), kTg[gs][:, :, :], r=[("kTg", gs)], key=("kTg", gs), is_out=True)

    ngr = NSEQ * 4
    norm_group(0)
    for gi in range(ngr):
        if gi + 1 < ngr:
            norm_group(gi + 1)
        mm_group(gi)
    P.close()


def phase_B(B):
    nc, d, NSEQ = B.nc, B.t, B.NSEQ
    P = Phase(nc, "B")
    identb = P.sb("identb", [128, 128], BF16)
    P.dma("sync", identb[:], d["ident_bf"], w=["identb"], key="identb")
    identf = P.sb("identf", [128, 128], F32)
    P.dma("sync", identf[:], d["ident_f32"], w=["identf"], key="identf")
    ones = P.sb("ones", [128, 128], F32)
    P.memset("vector", ones[:], 1.0, ["ones"])
    eps_t = P.sb("eps", [128, 1], F32)
    P.memset("vector", eps_t[:], EPS, ["eps"])
    cw = P.sb("cw", [31, 512], F32)
    P.dma("sync", cw[:], d["ab_conv_w"], w=["cw"], key="cw")
    prm = P.sb("prm", [128, 12], F32)
    P.dma("sync", prm[:], d["conv_prm"], w=["prm"], key="prm")
    ps_w = P.ps("psw", [128, 4, 32])
    for c in range(4):
        P.tr(ps_w[:, c, 0:31], cw[0:31, c * 128:(c + 1) * 128], identf[0:31, 0:31], ["cw", "identf"], ["psw"], mark=(c == 3))
    wT = P.sb("wT", [128, 4, 32], F32)
    P.copy("vector", wT[:, :, 0:31], ps_w[:, :, 0:31], ["psw"], ["wT"])
    diag = P.sb("diag", [128, 124, 128], BF16)
    for c in range(4):
        for j in range(31):
            i = c * 31 + j
            P.ts("vector" if i % 2 == 0 else "gpsimd", diag[:, i, :], identb[:], wT[:, c, j:j + 1], ALU.mult,
                 ["identb", "wT"], [("diag", c)])
    hbuf = P.sb("hbuf", [128, 4, 30 + SEQ], BF16)
    P.memset("gpsimd", hbuf[:, :, 0:30], 0.0, ["hbuf"])
    ps_c = [P.ps("psc%d" % i, [128, 512]) for i in range(2)]
    ps_s1 = P.ps("ps1", [128, 512])
    ps_s2 = P.ps("ps2", [128, 512])
    hcs = P.sb("hcs", [128, 4, 512], F32)
    sqs = P.sb("sqs", [128, 4, 512], F32)
    m = P.sb("m", [128, 512], F32)
    msq = P.sb("msq", [128, 512], F32)
    var = P.sb("var", [128, 512], F32)
    rstd = P.sb("rstd", [128, 512], F32)
    z = [P.sb("z%d" % i, [128, 512], F32) for i in range(2)]
    yst = [P.sb("yst%d" % i, [128, 4, 512], BF16) for i in range(2)]
    for b in range(NSEQ):
        for c in range(4):
            P.dma("sync", hbuf[:, c, 30:30 + SEQ], d["hconvT"][b, c, :, :], w=["hbuf"], key=("hbuf", c))
        for g in range(4):
            ys = g % 2
            for c in range(4):
                pc = ps_c[c % 2]
                pk = ("psc", c % 2)
                for j in range(31):
                    P.mm(pc[:, :], diag[:, c * 31 + j, :], hbuf[:, c, g * 512 + j:g * 512 + j + 512], j == 0, j == 30,
                         [("diag", c), "hbuf"], [pk])
                P.act(hcs[:, c, :], pc[:, :], AF.Identity, [pk, "prm"], [("hcs", c)], bias=prm[:, c:c + 1], scale=1.0)
                P.act(sqs[:, c, :], pc[:, :], AF.Square, [pk, "prm"], [("sqs", c)], bias=prm[:, c:c + 1], scale=1.0)
            for c in range(4):
                P.mm(ps_s1[:, :], ones[:], hcs[:, c, :], c == 0, c == 3, [("hcs", c), "ones"], ["ps1"])
            for c in range(4):
                P.mm(ps_s2[:, :], ones[:], sqs[:, c, :], c == 0, c == 3, [("sqs", c), "ones"], ["ps2"])
            P.act(m[:], ps_s1[:, :], AF.Copy, ["ps1"], ["m"], scale=1.0 / 512)
            P.tt("gpsimd", msq[:], m[:], m[:], ALU.mult, ["m"], ["msq"])
            P.stt(var[:], ps_s2[:, :], 1.0 / 512, msq[:], ALU.mult, ALU.subtract, ["ps2", "msq"], ["var"])
            P.act(var[:], var[:], AF.Sqrt, ["var", "eps"], ["var"], bias=eps_t[:, 0:1], scale=1.0)
            P.recip(rstd[:], var[:], ["var"], ["rstd"])
            for c in range(4):
                zz = z[c % 2]
                zk = ("z", c % 2)
                P.tt("gpsimd", zz[:], hcs[:, c, :], m[:], ALU.subtract, [("hcs", c), "m"], [zk])
                P.tt("vector", zz[:], zz[:], rstd[:], ALU.mult, [zk, "rstd"], [zk])
                P.act(yst[ys][:, c, :], zz[:], AF.Silu, [zk, "prm"], [("yst", ys)], scale=prm[:, 4 + c:5 + c], bias=prm[:, 8 + c:9 + c])
            P.dma("sync", d["yabT"][b, 0:4, :, g * 512:(g + 1) * 512].rearrange("c p t -> p c t"), yst[ys][:],
                  r=[("yst", ys)], key=("yst", ys), is_out=True)
    P.close()


def phase_C(B):
    nc, d, NSEQ = B.nc, B.t, B.NSEQ
    P = Phase(nc, "C")
    identb = P.sb("identb", [128, 128], BF16)
    P.dma("sync", identb[:], d["ident_bf"], w=["identb"], key="identb")
    negm = P.sb("negm", [128, 128], F32)
    P.dma("sync", negm[:], d["negmask"], w=["negm"], key="negm")
    pow2 = P.sb("pow2", [128, NIT], F32)
    P.dma("sync", pow2[:], d["pow2"], w=["pow2"], key="pow2")
    thr0 = P.sb("thr0", [128, 1], F32)
    P.memset("vector", thr0[:], -1e29, ["thr0"])
    bigI = P.sb("bigI", [128, 128], BF16)
    P.ts("vector", bigI[:], identb[:], 30000.0, ALU.mult, ["identb"], ["bigI"])
    qT = [P.sb("qT%d" % i, [128, 4, SEQ], BF16) for i in range(2)]
    iqT = [P.sb("iqT%d" % i, [128, 4, SEQ], BF16) for i in range(2)]
    kkT = [P.sb("kkT%d" % i, [128, 2, SEQ], BF16) for i in range(2)]
    vaug = [P.sb("vaug%d" % i, [128, NT, 256], BF16) for i in range(2)]
    iw = [P.sb("iw%d" % i, [128, NT, 8], F32) for i in range(2)]
    ybg = [P.sb("ybg%d" % i, [128, 512], BF16) for i in range(2)]
    maskT = [P.sb("maskT%d" % i, [128, NT, 512], BF16) for i in range(2)]
    score = [P.sb("score%d" % i, [128, SEQ], F32) for i in range(2)]
    junk = P.sb("junk", [128, SEQ], BF16)
    rl = [P.sb("rl%d" % i, [128, 512], F32) for i in range(4)]
    mk = [P.sb("mk%d" % i, [128, SEQ], BF16) for i in range(2)]
    st = [P.sb("st%d" % i, [128, 8 + NIT], F32) for i in range(2)]
    pe = [P.sb("pe%d" % i, [128, 512], BF16) for i in range(3)]
    rc = [P.sb("rc%d" % i, [128, 512], F32) for i in range(2)]
    ps_lg = [P.ps("plg%d" % i, [128, 512]) for i in range(3)]
    ps_tr = [P.ps("ptr%d" % i, [128, 8, 128], BF16) for i in range(1)]
    ps_s = [P.ps("pss%d" % i, [128, 512]) for i in range(2)]
    ps_o = [P.ps("pso%d" % i, [128, 512]) for i in range(2)]
    att_scale = 64 ** -0.5
    ctr = {"lg": 0, "s": 0, "o": 0, "tr": 0, "yb": 0, "g": 0}

    mslot = {}

    def load_seq(b):
        p = b % 2
        rs = slice(b * SEQ, (b + 1) * SEQ)
        P.dma("sync", iqT[p][:], d["iqT"][b].rearrange("c p t -> p c t"), w=[("iqT", p)], key=("iqT", p))
        P.dma("sync", kkT[p][:], d["kkT"][b].rearrange("c p t -> p c t"), w=[("kkT", p)], key=("kkT", p))
        P.dma("sync", iw[p][:], d["iw"][rs, :].rearrange("(t p) c -> p t c", p=128), w=[("iw", p)], key=("iw", p))
        P.dma("sync", qT[p][:], d["qT"][b].rearrange("c p t -> p c t"), w=[("qT", p)], key=("qT", p))
        P.dma("sync", vaug[p][:], d["vaug"][rs, :].rearrange("(t p) c -> p t c", p=128), w=[("vaug", p)], key=("vaug", p))

    junk2 = [junk, P.sb("junkb", [128, SEQ], BF16)]

    def score_pair(b, G, q4s):
        p = b % 2
        mG = mslot[(b, G)]
        info = []
        for q4 in q4s:
            qb = 4 * G + q4
            nk = 128 * (qb + 1)
            nkb = (nk + 511) // 512
            s2 = qb % 2
            sc = score[s2]
            sks = [("score", s2, kb) for kb in range(nkb)]
            qsl = slice(qb * 128, (qb + 1) * 128)
            for h in range(8):
                c, base = h // 2, 64 * (h % 2)
                for kb in range(nkb):
                    n = min(512, nk - kb * 512)
                    r = ctr["lg"] % 3
                    r4 = ctr["lg"] % 4
                    ctr["lg"] += 1
                    P.mm(ps_lg[r][:, 0:n], iqT[p][base:base + 64, c, qsl], kkT[p][base:base + 64, 1, kb * 512:kb * 512 + n],
                         True, True, [("iqT", p), ("kkT", p)], [("plg", r)])
                    P.act(rl[r4][:, 0:n], ps_lg[r][:, 0:n], AF.Relu, [("plg", r)], [("rl", r4)])
                    dst = sc[:, kb * 512:kb * 512 + n]
                    if h == 0:
                        P.ts("vector", dst, rl[r4][:, 0:n], iw[p][:, qb, 0:1], ALU.mult, [("rl", r4), ("iw", p)], [sks[kb]])
                    else:
                        P.stt(dst, rl[r4][:, 0:n], iw[p][:, qb, h:h + 1], dst, ALU.mult, ALU.add, [("rl", r4), ("iw", p), sks[kb]], [sks[kb]])
            info.append((q4, qb, nk, s2, sc, sks, qsl))
        for (q4, qb, nk, s2, sc, sks, qsl) in info:
            stt_ = st[s2]
            stk = ("st", s2)
            if qb >= 2:
                P.reduce(stt_[:, 0:1], sc[:, 0:nk], ALU.max, sks, [stk])
                P.reduce(stt_[:, 1:2], sc[:, 0:nk], ALU.min, sks, [(stk, "lo")])
            P.tt("vector", sc[:, qsl], sc[:, qsl], negm[:], ALU.add, [sks[qb // 4], "negm"], [sks[qb // 4]])
            if qb >= 2:
                P.tt("vector", stt_[:, 2:3], stt_[:, 0:1], stt_[:, 1:2], ALU.subtract, [stk, (stk, "lo")], [stk])
                P.ts("vector", stt_[:, 8:8 + NIT], pow2[:], stt_[:, 2:3], ALU.mult, ["pow2", stk], [(stk, "steps")])
        bis = [x for x in info if x[1] >= 2]
        for i in range(NIT):
            for (q4, qb, nk, s2, sc, sks, qsl) in bis:
                stt_, stk = st[s2], ("st", s2)
                P.tt("vector", stt_[:, 3:4], stt_[:, 1:2], stt_[:, 8 + i:9 + i], ALU.add, [(stk, "lo"), (stk, "steps")], [(stk, "mid")])
            for (q4, qb, nk, s2, sc, sks, qsl) in bis:
                stt_, stk = st[s2], ("st", s2)
                P.ts("vector", junk2[s2][:, 0:nk], sc[:, 0:nk], stt_[:, 3:4], ALU.is_ge, sks + [(stk, "mid")], [("junk", s2), (stk, "cnt")],
                     s2=0.0, op1=ALU.add, accum_out=stt_[:, 4:5])
            for (q4, qb, nk, s2, sc, sks, qsl) in bis:
                stt_, stk = st[s2], ("st", s2)
                P.stt(stt_[:, 5:6], stt_[:, 4:5], TOPK - 0.5, stt_[:, 8 + i:9 + i], ALU.is_ge, ALU.mult, [(stk, "cnt"), (stk, "steps")], [(stk, "sel")])
            for (q4, qb, nk, s2, sc, sks, qsl) in bis:
                stt_, stk = st[s2], ("st", s2)
                P.tt("vector", stt_[:, 1:2], stt_[:, 1:2], stt_[:, 5:6], ALU.add, [(stk, "lo"), (stk, "sel")], [(stk, "lo")])
        for (q4, qb, nk, s2, sc, sks, qsl) in info:
            stt_, stk = st[s2], ("st", s2)
            thr = stt_[:, 1:2] if qb >= 2 else thr0[:, 0:1]
            P.ts("vector", mk[s2][:, 0:nk], sc[:, 0:nk], thr, ALU.is_ge, sks + [(stk, "lo"), "thr0"], [("mk", s2)],
                 s2=-1.0, op1=ALU.add)
            kt = 0
            while kt <= qb:
                n = min(8, qb + 1 - kt)
                r = 0
                for j in range(n):
                    P.tr(ps_tr[r][:, j, :], mk[s2][:, (kt + j) * 128:(kt + j + 1) * 128], identb[:], [("mk", s2), "identb"],
                         [("ptr", r)], mark=(j == n - 1))
                P.copy("scalar", maskT[mG][:, kt:kt + n, q4 * 128:(q4 + 1) * 128], ps_tr[r][:, 0:n, :], [("ptr", r)], [("maskT", mG)])
                kt += n

    def attn_chunk(b, G, c):
        p = b % 2
        mG = mslot[(b, G)]
        nkt = 4 * G + 4
        gsl0 = G * 512
        ys = ctr["yb"] % 2
        ctr["yb"] += 1
        for e in range(2):
            base = 64 * e
            o = ctr["o"] % 2
            ctr["o"] += 1
            for kt in range(nkt):
                q0 = max(0, kt - 4 * G) * 128
                N = 512 - q0
                r = ctr["s"] % 2
                ctr["s"] += 1
                P.mm(ps_s[r][:, 0:N], kkT[p][base:base + 64, 0, kt * 128:(kt + 1) * 128], qT[p][base:base + 64, c, gsl0 + q0:gsl0 + 512],
                     True, False, [("kkT", p), ("qT", p)], [("pss", r)], mark=False)
                P.mm(ps_s[r][:, 0:N], bigI[:], maskT[mG][:, kt, q0:512], False, True, ["bigI", ("maskT", mG)], [("pss", r)])
                r3 = ctr["s"] % 3
                P.act(pe[r3][:, 0:N], ps_s[r][:, 0:N], AF.Exp, [("pss", r)], [("pe", r3)], scale=att_scale)
                P.mm(ps_o[o][:, q0:512], vaug[p][:, kt, e * 128:(e + 1) * 128], pe[r3][:, 0:N], kt == 0, kt == nkt - 1,
                     [("pe", r3), ("vaug", p)], [("pso", o)])
            so, ss_ = (slice(0, 64), slice(64, 128)) if e == 0 else (slice(64, 128), slice(0, 64))
            P.act(rc[o][ss_, :], ps_o[o][ss_, :], AF.Ln, [("pso", o)], [("rc", o)])
            P.act(rc[o][ss_, :], rc[o][ss_, :], AF.Exp, [("rc", o)], [("rc", o)], scale=-1.0)
            P.tt("vector", ybg[ys][so, :], ps_o[o][so, :], rc[o][ss_, :], ALU.mult, [("pso", o), ("rc", o)], [("ybg", ys)])
        P.dma("sync", d["yabT"][b, 4 + c, :, gsl0:gsl0 + 512], ybg[ys][:], r=[("ybg", ys)], key=("ybg", ys), is_out=True)

    groups = [(b, G) for G in range(4) for b in range(NSEQ)]
    for b in range(NSEQ):
        load_seq(b)
    for gi_, (b, G) in enumerate(groups):
        mslot[(b, G)] = gi_ % 2
        for pr in range(2):
            score_pair(b, G, (2 * pr, 2 * pr + 1))
            if gi_ >= 1:
                pb, pG = groups[gi_ - 1]
                attn_chunk(pb, pG, 2 * pr)
                attn_chunk(pb, pG, 2 * pr + 1)
    pb, pG = groups[-1]
    for c in range(4):
        attn_chunk(pb, pG, c)
    P.close()


def phase_D(B, tag, yT, wname, resid, gname, hmid, hnT_name):
    nc, d, NSEQ = B.nc, B.t, B.NSEQ
    P = Phase(nc, tag)
    Wo = P.sb("Wo", [128, 8, D], BF16)
    P.dma("gpsimd", Wo[:], d[wname].rearrange("(k p) n -> p k n", p=128), w=["Wo"], key="Wo")
    identb = P.sb("identb", [128, 128], BF16)
    P.dma("sync", identb[:], d["ident_bf"], w=["identb"], key="identb")
    gbc = P.sb("gbc", [128, D], F32)
    P.dma("sync", gbc[:], d[gname], w=["gbc"], key="gbc")
    eps_t = P.sb("eps", [128, 1], F32)
    P.memset("vector", eps_t[:], EPS, ["eps"])
    yt = [P.sb("yt%d" % i, [128, 8, 512], BF16) for i in range(2)]
    xt = [P.sb("xt%d" % i, [128, D], F32) for i in range(2)]
    hm = [P.sb("hm%d" % i, [128, D], F32) for i in range(2)]
    junk = P.sb("junk", [128, D], F32)
    stat = [P.sb("stat%d" % i, [128, 4], F32) for i in range(2)]
    hn = [P.sb("hn%d" % i, [128, D], BF16) for i in range(2)]
    hnTg = [P.sb("hnTg%d" % i, [128, 8, 512], BF16) for i in range(2)]
    ps_o = [P.ps("pso%d" % i, [128, 1024]) for i in range(2)]
    ps_tr = [P.ps("ptr%d" % i, [128, 8, 128], BF16) for i in range(2)]
    ngr = NSEQ * 4

    def load_y(gi):
        b, g = gi // 4, gi % 4
        P.dma("sync", yt[gi % 2][:], d[yT][b, :, :, g * 512:(g + 1) * 512].rearrange("c p t -> p c t"), w=[("yt", gi % 2)], key=("yt", gi % 2))

    load_y(0)

    def outproj(ti):
        gi, t4 = ti // 4, ti % 4
        b, g = gi // 4, gi % 4
        gs = gi % 2
        if t4 == 0 and gi + 1 < ngr:
            load_y(gi + 1)
        tt = g * 4 + t4
        r0 = b * SEQ + tt * 128
        tok = slice(t4 * 128, (t4 + 1) * 128)
        s2 = ti % 2
        P.dma("sync", xt[s2][:], d[resid][r0:r0 + 128, :], w=[("x", s2)], key=("x", s2))
        for half in range(2):
            for c in range(8):
                P.mm(ps_o[s2][:, half * 512:(half + 1) * 512], yt[gs][:, c, tok], Wo[:, c, half * 512:(half + 1) * 512],
                     c == 0, c == 7, [("yt", gs), "Wo"], [("pso", s2)], mark=(c == 7 and half == 1))

    def post(ti):
        gi, t4 = ti // 4, ti % 4
        b, g = gi // 4, gi % 4
        gs = gi % 2
        tt = g * 4 + t4
        r0 = b * SEQ + tt * 128
        tok = slice(t4 * 128, (t4 + 1) * 128)
        s2 = ti % 2
        for half in range(2):
            hs_ = slice(half * 512, (half + 1) * 512)
            P.tt("vector", hm[s2][:, hs_], ps_o[s2][:, hs_], xt[s2][:, hs_], ALU.add, [("pso", s2), ("x", s2)], [("hm", s2)])
        P.dma("sync", d[hmid][r0:r0 + 128, :], hm[s2][:], r=[("hm", s2)], key=("hm", s2), is_out=True)
        rms_to_hnT(P, hm[s2][:], ("hm", s2), gbc[:], eps_t, stat[s2], ("st", s2), junk, hn[s2][:], ("hn", s2),
                   ps_tr[s2], ("ptr", s2), identb, hnTg[gs], ("hnTg", gs), tok)
        if t4 == 3:
            P.dma("sync", d[hnT_name][:, :, gi * 512:(gi + 1) * 512].rearrange("c p t -> p c t"), hnTg[gs][:], r=[("hnTg", gs)],
                  key=("hnTg", gs), is_out=True)

    ntl = ngr * 4
    outproj(0)
    for ti in range(ntl):
        if ti + 1 < ntl:
            outproj(ti + 1)
        post(ti)
    P.close()


def phase_F(B, tag, layer, f0, h_in, h_out, hnT_name, final_g=None):
    nc, d, NSEQ = B.nc, B.t, B.NSEQ
    P = Phase(nc, tag)
    nf = 11
    Wg = P.sb("Wg", [128, 8, nf * 128], BF16)
    Wu = P.sb("Wu", [128, 8, nf * 128], BF16)
    Wd = P.sb("Wd", [128, nf, D], BF16)
    pieces = [(0, 3), (3, 6), (6, 9), (9, 11)]
    pf = {f: pi for pi, (fa, fb) in enumerate(pieces) for f in range(fa, fb)}
    srcg = d["ffn_w_gate%d" % layer].rearrange("(k p) n -> p k n", p=128)
    srcu = d["ffn_w_up%d" % layer].rearrange("(k p) n -> p k n", p=128)
    srcd = d["ffn_w_down%d" % layer].rearrange("(f p) n -> p f n", p=128)
    for pi, (fa, fb) in enumerate(pieces):
        P.dma("gpsimd", Wg[:, :, fa * 128:fb * 128], srcg[:, :, (f0 + fa) * 128:(f0 + fb) * 128], w=[("Wg", pi)], key=("Wg", pi))
        P.dma("gpsimd", Wu[:, :, fa * 128:fb * 128], srcu[:, :, (f0 + fa) * 128:(f0 + fb) * 128], w=[("Wu", pi)], key=("Wu", pi))
    for pi, (fa, fb) in enumerate(pieces):
        P.dma("gpsimd", Wd[:, fa:fb, :], srcd[:, f0 + fa:f0 + fb, :], w=[("Wd", pi)], key=("Wd", pi))
    if final_g is not None:
        gbc = P.sb("gbc", [128, D], F32)
        P.dma("sync", gbc[:], d[final_g], w=["gbc"], key="gbc")
        eps_t = P.sb("eps", [128, 1], F32)
        P.memset("vector", eps_t[:], EPS, ["eps"])
        junk = P.sb("junk", [128, D], F32)
        stat = [P.sb("stat%d" % i, [128, 4], F32) for i in range(2)]
    hnTg = [P.sb("hnTg%d" % i, [128, 8, 512], BF16) for i in range(2)]
    actT = [P.sb("actT%d" % i, [128, nf, 512], BF16) for i in range(2)]
    sg = [P.sb("sg%d" % i, [128, 512], F32) for i in range(2)]
    hin = [P.sb("hin%d" % i, [128, D], F32) for i in range(2)]
    hout = [P.sb("hout%d" % i, [128, D], F32) for i in range(2)]
    ps_g = [P.ps("psg%d" % i, [128, 512]) for i in range(2)]
    ps_u = [P.ps("psu%d" % i, [128, 512]) for i in range(2)]
    ps_d = [P.ps("psd%d" % i, [128, 1024]) for i in range(2)]
    ngr = NSEQ * 4

    def load_h(gi):
        P.dma("sync", hnTg[gi % 2][:], d[hnT_name][:, :, gi * 512:(gi + 1) * 512].rearrange("c p t -> p c t"),
              w=[("hnTg", gi % 2)], key=("hnTg", gi % 2))

    load_h(0)
    for gi in range(ngr):
        gs = gi % 2
        if gi + 1 < ngr:
            load_h(gi + 1)
        for f in range(nf):
            s2 = f % 2
            for k in range(8):
                P.mm(ps_g[s2][:, :], Wg[:, k, f * 128:(f + 1) * 128], hnTg[gs][:, k, :], k == 0, k == 7, [("hnTg", gs), ("Wg", pf[f])], [("psg", s2)])
            for k in range(8):
                P.mm(ps_u[s2][:, :], Wu[:, k, f * 128:(f + 1) * 128], hnTg[gs][:, k, :], k == 0, k == 7, [("hnTg", gs), ("Wu", pf[f])], [("psu", s2)])
            P.act(sg[s2][:], ps_g[s2][:, :], AF.Silu, [("psg", s2)], [("sg", s2)])
            P.tt("vector", actT[gs][:, f, :], ps_u[s2][:, :], sg[s2][:], ALU.mult, [("psu", s2), ("sg", s2)], [("actT", gs)])
        for t4 in range(4):
            r0 = gi * 512 + t4 * 128
            tok = slice(t4 * 128, (t4 + 1) * 128)
            s2 = t4 % 2
            P.dma("sync", hin[s2][:], d[h_in][r0:r0 + 128, :], w=[("hin", s2)], key=("hin", s2))
            for half in range(2):
                for f in range(nf):
                    P.mm(ps_d[s2][:, half * 512:(half + 1) * 512], actT[gs][:, f, tok], Wd[:, f, half * 512:(half + 1) * 512],
                         f == 0, f == nf - 1, [("actT", gs), ("Wd", pf[f])], [("psd", s2)], mark=(f == nf - 1 and half == 1))
            for half in range(2):
                hs_ = slice(half * 512, (half + 1) * 512)
                P.tt("vector", hout[s2][:, hs_], ps_d[s2][:, hs_], hin[s2][:, hs_], ALU.add, [("psd", s2), ("hin", s2)], [("hout", s2)])
            if final_g is not None:
                sk = ("st", s2)
                P.act(junk[:], hout[s2][:], AF.Square, [("hout", s2)], ["junkA", sk], accum_out=stat[s2][:, 0:1])
                P.act(stat[s2][:, 1:2], stat[s2][:, 0:1], AF.Sqrt, [sk], [sk], scale=1.0 / D, bias=eps_t[:, 0:1])
                P.recip(stat[s2][:, 2:3], stat[s2][:, 1:2], [sk], [sk])
                P.stt(hout[s2][:], hout[s2][:], stat[s2][:, 2:3], gbc[:], ALU.mult, ALU.mult, [("hout", s2), sk, "gbc"], [("hout", s2)])
            P.dma("sync", d[h_out][r0:r0 + 128, :], hout[s2][:], r=[("hout", s2)], key=("hout", s2), is_out=True)
    P.close()


def phase_E(B):
    nc, d, NSEQ = B.nc, B.t, B.NSEQ
    P = Phase(nc, "E")
    Wc = P.sb("Wc", [128, 8, 3104], BF16)
    Wsrc = d["c_w_in"].rearrange("(k p) n -> p k n", p=128)
    WK = []
    P.memset("gpsimd", Wc[:, :, 3088:3104], 0.0, [("W", 2)])
    for i, (c0, c1) in enumerate(((0, 1024), (1024, 2048), (2048, 3088))):
        P.dma("gpsimd", Wc[:, :, c0:c1], Wsrc[:, :, c0:c1], w=[("W", i)], key=("W", i))
        WK.append(("W", i))
    identb = P.sb("identb", [128, 128], BF16)
    P.dma("sync", identb[:], d["ident_bf"], w=["identb"], key="identb")
    gbc = P.sb("gbc", [128, D], F32)
    P.dma("sync", gbc[:], d["g_mix1"], w=["gbc"], key="gbc")
    g2w = P.sb("g2w", [128, 512], F32)
    P.memset("vector", g2w[:], 0.0, ["g2w"])
    P.dma("sync", g2w[0:16, :], d["c_gate_w"], w=["g2w"], key="g2w")
    g2b = P.sb("g2b", [128, 512], F32)
    P.dma("sync", g2b[:], d["c_gate_b_bc"], w=["g2b"], key="g2b")
    eps_t = P.sb("eps", [128, 1], F32)
    P.memset("vector", eps_t[:], EPS, ["eps"])
    one_t = P.sb("one", [128, 1], F32)
    P.memset("vector", one_t[:], 1.0, ["one"])
    xt = [P.sb("xt%d" % i, [128, D], F32) for i in range(2)]
    junk = P.sb("junk", [128, D], F32)
    stat = [P.sb("stat%d" % i, [128, 4], F32) for i in range(2)]
    hn = [P.sb("hn%d" % i, [128, D], BF16) for i in range(2)]
    hnT = [P.sb("hnT%d" % i, [128, 8, 512], BF16) for i in range(2)]
    qkT = [P.sb("qkT%d" % i, [128, 8, 512], BF16) for i in range(2)]
    glrT = [P.sb("glrT%d" % i, [128, 512], F32) for i in range(2)]
    for i in range(2):
        P.memset("vector", glrT[i][:], 0.0, [("glrT", i)])
    kst = [P.sb("kst%d" % i, [128, 512], BF16) for i in range(2)]
    vst = [P.sb("vst%d" % i, [128, 1024], BF16) for i in range(2)]
    rst = [P.sb("rst%d" % i, [128, 1024], F32) for i in range(2)]
    lat = [P.sb("lat%d" % i, [128, 512], F32) for i in range(2)]
    lt1 = P.sb("lt1", [128, 512], F32)
    lt2 = P.sb("lt2", [128, 512], F32)
    ps_tr = P.ps("ptr", [128, 8, 128], BF16)
    ps_f = [P.ps("psf%d" % i, [128, 512]) for i in range(2)]
    ps_t = [P.ps("pst%d" % i, [128, 512]) for i in range(3)]
    ps_l = P.ps("psl", [128, 512])
    n_t = 0
    n_f = 0
    n_tiles = NSEQ * NT

    def load_x(ti):
        P.dma("sync", xt[ti % 2][:], d["h1"][ti * 128:(ti + 1) * 128, :], w=[("x", ti % 2)], key=("x", ti % 2))

    load_x(0)

    def norm_group(gi):
        gs = gi % 2
        for t4 in range(4):
            ti = gi * 4 + t4
            if ti + 1 < n_tiles:
                load_x(ti + 1)
            xs = ti % 2
            rms_to_hnT(P, xt[xs][:], ("x", xs), gbc[:], eps_t, stat[xs], ("st", xs), junk, hn[xs][:], ("hn", xs),
                       ps_tr, "ptr", identb, hnT[gs], ("hnT", gs), slice(t4 * 128, (t4 + 1) * 128))

    def mm_group(gi):
        nonlocal n_t, n_f
        b, g = gi // 4, gi % 4
        gs = gi % 2
        for oc in range(8):
            r = n_f % 2
            n_f += 1
            for k in range(8):
                P.mm(ps_f[r][:, :], Wc[:, k, oc * 128:(oc + 1) * 128], hnT[gs][:, k, :], k == 0, k == 7, [("hnT", gs), ("W", 0)], [("psf", r)])
            if oc < 4:
                P.act(qkT[gs][:, oc, :], ps_f[r][:, :], AF.Copy, [("psf", r)], [("qkT", gs)], scale=128 ** -0.5)
            else:
                P.copy("vector", qkT[gs][:, oc, :], ps_f[r][:, :], [("psf", r)], [("qkT", gs)])
        r = n_f % 2
        n_f += 1
        for k in range(8):
            P.mm(ps_f[r][0:32, :], Wc[:, k, 3072:3104], hnT[gs][:, k, :], k == 0, k == 7, [("hnT", gs), ("W", 2)], [("psf", r)])
        P.copy("vector", glrT[gs][0:32, :], ps_f[r][0:32, :], [("psf", r)], [("glrT", gs)])
        gsl = slice(g * 512, (g + 1) * 512)
        P.dma("sync", d["c_qT"][b, :, :, gsl].rearrange("c p t -> p c t"), qkT[gs][:, 0:4, :], r=[("qkT", gs)], key=("qTo", gs), is_out=True)
        P.dma("sync", d["c_kT"][b, :, :, gsl].rearrange("c p t -> p c t"), qkT[gs][:, 4:8, :], r=[("qkT", gs)], key=("kTo", gs), is_out=True)
        for t4 in range(4):
            r0 = gi * 512 + t4 * 128
            tok = slice(t4 * 128, (t4 + 1) * 128)
            s2 = t4 % 2
            jobs = [(512, 1024, kst[s2][:, :], ("kst", s2), "k"),
                    (1024, 1536, vst[s2][:, 0:512], ("vst", s2), "v"), (1536, 2048, vst[s2][:, 512:1024], ("vst", s2), "v"),
                    (2048, 2560, rst[s2][:, 0:512], ("rst", s2), "r"), (2560, 3072, rst[s2][:, 512:1024], ("rst", s2), "r")]
            for (c0, c1, dst, dk, kind) in jobs:
                r = n_t % 3
                n_t += 1
                for k in range(8):
                    P.mm(ps_t[r][:, :], hnT[gs][:, k, tok], Wc[:, k, c0:c1], k == 0, k == 7, [("hnT", gs), ("W", c0 // 1024)], [("pst", r)])
                if kind == "r":
                    P.act(dst, ps_t[r][:, :], AF.Silu, [("pst", r)], [dk])
                elif kind == "v":
                    P.copy("vector", dst, ps_t[r][:, :], [("pst", r)], [dk])
                else:
                    P.copy("scalar", dst, ps_t[r][:, :], [("pst", r)], [dk])
            P.dma("sync", d["c_ktok"][r0:r0 + 128, :], kst[s2][:], r=[("kst", s2)], key=("kst", s2), is_out=True)
            P.dma("sync", d["c_v"][r0:r0 + 128, :], vst[s2][:], r=[("vst", s2)], key=("vst", s2), is_out=True)
            P.dma("sync", d["c_sr"][r0:r0 + 128, :], rst[s2][:], r=[("rst", s2)], key=("rst", s2), is_out=True)
            P.mm(ps_l[:, :], glrT[gs][:, tok], g2w[:, :], True, True, [("glrT", gs), "g2w"], ["psl"])
            P.tt("vector", lt1[:], ps_l[:, :], g2b[:], ALU.add, ["psl", "g2b"], ["lt1"])
            P.act(lt2[:], lt1[:], AF.Exp, ["lt1"], ["lt2"], scale=-1.0)
            P.act(lt1[:], lt2[:], AF.Ln, ["lt2", "one"], ["lt1"], bias=one_t[:, 0:1], scale=1.0)
            P.ts("gpsimd", lat[s2][:], lt1[:], -1.0 / 16.0, ALU.mult, ["lt1"], [("lat", s2)])
            P.dma("sync", d["c_la"][r0:r0 + 128, :], lat[s2][:], r=[("lat", s2)], key=("lat", s2), is_out=True)

    ngr = NSEQ * 4
    norm_group(0)
    for gi in range(ngr):
        if gi + 1 < ngr:
            norm_group(gi + 1)
        mm_group(gi)
    P.close()


def phase_G(B):
    nc, d, NSEQ = B.nc, B.t, B.NSEQ
    P = Phase(nc, "G")
    identb = P.sb("identb", [128, 128], BF16)
    P.dma("sync", identb[:], d["ident_bf"], w=["identb"], key="identb")
    U = P.sb("U", [128, 128], F32)
    SL = P.sb("SL", [128, 128], F32)
    P.dma("sync", U[:], d["tri_u"], w=["U"], key="U")
    P.dma("sync", SL[:], d["tri_sl"], w=["SL"], key="SL")
    gon = P.sb("gon", [128, D], F32)
    P.dma("sync", gon[:], d["c_onorm_g_bc"], w=["gon"], key="gon")
    eps_t = P.sb("eps", [128, 1], F32)
    P.memset("vector", eps_t[:], EPS, ["eps"])
    qT = P.sb("qT", [128, 4, SEQ], BF16)
    kT = P.sb("kT", [128, 4, SEQ], BF16)
    la = [P.sb("la%d" % i, [128, 512], F32) for i in range(2)]
    kt_ = [P.sb("ktk%d" % i, [128, 512], BF16) for i in range(2)]
    vt = [P.sb("vt%d" % i, [128, 1024], BF16) for i in range(3)]
    sr = [P.sb("sr%d" % i, [128, 1024], F32) for i in range(2)]
    eb = [P.sb("eb%d" % i, [128, 512], F32) for i in range(2)]
    enb = P.sb("enb", [128, 512], F32)
    erv = P.sb("erv", [128, 512], F32)
    qtT = [P.sb("qtT%d" % i, [128, 512], BF16) for i in range(2)]
    ktT = [P.sb("ktT%d" % i, [128, 512], BF16) for i in range(2)]
    ks = [P.sb("ks%d" % i, [128, 512], BF16) for i in range(2)]
    attm = [P.sb("attm%d" % i, [128, 128], BF16) for i in range(2)]
    state = P.sb("state", [128, 4, 256], F32)
    stb = P.sb("stb", [128, 4, 256], BF16)
    gr = [P.sb("gr%d" % i, [128, 1024], F32) for i in range(2)]
    stat = [P.sb("stat%d" % i, [128, 12], F32) for i in range(2)]
    junk = P.sb("junk", [128, 256], F32)
    yc = [P.sb("yc%d" % i, [128, 1024], BF16) for i in range(2)]
    ycT = [P.sb("ycT%d" % i, [128, 8, 512], BF16) for i in range(2)]
    ps_b = P.ps("psb", [128, 512])
    ps_r = P.ps("psr", [128, 512])
    ps_a = P.ps("psa", [128, 4, 128])
    ps_o = P.ps("pso", [128, 1024])
    ps_kv = P.ps("pskv", [128, 1024])
    ps_tr = P.ps("ptr", [128, 8, 128], BF16)
    nch = NSEQ * NT

    def load_chunk(ci):
        s2 = ci % 2
        r0 = ci * 128
        P.dma("sync", la[s2][:], d["c_la"][r0:r0 + 128, :], w=[("la", s2)], key=("la", s2))
        P.dma("sync", kt_[s2][:], d["c_ktok"][r0:r0 + 128, :], w=[("ktk", s2)], key=("ktk", s2))
        P.dma("sync", vt[ci % 3][:], d["c_v"][r0:r0 + 128, :], w=[("vt", ci % 3)], key=("vt", ci % 3))
        P.dma("sync", sr[s2][:], d["c_sr"][r0:r0 + 128, :], w=[("sr", s2)], key=("sr", s2))

    enb2 = [enb, P.sb("enbb", [128, 512], F32)]
    erv2 = [erv, P.sb("ervb", [128, 512], F32)]
    attm4 = attm + [P.sb("attm%d" % i, [128, 128], BF16) for i in range(2, 4)]

    def prep(ci):
        b, c = ci // NT, ci % NT
        s2 = ci % 2
        if c == 0:
            P.dma("sync", qT[:], d["c_qT"][b].rearrange("c p t -> p c t"), w=["qT"], key="qT")
            P.dma("sync", kT[:], d["c_kT"][b].rearrange("c p t -> p c t"), w=["kT"], key="kT")
        if ci + 1 < nch:
            load_chunk(ci + 1)
        csl = slice(c * 128, (c + 1) * 128)
        for h in range(4):
            P.mm(ps_b[:, h * 128:(h + 1) * 128], la[s2][:, h * 128:(h + 1) * 128], U[:], True, True, [("la", s2), "U"], ["psb"], mark=(h == 3))
        P.mm(ps_r[:, :], SL[:], la[s2][:, :], True, True, ["SL", ("la", s2)], ["psr"])
        P.act(eb[s2][:], ps_b[:, :], AF.Exp, ["psb"], [("eb", s2)])
        P.act(enb2[s2][:], ps_b[:, :], AF.Exp, ["psb"], [("enb", s2)], scale=-1.0)
        P.act(erv2[s2][:], ps_r[:, :], AF.Exp, ["psr"], [("erv", s2)])
        qv = qT[:, :, csl]
        kv_ = kT[:, :, csl]
        P.tt("vector", qtT[s2][:].rearrange("p (h t) -> p h t", t=128), qv, eb[s2][:].rearrange("p (h t) -> p h t", t=128), ALU.mult,
             ["qT", ("eb", s2)], [("qtT", s2)])
        P.tt("gpsimd", ktT[s2][:].rearrange("p (h t) -> p h t", t=128), kv_, enb2[s2][:].rearrange("p (h t) -> p h t", t=128), ALU.mult,
             ["kT", ("enb", s2)], [("ktT", s2)])
        P.tt("gpsimd", ks[s2][:], kt_[s2][:], erv2[s2][:], ALU.mult, [("ktk", s2), ("erv", s2)], [("ks", s2)])
        P.tt("gpsimd", gr[s2][:], sr[s2][:], gon[:], ALU.mult, [("sr", s2), "gon"], [("gr", s2)])

    def rec(ci):
        b, c = ci // NT, ci % NT
        s2 = ci % 2
        if c == 0:
            P.memset("vector", state[:], 0.0, [("state", h) for h in range(4)])
            P.memset("gpsimd", stb[:], 0.0, [("stb", h) for h in range(4)])
        HS = [slice(h * 128, (h + 1) * 128) for h in range(4)]
        VS = [slice(h * 256, (h + 1) * 256) for h in range(4)]
        for h in range(4):
            P.mm(ps_a[:, h, :], ktT[s2][:, HS[h]], qtT[s2][:, HS[h]], True, True, [("ktT", s2), ("qtT", s2)], ["psa"], mark=(h == 3))
        for h in range(4):
            P.tt("vector", attm4[h][:], ps_a[:, h, :], U[:], ALU.mult, ["psa", "U"], [("attm", h)])
        for h in range(4):
            P.mm(ps_o[:, VS[h]], attm4[h][:], vt[ci % 3][:, VS[h]], True, False, [("attm", h), ("vt", ci % 3)], ["pso"], mark=False)
            P.mm(ps_o[:, VS[h]], qtT[s2][:, HS[h]], stb[:, h, :], False, True, [("qtT", s2), ("stb", h)], ["pso"], mark=(h == 3))
        for h in range(4):
            P.mm(ps_kv[:, VS[h]], ks[s2][:, HS[h]], vt[ci % 3][:, VS[h]], True, True, [("ks", s2), ("vt", ci % 3)], ["pskv"], mark=(h == 3))
        for h in range(4):
            P.stt(state[:, h, :], state[:, h, :], eb[s2][:, h * 128 + 127:h * 128 + 128], ps_kv[:, VS[h]], ALU.mult, ALU.add,
                  [("state", h), ("eb", s2), "pskv"], [("state", h)])
        for h in range(4):
            P.copy("scalar", stb[:, h, :], state[:, h, :], [("state", h)], [("stb", h)])
        sk = ("st", s2)
        for h in range(4):
            P.act(junk[:], ps_o[:, VS[h]], AF.Square, ["pso"], ["junkA", sk], accum_out=stat[s2][:, h:h + 1])
        P.act(stat[s2][:, 4:8], stat[s2][:, 0:4], AF.Sqrt, [sk, "eps"], [sk], scale=1.0 / 256, bias=eps_t[:, 0:1])
        P.recip(stat[s2][:, 8:12], stat[s2][:, 4:8], [sk], [sk])
        for h in range(4):
            P.stt(yc[s2][:, VS[h]], ps_o[:, VS[h]], stat[s2][:, 8 + h:9 + h], gr[s2][:, VS[h]], ALU.mult, ALU.mult,
                  ["pso", sk, ("gr", s2)], [("yc", s2)])
        for k in range(8):
            P.tr(ps_tr[:, k, :], yc[s2][:, k * 128:(k + 1) * 128], identb[:], [("yc", s2), "identb"], ["ptr"], mark=(k == 7))
        g4 = (ci // 4) % 2
        P.copy("scalar", ycT[g4][:, 0:8, (ci % 4) * 128:(ci % 4 + 1) * 128], ps_tr[:, 0:8, :], ["ptr"], [("ycT", g4)])
        if ci % 4 == 3:
            g = c // 4
            P.dma("sync", d["ycT"][b, :, :, g * 512:(g + 1) * 512].rearrange("c p t -> p c t"), ycT[g4][:], r=[("ycT", g4)],
                  key=("ycT", g4), is_out=True)

    load_chunk(0)
    prep(0)
    for ci in range(nch):
        if ci + 1 < nch:
            prep(ci + 1)
        rec(ci)
    P.close()


def declare(B):
    NSEQ = B.NSEQ
    T = NSEQ * SEQ
    X = "ExternalInput"
    B.dram("x", [T, D], F32, X)
    B.dram("ab_w_in", [D, 2248], F32, X)
    B.dram("ab_conv_w", [31, 512], F32, X)
    B.dram("conv_prm", [128, 12], F32, X)
    B.dram("ab_w_out", [D, D], F32, X)
    B.dram("c_w_in", [D, 3088], F32, X)
    B.dram("c_gate_w", [16, 512], F32, X)
    B.dram("c_gate_b_bc", [128, 512], F32, X)
    B.dram("c_onorm_g_bc", [128, D], F32, X)
    B.dram("c_w_out", [D, D], F32, X)
    for l in range(2):
        B.dram("ffn_w_gate%d" % l, [D, DFF], F32, X)
        B.dram("ffn_w_up%d" % l, [D, DFF], F32, X)
        B.dram("ffn_w_down%d" % l, [DFF, D], F32, X)
        B.dram("g_mix%d" % l, [128, D], F32, X)
        B.dram("g_ffn%d" % l, [128, D], F32, X)
    B.dram("g_final", [128, D], F32, X)
    B.dram("ident_bf", [128, 128], BF16, X)
    B.dram("ident_f32", [128, 128], F32, X)
    B.dram("cos128", [SEQ, 128], F32, X)
    B.dram("sin128", [SEQ, 128], F32, X)
    B.dram("negmask", [128, 128], F32, X)
    B.dram("pow2", [128, NIT], F32, X)
    B.dram("tri_u", [128, 128], F32, X)
    B.dram("tri_sl", [128, 128], F32, X)
    B.dram("hconvT", [NSEQ, 4, 128, SEQ], BF16)
    B.dram("qT", [NSEQ, 4, 128, SEQ], BF16)
    B.dram("iqT", [NSEQ, 4, 128, SEQ], BF16)
    B.dram("kkT", [NSEQ, 2, 128, SEQ], BF16)
    B.dram("vaug", [T, 256], BF16)
    B.dram("iw", [T, 8], F32)
    B.dram("yabT", [NSEQ, 8, 128, SEQ], BF16)
    B.dram("h_mid0", [T, D], F32)
    B.dram("hnT", [8, 128, T], BF16)
    B.dram("h_half", [T, D], F32)
    B.dram("h1", [T, D], F32)
    B.dram("c_qT", [NSEQ, 4, 128, SEQ], BF16)
    B.dram("c_kT", [NSEQ, 4, 128, SEQ], BF16)
    B.dram("c_ktok", [T, 512], BF16)
    B.dram("c_v", [T, D], BF16)
    B.dram("c_sr", [T, D], F32)
    B.dram("c_la", [T, 512], F32)
    B.dram("ycT", [NSEQ, 8, 128, SEQ], BF16)
    B.dram("h_mid1", [T, D], F32)
    B.dram("out", [T, D], F32, "ExternalOutput")
    if DEBUG_STOP == 77:
        B.dram("dbg", [128, 16], F32, "ExternalOutput")
        B.dram("dbg2", [4, 128, D], BF16, "ExternalOutput")


PHASES = {
    "A": phase_A,
    "B": phase_B,
    "C": phase_C,
    "D0": lambda B: phase_D(B, "D0", "yabT", "ab_w_out", "x", "g_ffn0", "h_mid0", "hnT"),
    "F0a": lambda B: phase_F(B, "F0a", 0, 0, "h_mid0", "h_half", "hnT"),
    "F0b": lambda B: phase_F(B, "F0b", 0, 11, "h_half", "h1", "hnT"),
    "E": phase_E,
    "G": phase_G,
    "D1": lambda B: phase_D(B, "D1", "ycT", "c_w_out", "h1", "g_ffn1", "h_mid1", "hnT"),
    "F1a": lambda B: phase_F(B, "F1a", 1, 0, "h_mid1", "h_half", "hnT"),
    "F1b": lambda B: phase_F(B, "F1b", 1, 11, "h_half", "out", "hnT", final_g="g_final"),
}
ORDER = ["A", "B", "C", "D0", "F0a", "F0b", "E", "G", "D1", "F1a", "F1b"]


def host_consts():
    bf = ml_dtypes.bfloat16
    c = {}
    c["ident_bf"] = np.eye(128, dtype=np.float32).astype(bf)
    c["ident_f32"] = np.eye(128, dtype=np.float32)
    rot = 16
    inv = (500000.0 ** (-np.arange(0, rot, 2, dtype=np.float32) / np.float32(rot))).astype(np.float32)
    ang = (np.arange(SEQ, dtype=np.float32)[:, None] * inv[None, :]).astype(np.float32)
    c["cos128"] = np.ascontiguousarray(np.tile(np.cos(ang).astype(np.float32), (1, 16)))
    c["sin128"] = np.ascontiguousarray(np.tile(np.sin(ang).astype(np.float32), (1, 16)))
    i = np.arange(128)
    c["negmask"] = np.where(i[None, :] <= i[:, None], 0.0, -1e30).astype(np.float32)
    c["pow2"] = np.ascontiguousarray(np.broadcast_to((0.5 ** np.arange(1, NIT + 1)).astype(np.float32), (128, NIT)))
    c["tri_u"] = (i[:, None] <= i[None, :]).astype(np.float32)
    c["tri_sl"] = (i[:, None] > i[None, :]).astype(np.float32)
    return c


def host_weights(inp):
    f = lambda a: np.ascontiguousarray(np.asarray(a, dtype=np.float32))
    bc = lambda v, n: np.ascontiguousarray(np.broadcast_to(np.asarray(v, np.float32).reshape(1, -1), (128, n)))
    w = {}
    w["ab_w_in"] = f(inp["ab_w_in"][0])
    w["ab_conv_w"] = f(inp["ab_conv_w"][0].reshape(31, 512))
    prm = np.concatenate([np.asarray(inp[k][0], np.float32).reshape(4, 128).T for k in ("ab_conv_b", "ab_ln_g", "ab_ln_b")], axis=1)
    w["conv_prm"] = f(prm)
    w["ab_w_out"] = f(inp["ab_w_out"][0])
    w["c_w_in"] = f(inp["c_w_in"][0])
    w["c_gate_w"] = f(inp["c_gate_w"][0])
    w["c_gate_b_bc"] = bc(inp["c_gate_b"][0], 512)
    w["c_onorm_g_bc"] = bc(inp["c_onorm_g"][0], D)
    w["c_w_out"] = f(inp["c_w_out"][0])
    for l in range(2):
        w["ffn_w_gate%d" % l] = f(inp["ffn_w_gate"][l])
        w["ffn_w_up%d" % l] = f(inp["ffn_w_up"][l])
        w["ffn_w_down%d" % l] = f(inp["ffn_w_down"][l])
        w["g_mix%d" % l] = bc(inp["norm_mix_g"][l], D)
        w["g_ffn%d" % l] = bc(inp["norm_ffn_g"][l], D)
    w["g_final"] = bc(inp["final_norm_g"], D)
    return w


def build_program(nseq, phases, ext=None):
    B = Build(nseq, ext)
    declare(B)
    for p in phases:
        PHASES[p](B)
    return B


def kernel(**inp):
    n = 8
    nseq = 2
    x = np.asarray(inp["x"], dtype=np.float32)
    B = build_program(nseq, ORDER)
    shared = host_consts()
    shared.update(host_weights(inp))
    in_maps = []
    for c in range(n):
        m = dict(shared)
        m["x"] = np.ascontiguousarray(x[c * nseq:(c + 1) * nseq].reshape(nseq * SEQ, D))
        in_maps.append(m)
    res = run_bass_kernel_spmd(B.nc, in_maps, core_ids=list(range(n)))
    out = np.stack([np.asarray(r["out"], dtype=np.float32).reshape(nseq, SEQ, D) for r in res.results], axis=0)
    return out.reshape(16, SEQ, D)
```
